# Optimizing a Trainium2 kernel written in Bass

```python
import jax, jax.numpy as jnp
from jax import lax
import numpy as np


D_MODEL = 1024
BATCH = 1
SEQ = 16384
DEPTH = 1

NORM_EPS = 1e-6
PLE_DIM = 256
RW_HEADS = 8
RW_HEAD_DIM = 64
RW_WIDTH = RW_HEADS * RW_HEAD_DIM
RW_DECAY_LORA = 64
RW_ICLR_LORA = 64
RW_GATE_LORA = 128
RW_GN_EPS = 64e-5
RW_COLS = 3 * RW_WIDTH + 2 * RW_DECAY_LORA + 2 * RW_ICLR_LORA + RW_GATE_LORA
MLA_HEADS = 8
MLA_NOPE_DIM = 64
MLA_ROPE_DIM = 32
MLA_V_DIM = 64
MLA_QK_DIM = MLA_NOPE_DIM + MLA_ROPE_DIM
MLA_Q_RANK = 256
MLA_KV_RANK = 128
MLA_COLS = MLA_Q_RANK + MLA_KV_RANK + MLA_ROPE_DIM
MLA_WIDTH = MLA_HEADS * MLA_V_DIM
ROPE_THETA = 10000.0
Q_BLOCK = 128
N_BRANCHES = 2
GATE_COLS = N_BRANCHES * D_MODEL
IN_COLS = RW_COLS + MLA_COLS + GATE_COLS
PEER_HEADS = 8
PEER_N_KEYS = 128
PEER_N_EXPERTS = PEER_N_KEYS * PEER_N_KEYS
PEER_TOPK = 16
PEER_QUERY_DIM = 256
PEER_HALF = PEER_QUERY_DIM // 2
TOKEN_BLOCK = 128

kernel_name = 'hybrid_rwkv7_mla_peer_block'


def rmsnorm(x, g):
    xf = x.astype(jnp.float32)
    y = xf * lax.rsqrt(jnp.mean(xf * xf, axis=-1, keepdims=True) + NORM_EPS)
    return (y * g).astype(x.dtype)


def centred_shift(u):
    prev = jnp.pad(u[:, :-1], ((0, 0), (1, 0), (0, 0)))
    nxt = jnp.pad(u[:, 1:], ((0, 0), (0, 1), (0, 0)))
    return 0.5 * (prev + nxt) - u


def _heads(t, n_heads, head_dim):
    return t.reshape(t.shape[:-1] + (n_heads, head_dim))


def rope_tables(positions):
    inv = ROPE_THETA ** (-jnp.arange(0, MLA_ROPE_DIM, 2, dtype=jnp.float32) / MLA_ROPE_DIM)
    ang = positions.astype(jnp.float32)[..., None] * inv
    return jnp.cos(ang), jnp.sin(ang)


def apply_rope(x, cos, sin):
    xf = x.astype(jnp.float32)
    half = xf.shape[-1] // 2
    x1, x2 = xf[..., :half], xf[..., half:]
    return jnp.concatenate([x1 * cos - x2 * sin, x2 * cos + x1 * sin], axis=-1).astype(x.dtype)


def rwkv7_scan(r, a_vec, b_vec, v, k, decay):
    def step(state, inp):
        r_t, a_t, b_t, v_t, k_t, w_t = inp
        sa = jnp.einsum('bdhvk,bdhk->bdhv', state, a_t)
        state = (state * w_t[..., None, :] + sa[..., :, None] * b_t[..., None, :]
                 + v_t[..., :, None] * k_t[..., None, :])
        return state, jnp.einsum('bdhvk,bdhk->bdhv', state, r_t)
    B = r.shape[1]
    init = jnp.zeros((B, 2, RW_HEADS, RW_HEAD_DIM, RW_HEAD_DIM), jnp.float32)
    _, out = lax.scan(step, init, (r, a_vec, b_vec, v, k, decay))
    return out


def rwkv7_time_mix(u, mu, w0, w2, a0, a2, g2, k_k, k_a, r_k, gn_w, gn_b):
    f32 = jnp.float32
    B, T, _ = u.shape
    H, N, W = RW_HEADS, RW_HEAD_DIM, RW_WIDTH
    u = u + mu * centred_shift(u)
    o0 = 3 * W
    o1 = o0 + 2 * RW_DECAY_LORA
    o2 = o1 + 2 * RW_ICLR_LORA
    r, k, v = u[..., :W], u[..., W:2 * W], u[..., 2 * W:o0]
    lw = u[..., o0:o1].reshape(B, T, 2, RW_DECAY_LORA)
    la = u[..., o1:o2].reshape(B, T, 2, RW_ICLR_LORA)
    lg = u[..., o2:]
    w_log = -jax.nn.softplus(-(w0 + jnp.einsum('btdl,dlc->btdc', jnp.tanh(lw), w2)).astype(f32)) - 0.5
    decay = jnp.exp(-jnp.exp(w_log))
    a = jax.nn.sigmoid((a0 + jnp.einsum('btdl,dlc->btdc', la, a2)).astype(f32))
    g = jax.nn.sigmoid(lg) @ g2
    kk = _heads((k * k_k).astype(f32), H, N)
    kk = kk / jnp.maximum(jnp.sqrt(jnp.sum(kk * kk, axis=-1, keepdims=True)), 1e-12)
    k_dir = k.astype(f32)[:, :, None, :] * (1.0 + (a - 1.0) * k_a.astype(f32))
    r_h = _heads(r.astype(f32), H, N)
    v_h = _heads(v.astype(f32), H, N)
    k_h = _heads(k_dir, H, N)
    a_h = _heads(a, H, N)
    both = lambda t: jnp.stack([t, t], axis=2)

    def time_major(t):
        t = jnp.concatenate([t[:, :, :1], jnp.flip(t[:, :, 1:], axis=1)], axis=2)
        return jnp.moveaxis(t, 1, 0)

    out = rwkv7_scan(time_major(both(r_h)), time_major(both(-kk)),
                     time_major(kk[:, :, None] * a_h), time_major(both(v_h)),
                     time_major(k_h), time_major(_heads(decay, H, N)))
    out = jnp.moveaxis(out, 0, 1)
    o = out[:, :, 0] + jnp.flip(out[:, :, 1], axis=1)
    m = jnp.mean(o, axis=-1, keepdims=True)
    var = jnp.mean(jnp.square(o - m), axis=-1, keepdims=True)
    o = ((o - m) * lax.rsqrt(var + RW_GN_EPS)).reshape(B, T, W) * gn_w + gn_b
    bonus = jnp.sum(jnp.sum(r_h[:, :, None] * k_h * _heads(r_k, H, N), axis=-1, keepdims=True), axis=2) * v_h
    o = o + bonus.reshape(B, T, W)
    return (o * g).astype(u.dtype)


def bidir_attention(q, k, v):
    B, T, H, Dq = q.shape
    scale = Dq ** -0.5
    qb = jnp.moveaxis(q.reshape(B, T // Q_BLOCK, Q_BLOCK, H, Dq), 1, 0)

    def block(qi):
        s = jnp.einsum('bqhd,bkhd->bhqk', qi, k).astype(jnp.float32) * scale
        p = jax.nn.softmax(s, axis=-1)
        return jnp.einsum('bhqk,bkhd->bqhd', p.astype(v.dtype), v)

    o = lax.map(block, qb)
    return jnp.moveaxis(o, 0, 1).reshape(B, T, H, v.shape[-1])


def mla_mix(u, positions, g_qa, w_qup, g_kva, w_kvup):
    B, T, _ = u.shape
    H = MLA_HEADS
    cq = u[..., :MLA_Q_RANK]
    ckv = u[..., MLA_Q_RANK:MLA_Q_RANK + MLA_KV_RANK]
    kr = u[..., MLA_Q_RANK + MLA_KV_RANK:]
    q = (rmsnorm(cq, g_qa) @ w_qup).reshape(B, T, H, MLA_QK_DIM)
    kv = (rmsnorm(ckv, g_kva) @ w_kvup).reshape(B, T, H, MLA_NOPE_DIM + MLA_V_DIM)
    cos, sin = rope_tables(positions)
    q_rope = apply_rope(q[..., MLA_NOPE_DIM:], cos[:, :, None], sin[:, :, None])
    k_rope = apply_rope(kr, cos, sin)
    qh = jnp.concatenate([q[..., :MLA_NOPE_DIM], q_rope], axis=-1)
    kh = jnp.concatenate([kv[..., :MLA_NOPE_DIM],
                          jnp.broadcast_to(k_rope[:, :, None, :], (B, T, H, MLA_ROPE_DIM))], axis=-1)
    o = bidir_attention(qh, kh, kv[..., MLA_NOPE_DIM:])
    return o.reshape(B, T, MLA_WIDTH)


def peer_ffn(h, w_q, sub_keys, expert_u, expert_v):
    B, T, D = h.shape
    hb_all = h.reshape(-1, TOKEN_BLOCK, D)

    def block(hb):
        n = hb.shape[0]
        q = (hb @ w_q).reshape(n, PEER_HEADS, 2, PEER_HALF)
        s = jnp.einsum('nhcd,hckd->nhck', q, sub_keys).astype(jnp.float32)
        sc, ix = lax.top_k(s, PEER_TOPK)
        cand = sc[:, :, 0, :, None] + sc[:, :, 1, None, :]
        cidx = ix[:, :, 0, :, None] * PEER_N_KEYS + ix[:, :, 1, None, :]
        top, pos = lax.top_k(cand.reshape(n, PEER_HEADS, PEER_TOPK * PEER_TOPK), PEER_TOPK)
        eidx = jnp.take_along_axis(cidx.reshape(n, PEER_HEADS, -1), pos, axis=-1)
        gate = jax.nn.softmax(top, axis=-1)
        act = jax.nn.gelu(jnp.einsum('nhed,nd->nhe', expert_u[eidx], hb), approximate=False)
        return jnp.einsum('nhe,nhed->nd', (gate * act).astype(hb.dtype), expert_v[eidx])

    return lax.map(block, hb_all).reshape(B, T, D)


def setup_inputs(seed: int = 0) -> dict:
    key = jax.random.key(seed)
    ks = jax.random.split(key, 32)
    nrm = lambda k, shape, s: jax.random.normal(k, shape, jnp.float32) * s
    gain = lambda k, shape: 1.0 + 0.05 * jax.random.normal(k, shape, jnp.float32)
    L, D, W = DEPTH, D_MODEL, RW_WIDTH
    return {
        'x': nrm(ks[0], (BATCH, SEQ, D), 1.0),
        'p': nrm(ks[1], (DEPTH, BATCH, SEQ, PLE_DIM), 1.0),
        'positions': jnp.broadcast_to(jnp.arange(SEQ, dtype=jnp.int32)[None, :], (BATCH, SEQ)),
        'g_mix': gain(ks[2], (L, D)),
        'w_in': nrm(ks[3], (L, D, IN_COLS), D ** -0.5),
        'rw_mu': jax.random.uniform(ks[4], (L, RW_COLS), jnp.float32),
        'rw_w0': jax.random.uniform(ks[5], (L, 2, W), jnp.float32, -6.0, -1.0),
        'rw_w2': nrm(ks[6], (L, 2, RW_DECAY_LORA, W), 0.1 * RW_DECAY_LORA ** -0.5),
        'rw_a0': nrm(ks[7], (L, 2, W), 0.1),
        'rw_a2': nrm(ks[8], (L, 2, RW_ICLR_LORA, W), 0.5 * RW_ICLR_LORA ** -0.5),
        'rw_g2': nrm(ks[9], (L, RW_GATE_LORA, W), RW_GATE_LORA ** -0.5),
        'rw_k_k': 0.85 + nrm(ks[10], (L, W), 0.05),
        'rw_k_a': gain(ks[11], (L, W)),
        'rw_r_k': nrm(ks[12], (L, W), 0.1),
        'rw_gn_w': gain(ks[13], (L, W)),
        'rw_gn_b': nrm(ks[14], (L, W), 0.01),
        'mla_g_qa': gain(ks[15], (L, MLA_Q_RANK)),
        'mla_w_qup': nrm(ks[16], (L, MLA_Q_RANK, MLA_HEADS * MLA_QK_DIM), MLA_Q_RANK ** -0.5),
        'mla_g_kva': gain(ks[17], (L, MLA_KV_RANK)),
        'mla_w_kvup': nrm(ks[18], (L, MLA_KV_RANK, MLA_HEADS * (MLA_NOPE_DIM + MLA_V_DIM)), MLA_KV_RANK ** -0.5),
        'w_br_rwkv': nrm(ks[19], (L, W, D), W ** -0.5),
        'w_br_mla': nrm(ks[20], (L, MLA_WIDTH, D), MLA_WIDTH ** -0.5),
        'w_out': nrm(ks[21], (L, D, D), D ** -0.5),
        'g_ffn': gain(ks[22], (L, D)),
        'peer_w_q': nrm(ks[23], (L, D, PEER_HEADS * PEER_QUERY_DIM), D ** -0.5),
        'peer_sub_keys': nrm(ks[24], (L, PEER_HEADS, 2, PEER_N_KEYS, PEER_HALF), PEER_HALF ** -0.5),
        'peer_u': nrm(ks[25], (L, PEER_N_EXPERTS, D), D ** -0.5),
        'peer_v': nrm(ks[26], (L, PEER_N_EXPERTS, D), PEER_HEADS ** -0.5),
        'g_ple': gain(ks[27], (L, D)),
        'w_ple_gate': nrm(ks[28], (L, D, D), D ** -0.5),
        'w_ple_proj': nrm(ks[29], (L, PLE_DIM, D), PLE_DIM ** -0.5),
        'g_final': gain(ks[30], (D,)),
    }


def reference(x, p, positions, g_mix, w_in, rw_mu, rw_w0, rw_w2, rw_a0, rw_a2, rw_g2,
              rw_k_k, rw_k_a, rw_r_k, rw_gn_w, rw_gn_b, mla_g_qa, mla_w_qup, mla_g_kva,
              mla_w_kvup, w_br_rwkv, w_br_mla, w_out, g_ffn, peer_w_q, peer_sub_keys,
              peer_u, peer_v, g_ple, w_ple_gate, w_ple_proj, g_final):
    B, T, D = x.shape
    for i in range(DEPTH):
        h = rmsnorm(x, g_mix[i])
        u = h @ w_in[i]
        u_rw = u[..., :RW_COLS]
        u_mla = u[..., RW_COLS:RW_COLS + MLA_COLS]
        gates = jax.nn.sigmoid(u[..., RW_COLS + MLA_COLS:]).reshape(B, T, N_BRANCHES, D)
        y_rw = rwkv7_time_mix(u_rw, rw_mu[i], rw_w0[i], rw_w2[i], rw_a0[i], rw_a2[i], rw_g2[i],
                              rw_k_k[i], rw_k_a[i], rw_r_k[i], rw_gn_w[i], rw_gn_b[i])
        y_mla = mla_mix(u_mla, positions, mla_g_qa[i], mla_w_qup[i], mla_g_kva[i], mla_w_kvup[i])
        merged = gates[:, :, 0] * (y_rw @ w_br_rwkv[i]) + gates[:, :, 1] * (y_mla @ w_br_mla[i])
        x = x + merged @ w_out[i]
        x = x + peer_ffn(rmsnorm(x, g_ffn[i]), peer_w_q[i], peer_sub_keys[i], peer_u[i], peer_v[i])
        x = x + (p[i] @ w_ple_proj[i]) * jax.nn.sigmoid(rmsnorm(x, g_ple[i]) @ w_ple_gate[i])
    return rmsnorm(x, g_final)
```

```python
import contextlib
import numpy as np
import concourse.bass as bass
import concourse.mybir as mybir
from concourse.bass_utils import run_bass_kernel_spmd

F32 = mybir.dt.float32
BF16 = mybir.dt.bfloat16
I32 = mybir.dt.int32
AF = mybir.ActivationFunctionType
ALU = mybir.AluOpType
AX = mybir.AxisListType

ENGS = ['pe', 'act', 'dve', 'pool', 'sp']


class Sched:
    def __init__(self, nc, n_dma_sems=48):
        self.nc = nc
        self.stack = contextlib.ExitStack()
        self.streams = {e: [] for e in ENGS}
        self.cnt = {}
        self.seen = {e: {} for e in ENGS}
        self.last_write = {}
        self.readers = {}
        self.n_dma_sems = n_dma_sems
        self.dma_keys = {}
        self.sems = {}
        self.scopes = [self.stack]
        self.cap = None

    def __enter__(self):
        self.stack.__enter__()
        for e in ENGS:
            self.sems[e] = self.stack.enter_context(self.nc.semaphore("s_" + e))
            self.cnt[e] = 0
        self.dma_pool = [self.stack.enter_context(self.nc.semaphore("d%d" % i)) for i in range(self.n_dma_sems)]
        return self

    def __exit__(self, *a):
        return self.stack.__exit__(*a)

    def sb(self, name, shape, dt):
        self.uid = getattr(self, 'uid', 0) + 1
        return self.scopes[-1].enter_context(self.nc.sbuf_tensor("sb%d_%s" % (self.uid, name), list(shape), dt))

    def ps(self, name, shape, dt):
        self.uid = getattr(self, 'uid', 0) + 1
        return self.scopes[-1].enter_context(self.nc.psum_tensor("ps%d_%s" % (self.uid, name), list(shape), dt))

    def push(self):
        st = contextlib.ExitStack()
        st.__enter__()
        self.scopes.append(st)

    def pop(self):
        self.barrier()
        st = self.scopes.pop()
        st.__exit__(None, None, None)

    def _deps(self, eng, reads, writes):
        deps = {}
        def add(tok):
            if tok is None:
                return
            k, v = tok
            if deps.get(k, 0) < v:
                deps[k] = v
        for r in reads:
            add(self.last_write.get(r))
        for w in writes:
            add(self.last_write.get(w))
            for t in self.readers.get(w, ()):
                add(t)
        waits = []
        seen = self.seen[eng]
        for k, v in deps.items():
            if k == 'pe' and eng == 'pe':
                continue
            if seen.get(k, 0) >= v:
                continue
            seen[k] = v
            waits.append((k, v))
        return waits

    def _commit(self, tok, reads, writes):
        for w in writes:
            self.last_write[w] = tok
            self.readers[w] = []
        for r in reads:
            if r in writes:
                continue
            self.readers.setdefault(r, []).append(tok)

    @staticmethod
    def _excl(reads, writes):
        pr = [r for r in reads if isinstance(r, str) and r.startswith('pb')]
        if pr:
            reads = [r for r in reads if r not in pr]
            writes = list(writes) + [r for r in pr if r not in writes]
        return reads, writes

    def op(self, eng, fn, reads=(), writes=()):
        if self.cap is not None:
            self.cap.append(('op', (eng, fn, tuple(reads), tuple(writes)), {}))
            return
        reads, writes = self._excl(reads, writes)
        waits = self._deps(eng, reads, writes)
        self.cnt[eng] += 1
        tok = (eng, self.cnt[eng])
        self.streams[eng].append((waits, fn, (eng, 1)))
        self._commit(tok, reads, writes)

    def capture(self, fn, *a):
        self.cap = []
        fn(*a)
        lst, self.cap = self.cap, None
        return lst

    def emit_interleaved(self, A, B):
        ia = ib = 0
        na, nb = len(A), len(B)
        while ia < na or ib < nb:
            if ib >= nb or (ia < na and ia * max(nb, 1) <= ib * max(na, 1)):
                kind, args, kw = A[ia]; ia += 1
            else:
                kind, args, kw = B[ib]; ib += 1
            getattr(self, kind)(*args, **kw)

    def _dkey(self, key):
        if key not in self.dma_keys:
            idx = len(self.dma_keys)
            assert idx < self.n_dma_sems, "out of dma semaphores"
            self.dma_keys[key] = ('dma', idx)
            self.cnt.setdefault(('dma', idx), 0)
        return self.dma_keys[key]

    def dma(self, eng, out, in_, reads=(), writes=(), key=None, **kw):
        if self.cap is not None:
            self.cap.append(('dma', (eng, out, in_), dict(reads=tuple(reads), writes=tuple(writes), key=key, **kw)))
            return
        k = self._dkey(key)
        waits = self._deps(eng, reads, writes)
        self.cnt[k] += 16
        tok = (k, self.cnt[k])
        self.streams[eng].append((waits, (lambda e, o=out, i=in_, kw=kw: e.dma_start(out=o, in_=i, **kw)), (k, 16)))
        self._commit(tok, reads, writes)

    def gather(self, out, in_rows, idx_ap, reads=(), writes=(), key=None):
        kk_ = self._dkey(key)
        waits = self._deps('pool', reads, writes)
        self.cnt[kk_] += 16
        tok = (kk_, self.cnt[kk_])
        self.streams['pool'].append((waits, (lambda e: e.indirect_dma_start(out=out, out_offset=None, in_=in_rows, in_offset=bass.IndirectOffsetOnAxis(ap=idx_ap, axis=0))), (kk_, 16)))
        self._commit(tok, reads, writes)

    def coll(self, kind, op, groups, ins, outs, reads=(), writes=()):
        key = 'coll'
        kk_ = self._dkey(key)
        waits = self._deps('pool', reads, writes)
        self.cnt[kk_] += 16
        tok = (kk_, self.cnt[kk_])
        self.streams['pool'].append((waits, (lambda e: e.collective_compute(kind, op, replica_groups=groups, ins=ins, outs=outs)), (kk_, 16)))
        self._commit(tok, reads, writes)

    def barrier(self):
        for e in ENGS:
            waits = []
            for k, v in self.cnt.items():
                if v == 0 or k == e:
                    continue
                if self.seen[e].get(k, 0) >= v:
                    continue
                self.seen[e][k] = v
                waits.append((k, v))
            if waits:
                self.streams[e].append((waits, None, None))
        for e in ENGS:
            if self.cnt[e] and self.seen[e].get(e, 0) < self.cnt[e]:
                self.seen[e][e] = self.cnt[e]
                self.streams[e].append(([(e, self.cnt[e])], None, None))
        self.last_write.clear()
        self.readers.clear()
        self.dma_keys = {}

    def _sem(self, k):
        if isinstance(k, tuple):
            return self.dma_pool[k[1]]
        return self.sems[k]

    def finish(self):
        self.barrier()
        nc = self.nc
        streams = self.streams
        sem = self._sem

        def replay(engname):
            def run(eng):
                for waits, fn, inc in streams[engname]:
                    for k, v in waits:
                        eng.wait_ge(sem(k), v)
                    if fn is not None:
                        ins = fn(eng)
                        ins.then_inc(sem(inc[0]), inc[1])
            return run

        with nc.Block() as block:
            block.tensor(replay('pe'))
            block.scalar(replay('act'))
            block.vector(replay('dve'))
            block.gpsimd(replay('pool'))
            block.sync(replay('sp'))


T = 16384
D = 1024
NB = 32
CL = 128
NCH = T // CL
CDEC = 0.6065306597126334
NPP = 32


class K:
    def __init__(self, S):
        self.S = S
        self.nbank = 0

    def mm(self, out, lhsT, rhs, start=True, stop=True, reads=(), writes=()):
        self.S.op('pe', lambda e: e.matmul(out, lhsT=lhsT, rhs=rhs, start=start, stop=stop), reads=reads, writes=writes)

    def act(self, out, in_, func, bias=None, scale=None, reads=(), writes=()):
        kw = {}
        if bias is not None:
            kw['bias'] = bias
        if scale is not None:
            kw['scale'] = scale
        self.S.op('act', lambda e: e.activation(out=out, in_=in_, func=func, **kw), reads=reads, writes=writes)

    def tt(self, eng, out, in0, in1, op, reads=(), writes=()):
        self.S.op(eng, lambda e: e.tensor_tensor(out=out, in0=in0, in1=in1, op=op), reads=reads, writes=writes)

    def ts(self, eng, out, in0, s1, s2, op0, op1=None, reads=(), writes=()):
        if op1 is None and op0 == ALU.pow:
            self.S.op(eng, lambda e: e.tensor_scalar(out=out, in0=in0, scalar1=1.0, scalar2=s1, op0=ALU.mult, op1=ALU.pow), reads=reads, writes=writes)
        elif op1 is None:
            self.S.op(eng, lambda e: e.tensor_scalar(out=out, in0=in0, scalar1=s1, scalar2=0.0, op0=op0, op1=ALU.add), reads=reads, writes=writes)
        else:
            self.S.op(eng, lambda e: e.tensor_scalar(out=out, in0=in0, scalar1=s1, scalar2=s2, op0=op0, op1=op1), reads=reads, writes=writes)

    def rsqrt(self, out, in_, scale, eps, reads=(), writes=()):
        self.S.op('act', lambda e: e.activation(out=out, in_=in_, func=AF.Sqrt, bias=self.eps_ap(eps, out), scale=scale), reads=list(reads) + ['epsc'], writes=writes)
        self.S.op('dve', lambda e: e.reciprocal(out=out, in_=out), reads=writes, writes=writes)

    def eps_ap(self, eps, out):
        n = out.shape[0]
        return self.epsc[eps][0:n, 0:1]

    def stt(self, eng, out, in0, scalar, in1, op0, op1, reads=(), writes=()):
        self.S.op(eng, lambda e: e.scalar_tensor_tensor(out=out, in0=in0, scalar=scalar, in1=in1, op0=op0, op1=op1), reads=reads, writes=writes)

    def cp(self, eng, out, in_, reads=(), writes=()):
        if eng == 'act':
            self.S.op('act', lambda e: e.activation(out=out, in_=in_, func=AF.Copy), reads=reads, writes=writes)
        else:
            self.S.op(eng, lambda e: e.tensor_copy(out=out, in_=in_), reads=reads, writes=writes)

    def memset(self, eng, ap, val, writes=()):
        self.S.op(eng, lambda e: e.memset(ap, val), reads=(), writes=writes)


def rwkv_phase(nc, S, k, dr, core_dbg=None):
    x_d = dr['x']
    nblk = dr.get('_nblk', NB)
    lvl = dr.get('_lvl', 99)
    banks = dr.get('_banks') or [(S.ps("pb%d" % i, [128, 512], F32), "pb%d" % i) for i in range(8)]
    st = {'b': 0, 'lo': 0, 'hi': 8}

    def bank():
        b = banks[st['lo'] + st['b'] % (st['hi'] - st['lo'])]
        st['b'] += 1
        return b

    wa = S.sb("wa", [128, 8, 384], BF16)
    wst_ = S.sb("wst_", [128, 384], F32)
    for c in range(8):
        S.dma('sp', wst_[:], dr['wa'][c * 128:(c + 1) * 128, 0:384], writes=['wst_'], key='c0')
        k.cp('dve', wa[:, c, :], wst_[:], reads=['wst_'], writes=['wa'])
    pp = S.sb("pp", [128, NPP], F32)
    S.dma('sp', pp[:], dr['pp'], writes=['pp'], key='c1')
    cst_st = S.sb("cst_st", [128, 5, 128], F32)
    S.dma('sp', cst_st[:], dr['cst'].rearrange("m p n -> p m n"), writes=['cst_st'], key='c2')
    cst = S.sb("cst", [128, 5, 128], BF16)
    k.cp('dve', cst[:], cst_st[:], reads=['cst_st'], writes=['cst'])
    ident = cst[:, 0, :]
    m4 = S.sb("m4", [128, 4, 4, 128], BF16)
    for mi in range(4):
        for j in range(4):
            k.cp('pool', m4[:, mi, j, :], cst[:, 1 + mi, :], reads=['cst'], writes=['m4'])
    id4 = S.sb("id4", [128, 4, 128], BF16)
    for j in range(4):
        k.cp('pool', id4[:, j, :], cst[:, 0, :], reads=['cst'], writes=['id4'])
    ones_bd = S.sb("ones_bd", [128, 128], BF16)
    k.memset('pool', ones_bd[:], 0.0, writes=['ones_bd'])
    k.memset('pool', ones_bd[0:64, 0:64], 1.0, writes=['ones_bd'])
    k.memset('pool', ones_bd[64:128, 64:128], 1.0, writes=['ones_bd'])
    ones_f = S.sb("ones_f", [128, 128], BF16)
    k.memset('pool', ones_f[:], 1.0, writes=['ones_f'])
    bd_st = S.sb("bd_st", [128, 2, 128], F32)
    k.memset('pool', bd_st[:], 0.0, writes=['bd_st'])
    S.dma('sp', bd_st[0:64, 0, 0:64], dr['w2s'][0:64, :], reads=['bd_st'], writes=['bd_st'], key='c3')
    S.dma('sp', bd_st[64:128, 0, 64:128], dr['w2s'][64:128, :], reads=['bd_st'], writes=['bd_st'], key='c3')
    S.dma('sp', bd_st[0:64, 1, 0:64], dr['a2s'][0:64, :], reads=['bd_st'], writes=['bd_st'], key='c3')
    S.dma('sp', bd_st[64:128, 1, 64:128], dr['a2s'][64:128, :], reads=['bd_st'], writes=['bd_st'], key='c3')
    bd = S.sb("bd", [128, 2, 128], BF16)
    k.cp('dve', bd[:], bd_st[:], reads=['bd_st'], writes=['bd'])
    g2_st = S.sb("g2_st", [128, 64], F32)
    S.dma('sp', g2_st[:], dr['g2h'], writes=['g2_st'], key='c4')
    g2h = S.sb("g2h", [128, 64], BF16)
    k.cp('dve', g2h[:], g2_st[:], reads=['g2_st'], writes=['g2h'])
    w0r_st = S.sb("w0r_st", [1, 128], F32)
    S.dma('sp', w0r_st[:], dr['w0row'], writes=['w0r_st'], key='c5')
    w0row = S.sb("w0row", [1, 128], BF16)
    k.cp('dve', w0row[:], w0r_st[:], reads=['w0r_st'], writes=['w0row'])
    epst = S.sb("epst", [128, 4], F32)
    k.epsc = {}
    for i_, ev in enumerate([1e-6, 1e-24, 64e-5]):
        k.memset('pool', epst[:, i_:i_ + 1], ev, writes=['epsc'])
        k.epsc[ev] = epst[:, i_:i_ + 1]
    omka = S.sb("omka", [128, 1], F32)
    k.ts('dve', omka[:], pp[:, 9:10], -1.0, 1.0, ALU.mult, ALU.add, reads=['pp'], writes=['omka'])

    MT_all = S.sb("MT_all", [128, NCH, 128], BF16)
    k.memset('pool', MT_all[:], 0.0, writes=['MT_all'])
    N_all = S.sb("N_all", [128, NCH, 64], BF16)
    gamL = S.sb("gamL", [128, NCH], F32)
    Hh = S.sb("Hh", [128, NCH + 1, 64], BF16)

    hT = [S.sb("hT%d" % i, [128, 8, 512], BF16) for i in range(2)]
    U = [S.sb("U%d" % i, [128, 6, 514], BF16) for i in range(3)]
    for i in range(3):
        k.memset('pool', U[i][:], 0.0, writes=['U%d' % i])

    def w(name, shape, dt=BF16):
        return S.sb(name, shape, dt)
    tsum6 = w("tsum6", [128, 6, 512], BF16)
    us2 = [w("us%d" % i, [128, 6, 512], BF16) for i in range(2)]
    us = us2[0]
    hmu = w("hmu", [128, 6], F32); omu = w("omu", [128, 6], F32)
    k.ts('dve', hmu[:], pp[:, 0:6], 0.5, None, ALU.mult, reads=['pp'], writes=['hmu'])
    k.ts('dve', omu[:], pp[:, 0:6], -1.0, 1.0, ALU.mult, ALU.add, reads=['pp'], writes=['omu'])
    tl = w("tl", [128, 512]); sl = w("sl", [128, 512])
    sg_tok = w("sg_tok", [128, 4, 128])
    Gi = w("Gi", [128, 512], F32); Ginv = w("Ginv", [128, 512], F32); Ge = w("Ge", [128, 512], F32); Gh = w("Gh", [128, 512], F32)
    tot = w("tot", [128, 4], F32); nct = w("nct", [128, 4], F32)
    a_t = w("a_t", [128, 512], F32)
    kk = w("kk", [128, 512], F32); kk2 = w("kk2", [128, 512]); rn = w("rn", [128, 512], F32); kkn = w("kkn", [128, 512], F32)
    t1 = rn; kdir = kk; bvec = a_t
    At2 = [w("At%d" % i, [128, 512]) for i in range(2)]; Bt2 = [w("Bt%d" % i, [128, 512]) for i in range(2)]
    Kt2 = [w("Kt%d" % i, [128, 512]) for i in range(2)]; Rt2 = [w("Rt%d" % i, [128, 512]) for i in range(2)]
    Bht2 = [w("Bht%d" % i, [128, 512]) for i in range(2)]; Kht2 = [w("Kht%d" % i, [128, 512]) for i in range(2)]
    At, Bt, Kt, Rt, Bht, Kht = At2[0], Bt2[0], Kt2[0], Rt2[0], Bht2[0], Kht2[0]
    rk = w("rk", [128, 512]); bon = w("bon", [64, 512]); g_t = w("g_t", [64, 512])
    Sm = [w("Sm%d" % i, [128, 8, 128]) for i in range(2)]
    SmT = [w("SmT%d" % i, [128, 8, 128]) for i in range(2)]
    Qm = [w("Qm%d" % i, [128, 8, 128]) for i in range(2)]
    AakT = w("AakT", [128, 8, 128]); TrbT = w("TrbT", [128, 8, 128]); TrkT = w("TrkT", [128, 8, 128])
    AXm = w("AXm", [128, 8, 128]); WU = w("WU", [128, 8, 128])
    Bh_tok = w("Bh_tok", [128, 8, 64]); Kh_tok = w("Kh_tok", [128, 8, 64]); V_tok = w("V_tok", [128, 4, 64])
    QhT = w("QhT", [128, 512]); Oloc = w("Oloc", [64, 512])

    MASK = {0: {'ss': 0, 'si': 1}, 1: {'ss': 2, 'si': 3}}
    MASK_TS = {0: 2, 1: 0}

    def load_h(b):
        S.dma('sp', hT[b % 2][:], dr['hT_d'][:, :, b * 512:(b + 1) * 512], reads=['hT_d'], writes=['hT%d' % (b % 2)], key='lh%d' % (b % 2))

    def project(b):
        if b == 0:
            load_h(0)
        if b + 1 < nblk:
            load_h(b + 1)
        h = hT[b % 2]; hn = 'hT%d' % (b % 2)
        Ub = U[b % 3]; un = 'U%d' % (b % 3)
        S.dma('sp', Ub[:, 3:6, 1:513], dr['ush_d'][:, :, b * 512:(b + 1) * 512].rearrange("a p n -> p a n"), reads=['ush_d'], writes=[un], key='lu%d' % (b % 3))
        for tI in range(3):
            pb, pn = bank()
            for c in range(8):
                k.mm(pb[:, :], lhsT=wa[:, c, tI * 128:(tI + 1) * 128], rhs=h[:, c, :], start=(c == 0), stop=(c == 7), reads=['wa', hn], writes=[pn])
            if tI % 2 == 0:
                k.cp('act', Ub[:, tI, 1:513], pb[:, :], reads=[pn], writes=[un])
            else:
                k.cp('dve', Ub[:, tI, 1:513], pb[:, :], reads=[pn], writes=[un])
        if b > 0:
            pu = U[(b - 1) % 3]; pun = 'U%d' % ((b - 1) % 3)
            k.cp('pool', pu[:, :, 513:514], Ub[:, :, 1:2], reads=[un], writes=[pun])
            k.cp('pool', Ub[:, :, 0:1], pu[:, :, 512:513], reads=[pun], writes=[un])
        else:
            k.memset('pool', Ub[:, :, 0:1], 0.0, writes=[un])
        if b == NB - 1:
            k.memset('pool', Ub[:, :, 513:514], 0.0, writes=[un])

    def prep(b):
        Ub = U[b % 3]; un = 'U%d' % (b % 3)
        tok0 = b * 512
        sfx = str(b % 2)
        At = At2[b % 2]; Bt = Bt2[b % 2]; Kt = Kt2[b % 2]; Rt = Rt2[b % 2]; Bht = Bht2[b % 2]; Kht = Kht2[b % 2]; us = us2[b % 2]
        k.tt('pool', tsum6[:], Ub[:, :, 0:512], Ub[:, :, 2:514], ALU.add, reads=[un], writes=['tsum6'])
        k.tt('dve', tsum6[:], tsum6[:], hmu[:, 0:6].unsqueeze(2).to_broadcast([128, 6, 512]), ALU.mult, reads=['tsum6', 'hmu'], writes=['tsum6'])
        k.tt('pool', us[:], Ub[:, :, 1:513], omu[:, 0:6].unsqueeze(2).to_broadcast([128, 6, 512]), ALU.mult, reads=[un, 'omu'], writes=['us' + sfx])
        k.tt('dve', us[:], us[:], tsum6[:], ALU.add, reads=['us' + sfx, 'tsum6'], writes=['us' + sfx])
        r2 = us[:, 0, :]; k2 = us[:, 1, :]; v2 = us[:, 2, :]
        k.act(tl[:], us[:, 3, :], AF.Tanh, reads=['us' + sfx], writes=['tl'])
        pb, pn = bank()
        for j in range(4):
            k.mm(pb[:, j * 128:(j + 1) * 128], lhsT=tl[:, j * 128:(j + 1) * 128], rhs=bd[:, 0, :], start=True, stop=False, reads=['tl', 'bd'], writes=[pn])
            k.mm(pb[:, j * 128:(j + 1) * 128], lhsT=ones_f[0:1, :], rhs=w0row[0:1, :], start=False, stop=True, reads=['ones_f', 'w0row'], writes=[pn])
        k.act(sg_tok[:].rearrange("p j n -> p (j n)"), pb[:, :], AF.Sigmoid, reads=[pn], writes=['sg_tok'])
        pI, pIn = bank(); pE, pEn = bank()
        for j in range(4):
            for d in range(2):
                P = slice(64 * d, 64 * d + 64)
                k.mm(pI[P, j * 128:(j + 1) * 128], lhsT=sg_tok[:, j, P], rhs=cst[:, 2 + 2 * d, :], reads=['sg_tok', 'cst'], writes=[pIn])
                k.mm(pE[P, j * 128:(j + 1) * 128], lhsT=sg_tok[:, j, P], rhs=cst[:, 1 + 2 * d, :], reads=['sg_tok', 'cst'], writes=[pEn])
        k.act(Gi[:], pI[:, :], AF.Exp, scale=-CDEC, reads=[pIn], writes=['Gi'])
        k.act(Ginv[:], pI[:, :], AF.Exp, scale=CDEC, reads=[pIn], writes=['Ginv'])
        k.act(Ge[:], pE[:, :], AF.Exp, scale=-CDEC, reads=[pEn], writes=['Ge'])
        pI3 = pI[:, :].rearrange("p (j n) -> p j n", n=128)
        k.cp('dve', tot[0:64, :], pI3[0:64, :, 127], reads=[pIn], writes=['tot'])
        k.cp('dve', tot[64:128, :], pI3[64:128, :, 0], reads=[pIn], writes=['tot'])
        k.ts('dve', nct[:], tot[:], -CDEC, None, ALU.mult, reads=['tot'], writes=['nct'])
        k.act(gamL[:, b * 4:(b + 1) * 4], tot[:], AF.Exp, scale=-CDEC, reads=['tot'], writes=['gamL'])
        for j in range(4):
            k.act(Gh[:, j * 128:(j + 1) * 128], pI[:, j * 128:(j + 1) * 128], AF.Exp, bias=nct[:, j:j + 1], scale=CDEC, reads=[pIn, 'nct'], writes=['Gh'])
        pb, pn = bank()
        k.mm(pb[:, :], lhsT=bd[:, 1, :], rhs=us[:, 4, :], reads=['bd', 'us' + sfx], writes=[pn])
        k.act(a_t[:], pb[:, :], AF.Sigmoid, bias=pp[:, 7:8], reads=[pn, 'pp'], writes=['a_t'])
        k.ts('dve', kk[:], k2, pp[:, 8:9], None, ALU.mult, reads=['us' + sfx, 'pp'], writes=['kk'])
        k.tt('pool', kk2[:], kk[:], kk[:], ALU.mult, reads=['kk'], writes=['kk2'])
        pb, pn = bank()
        k.mm(pb[:, :], lhsT=ones_bd[:], rhs=kk2[:], reads=['ones_bd', 'kk2'], writes=[pn])
        k.rsqrt(rn[:], pb[:, :], 1.0, 1e-24, reads=[pn], writes=['rn'])
        k.tt('dve', kkn[:], kk[:], rn[:], ALU.mult, reads=['kk', 'rn'], writes=['kkn'])
        k.ts('dve', t1[:], a_t[:], pp[:, 9:10], omka[:, 0:1], ALU.mult, ALU.add, reads=['a_t', 'pp', 'omka', 'rn', 'kkn'], writes=['rn'])
        k.tt('pool', kdir[:], k2, t1[:], ALU.mult, reads=['us' + sfx, 'rn', 'kkn'], writes=['kk'])
        k.tt('pool', bvec[:], kkn[:], a_t[:], ALU.mult, reads=['kkn', 'a_t', 'rn'], writes=['a_t'])
        k.stt('dve', At[:], kkn[:], -1.0, Ge[:], ALU.mult, ALU.mult, reads=['kkn', 'Ge'], writes=['At' + sfx])
        k.tt('pool', Bt[:], bvec[:], Ginv[:], ALU.mult, reads=['a_t', 'Ginv'], writes=['Bt' + sfx])
        k.tt('dve', Kt[:], kdir[:], Ginv[:], ALU.mult, reads=['kk', 'Ginv'], writes=['Kt' + sfx])
        k.tt('pool', Rt[:], r2, Gi[:], ALU.mult, reads=['us' + sfx, 'Gi'], writes=['Rt' + sfx])
        k.tt('dve', Bht[:], bvec[:], Gh[:], ALU.mult, reads=['a_t', 'Gh'], writes=['Bht' + sfx])
        k.tt('pool', Kht[:], kdir[:], Gh[:], ALU.mult, reads=['kk', 'Gh'], writes=['Kht' + sfx])
        k.stt('dve', rk[:], r2, pp[:, 10:11], kdir[:], ALU.mult, ALU.mult, reads=['us' + sfx, 'pp', 'kk'], writes=['rk'])
        pb, pn = bank()
        k.mm(pb[0:64, :], lhsT=ones_f[:, 0:64], rhs=rk[:], reads=['ones_f', 'rk'], writes=[pn])
        k.tt('dve', bon[:], pb[0:64, :], us[0:64, 2, :], ALU.mult, reads=[pn, 'us' + sfx], writes=['bon'])
        S.dma('sp', dr['bon_d'][:, tok0:tok0 + 512], bon[:], reads=['bon'], writes=['bon_d'], key='bon')
        k.act(sl[:], us[:, 5, :], AF.Sigmoid, reads=['us' + sfx], writes=['sl'])
        pb, pn = bank()
        k.mm(pb[0:64, :], lhsT=g2h[:], rhs=sl[:], reads=['g2h', 'sl'], writes=[pn])
        k.cp('act', g_t[:], pb[0:64, :], reads=[pn], writes=['g_t'])
        S.dma('sp', dr['g_d'][:, tok0:tok0 + 512], g_t[:], reads=['g_t'], writes=['g_d'], key='gd')


    def stages(b):
        Ub = U[b % 3]; un = 'U%d' % (b % 3)
        tok0 = b * 512
        sfx = str(b % 2)
        At = At2[b % 2]; Bt = Bt2[b % 2]; Kt = Kt2[b % 2]; Rt = Rt2[b % 2]; Bht = Bht2[b % 2]; Kht = Kht2[b % 2]; us = us2[b % 2]
        def scores(dst, dstn, L, Ln, R, Rn, mask_of_dir, ts_layout=False):
            for d in range(2):
                P = slice(64 * d, 64 * d + 64)
                pb, pn = bank()
                for j in range(4):
                    C = slice(j * 128, (j + 1) * 128)
                    k.mm(pb[:, C], lhsT=L[P, C], rhs=R[P, C], reads=[Ln, Rn], writes=[pn])
                mi = mask_of_dir[d]
                k.tt('dve', dst[:, 4 * d:4 * d + 4, :].rearrange("p j n -> p (j n)"), pb[:, :], m4[:, mi, :, :].rearrange("p j n -> p (j n)"), ALU.mult, reads=[pn, 'm4'], writes=[dstn + '_%d' % d])
        scores(SmT[0], 'SmT0', Bt, 'Bt' + sfx, At, 'At' + sfx, {0: 0, 1: 2})
        scores(Sm[0], 'Sm0', At, 'At' + sfx, Bt, 'Bt' + sfx, {0: 2, 1: 0})
        scores(AakT, 'AakT', Kt, 'Kt' + sfx, At, 'At' + sfx, {0: 0, 1: 2})
        scores(TrbT, 'TrbT', Bt, 'Bt' + sfx, Rt, 'Rt' + sfx, {0: 1, 1: 3})
        scores(TrkT, 'TrkT', Kt, 'Kt' + sfx, Rt, 'Rt' + sfx, {0: 1, 1: 3})
        for d in range(2):
            k.tt('pool', Qm[0][:, 4 * d:4 * d + 4, :], SmT[0][:, 4 * d:4 * d + 4, :], id4[:], ALU.add, reads=['SmT0_%d' % d, 'id4'], writes=['Qm0_%d' % d])
        if lvl < 4:
            return
        cur = 0
        for dl in range(1, dr.get('_ndl', 7)):
            nxt = 1 - cur
            sc, scn = Sm[cur], 'Sm%d' % cur
            stc, stcn = SmT[cur], 'SmT%d' % cur
            sn, snn = Sm[nxt], 'Sm%d' % nxt
            stn, stnn = SmT[nxt], 'SmT%d' % nxt
            for d in range(2):
                pb, pn = bank()
                for j in range(4):
                    c8 = 4 * d + j
                    k.mm(pb[:, j * 128:(j + 1) * 128], lhsT=stc[:, c8, :], rhs=sc[:, c8, :], reads=[stcn + '_%d' % d, scn + '_%d' % d], writes=[pn])
                k.cp(dr.get('_e1', 'act'), sn[:, 4 * d:4 * d + 4, :].rearrange("p j n -> p (j n)"), pb[:, :], reads=[pn], writes=[snn + '_%d' % d])
            if dr.get('_sub', 9) < 1:
                break
            if dl < 6:
                for d in range(2):
                    pb, pn = bank()
                    for j in range(4):
                        c8 = 4 * d + j
                        k.mm(pb[:, j * 128:(j + 1) * 128], lhsT=sc[:, c8, :], rhs=stc[:, c8, :], reads=[stcn + '_%d' % d, scn + '_%d' % d], writes=[pn])
                    k.cp('dve', stn[:, 4 * d:4 * d + 4, :].rearrange("p j n -> p (j n)"), pb[:, :], reads=[pn], writes=[stnn + '_%d' % d])
            qc, qcn = Qm[cur], 'Qm%d' % cur
            qn, qnn = Qm[nxt], 'Qm%d' % nxt
            if dr.get('_sub', 9) < 2:
                break
            for d in range(2):
                pb, pn = bank()
                for j in range(4):
                    c8 = 4 * d + j
                    k.mm(pb[:, j * 128:(j + 1) * 128], lhsT=sn[:, c8, :], rhs=qc[:, c8, :], reads=[snn + '_%d' % d, qcn + '_%d' % d], writes=[pn])
                k.tt('dve', qn[:, 4 * d:4 * d + 4, :].rearrange("p j n -> p (j n)"), pb[:, :], qc[:, 4 * d:4 * d + 4, :].rearrange("p j n -> p (j n)"), ALU.add, reads=[pn, qcn + '_%d' % d], writes=[qnn + '_%d' % d])
            cur = nxt
        Qf, Qfn = Qm[cur], 'Qm%d' % cur
        if lvl < 6:
            return
        def tokmajor(dst, dstn, src, srcn, col0, eng):
            pb, pn = bank()
            for j in range(4):
                k.mm(pb[:, j * 128:(j + 1) * 128], lhsT=src[:, j * 128:(j + 1) * 128], rhs=ident, reads=[srcn, 'cst'], writes=[pn])
            pv = pb[:, :].rearrange("p (j d n) -> p j d n", j=4, d=2, n=64)
            for d in range(2):
                k.cp(eng, dst[:, 4 * d:4 * d + 4, col0:col0 + 64], pv[:, :, d, :], reads=[pn], writes=[dstn + '_%d' % d])
        sub = dr.get('_sub', 9)
        if sub in (0, 9):
            tokmajor(AXm, 'AXm', At, 'At' + sfx, 0, 'act' if sub == 9 else 'dve')
        if sub in (1, 9):
            tokmajor(Bh_tok, 'Bh_tok', Bht, 'Bht' + sfx, 0, 'dve')
        if sub in (2, 9):
            tokmajor(Kh_tok, 'Kh_tok', Kht, 'Kht' + sfx, 0, 'act')
        if sub < 9:
            return
        pb, pn = bank()
        for j in range(4):
            k.mm(pb[:, j * 64:(j + 1) * 64], lhsT=us[0:64, 2, j * 128:(j + 1) * 128], rhs=cst[0:64, 0, 0:64], reads=['us' + sfx, 'cst'], writes=[pn])
        k.cp('dve', V_tok[:], pb[:, 0:256].rearrange("p (c n) -> p c n", n=64), reads=[pn], writes=['V_tok'])
        if lvl < 7:
            return
        pb, pn = bank()
        for d in range(2):
            for j in range(4):
                c8 = 4 * d + j
                k.mm(pb[:, c8 * 64:(c8 + 1) * 64], lhsT=AakT[:, c8, :], rhs=V_tok[:, j, :], reads=['AakT_%d' % d, 'V_tok'], writes=[pn])
        k.cp('act', AXm[:, :, 64:128], pb[:, :].rearrange("p (c n) -> p c n", n=64), reads=[pn], writes=['AXm_0', 'AXm_1'])
        if lvl < 8:
            return
        for d in range(2):
            pb, pn = bank()
            for j in range(4):
                c8 = 4 * d + j
                k.mm(pb[:, j * 128:(j + 1) * 128], lhsT=Qf[:, c8, :], rhs=AXm[:, c8, :], reads=[Qfn + '_%d' % d, 'AXm_%d' % d], writes=[pn])
            k.cp('act' if d == 0 else 'dve', WU[:, 4 * d:4 * d + 4, :].rearrange("p j n -> p (j n)"), pb[:, :], reads=[pn], writes=['WU_%d' % d])
        if lvl < 9:
            return
        pb, pn = bank()
        for d in range(2):
            P = slice(64 * d, 64 * d + 64)
            for j in range(4):
                c8 = 4 * d + j
                k.mm(pb[P, j * 128:(j + 1) * 128], lhsT=WU[:, c8, 0:64], rhs=TrbT[:, c8, :], reads=['WU_%d' % d, 'TrbT_%d' % d], writes=[pn])
        k.tt('dve', QhT[:], pb[:, :], Rt[:], ALU.add, reads=[pn, 'Rt' + sfx], writes=['QhT'])
        S.dma('sp', dr['qh_d'][:, tok0:tok0 + 512], QhT[:], reads=['QhT'], writes=['qh_d'], key='qh')
        pb, pn = bank()
        for j in range(4):
            for d in range(2):
                c8 = 4 * d + j
                k.mm(pb[0:64, j * 128:(j + 1) * 128], lhsT=WU[:, c8, 64:128], rhs=TrbT[:, c8, :], start=(d == 0), stop=False, reads=['WU_%d' % d, 'TrbT_%d' % d], writes=[pn])
                k.mm(pb[0:64, j * 128:(j + 1) * 128], lhsT=V_tok[:, j, :], rhs=TrkT[:, c8, :], start=False, stop=(d == 1), reads=['V_tok', 'TrkT_%d' % d], writes=[pn])
        k.cp('act', Oloc[:], pb[0:64, :], reads=[pn], writes=['Oloc'])
        S.dma('sp', dr['ol_d'][:, tok0:tok0 + 512], Oloc[:], reads=['Oloc'], writes=['ol_d'], key='ol')
        pb, pn = bank()
        for d in range(2):
            P = slice(64 * d, 64 * d + 64)
            for j in range(4):
                c8 = 4 * d + j
                k.mm(pb[P, j * 64:(j + 1) * 64], lhsT=WU[:, c8, 0:64], rhs=Bh_tok[:, c8, :], reads=['WU_%d' % d, 'Bh_tok_%d' % d], writes=[pn])
        k.cp('dve', MT_all[0:64, b * 4:(b + 1) * 4, 0:64], pb[0:64, 0:256].rearrange("p (c n) -> p c n", n=64), reads=[pn], writes=['MT_all'])
        for j in range(4):
            st1 = nblk * 4 - 1 - (b * 4 + j)
            k.cp('dve', MT_all[64:128, st1, 64:128], pb[64:128, j * 64:(j + 1) * 64], reads=[pn], writes=['MT_all'])
        pb, pn = bank()
        for d in range(2):
            P = slice(64 * d, 64 * d + 64)
            for j in range(4):
                c8 = 4 * d + j
                k.mm(pb[P, j * 64:(j + 1) * 64], lhsT=Bh_tok[:, c8, :], rhs=WU[:, c8, 64:128], start=True, stop=False, reads=['WU_%d' % d, 'Bh_tok_%d' % d], writes=[pn])
                k.mm(pb[P, j * 64:(j + 1) * 64], lhsT=Kh_tok[:, c8, :], rhs=V_tok[:, j, :], start=False, stop=True, reads=['Kh_tok_%d' % d, 'V_tok'], writes=[pn])
        k.cp('act', N_all[:, b * 4:(b + 1) * 4, :], pb[:, 0:256].rearrange("p (c n) -> p c n", n=64), reads=[pn], writes=['N_all'])

    def run_direct(fn, b, lo, hi):
        st['lo'], st['hi'] = lo, hi
        fn(b)
        st['lo'], st['hi'] = 0, 8

    def cap(fn, b, lo, hi):
        st['lo'], st['hi'] = lo, hi
        lst = S.capture(fn, b)
        st['lo'], st['hi'] = 0, 8
        return lst
    run_direct(project, 0, 0, 3)
    if nblk > 1:
        run_direct(project, 1, 0, 3)
    run_direct(prep, 0, 0, 3)
    for b in range(nblk):
        if b + 2 < nblk:
            run_direct(project, b + 2, 0, 3)
        A = cap(stages, b, *dr.get('_rgA', (3, 8)))
        Bp = cap(prep, b + 1, *dr.get('_rgB', (0, 3))) if b + 1 < nblk else []
        if not dr.get('_int'):
            S.emit_interleaved(A, []); S.emit_interleaved(Bp, [])
        else:
            S.emit_interleaved(A, Bp)
    if lvl < 10:
        return

    nch = nblk * 4
    S.barrier()
    Hf = Gi[:, 0:64]
    T1 = Gi[:, 64:128]
    k.memset('pool', Hf[:], 0.0, writes=['Hf'])
    k.memset('pool', Hh[:, 0, :], 0.0, writes=['Hh0'])
    def t1_for(s_):
        c0 = s_; c1 = nch - 1 - s_
        k.stt('dve', T1[0:64, :], Hf[0:64, :], gamL[0:64, c0:c0 + 1], N_all[0:64, c0, :], ALU.mult, ALU.add, reads=['Hf', 'gamL', 'N_all'], writes=['T1'])
        k.stt('dve', T1[64:128, :], Hf[64:128, :], gamL[64:128, c1:c1 + 1], N_all[64:128, c1, :], ALU.mult, ALU.add, reads=['Hf', 'gamL', 'N_all'], writes=['T1'])
    t1_for(0)
    for s in range(nch):
        pb, pn = bank()
        k.mm(pb[:, 0:64], lhsT=MT_all[:, s, :], rhs=Hh[:, s, :], reads=['MT_all', 'Hh%d' % s], writes=[pn])
        k.tt('dve', Hh[:, s + 1, :], pb[:, 0:64], T1[:], ALU.add, reads=[pn, 'T1'], writes=['Hh%d' % (s + 1)])
        k.tt('dve', Hf[:], pb[:, 0:64], T1[:], ALU.add, reads=[pn, 'T1'], writes=['Hf'])
        if s + 1 < nch:
            t1_for(s + 1)
    if lvl < 11:
        return
    S.barrier()
    qh = [At, Bt]; ol = [Kt[0:64, :], Rt[0:64, :]]; bo = [Bht[0:64, :], Kht[0:64, :]]; gg = [rk[0:64, :], kk2[0:64, :]]
    of = kkn[0:64, :]; ob = tl[0:64, :]; dd = Ginv[0:64, :]; d2 = sl[0:64, :]; rs = Ge[0:64, :]; yy = Gh[0:64, :]
    yo = [bon, g_t]
    mean_m = AakT[0:64, 0, 0:64]
    k.memset('pool', mean_m[:], 1.0 / 64.0, writes=['mean_m'])
    ridx = S.sb("ridx", [128, 2], I32)
    S.dma('sp', ridx[:], dr['ridx'], writes=['ridx'], key='rix')
    S.dma('sp', dr['hh_d'], Hh[:, 0:NCH, :], reads=['Hh%d' % i for i in range(NCH)], writes=['hh_d'], key='shh')
    rows2k = lambda ap: ap.rearrange("p (b n) -> (p b) n", n=TO)
    qh_o = us[:, 0:4, :].rearrange("p a n -> p (a n)")
    ol_o = hT[0][0:64, 0:4, :].rearrange("p a n -> p (a n)"); bo_o = hT[0][0:64, 4:8, :].rearrange("p a n -> p (a n)")
    gg_o = hT[1][0:64, 0:4, :].rearrange("p a n -> p (a n)")
    HhA = hT[1][:, 4:6, :].rearrange("p a n -> p (a n)"); HhB = hT[1][:, 6:8, :].rearrange("p a n -> p (a n)")
    S.gather(qh_o, rows2k(dr['qh_d']), ridx[:, 0:1], reads=['ridx', 'qh_d'], writes=['qh_o'], key='g1')
    S.gather(ol_o, rows2k(dr['ol_d']), ridx[0:64, 0:1], reads=['ridx', 'ol_d'], writes=['ol_o'], key='g2')
    S.gather(bo_o, rows2k(dr['bon_d']), ridx[0:64, 0:1], reads=['ridx', 'bon_d'], writes=['bo_o'], key='g3')
    S.gather(gg_o, rows2k(dr['g_d']), ridx[0:64, 0:1], reads=['ridx', 'g_d'], writes=['gg_o'], key='g4')
    hrows = dr['hh_d'].rearrange("p (b c) n -> (p b) (c n)", c=16)
    S.gather(HhA, hrows, ridx[:, 0:1], reads=['ridx', 'hh_d'], writes=['HhA'], key='g5')
    S.gather(HhB, hrows, ridx[:, 1:2], reads=['ridx', 'hh_d'], writes=['HhB'], key='g6')
    HhA3 = HhA.rearrange("p (c n) -> p c n", n=64); HhB3 = HhB.rearrange("p (c n) -> p c n", n=64)
    for b in range(4):
        i2 = b % 2
        TS = slice(b * 512, (b + 1) * 512)
        pb, pn = bank()
        for j in range(4):
            cl = b * 4 + j
            C = slice(j * 128, (j + 1) * 128)
            k.cp('dve', TrbT[0:64, j, 0:64], HhA3[0:64, cl, :], reads=['HhA'], writes=['Hc'])
            k.cp('pool', TrbT[64:128, j, 0:64], HhB3[64:128, 15 - cl, :], reads=['HhB'], writes=['Hc'])
            k.mm(pb[0:64, C], lhsT=TrbT[:, j, 0:64], rhs=qh_o[:, b * 512 + j * 128:b * 512 + (j + 1) * 128], reads=['Hc', 'qh_o'], writes=[pn])
        k.tt('dve', of[:], pb[0:64, :], ol_o[:, TS], ALU.add, reads=[pn, 'ol_o'], writes=['of'])
        k.cp('act', ob[:], of[:], reads=['of'], writes=['ob'])
        pb, pn = bank()
        k.mm(pb[0:64, :], lhsT=mean_m[:], rhs=ob[:], reads=['mean_m', 'ob'], writes=[pn])
        k.tt('dve', dd[:], of[:], pb[0:64, :], ALU.subtract, reads=['of', pn], writes=['dd'])
        k.act(d2[:], dd[:], AF.Square, reads=['dd'], writes=['d2'])
        pb, pn = bank()
        k.mm(pb[0:64, :], lhsT=mean_m[:], rhs=d2[:], reads=['mean_m', 'd2'], writes=[pn])
        k.rsqrt(rs[:], pb[0:64, :], 1.0, 64e-5, reads=[pn], writes=['rs'])
        k.tt('dve', yy[:], dd[:], rs[:], ALU.mult, reads=['dd', 'rs'], writes=['yy'])
        k.ts('dve', yy[:], yy[:], pp[0:64, 11:12], pp[0:64, 12:13], ALU.mult, ALU.add, reads=['yy', 'pp'], writes=['yy'])
        k.tt('pool', yy[:], yy[:], bo_o[:, TS], ALU.add, reads=['yy', 'bo_o'], writes=['yy'])
        k.tt('pool', yo[i2][:], yy[:], gg_o[:, TS], ALU.mult, reads=['yy', 'gg_o'], writes=['yo%d' % i2])
        S.dma('sp', dr['yrw_own'][:, TS], yo[i2][:], reads=['yo%d' % i2], writes=['yrw_own'], key='sy%d' % i2)


def host_consts():
    p = np.arange(128)[:, None]; f = np.arange(128)[None, :]
    cst = np.stack([(p == f), (p < f), (p <= f), (p > f), (p >= f)]).astype(np.float32)
    return cst


def prep_core(inp, hd):
    hc = slice(hd * 64, (hd + 1) * 64)
    w_in = inp['w_in'][0]
    o = {}
    r_c = np.arange(hd * 64, (hd + 1) * 64)
    cols = np.concatenate([r_c, r_c, 512 + r_c, 512 + r_c, 1024 + r_c, 1024 + r_c,
                           np.arange(1536, 1920),
                           np.arange(1920 + 256, 1920 + 384),
                           1920 + 384 + np.arange(16), 1920 + 384 + np.arange(16),
                           1920 + 400 + np.arange(16), 1920 + 400 + np.arange(16)])
    o['wa'] = np.ascontiguousarray(w_in[:, cols])
    mu = inp['rw_mu'][0]
    pp = np.zeros((128, NPP), np.float32)
    mucols = cols[:768]
    for tI in range(6):
        pp[:, tI] = mu[mucols[tI * 128:(tI + 1) * 128]]
    st2 = lambda v: np.concatenate([v[hc], v[hc]])
    pp[:, 6] = np.concatenate([inp['rw_w0'][0, 0, hc], inp['rw_w0'][0, 1, hc]])
    pp[:, 7] = np.concatenate([inp['rw_a0'][0, 0, hc], inp['rw_a0'][0, 1, hc]])
    pp[:, 8] = st2(inp['rw_k_k'][0]); pp[:, 9] = st2(inp['rw_k_a'][0]); pp[:, 10] = st2(inp['rw_r_k'][0])
    pp[:, 11] = st2(inp['rw_gn_w'][0]); pp[:, 12] = st2(inp['rw_gn_b'][0])
    pp[:, 13:21] = inp['g_mix'][0].reshape(8, 128).T
    o['pp'] = pp
    o['w2s'] = np.ascontiguousarray(np.concatenate([inp['rw_w2'][0, 0][:, hc], inp['rw_w2'][0, 1][:, hc]], 0))
    o['a2s'] = np.ascontiguousarray(np.concatenate([inp['rw_a2'][0, 0][:, hc], inp['rw_a2'][0, 1][:, hc]], 0))
    o['g2h'] = np.ascontiguousarray(inp['rw_g2'][0][:, hc])
    o['w0row'] = np.ascontiguousarray(pp[:, 6][None, :])
    o['cst'] = host_consts()
    return o


TO = 2048
TWO_PI = 6.283185307179586
ATT_SCALE = 96.0 ** -0.5


def make_banks(S, n=8):
    banks = [(S.ps("pb%d" % i, [128, 512], F32), "pb%d" % i) for i in range(n)]
    st = {'b': 0}

    def bank(lo=0, hi=n):
        b = banks[lo + st['b'] % (hi - lo)]
        st['b'] += 1
        return b
    return banks, bank


def norm_T(S, k, bank, x_rows, gcol, bufs, eps=1e-6):
    xt, sq, ss, rstd, xb, h = bufs['xt'], bufs['sq'], bufs['ss'], bufs['rstd'], bufs['xb'], bufs['hT']
    ident = bufs['ident']
    if x_rows is not None:
        S.dma('sp', xt[:], x_rows.rearrange("(j p) d -> p j d", p=128), writes=['xt'], key='xt')
    for j in range(4):
        k.act(sq[:], xt[:, j, :], AF.Square, reads=['xt'], writes=['sq'])
        S.op('dve', lambda e, j=j: e.reduce_sum(out=ss[:, j:j + 1], in_=sq[:], axis=AX.X), reads=['sq'], writes=['ss'])
    k.rsqrt(rstd[:], ss[:], 1.0 / D, eps, reads=['ss'], writes=['rstd'])
    for j in range(4):
        k.ts('dve' if j % 2 else 'pool', xb[:, j, :], xt[:, j, :], rstd[:, j:j + 1], None, ALU.mult, reads=['xt', 'rstd'], writes=['xb'])
    for c in range(8):
        pb, pn = bank()
        for j in range(4):
            k.mm(pb[:, j * 128:(j + 1) * 128], lhsT=xb[:, j, c * 128:(c + 1) * 128], rhs=ident, reads=['xb', 'cst'], writes=[pn])
        k.ts('dve', h[:, c, :], pb[:, :], gcol[:, c:c + 1], None, ALU.mult, reads=[pn, 'pp2'], writes=[bufs.get('hTn', 'hT')])


def load_w_bf16(S, k, dst, dstn, src_ap, stage, stagen, nk, ncols, key):
    for kt in range(nk):
        c0 = 0
        while c0 < ncols:
            w = min(stage.shape[-1], ncols - c0)
            S.dma('sp', stage[:, 0:w], src_ap[kt * 128:(kt + 1) * 128, c0:c0 + w], writes=[stagen], key=key)
            k.cp('dve', dst[:, kt, c0:c0 + w], stage[:, 0:w], reads=[stagen], writes=[dstn])
            c0 += w


def phase_attn(nc, S, k, dr, bank, cm):
    ident = cm['ident']; ones_f = cm['ones_f']; pp2 = cm['pp2']
    S.push()
    bufs = dict(xt=S.sb("xt", [128, 4, 1024], F32), sq=S.sb("sq", [128, 1024], BF16), ss=S.sb("ss", [128, 4], F32),
                rstd=S.sb("rstd", [128, 4], F32), xb=S.sb("xb", [128, 4, 1024], BF16), hT=S.sb("hT", [128, 8, 512], BF16), ident=ident)
    stage = S.sb("stage", [128, 1024], F32)
    wcq = S.sb("wcq", [128, 8, 256], BF16)
    load_w_bf16(S, k, wcq, 'wcq', dr['w_cq'], stage, 'stage', 8, 256, 'wst')
    wq = S.sb("wq", [128, 2, 768], BF16)
    load_w_bf16(S, k, wq, 'wq', dr['w_q'], stage, 'stage', 2, 768, 'wst')
    posi = S.sb("posi", [128, TO], I32)
    S.dma('sp', posi[:], dr['pos'].partition_broadcast(128), writes=['posi'], key='pos')
    ang = S.sb("ang", [128, TO], F32)
    k.cp('dve', ang[:], posi[:], reads=['posi'], writes=['ang'])
    cosT = S.sb("cosT", [128, TO], F32); sinT = S.sb("sinT", [128, TO], F32)
    tnf = S.sb("tnf", [128, TO], F32)
    PI = 3.141592653589793
    k.ts('dve', ang[:], ang[:], pp2[:, 16:17], None, ALU.mult, reads=['ang', 'pp2'], writes=['ang'])
    for (dst, dn, shift) in ((sinT, 'sinT', 0.0), (cosT, 'cosT', PI / 2)):
        k.ts('dve', dst[:], ang[:], shift, 1.0 / TWO_PI, ALU.add, ALU.mult, reads=['ang'], writes=[dn])
        k.cp('dve', posi[:], dst[:], reads=[dn], writes=['posi'])
        k.cp('dve', tnf[:], posi[:], reads=['posi'], writes=['tnf'])
        k.ts('dve', dst[:], ang[:], shift, None, ALU.add, reads=['ang'], writes=[dn])
        k.stt('dve', dst[:], tnf[:], -TWO_PI, dst[:], ALU.mult, ALU.add, reads=['tnf', dn], writes=[dn])
        k.ts('dve', dst[:], dst[:], -PI, PI, ALU.max, ALU.min, reads=[dn], writes=[dn])
        k.act(dst[:], dst[:], AF.Sin, reads=[dn], writes=[dn])
    QT = cm['QT']
    cq = S.sb("cq", [128, 2, 512], F32); cqs = S.sb("cqs", [128, 2, 512], BF16); cqn = S.sb("cqn", [128, 2, 512], BF16)
    rq = S.sb("rq", [128, 512], F32)
    x1s = S.sb("x1s", [128, 512], F32); x2s = S.sb("x2s", [128, 512], F32)
    ta = S.sb("ta", [128, 512], F32); tb = S.sb("tb", [128, 512], F32)
    x1p = S.sb("x1p", [128, 512], BF16); x2p = S.sb("x2p", [128, 512], BF16)
    for blk in range(4):
        T0 = blk * 512
        norm_T(S, k, bank, dr['x'][T0:T0 + 512, :], pp2[:, 0:8], bufs)
        hT = bufs['hT']
        pbs = []
        for t in range(2):
            pb, pn = bank()
            for c in range(8):
                k.mm(pb[:, :], lhsT=wcq[:, c, t * 128:(t + 1) * 128], rhs=hT[:, c, :], start=(c == 0), stop=(c == 7), reads=['wcq', 'hT'], writes=[pn])
            k.cp('act', cq[:, t, :], pb[:, :], reads=[pn], writes=['cq'])
        k.act(cqs[:], cq[:], AF.Square, reads=['cq'], writes=['cqs'])
        pb, pn = bank()
        for t in range(2):
            k.mm(pb[:, :], lhsT=ones_f[:], rhs=cqs[:, t, :], start=(t == 0), stop=(t == 1), reads=['ones_f', 'cqs'], writes=[pn])
        k.rsqrt(rq[:], pb[:, :], 1.0 / 256, 1e-6, reads=[pn], writes=['rq'])
        for t in range(2):
            k.stt('dve', cqn[:, t, :], cq[:, t, :], pp2[:, 8 + t:9 + t], rq[:], ALU.mult, ALU.mult, reads=['cq', 'pp2', 'rq'], writes=['cqn'])
        for hp in range(4):
            pb, pn = bank()
            for t in range(2):
                k.mm(pb[:, :], lhsT=wq[:, t, hp * 128:(hp + 1) * 128], rhs=cqn[:, t, :], start=(t == 0), stop=(t == 1), reads=['wq', 'cqn'], writes=[pn])
            k.cp('act', QT[0:64, 2 * hp, T0:T0 + 512], pb[0:64, :], reads=[pn], writes=['QT'])
            k.cp('dve', QT[0:64, 2 * hp + 1, T0:T0 + 512], pb[64:128, :], reads=[pn], writes=['QT'])
        for (dst, c0) in ((x1s, 512), (x2s, 640)):
            pb, pn = bank()
            for t in range(2):
                k.mm(pb[:, :], lhsT=wq[:, t, c0:c0 + 128], rhs=cqn[:, t, :], start=(t == 0), stop=(t == 1), reads=['wq', 'cqn'], writes=[pn])
            k.cp('act', dst[:], pb[:, :], reads=[pn], writes=['x1s' if c0 == 512 else 'x2s'])
        nc_ = cosT[:, T0:T0 + 512]; ns_ = sinT[:, T0:T0 + 512]
        k.tt('dve', ta[:], x1s[:], nc_, ALU.mult, reads=['x1s', 'cosT'], writes=['ta'])
        k.tt('pool', tb[:], x2s[:], ns_, ALU.mult, reads=['x2s', 'sinT'], writes=['tb'])
        k.tt('dve', x1p[:], ta[:], tb[:], ALU.subtract, reads=['ta', 'tb'], writes=['x1p'])
        k.tt('dve', ta[:], x2s[:], nc_, ALU.mult, reads=['x2s', 'cosT', 'x1p'], writes=['ta'])
        k.tt('pool', tb[:], x1s[:], ns_, ALU.mult, reads=['x1s', 'sinT', 'x1p'], writes=['tb'])
        k.tt('dve', x2p[:], ta[:], tb[:], ALU.add, reads=['ta', 'tb'], writes=['x2p'])
        for h in range(8):
            S.dma('sp', QT[64:80, h, T0:T0 + 512], x1p[h * 16:(h + 1) * 16, :], reads=['x1p'], writes=['QT'], key='qr')
            S.dma('sp', QT[80:96, h, T0:T0 + 512], x2p[h * 16:(h + 1) * 16, :], reads=['x2p'], writes=['QT'], key='qr')
    S.pop()

    S.push()
    ymla = cm['ymlaT']
    Kh = S.sb("Kh", [96, T], BF16)
    Vh = S.sb("Vh", [128, 128, 128], BF16)
    k.memset('pool', Vh[:, :, 64:128], 1.0, writes=['Vh'])
    PT = [S.sb("PT%d" % i, [128, 512], BF16) for i in range(3)]
    osb = S.sb("osb", [128, 512], F32); rden = S.sb("rden", [64, 512], F32)
    nh = dr.get('_nh', 8)
    it = 0
    S.dma('sp', Kh[64:96, :], dr['kr_d'], reads=['kr_d'], writes=['Kh'], key='kh')
    for h in range(nh):
        S.dma('sp', Kh[0:64, :], dr['kTn_d'][h], reads=['kTn_d'], writes=['Kh'], key='kh')
        S.dma('sp', Vh[:, :, 0:64], dr['vtok_d'][h], reads=['vtok_d'], writes=['Vh'], key='vh')
        for qg in range(4):
            acc, accn = cm['banks'][6 + (qg % 2)]
            def tail(pb, pn, kt):
                nonlocal it
                pt = PT[it % 3]; ptn = 'PT%d' % (it % 3); it += 1
                k.act(pt[:], pb[:, :], AF.Exp, scale=ATT_SCALE, reads=[pn], writes=[ptn])
                k.mm(acc[:, :], lhsT=Vh[:, kt, :], rhs=pt[:], start=(kt == 0), stop=(kt == 127), reads=['Vh', ptn], writes=[accn])
            pend = []
            for kt in range(128):
                pb, pn = bank(0, 6)
                k.mm(pb[:, :], lhsT=Kh[:, kt * 128:(kt + 1) * 128], rhs=QT[:, h, qg * 512:(qg + 1) * 512], reads=['Kh', 'QT'], writes=[pn])
                pend.append((pb, pn, kt))
                if len(pend) > 2:
                    tail(*pend.pop(0))
            while pend:
                tail(*pend.pop(0))
            k.cp('dve', osb[:], acc[:, :], reads=[accn], writes=['osb'])
            S.op('dve', lambda e: e.reciprocal(out=osb[64:128, :], in_=osb[64:128, :]), reads=['osb'], writes=['osb'])
            k.cp('dve', rden[:], osb[64:128, :], reads=['osb'], writes=['rden'])
            k.tt('dve', ymla[(h % 2) * 64:(h % 2) * 64 + 64, h // 2, qg * 512:(qg + 1) * 512], osb[0:64, :], rden[:], ALU.mult, reads=['osb', 'rden'], writes=['ymlaT'])
    S.pop()


NPP2 = 48


def common2(nc, S, k, dr):
    cm = {}
    banks, bank = make_banks(S)
    cm['banks'] = banks
    cst_st = S.sb("cst_st", [128, 128], F32)
    S.dma('sp', cst_st[:], dr['cst'][0], writes=['cst_st'], key='c2')
    ident = S.sb("ident", [128, 128], BF16)
    k.cp('dve', ident[:], cst_st[:], reads=['cst_st'], writes=['cst'])
    cm['ident'] = ident[:]
    ones_f = S.sb("ones_f", [128, 128], BF16)
    k.memset('pool', ones_f[:], 1.0, writes=['ones_f'])
    cm['ones_f'] = ones_f
    pp2 = S.sb("pp2", [128, NPP2], F32)
    S.dma('sp', pp2[:], dr['pp2'], writes=['pp2'], key='c1')
    cm['pp2'] = pp2
    epst = S.sb("epst", [128, 4], F32)
    k.epsc = {}
    for i_, ev in enumerate([1e-6, 1e-24, 64e-5]):
        k.memset('pool', epst[:, i_:i_ + 1], ev, writes=['epsc'])
        k.epsc[ev] = epst[:, i_:i_ + 1]
    cm['epsc'] = k.epsc
    negpi = S.sb("negpi", [128, 1], F32)
    k.memset('pool', negpi[:], -3.141592653589793, writes=['negpi'])
    cm['negpi'] = negpi
    return cm, bank


def alloc_attn(S, cm):
    cm['QT'] = S.sb("QT", [96, 8, TO], BF16)
    cm['ymlaT'] = S.sb("ymlaT", [128, 4, TO], BF16)


def prep2_core(inp, c):
    o = {}
    tok = slice(c * TO, (c + 1) * TO)
    w_in = inp['w_in'][0]
    o['x'] = np.ascontiguousarray(inp['x'][0, tok])
    o['pos'] = np.ascontiguousarray(inp['positions'][0, tok]).astype(np.int32)
    o['w_cq'] = np.ascontiguousarray(w_in[:, 1920:2176])
    wq = inp['mla_w_qup'][0].reshape(256, 8, 96)
    o['w_q'] = np.ascontiguousarray(np.concatenate([wq[:, :, 0:64].reshape(256, 512), wq[:, :, 64:80].reshape(256, 128), wq[:, :, 80:96].reshape(256, 128)], 1))
    pp2 = np.zeros((128, NPP2), np.float32)
    pp2[:, 0:8] = inp['g_mix'][0].reshape(8, 128).T
    pp2[:, 8:10] = inp['mla_g_qa'][0].reshape(2, 128).T
    inv = (10000.0 ** (-np.arange(0, 32, 2, dtype=np.float32) / 32)).astype(np.float32)
    pp2[:, 16] = np.tile(inv, 8)
    pp2[:, 17:25] = inp['g_ffn'][0].reshape(8, 128).T
    pp2[:, 25:33] = inp['g_ple'][0].reshape(8, 128).T
    pp2[:, 33:41] = inp['g_final'].reshape(8, 128).T
    pp2[0:64, 41] = np.tile(inv, 4)
    pp2[0:32, 42] = -1.0; pp2[32:64, 42] = 1.0
    pp2[:, 43] = inp['mla_g_kva'][0]
    o['pp2'] = pp2
    o['cst'] = host_consts()
    o['w_gate'] = np.ascontiguousarray(w_in[:, 2336:4384])
    o['w_a'] = np.ascontiguousarray(inp['w_br_rwkv'][0]); o['w_b'] = np.ascontiguousarray(inp['w_br_mla'][0])
    o['w_o'] = np.ascontiguousarray(inp['w_out'][0])
    o['w_pq'] = np.ascontiguousarray(inp['peer_w_q'][0])
    o['sk'] = np.ascontiguousarray(inp['peer_sub_keys'][0].reshape(16, 128, 128))
    o['w_pg'] = np.ascontiguousarray(inp['w_ple_gate'][0]); o['w_pp'] = np.ascontiguousarray(inp['w_ple_proj'][0])
    o['g_fin'] = np.ascontiguousarray(inp['g_final'])
    o['p'] = np.ascontiguousarray(inp['p'][0, 0, tok])
    kr = w_in[:, 1920 + 384:1920 + 416]
    x1c, x2c = kr[:, 0:16], kr[:, 16:32]
    o['w_kvin'] = np.ascontiguousarray(np.concatenate([w_in[:, 1920 + 256:1920 + 384], x1c, x1c, x2c, x2c, x2c, x2c, x1c, x1c], 1))
    wk = inp['mla_w_kvup'][0].reshape(128, 8, 128)
    o['w_kvup'] = np.ascontiguousarray(np.concatenate([wk[:, :, 0:64].reshape(128, 512), wk[:, :, 64:128].reshape(128, 512)], 1))
    o['u_sh'] = np.ascontiguousarray(inp['peer_u'][0, c * 2048:(c + 1) * 2048])
    o['v_sh'] = np.ascontiguousarray(inp['peer_v'][0, c * 2048:(c + 1) * 2048])
    return o


def phase_merge(nc, S, k, dr, bank, cm):
    ident = cm['ident']; pp2 = cm['pp2']
    S.push()
    bufs = dict(xt=S.sb("xt", [128, 4, 1024], F32), sq=S.sb("sq", [128, 1024], BF16), ss=S.sb("ss", [128, 4], F32),
                rstd=S.sb("rstd", [128, 4], F32), xb=S.sb("xb", [128, 4, 1024], BF16), hT=S.sb("hT", [128, 8, 512], BF16), ident=ident)
    stage = S.sb("stage", [128, 1024], F32)
    wg = S.sb("wg", [128, 8, 2048], BF16)
    load_w_bf16(S, k, wg, 'wg', dr['w_gate'], stage, 'stage', 8, 2048, 'wst')
    WA = S.sb("WA", [128, 4, 1024], BF16); WB = S.sb("WB", [128, 4, 1024], BF16); WO = S.sb("WO", [128, 8, 1024], BF16)
    load_w_bf16(S, k, WA, 'WA', dr['w_a'], stage, 'stage', 4, 1024, 'wst')
    load_w_bf16(S, k, WB, 'WB', dr['w_b'], stage, 'stage', 4, 1024, 'wst')
    load_w_bf16(S, k, WO, 'WO', dr['w_o'], stage, 'stage', 8, 1024, 'wst')
    yrw = S.sb("yrw", [128, 4, TO], BF16)
    S.dma('sp', yrw[:], dr['yrw_own'].rearrange("(c p) n -> p c n", p=128), reads=['yrw_own'], writes=['yrw'], key='yrw')
    ymla = cm['ymlaT']
    mT = S.sb("mT", [128, 8, 512], BF16)
    sgA = S.sb("sgA", [128, 512], F32); sgB = S.sb("sgB", [128, 512], F32)
    m1 = S.sb("m1", [128, 512], F32); m2 = S.sb("m2", [128, 512], F32)
    xt = bufs['xt']
    for blk in range(4):
        T0 = blk * 512
        TS = slice(T0, T0 + 512)
        norm_T(S, k, bank, dr['x'][T0:T0 + 512, :], pp2[:, 0:8], bufs)
        hT = bufs['hT']
        for dt in range(8):
            pA, pAn = bank(); pB, pBn = bank(); qA, qAn = bank(); qB, qBn = bank()
            for c in range(8):
                k.mm(pA[:, :], lhsT=wg[:, c, dt * 128:(dt + 1) * 128], rhs=hT[:, c, :], start=(c == 0), stop=(c == 7), reads=['wg', 'hT'], writes=[pAn])
            for c in range(8):
                k.mm(pB[:, :], lhsT=wg[:, c, 1024 + dt * 128:1024 + (dt + 1) * 128], rhs=hT[:, c, :], start=(c == 0), stop=(c == 7), reads=['wg', 'hT'], writes=[pBn])
            for c in range(4):
                k.mm(qA[:, :], lhsT=WA[:, c, dt * 128:(dt + 1) * 128], rhs=yrw[:, c, TS], start=(c == 0), stop=(c == 3), reads=['WA', 'yrw'], writes=[qAn])
            for c in range(4):
                k.mm(qB[:, :], lhsT=WB[:, c, dt * 128:(dt + 1) * 128], rhs=ymla[:, c, TS], start=(c == 0), stop=(c == 3), reads=['WB', 'ymlaT'], writes=[qBn])
            k.act(sgA[:], pA[:, :], AF.Sigmoid, reads=[pAn], writes=['sgA'])
            k.act(sgB[:], pB[:, :], AF.Sigmoid, reads=[pBn], writes=['sgB'])
            k.tt('dve', m1[:], qA[:, :], sgA[:], ALU.mult, reads=[qAn, 'sgA'], writes=['m1'])
            k.tt('dve', m2[:], qB[:, :], sgB[:], ALU.mult, reads=[qBn, 'sgB'], writes=['m2'])
            k.tt('pool', mT[:, dt, :], m1[:], m2[:], ALU.add, reads=['m1', 'm2'], writes=['mT'])
        for j in range(4):
            for hf in range(2):
                pb, pn = bank()
                for m in range(8):
                    k.mm(pb[:, :], lhsT=mT[:, m, j * 128:(j + 1) * 128], rhs=WO[:, m, hf * 512:(hf + 1) * 512], start=(m == 0), stop=(m == 7), reads=['mT', 'WO'], writes=[pn])
                k.tt('dve', xt[:, j, hf * 512:(hf + 1) * 512], pb[:, :], xt[:, j, hf * 512:(hf + 1) * 512], ALU.add, reads=[pn, 'xt'], writes=['xt'])
        S.dma('sp', dr['x1_d'][T0:T0 + 512, :].rearrange("(j p) d -> p j d", p=128), xt[:], reads=['xt'], writes=['x1_d'], key='x1s')
    S.pop()


def phase_peer(nc, S, k, dr, bank, cm):
    ident = cm['ident']; pp2 = cm['pp2']
    banks = cm['banks']
    S.push()
    h2T = S.sb("h2T", [128, 8, TO], BF16)
    S.push()
    bufs = dict(xt=S.sb("xt", [128, 4, 1024], F32), sq=S.sb("sq", [128, 1024], BF16), ss=S.sb("ss", [128, 4], F32),
                rstd=S.sb("rstd", [128, 4], F32), xb=S.sb("xb", [128, 4, 1024], BF16), hT=None, ident=ident)
    stage = S.sb("stage", [128, 1024], F32)
    wpq = S.sb("wpq", [128, 8, 2048], BF16)
    load_w_bf16(S, k, wpq, 'wpq', dr['w_pq'], stage, 'stage', 8, 2048, 'wst')
    skb = S.sb("skb", [128, 16, 128], BF16)
    skT = S.sb("skT", [128, 16, 128], BF16)
    for g4 in range(4):
        S.dma('sp', stage[:, 0:512].rearrange("p (a n) -> p a n", n=128), dr['sk'][g4 * 4:(g4 + 1) * 4].rearrange("a p n -> p a n"), writes=['stage'], key='wst')
        k.cp('dve', skb[:, g4 * 4:(g4 + 1) * 4, :], stage[:, 0:512].rearrange("p (a n) -> p a n", n=128), reads=['stage'], writes=['skb'])
    for g4 in range(4):
        pb, pn = bank()
        for a in range(4):
            k.mm(pb[:, a * 128:(a + 1) * 128], lhsT=skb[:, g4 * 4 + a, :], rhs=ident, reads=['skb', 'cst'], writes=[pn])
        k.cp('dve', skT[:, g4 * 4:(g4 + 1) * 4, :].rearrange("p a n -> p (a n)"), pb[:, :], reads=[pn], writes=['skT'])
    qpT = [S.sb("qpT%d" % i, [128, 512], BF16) for i in range(2)]
    s_sb = S.sb("s_sb", [128, 4, 16, 128], F32)
    for blk in range(4):
        T0 = blk * 512
        bufs['hT'] = h2T[:, :, T0:T0 + 512]
        norm_T(S, k, (lambda: bank(0, 4)), dr['x1_d'][T0:T0 + 512, :], pp2[:, 17:25], bufs)
        hT = bufs['hT']
        for hc in range(16):
            pb, pn = bank(0, 4)
            for c in range(8):
                k.mm(pb[:, :], lhsT=wpq[:, c, hc * 128:(hc + 1) * 128], rhs=hT[:, c, :], start=(c == 0), stop=(c == 7), reads=['wpq', 'hT'], writes=[pn])
            qp = qpT[hc % 2]; qpn = 'qpT%d' % (hc % 2)
            k.cp('act', qp[:], pb[:, :], reads=[pn], writes=[qpn])
            for j in range(4):
                sb_, sn_ = banks[4 + j]
                k.mm(sb_[:, (hc % 4) * 128:(hc % 4 + 1) * 128], lhsT=qp[:, j * 128:(j + 1) * 128], rhs=skT[:, hc, :], reads=[qpn, 'skT'], writes=[sn_])
            if hc % 4 == 3:
                for j in range(4):
                    sb_, sn_ = banks[4 + j]
                    k.cp('dve' if j % 2 else 'act', s_sb[:, j, hc - 3:hc + 1, :].rearrange("p a n -> p (a n)"), sb_[:, :], reads=[sn_], writes=['s_sb'])
        S.dma('sp', dr['s_d'][T0:T0 + 512].rearrange("(j p) a n -> p j a n", p=128), s_sb[:], reads=['s_sb'], writes=['s_d'], key='ssd')
    S.pop()

    S.push()
    st = S.sb("st", [128, 16, 128], F32)
    m16 = S.sb("m16", [128, 16, 16], F32)
    tmp = S.sb("tmp", [128, 256], F32)
    cand = S.sb("cand", [128, 8, 256], F32)
    top16 = S.sb("top16", [128, 8, 16], F32)
    thr = S.sb("thr", [128, 8], F32); mx = S.sb("mx", [128, 8], F32); negm = S.sb("negm", [128, 8], F32)
    e16 = S.sb("e16", [128, 8, 16], F32); Zs = S.sb("Zs", [128, 8], F32); rZ = S.sb("rZ", [128, 8], F32)
    Gb = [S.sb("G%d" % i, [128, 16384], BF16) for i in range(2)]
    RC = 16
    Cb = [S.sb("Cb%d" % i, [128, RC, 128], F32) for i in range(2)]
    Eb = [S.sb("Eb%d" % i, [128, RC, 128], BF16) for i in range(2)]
    Mb = [S.sb("Mb%d" % i, [128, RC, 128], BF16) for i in range(2)]
    UTg = [S.sb("UTg%d" % i, [128, 8, 512], BF16) for i in range(2)]
    Vg = [S.sb("Vg%d" % i, [128, 4, 1024], BF16) for i in range(3)]
    a_sb = [S.sb("a_sb%d" % i, [128, 512], F32) for i in range(2)]
    ga = [S.sb("ga%d" % i, [128, 512], BF16) for i in range(2)]
    gaT = [S.sb("gaT%d" % i, [128, 4, 128], BF16) for i in range(2)]
    x1t = S.sb("x1t", [128, 1024], F32)
    ntile = dr.get('_ntile', 16)
    cnt = {'c': 0}

    def topk(nt):
        N0 = nt * 128
        S.dma('sp', st[:], dr['s_d'][N0:N0 + 128], reads=['s_d'], writes=['st'], key='lst')
        for hc in range(16):
            S.op('dve', lambda e, hc=hc: e.max(out=m16[:, hc, 0:8], in_=st[:, hc, :]), reads=['st'], writes=['m16'])
            S.op('dve', lambda e, hc=hc: e.match_replace(out=tmp[:, 0:128], in_to_replace=m16[:, hc, 0:8], in_values=st[:, hc, :], imm_value=-1e30), reads=['st', 'm16'], writes=['tmp'])
            S.op('dve', lambda e, hc=hc: e.max(out=m16[:, hc, 8:16], in_=tmp[:, 0:128]), reads=['tmp'], writes=['m16'])
        for h in range(8):
            k.tt('pool', cand[:, h, :].rearrange("p (a b) -> p a b", b=16),
                 m16[:, 2 * h, :].unsqueeze(2).to_broadcast([128, 16, 16]),
                 m16[:, 2 * h + 1, :].unsqueeze(1).to_broadcast([128, 16, 16]), ALU.add, reads=['m16'], writes=['cand'])
        for h in range(8):
            S.op('dve', lambda e, h=h: e.max(out=top16[:, h, 0:8], in_=cand[:, h, :]), reads=['cand'], writes=['top16'])
            S.op('dve', lambda e, h=h: e.match_replace(out=tmp[:, :], in_to_replace=top16[:, h, 0:8], in_values=cand[:, h, :], imm_value=-1e30), reads=['cand', 'top16'], writes=['tmp'])
            S.op('dve', lambda e, h=h: e.max(out=top16[:, h, 8:16], in_=tmp[:, :]), reads=['tmp'], writes=['top16'])
        S.op('dve', lambda e: e.tensor_reduce(out=thr[:], in_=top16[:], axis=AX.X, op=ALU.min), reads=['top16'], writes=['thr'])
        S.op('dve', lambda e: e.tensor_reduce(out=mx[:], in_=top16[:], axis=AX.X, op=ALU.max), reads=['top16'], writes=['mx'])
        k.ts('dve', negm[:], mx[:], -1.0, None, ALU.mult, reads=['mx'], writes=['negm'])
        for h in range(8):
            k.act(e16[:, h, :], top16[:, h, :], AF.Exp, bias=negm[:, h:h + 1], reads=['top16', 'negm'], writes=['e16'])
        S.op('dve', lambda e: e.reduce_sum(out=Zs[:], in_=e16[:], axis=AX.X), reads=['e16'], writes=['Zs'])
        S.op('dve', lambda e: e.reciprocal(out=rZ[:], in_=Zs[:]), reads=['Zs'], writes=['rZ'])

    def gbuild(nt):
        G = Gb[nt % 2]; gn = 'G%d' % (nt % 2)
        k.memset('pool', G[:], 0.0, writes=[gn])
        yield
        for h in range(8):
            for ic in range(128 // RC):
                b2 = cnt['c'] % 2; cnt['c'] += 1
                C = Cb[b2]; E = Eb[b2]; M = Mb[b2]
                cn, en, mn = 'Cb%d' % b2, 'Eb%d' % b2, 'Mb%d' % b2
                k.tt('pool', C[:], st[:, 2 * h, ic * RC:(ic + 1) * RC].unsqueeze(2).to_broadcast([128, RC, 128]),
                     st[:, 2 * h + 1, :].unsqueeze(1).to_broadcast([128, RC, 128]), ALU.add, reads=['st'], writes=[cn])
                k.act(E[:], C[:], AF.Exp, bias=negm[:, h:h + 1], reads=[cn, 'negm'], writes=[en])
                k.stt('dve', M[:], C[:], thr[:, h:h + 1], E[:], ALU.is_ge, ALU.mult, reads=[cn, en, 'thr'], writes=[mn])
                Gs = G[:, ic * RC * 128:(ic + 1) * RC * 128].rearrange("p (a b) -> p a b", b=128)
                k.stt('dve', Gs, M[:], rZ[:, h:h + 1], Gs, ALU.mult, ALU.add, reads=[mn, 'rZ', gn], writes=[gn])
                yield

    def dense(nt):
        N0 = nt * 128
        G = Gb[nt % 2]; gn = 'G%d' % (nt % 2)
        acc = [banks[6], banks[7]]
        pre = {}; tr = {}

        def stA(eg):
            b2 = eg % 2; v3 = eg % 3
            if not (dr.get('_nodma') and (nt > 0 or eg > 2)):
                S.dma('sp', UTg[b2][:], dr['UT'][:, :, eg * 512:(eg + 1) * 512], reads=['UT'], writes=['UTg%d' % b2], key='ut%d' % b2)
                S.dma('sp', Vg[v3][:], dr['Vb'][eg * 512:(eg + 1) * 512, :].rearrange("(q p) d -> p q d", p=128), reads=['Vb'], writes=['Vg%d' % v3], key='vg%d' % v3)
            pb, pn = bank(0, 3)
            for c in range(8):
                k.mm(pb[:, :], lhsT=h2T[:, c, N0:N0 + 128], rhs=UTg[b2][:, c, :], start=(c == 0), stop=(c == 7), reads=['h2T', 'UTg%d' % b2], writes=[pn])
            k.act(a_sb[b2][:], pb[:, :], AF.Gelu, reads=[pn], writes=['a_sb%d' % b2])
            k.tt('dve', ga[b2][:], a_sb[b2][:], G[:, eg * 512:(eg + 1) * 512], ALU.mult, reads=['a_sb%d' % b2, gn], writes=['ga%d' % b2])

        def stB(eg):
            b2 = eg % 2
            pt, ptn = bank(3, 6)
            for q in range(4):
                k.mm(pt[:, q * 128:(q + 1) * 128], lhsT=ga[b2][:, q * 128:(q + 1) * 128], rhs=ident, reads=['ga%d' % b2, 'cst'], writes=[ptn])
            k.cp('act', gaT[b2][:].rearrange("p q n -> p (q n)"), pt[:, :], reads=[ptn], writes=['gaT%d' % b2])

        def stC(eg):
            b2 = eg % 2; v3 = eg % 3
            for q in range(4):
                for hf in range(2):
                    k.mm(acc[hf][0][:, :], lhsT=gaT[b2][:, q, :], rhs=Vg[v3][:, q, hf * 512:(hf + 1) * 512],
                         start=(eg == 0 and q == 0), stop=(eg == 31 and q == 3), reads=['gaT%d' % b2, 'Vg%d' % v3], writes=[acc[hf][1]])
        for g in range(34):
            if g < 32:
                stA(g)
            if 0 <= g - 1 < 32:
                stB(g - 1)
            if 0 <= g - 2 < 32:
                stC(g - 2)
            yield
        S.dma('sp', x1t[:], dr['x1_d'][N0:N0 + 128, :], reads=['x1_d'], writes=['x1t'], key='lx1')
        for hf in range(2):
            k.tt('dve', x1t[:, hf * 512:(hf + 1) * 512], acc[hf][0][:, :], x1t[:, hf * 512:(hf + 1) * 512], ALU.add, reads=[acc[hf][1], 'x1t'], writes=['x1t'])
        S.dma('sp', dr['x2_d'][N0:N0 + 128, :], x1t[:], reads=['x1t'], writes=['x2_d'], key='sx2')
        yield

    topk(0)
    if dr.get('_nog'):
        def gbuild(nt):
            yield
    for _ in gbuild(0):
        pass
    for nt in range(ntile):
        gb = None
        if nt + 1 < ntile:
            topk(nt + 1)
            gb = gbuild(nt + 1)
        for _ in dense(nt):
            if gb is not None:
                for _r in range(2):
                    try:
                        next(gb)
                    except StopIteration:
                        gb = None
                        break
        if gb is not None:
            for _ in gb:
                pass
    S.pop()
    S.pop()


def phase_final(nc, S, k, dr, bank, cm):
    ident = cm['ident']; pp2 = cm['pp2']
    S.push()
    bufs = dict(xt=S.sb("xt", [128, 4, 1024], F32), sq=S.sb("sq", [128, 1024], BF16), ss=S.sb("ss", [128, 4], F32),
                rstd=S.sb("rstd", [128, 4], F32), xb=S.sb("xb", [128, 4, 1024], BF16), hT=S.sb("hT", [128, 8, 512], BF16), ident=ident)
    stage = S.sb("stage", [128, 1024], F32)
    Wpg = S.sb("Wpg", [128, 8, 1024], BF16); Wpp = S.sb("Wpp", [128, 2, 1024], BF16)
    load_w_bf16(S, k, Wpg, 'Wpg', dr['w_pg'], stage, 'stage', 8, 1024, 'wst')
    load_w_bf16(S, k, Wpp, 'Wpp', dr['w_pp'], stage, 'stage', 2, 1024, 'wst')
    gfin = S.sb("gfin", [128, 1024], F32)
    S.dma('sp', gfin[:], dr['g_fin'].partition_broadcast(128), writes=['gfin'], key='gf')
    pt = S.sb("pt", [128, 4, 256], F32); pb16 = S.sb("pb16", [128, 4, 256], BF16); pT = S.sb("pT", [128, 2, 512], BF16)
    sg = S.sb("sg", [128, 512], F32); tq = S.sb("tq", [128, 512], F32)
    sq2 = S.sb("sq2", [128, 1024], F32); ss2 = S.sb("ss2", [128, 4], F32); rs2 = S.sb("rs2", [128, 4], F32)
    ot = S.sb("ot", [128, 4, 1024], F32)
    xt = bufs['xt']
    for blk in range(4):
        T0 = blk * 512
        norm_T(S, k, bank, dr['x2_d'][T0:T0 + 512, :], pp2[:, 25:33], bufs)
        hT = bufs['hT']
        S.dma('sp', pt[:], dr['p'][T0:T0 + 512, :].rearrange("(j p) d -> p j d", p=128), writes=['pt'], key='lp')
        k.cp('pool', pb16[:], pt[:], reads=['pt'], writes=['pb16'])
        for kt in range(2):
            pb, pn = bank()
            for j in range(4):
                k.mm(pb[:, j * 128:(j + 1) * 128], lhsT=pb16[:, j, kt * 128:(kt + 1) * 128], rhs=ident, reads=['pb16', 'cst'], writes=[pn])
            k.cp('act', pT[:, kt, :], pb[:, :], reads=[pn], writes=['pT'])
        for j in range(4):
            for hf in range(2):
                HS = slice(hf * 512, (hf + 1) * 512)
                pg, pgn = bank(); pq, pqn = bank()
                for c in range(8):
                    k.mm(pg[:, :], lhsT=hT[:, c, j * 128:(j + 1) * 128], rhs=Wpg[:, c, HS], start=(c == 0), stop=(c == 7), reads=['hT', 'Wpg'], writes=[pgn])
                for kt in range(2):
                    k.mm(pq[:, :], lhsT=pT[:, kt, j * 128:(j + 1) * 128], rhs=Wpp[:, kt, HS], start=(kt == 0), stop=(kt == 1), reads=['pT', 'Wpp'], writes=[pqn])
                k.act(sg[:], pg[:, :], AF.Sigmoid, reads=[pgn], writes=['sg'])
                k.tt('dve', tq[:], pq[:, :], sg[:], ALU.mult, reads=[pqn, 'sg'], writes=['tq'])
                k.tt('pool', xt[:, j, HS], xt[:, j, HS], tq[:], ALU.add, reads=['xt', 'tq'], writes=['xt'])
        for j in range(4):
            k.act(sq2[:], xt[:, j, :], AF.Square, reads=['xt'], writes=['sq2'])
            S.op('dve', lambda e, j=j: e.reduce_sum(out=ss2[:, j:j + 1], in_=sq2[:], axis=AX.X), reads=['sq2'], writes=['ss2'])
        k.rsqrt(rs2[:], ss2[:], 1.0 / D, 1e-6, reads=['ss2'], writes=['rs2'])
        for j in range(4):
            k.stt('dve', ot[:, j, :], xt[:, j, :], rs2[:, j:j + 1], gfin[:], ALU.mult, ALU.mult, reads=['xt', 'rs2', 'gfin'], writes=['ot'])
        S.dma('sp', dr['out'][T0:T0 + 512, :].rearrange("(j p) d -> p j d", p=128), ot[:], reads=['ot'], writes=['out'], key='so')
    S.pop()


def phase_h(nc, S, k, dr, bank, cm):
    ident = cm['ident']; pp2 = cm['pp2']
    S.push()
    bufs = dict(xt=S.sb("xt", [128, 4, 1024], F32), sq=S.sb("sq", [128, 1024], BF16), ss=S.sb("ss", [128, 4], F32),
                rstd=S.sb("rstd", [128, 4], F32), xb=S.sb("xb", [128, 4, 1024], BF16), hT=None, ident=ident)
    hTb = [S.sb("hTb%d" % i, [128, 8, 512], BF16) for i in range(2)]
    stage = S.sb("stage", [128, 1024], F32)
    wsh = S.sb("wsh", [128, 8, 384], BF16)
    load_w_bf16(S, k, wsh, 'wsh', dr['w_sh'], stage, 'stage', 8, 384, 'wst')
    ush = [S.sb("ush%d" % i, [128, 3, 512], BF16) for i in range(2)]
    for blk in range(NB):
        T0 = blk * 512
        b2 = blk % 2
        bufs['hT'] = hTb[b2]; bufs['hTn'] = 'hTb%d' % b2
        norm_T(S, k, bank, dr['x'][T0:T0 + 512, :], pp2[:, 0:8], bufs)
        S.dma('sp', dr['hT_d'][:, :, T0:T0 + 512], hTb[b2][:], reads=['hTb%d' % b2], writes=['hT_d'], key='sh%d' % b2)
        for tI in range(3):
            pb, pn = bank()
            for c in range(8):
                k.mm(pb[:, :], lhsT=wsh[:, c, tI * 128:(tI + 1) * 128], rhs=hTb[b2][:, c, :], start=(c == 0), stop=(c == 7), reads=['wsh', 'hTb%d' % b2], writes=[pn])
            k.cp('act' if tI % 2 else 'dve', ush[b2][:, tI, :], pb[:, :], reads=[pn], writes=['ush%d' % b2])
        S.dma('sp', dr['ush_d'][:, :, T0:T0 + 512].rearrange("a p n -> p a n"), ush[b2][:], reads=['ush%d' % b2], writes=['ush_d'], key='su%d' % b2)
    S.pop()


def phase_kv(nc, S, k, dr, bank, cm):
    ident = cm['ident']; pp2 = cm['pp2']; ones_f = cm['ones_f']
    S.push()
    hTk = [S.sb("hTk%d" % i, [128, 8, 512], BF16) for i in range(2)]
    stage = S.sb("stage", [128, 1024], F32)
    wki = S.sb("wki", [128, 8, 256], BF16)
    load_w_bf16(S, k, wki, 'wki', dr['w_kvin'], stage, 'stage', 8, 256, 'wst')
    wku = S.sb("wku", [128, 1, 1024], BF16)
    load_w_bf16(S, k, wku, 'wku', dr['w_kvup'], stage, 'stage', 1, 1024, 'wst')
    posi = S.sb("posi", [64, 512], I32); ang = S.sb("ang", [64, 512], F32)
    cosT = S.sb("cosT", [64, 512], F32); sinT = S.sb("sinT", [64, 512], F32); tnf = S.sb("tnf", [64, 512], F32)
    PI = 3.141592653589793
    cks = S.sb("cks", [128, 512], F32); ck2 = S.sb("ck2", [128, 512], BF16); rk_ = S.sb("rk_", [128, 512], F32); ckn = S.sb("ckn", [128, 512], BF16)
    krA = S.sb("krA", [64, 512], F32); krB = S.sb("krB", [64, 512], F32); krR = S.sb("krR", [64, 512], BF16)
    kTs = [S.sb("kTs%d" % i, [128, 512], BF16) for i in range(2)]
    vts = [S.sb("vts%d" % i, [128, 512], BF16) for i in range(2)]
    for blk in range(NB):
        T0 = blk * 512
        S.dma('sp', posi[:], dr['pos_all'][T0:T0 + 512].partition_broadcast(64), writes=['posi'], key='pos')
        k.cp('dve', ang[:], posi[:], reads=['posi'], writes=['ang'])
        k.ts('dve', ang[:], ang[:], pp2[0:64, 41:42], None, ALU.mult, reads=['ang', 'pp2'], writes=['ang'])
        for (dst, dn, shift) in ((sinT, 'sinT', 0.0), (cosT, 'cosT', PI / 2)):
            k.ts('dve', dst[:], ang[:], shift, 1.0 / TWO_PI, ALU.add, ALU.mult, reads=['ang'], writes=[dn])
            k.cp('dve', posi[:], dst[:], reads=[dn], writes=['posi'])
            k.cp('dve', tnf[:], posi[:], reads=['posi'], writes=['tnf'])
            k.ts('dve', dst[:], ang[:], shift, None, ALU.add, reads=['ang'], writes=[dn])
            k.stt('dve', dst[:], tnf[:], -TWO_PI, dst[:], ALU.mult, ALU.add, reads=['tnf', dn], writes=[dn])
            k.ts('dve', dst[:], dst[:], -PI, PI, ALU.max, ALU.min, reads=[dn], writes=[dn])
            k.act(dst[:], dst[:], AF.Sin, reads=[dn], writes=[dn])
        k.ts('dve', sinT[:], sinT[:], pp2[0:64, 42:43], None, ALU.mult, reads=['sinT', 'pp2'], writes=['sinT'])
        hT = hTk[blk % 2]; hTn = 'hTk%d' % (blk % 2)
        S.dma('sp', hT[:], dr['hT_d'][:, :, T0:T0 + 512], reads=['hT_d'], writes=[hTn], key='lh%d' % (blk % 2))
        pb, pn = bank()
        for c in range(8):
            k.mm(pb[:, :], lhsT=wki[:, c, 0:128], rhs=hT[:, c, :], start=(c == 0), stop=(c == 7), reads=['wki', hTn], writes=[pn])
        k.cp('act', cks[:], pb[:, :], reads=[pn], writes=['cks'])
        for (dst, dn, c0) in ((krA, 'krA', 128), (krB, 'krB', 192)):
            pb, pn = bank()
            for c in range(8):
                k.mm(pb[0:64, :], lhsT=wki[:, c, c0:c0 + 64], rhs=hT[:, c, :], start=(c == 0), stop=(c == 7), reads=['wki', hTn], writes=[pn])
            k.cp('act', dst[:], pb[0:64, :], reads=[pn], writes=[dn])
        k.act(ck2[:], cks[:], AF.Square, reads=['cks'], writes=['ck2'])
        pb, pn = bank()
        k.mm(pb[:, :], lhsT=ones_f[:], rhs=ck2[:], reads=['ones_f', 'ck2'], writes=[pn])
        k.rsqrt(rk_[:], pb[:, :], 1.0 / 128, 1e-6, reads=[pn], writes=['rk_'])
        k.stt('dve', ckn[:], cks[:], pp2[:, 43:44], rk_[:], ALU.mult, ALU.mult, reads=['cks', 'pp2', 'rk_'], writes=['ckn'])
        for hp in range(4):
            pb, pn = bank()
            k.mm(pb[:, :], lhsT=wku[:, 0, hp * 128:(hp + 1) * 128], rhs=ckn[:], reads=['wku', 'ckn'], writes=[pn])
            kt_ = kTs[hp % 2]; ktn = 'kTs%d' % (hp % 2)
            k.cp('act' if hp % 2 else 'dve', kt_[:], pb[:, :], reads=[pn], writes=[ktn])
            S.dma('sp', dr['kTn_d'][2 * hp, :, T0:T0 + 512], kt_[0:64, :], reads=[ktn], writes=['kTn_d'], key='sk%d' % (hp % 2))
            S.dma('sp', dr['kTn_d'][2 * hp + 1, :, T0:T0 + 512], kt_[64:128, :], reads=[ktn], writes=['kTn_d'], key='sk%d' % (hp % 2))
        for j in range(4):
            pb, pn = bank()
            k.mm(pb[:, :], lhsT=ckn[:, j * 128:(j + 1) * 128], rhs=wku[:, 0, 512:1024], reads=['wku', 'ckn'], writes=[pn])
            vt_ = vts[j % 2]; vtn = 'vts%d' % (j % 2)
            k.cp('act' if j % 2 else 'dve', vt_[:], pb[:, :], reads=[pn], writes=[vtn])
            S.dma('sp', dr['vtok_d'][:, :, blk * 4 + j, :].rearrange("h p d -> p h d"), vt_[:].rearrange("p (h d) -> p h d", d=64), reads=[vtn], writes=['vtok_d'], key='sv%d' % (j % 2))
        k.tt('dve', krA[:], krA[:], cosT[:], ALU.mult, reads=['krA', 'cosT'], writes=['krA'])
        k.tt('pool', krB[:], krB[:], sinT[:], ALU.mult, reads=['krB', 'sinT'], writes=['krB'])
        k.tt('dve', krR[:], krA[:], krB[:], ALU.add, reads=['krA', 'krB'], writes=['krR'])
        S.dma('sp', dr['kr_d'][0:16, T0:T0 + 512], krR[0:16, :], reads=['krR'], writes=['kr_d'], key='skr')
        S.dma('sp', dr['kr_d'][16:32, T0:T0 + 512], krR[32:48, :], reads=['krR'], writes=['kr_d'], key='skr')
    S.pop()


def phase_experts(nc, S, k, dr, bank, cm):
    ident = cm['ident']
    S.push()
    uf = [S.sb("uf%d" % i, [128, 1024], F32) for i in range(2)]
    ub = S.sb("ub", [128, 1024], BF16)
    utt = [S.sb("utt%d" % i, [128, 8, 128], BF16) for i in range(2)]
    vf = [S.sb("vf%d" % i, [128, 1024], F32) for i in range(2)]
    vb = [S.sb("vb%d" % i, [128, 1024], BF16) for i in range(2)]
    for et in range(128):
        b2 = et % 2
        S.dma('sp', uf[b2][:], dr['u_sh'][et * 128:(et + 1) * 128, :], writes=['uf%d' % b2], key='lu%d' % b2)
        k.cp('dve', ub[:], uf[b2][:], reads=['uf%d' % b2], writes=['ub'])
        for g in range(2):
            pb, pn = bank()
            for c4 in range(4):
                c = g * 4 + c4
                k.mm(pb[:, c4 * 128:(c4 + 1) * 128], lhsT=ub[:, c * 128:(c + 1) * 128], rhs=ident, reads=['ub', 'cst'], writes=[pn])
            k.cp('act', utt[b2][:, g * 4:(g + 1) * 4, :].rearrange("p a n -> p (a n)"), pb[:, :], reads=[pn], writes=['utt%d' % b2])
        S.dma('sp', dr['UT'][:, :, et * 128:(et + 1) * 128], utt[b2][:], reads=['utt%d' % b2], writes=['UT'], key='su%d' % b2)
        S.dma('sp', vf[b2][:], dr['v_sh'][et * 128:(et + 1) * 128, :], writes=['vf%d' % b2], key='lv%d' % b2)
        k.cp('pool', vb[b2][:], vf[b2][:], reads=['vf%d' % b2], writes=['vb%d' % b2])
        S.dma('sp', dr['Vb'][et * 128:(et + 1) * 128, :], vb[b2][:], reads=['vb%d' % b2], writes=['Vb'], key='svb%d' % b2)
    S.pop()


F_IN = [('x', [T, D], F32), ('pos_all', [T], I32),
        ('wa_all', [8, 1024, 384], F32), ('w_sh', [1024, 384], F32), ('pp_all', [8, 128, NPP], F32), ('w2s_all', [8, 128, 64], F32), ('a2s_all', [8, 128, 64], F32),
        ('g2h_all', [8, 128, 64], F32), ('w0row_all', [8, 1, 128], F32), ('cst', [5, 128, 128], F32),
        ('pp2', [128, NPP2], F32), ('w_kvin', [1024, 256], F32), ('w_kvup', [128, 1024], F32), ('u_sh', [16384, 1024], F32), ('v_sh', [16384, 1024], F32),
        ('x_own', [TO, D], F32), ('pos', [TO], I32), ('p', [TO, 256], F32), ('ridx', [128, 2], I32),
        ('w_cq', [1024, 256], F32), ('w_q', [256, 768], F32), ('w_gate', [1024, 2048], F32), ('w_a', [512, 1024], F32), ('w_b', [512, 1024], F32),
        ('w_o', [1024, 1024], F32), ('w_pq', [1024, 2048], F32), ('sk', [16, 128, 128], F32), ('w_pg', [1024, 1024], F32), ('w_pp', [256, 1024], F32),
        ('g_fin', [1024], F32)]


def build_nc():
    nc = bass.Bass("TRN2", target_bir_lowering=False)
    dr = {}
    for nm, sh, dt in F_IN:
        dr[nm] = nc.dram_tensor(nm, list(sh), dt, kind="ExternalInput").ap()
    for nm, sh in [('bon_d', [64, T]), ('g_d', [64, T]), ('qh_d', [128, T]), ('ol_d', [64, T]), ('yrw_own', [512, TO]), ('hh_d', [128, NCH, 64]), ('hT_d', [128, 8, T]), ('ush_d', [3, 128, T]),
                   ('kTn_d', [8, 64, T]), ('kr_d', [32, T]), ('vtok_d', [8, 128, 128, 64]), ('UT', [128, 8, 16384]), ('Vb', [16384, 1024])]:
        dr[nm] = nc.dram_tensor(nm, sh, BF16, kind="Internal").ap()
    dr['x1_d'] = nc.dram_tensor('x1_d', [TO, D], F32, kind="Internal").ap()
    dr['x2_d'] = nc.dram_tensor('x2_d', [TO, D], F32, kind="Internal").ap()
    dr['s_d'] = nc.dram_tensor('s_d', [TO, 16, 128], F32, kind="Internal").ap()
    dr['out'] = nc.dram_tensor('out', [TO, D], F32, kind="ExternalOutput").ap()
    S = Sched(nc)
    with S:
        k = K(S)
        cm, bank = common2(nc, S, k, dr)
        phase_h(nc, S, k, dr, bank, cm)
        for hd in range(8):
            d2 = dict(dr)
            d2['wa'] = dr['wa_all'][hd]; d2['pp'] = dr['pp_all'][hd]; d2['w2s'] = dr['w2s_all'][hd]; d2['a2s'] = dr['a2s_all'][hd]
            d2['g2h'] = dr['g2h_all'][hd]; d2['w0row'] = dr['w0row_all'][hd]
            d2['yrw_own'] = dr['yrw_own'][hd * 64:(hd + 1) * 64, :]
            d2['_banks'] = cm['banks']
            S.push()
            rwkv_phase(nc, S, k, d2)
            S.pop()
            k.epsc = cm['epsc']
        phase_kv(nc, S, k, dr, bank, cm)
        phase_experts(nc, S, k, dr, bank, cm)
        dr2 = dict(dr); dr2['x'] = dr['x_own']
        S.push()
        alloc_attn(S, cm)
        phase_attn(nc, S, k, dr2, bank, cm)
        phase_merge(nc, S, k, dr2, bank, cm)
        S.pop()
        phase_peer(nc, S, k, dr2, bank, cm)
        phase_final(nc, S, k, dr2, bank, cm)
        S.finish()
    return nc


def kernel(**inputs):
    inp = {k_: np.asarray(v) for k_, v in inputs.items()}
    x2d = np.ascontiguousarray(inp['x'][0])
    p1 = [prep_core(inp, h) for h in range(8)]
    shared = {'x': x2d, 'pos_all': np.ascontiguousarray(inp['positions'][0]).astype(np.int32),
              'wa_all': np.stack([np.ascontiguousarray(p['wa'][:, 0:384]) for p in p1]), 'w_sh': np.ascontiguousarray(inp['w_in'][0][:, 1536:1920]), 'pp_all': np.stack([p['pp'] for p in p1]),
              'w2s_all': np.stack([p['w2s'] for p in p1]), 'a2s_all': np.stack([p['a2s'] for p in p1]),
              'g2h_all': np.stack([p['g2h'] for p in p1]), 'w0row_all': np.stack([p['w0row'] for p in p1]),
              'u_sh': np.ascontiguousarray(inp['peer_u'][0]), 'v_sh': np.ascontiguousarray(inp['peer_v'][0])}
    nc = build_nc()
    in_maps = []
    for c in range(8):
        p2 = prep2_core(inp, c)
        m = dict(shared)
        for nm, _, _ in F_IN:
            if nm in m:
                continue
            if nm == 'x_own':
                m[nm] = p2['x']
            elif nm == 'ridx':
                m[nm] = np.stack([np.arange(128) * 8 + c, np.arange(128) * 8 + 7 - c], 1).astype(np.int32)
            else:
                m[nm] = p2[nm]
        in_maps.append(m)
    res = run_bass_kernel_spmd(nc, in_maps, core_ids=list(range(8))).results
    out = np.concatenate([np.asarray(r['out']) for r in res], axis=0)
    return out.reshape(1, T, D).astype(np.float32)
```

```python
import contextlib
import numpy as np
import concourse.bass as bass
import concourse.mybir as mybir
from concourse.bass_utils import run_bass_kernel_spmd

F32 = mybir.dt.float32
BF16 = mybir.dt.bfloat16
I32 = mybir.dt.int32
AF = mybir.ActivationFunctionType
ALU = mybir.AluOpType
AX = mybir.AxisListType

ENGS = ['pe', 'act', 'dve', 'pool', 'sp']


class Sched:
    def __init__(self, nc, n_dma_sems=48):
        self.nc = nc
        self.stack = contextlib.ExitStack()
        self.streams = {e: [] for e in ENGS}
        self.cnt = {}
        self.seen = {e: {} for e in ENGS}
        self.last_write = {}
        self.readers = {}
        self.n_dma_sems = n_dma_sems
        self.dma_keys = {}
        self.sems = {}
        self.scopes = [self.stack]
        self.cap = None

    def __enter__(self):
        self.stack.__enter__()
        for e in ENGS:
            self.sems[e] = self.stack.enter_context(self.nc.semaphore("s_" + e))
            self.cnt[e] = 0
        self.dma_pool = [self.stack.enter_context(self.nc.semaphore("d%d" % i)) for i in range(self.n_dma_sems)]
        return self

    def __exit__(self, *a):
        return self.stack.__exit__(*a)

    def sb(self, name, shape, dt):
        self.uid = getattr(self, 'uid', 0) + 1
        return self.scopes[-1].enter_context(self.nc.sbuf_tensor("sb%d_%s" % (self.uid, name), list(shape), dt))

    def ps(self, name, shape, dt):
        self.uid = getattr(self, 'uid', 0) + 1
        return self.scopes[-1].enter_context(self.nc.psum_tensor("ps%d_%s" % (self.uid, name), list(shape), dt))

    def push(self):
        st = contextlib.ExitStack()
        st.__enter__()
        self.scopes.append(st)

    def pop(self):
        self.barrier()
        st = self.scopes.pop()
        st.__exit__(None, None, None)

    def _deps(self, eng, reads, writes):
        deps = {}
        def add(tok):
            if tok is None:
                return
            k, v = tok
            if deps.get(k, 0) < v:
                deps[k] = v
        for r in reads:
            add(self.last_write.get(r))
        for w in writes:
            add(self.last_write.get(w))
            for t in self.readers.get(w, ()):
                add(t)
        waits = []
        seen = self.seen[eng]
        for k, v in deps.items():
            if k == 'pe' and eng == 'pe':
                continue
            if seen.get(k, 0) >= v:
                continue
            seen[k] = v
            waits.append((k, v))
        return waits

    def _commit(self, tok, reads, writes):
        for w in writes:
            self.last_write[w] = tok
            self.readers[w] = []
        for r in reads:
            if r in writes:
                continue
            self.readers.setdefault(r, []).append(tok)

    @staticmethod
    def _excl(reads, writes):
        pr = [r for r in reads if isinstance(r, str) and r.startswith('pb')]
        if pr:
            reads = [r for r in reads if r not in pr]
            writes = list(writes) + [r for r in pr if r not in writes]
        return reads, writes

    def op(self, eng, fn, reads=(), writes=()):
        if self.cap is not None:
            self.cap.append(('op', (eng, fn, tuple(reads), tuple(writes)), {}))
            return
        reads, writes = self._excl(reads, writes)
        waits = self._deps(eng, reads, writes)
        self.cnt[eng] += 1
        tok = (eng, self.cnt[eng])
        self.streams[eng].append((waits, fn, (eng, 1)))
        self._commit(tok, reads, writes)

    def capture(self, fn, *a):
        self.cap = []
        fn(*a)
        lst, self.cap = self.cap, None
        return lst

    def emit_interleaved(self, A, B):
        ia = ib = 0
        na, nb = len(A), len(B)
        while ia < na or ib < nb:
            if ib >= nb or (ia < na and ia * max(nb, 1) <= ib * max(na, 1)):
                kind, args, kw = A[ia]; ia += 1
            else:
                kind, args, kw = B[ib]; ib += 1
            getattr(self, kind)(*args, **kw)

    def _dkey(self, key):
        if key not in self.dma_keys:
            idx = len(self.dma_keys)
            assert idx < self.n_dma_sems, "out of dma semaphores"
            self.dma_keys[key] = ('dma', idx)
            self.cnt.setdefault(('dma', idx), 0)
        return self.dma_keys[key]

    def dma(self, eng, out, in_, reads=(), writes=(), key=None, **kw):
        if self.cap is not None:
            self.cap.append(('dma', (eng, out, in_), dict(reads=tuple(reads), writes=tuple(writes), key=key, **kw)))
            return
        k = self._dkey(key)
        waits = self._deps(eng, reads, writes)
        self.cnt[k] += 16
        tok = (k, self.cnt[k])
        self.streams[eng].append((waits, (lambda e, o=out, i=in_, kw=kw: e.dma_start(out=o, in_=i, **kw)), (k, 16)))
        self._commit(tok, reads, writes)

    def gather(self, out, in_rows, idx_ap, reads=(), writes=(), key=None):
        kk_ = self._dkey(key)
        waits = self._deps('pool', reads, writes)
        self.cnt[kk_] += 16
        tok = (kk_, self.cnt[kk_])
        self.streams['pool'].append((waits, (lambda e: e.indirect_dma_start(out=out, out_offset=None, in_=in_rows, in_offset=bass.IndirectOffsetOnAxis(ap=idx_ap, axis=0))), (kk_, 16)))
        self._commit(tok, reads, writes)

    def coll(self, kind, op, groups, ins, outs, reads=(), writes=()):
        key = 'coll'
        kk_ = self._dkey(key)
        waits = self._deps('pool', reads, writes)
        self.cnt[kk_] += 16
        tok = (kk_, self.cnt[kk_])
        self.streams['pool'].append((waits, (lambda e: e.collective_compute(kind, op, replica_groups=groups, ins=ins, outs=outs)), (kk_, 16)))
        self._commit(tok, reads, writes)

    def barrier(self):
        for e in ENGS:
            waits = []
            for k, v in self.cnt.items():
                if v == 0 or k == e:
                    continue
                if self.seen[e].get(k, 0) >= v:
                    continue
                self.seen[e][k] = v
                waits.append((k, v))
            if waits:
                self.streams[e].append((waits, None, None))
        for e in ENGS:
            if self.cnt[e] and self.seen[e].get(e, 0) < self.cnt[e]:
                self.seen[e][e] = self.cnt[e]
                self.streams[e].append(([(e, self.cnt[e])], None, None))
        self.last_write.clear()
        self.readers.clear()
        self.dma_keys = {}

    def _sem(self, k):
        if isinstance(k, tuple):
            return self.dma_pool[k[1]]
        return self.sems[k]

    def finish(self):
        self.barrier()
        nc = self.nc
        streams = self.streams
        sem = self._sem

        def replay(engname):
            def run(eng):
                for waits, fn, inc in streams[engname]:
                    for k, v in waits:
                        eng.wait_ge(sem(k), v)
                    if fn is not None:
                        ins = fn(eng)
                        ins.then_inc(sem(inc[0]), inc[1])
            return run

        with nc.Block() as block:
            block.tensor(replay('pe'))
            block.scalar(replay('act'))
            block.vector(replay('dve'))
            block.gpsimd(replay('pool'))
            block.sync(replay('sp'))


T = 16384
D = 1024
NB = 32
CL = 128
NCH = T // CL
CDEC = 0.6065306597126334
NPP = 32


class K:
    def __init__(self, S):
        self.S = S
        self.nbank = 0

    def mm(self, out, lhsT, rhs, start=True, stop=True, reads=(), writes=()):
        self.S.op('pe', lambda e: e.matmul(out, lhsT=lhsT, rhs=rhs, start=start, stop=stop), reads=reads, writes=writes)

    def act(self, out, in_, func, bias=None, scale=None, reads=(), writes=()):
        kw = {}
        if bias is not None:
            kw['bias'] = bias
        if scale is not None:
            kw['scale'] = scale
        self.S.op('act', lambda e: e.activation(out=out, in_=in_, func=func, **kw), reads=reads, writes=writes)

    def tt(self, eng, out, in0, in1, op, reads=(), writes=()):
        self.S.op(eng, lambda e: e.tensor_tensor(out=out, in0=in0, in1=in1, op=op), reads=reads, writes=writes)

    def ts(self, eng, out, in0, s1, s2, op0, op1=None, reads=(), writes=()):
        if op1 is None and op0 == ALU.pow:
            self.S.op(eng, lambda e: e.tensor_scalar(out=out, in0=in0, scalar1=1.0, scalar2=s1, op0=ALU.mult, op1=ALU.pow), reads=reads, writes=writes)
        elif op1 is None:
            self.S.op(eng, lambda e: e.tensor_scalar(out=out, in0=in0, scalar1=s1, scalar2=0.0, op0=op0, op1=ALU.add), reads=reads, writes=writes)
        else:
            self.S.op(eng, lambda e: e.tensor_scalar(out=out, in0=in0, scalar1=s1, scalar2=s2, op0=op0, op1=op1), reads=reads, writes=writes)

    def rsqrt(self, out, in_, scale, eps, reads=(), writes=()):
        self.S.op('act', lambda e: e.activation(out=out, in_=in_, func=AF.Sqrt, bias=self.eps_ap(eps, out), scale=scale), reads=list(reads) + ['epsc'], writes=writes)
        self.S.op('dve', lambda e: e.reciprocal(out=out, in_=out), reads=writes, writes=writes)

    def eps_ap(self, eps, out):
        n = out.shape[0]
        return self.epsc[eps][0:n, 0:1]

    def stt(self, eng, out, in0, scalar, in1, op0, op1, reads=(), writes=()):
        self.S.op(eng, lambda e: e.scalar_tensor_tensor(out=out, in0=in0, scalar=scalar, in1=in1, op0=op0, op1=op1), reads=reads, writes=writes)

    def cp(self, eng, out, in_, reads=(), writes=()):
        if eng == 'act':
            self.S.op('act', lambda e: e.activation(out=out, in_=in_, func=AF.Copy), reads=reads, writes=writes)
        else:
            self.S.op(eng, lambda e: e.tensor_copy(out=out, in_=in_), reads=reads, writes=writes)

    def memset(self, eng, ap, val, writes=()):
        self.S.op(eng, lambda e: e.memset(ap, val), reads=(), writes=writes)


def rwkv_phase(nc, S, k, dr, core_dbg=None):
    x_d = dr['x']
    nblk = dr.get('_nblk', NB)
    lvl = dr.get('_lvl', 99)
    banks = dr.get('_banks') or [(S.ps("pb%d" % i, [128, 512], F32), "pb%d" % i) for i in range(8)]
    st = {'b': 0, 'lo': 0, 'hi': 8}

    def bank():
        b = banks[st['lo'] + st['b'] % (st['hi'] - st['lo'])]
        st['b'] += 1
        return b

    wa = S.sb("wa", [128, 8, 384], BF16)
    wst_ = S.sb("wst_", [128, 384], F32)
    for c in range(8):
        S.dma('sp', wst_[:], dr['wa'][c * 128:(c + 1) * 128, 0:384], writes=['wst_'], key='c0')
        k.cp('dve', wa[:, c, :], wst_[:], reads=['wst_'], writes=['wa'])
    pp = S.sb("pp", [128, NPP], F32)
    S.dma('sp', pp[:], dr['pp'], writes=['pp'], key='c1')
    cst_st = S.sb("cst_st", [128, 5, 128], F32)
    S.dma('sp', cst_st[:], dr['cst'].rearrange("m p n -> p m n"), writes=['cst_st'], key='c2')
    cst = S.sb("cst", [128, 5, 128], BF16)
    k.cp('dve', cst[:], cst_st[:], reads=['cst_st'], writes=['cst'])
    ident = cst[:, 0, :]
    m4 = S.sb("m4", [128, 4, 4, 128], BF16)
    for mi in range(4):
        for j in range(4):
            k.cp('pool', m4[:, mi, j, :], cst[:, 1 + mi, :], reads=['cst'], writes=['m4'])
    id4 = S.sb("id4", [128, 4, 128], BF16)
    for j in range(4):
        k.cp('pool', id4[:, j, :], cst[:, 0, :], reads=['cst'], writes=['id4'])
    ones_bd = S.sb("ones_bd", [128, 128], BF16)
    k.memset('pool', ones_bd[:], 0.0, writes=['ones_bd'])
    k.memset('pool', ones_bd[0:64, 0:64], 1.0, writes=['ones_bd'])
    k.memset('pool', ones_bd[64:128, 64:128], 1.0, writes=['ones_bd'])
    ones_f = S.sb("ones_f", [128, 128], BF16)
    k.memset('pool', ones_f[:], 1.0, writes=['ones_f'])
    bd_st = S.sb("bd_st", [128, 2, 128], F32)
    k.memset('pool', bd_st[:], 0.0, writes=['bd_st'])
    S.dma('sp', bd_st[0:64, 0, 0:64], dr['w2s'][0:64, :], reads=['bd_st'], writes=['bd_st'], key='c3')
    S.dma('sp', bd_st[64:128, 0, 64:128], dr['w2s'][64:128, :], reads=['bd_st'], writes=['bd_st'], key='c3')
    S.dma('sp', bd_st[0:64, 1, 0:64], dr['a2s'][0:64, :], reads=['bd_st'], writes=['bd_st'], key='c3')
    S.dma('sp', bd_st[64:128, 1, 64:128], dr['a2s'][64:128, :], reads=['bd_st'], writes=['bd_st'], key='c3')
    bd = S.sb("bd", [128, 2, 128], BF16)
    k.cp('dve', bd[:], bd_st[:], reads=['bd_st'], writes=['bd'])
    g2_st = S.sb("g2_st", [128, 64], F32)
    S.dma('sp', g2_st[:], dr['g2h'], writes=['g2_st'], key='c4')
    g2h = S.sb("g2h", [128, 64], BF16)
    k.cp('dve', g2h[:], g2_st[:], reads=['g2_st'], writes=['g2h'])
    w0r_st = S.sb("w0r_st", [1, 128], F32)
    S.dma('sp', w0r_st[:], dr['w0row'], writes=['w0r_st'], key='c5')
    w0row = S.sb("w0row", [1, 128], BF16)
    k.cp('dve', w0row[:], w0r_st[:], reads=['w0r_st'], writes=['w0row'])
    epst = S.sb("epst", [128, 4], F32)
    k.epsc = {}
    for i_, ev in enumerate([1e-6, 1e-24, 64e-5]):
        k.memset('pool', epst[:, i_:i_ + 1], ev, writes=['epsc'])
        k.epsc[ev] = epst[:, i_:i_ + 1]
    omka = S.sb("omka", [128, 1], F32)
    k.ts('dve', omka[:], pp[:, 9:10], -1.0, 1.0, ALU.mult, ALU.add, reads=['pp'], writes=['omka'])

    MT_all = S.sb("MT_all", [128, NCH, 128], BF16)
    k.memset('pool', MT_all[:], 0.0, writes=['MT_all'])
    N_all = S.sb("N_all", [128, NCH, 64], BF16)
    gamL = S.sb("gamL", [128, NCH], F32)
    Hh = S.sb("Hh", [128, NCH + 1, 64], BF16)

    hT = [S.sb("hT%d" % i, [128, 8, 512], BF16) for i in range(2)]
    U = [S.sb("U%d" % i, [128, 6, 514], BF16) for i in range(3)]
    for i in range(3):
        k.memset('pool', U[i][:], 0.0, writes=['U%d' % i])

    def w(name, shape, dt=BF16):
        return S.sb(name, shape, dt)
    tsum6 = w("tsum6", [128, 6, 512], BF16)
    us2 = [w("us%d" % i, [128, 6, 512], BF16) for i in range(2)]
    us = us2[0]
    hmu = w("hmu", [128, 6], F32); omu = w("omu", [128, 6], F32)
    k.ts('dve', hmu[:], pp[:, 0:6], 0.5, None, ALU.mult, reads=['pp'], writes=['hmu'])
    k.ts('dve', omu[:], pp[:, 0:6], -1.0, 1.0, ALU.mult, ALU.add, reads=['pp'], writes=['omu'])
    tl = w("tl", [128, 512]); sl = w("sl", [128, 512])
    sg_tok = w("sg_tok", [128, 4, 128])
    Gi = w("Gi", [128, 512], F32); Ginv = w("Ginv", [128, 512], F32); Ge = w("Ge", [128, 512], F32); Gh = w("Gh", [128, 512], F32)
    tot = w("tot", [128, 4], F32); nct = w("nct", [128, 4], F32)
    a_t = w("a_t", [128, 512], F32)
    kk = w("kk", [128, 512], F32); kk2 = w("kk2", [128, 512]); rn = w("rn", [128, 512], F32); kkn = w("kkn", [128, 512], F32)
    t1 = rn; kdir = kk; bvec = a_t
    At2 = [w("At%d" % i, [128, 512]) for i in range(2)]; Bt2 = [w("Bt%d" % i, [128, 512]) for i in range(2)]
    Kt2 = [w("Kt%d" % i, [128, 512]) for i in range(2)]; Rt2 = [w("Rt%d" % i, [128, 512]) for i in range(2)]
    Bht2 = [w("Bht%d" % i, [128, 512]) for i in range(2)]; Kht2 = [w("Kht%d" % i, [128, 512]) for i in range(2)]
    At, Bt, Kt, Rt, Bht, Kht = At2[0], Bt2[0], Kt2[0], Rt2[0], Bht2[0], Kht2[0]
    rk = w("rk", [128, 512]); bon = w("bon", [64, 512]); g_t = w("g_t", [64, 512])
    Sm = [w("Sm%d" % i, [128, 8, 128]) for i in range(2)]
    SmT = [w("SmT%d" % i, [128, 8, 128]) for i in range(2)]
    Qm = [w("Qm%d" % i, [128, 8, 128]) for i in range(2)]
    AakT = w("AakT", [128, 8, 128]); TrbT = w("TrbT", [128, 8, 128]); TrkT = w("TrkT", [128, 8, 128])
    AXm = w("AXm", [128, 8, 128]); WU = w("WU", [128, 8, 128])
    Bh_tok = w("Bh_tok", [128, 8, 64]); Kh_tok = w("Kh_tok", [128, 8, 64]); V_tok = w("V_tok", [128, 4, 64])
    QhT = w("QhT", [128, 512]); Oloc = w("Oloc", [64, 512])

    MASK = {0: {'ss': 0, 'si': 1}, 1: {'ss': 2, 'si': 3}}
    MASK_TS = {0: 2, 1: 0}

    def load_h(b):
        S.dma('sp', hT[b % 2][:], dr['hT_d'][:, :, b * 512:(b + 1) * 512], reads=['hT_d'], writes=['hT%d' % (b % 2)], key='lh%d' % (b % 2))

    def project(b):
        if b == 0:
            load_h(0)
        if b + 1 < nblk:
            load_h(b + 1)
        h = hT[b % 2]; hn = 'hT%d' % (b % 2)
        Ub = U[b % 3]; un = 'U%d' % (b % 3)
        S.dma('sp', Ub[:, 3:6, 1:513], dr['ush_d'][:, :, b * 512:(b + 1) * 512].rearrange("a p n -> p a n"), reads=['ush_d'], writes=[un], key='lu%d' % (b % 3))
        for tI in range(3):
            pb, pn = bank()
            for c in range(8):
                k.mm(pb[:, :], lhsT=wa[:, c, tI * 128:(tI + 1) * 128], rhs=h[:, c, :], start=(c == 0), stop=(c == 7), reads=['wa', hn], writes=[pn])
            if tI % 2 == 0:
                k.cp('act', Ub[:, tI, 1:513], pb[:, :], reads=[pn], writes=[un])
            else:
                k.cp('dve', Ub[:, tI, 1:513], pb[:, :], reads=[pn], writes=[un])
        if b > 0:
            pu = U[(b - 1) % 3]; pun = 'U%d' % ((b - 1) % 3)
            k.cp('pool', pu[:, :, 513:514], Ub[:, :, 1:2], reads=[un], writes=[pun])
            k.cp('pool', Ub[:, :, 0:1], pu[:, :, 512:513], reads=[pun], writes=[un])
        else:
            k.memset('pool', Ub[:, :, 0:1], 0.0, writes=[un])
        if b == NB - 1:
            k.memset('pool', Ub[:, :, 513:514], 0.0, writes=[un])

    def prep(b):
        Ub = U[b % 3]; un = 'U%d' % (b % 3)
        tok0 = b * 512
        sfx = str(b % 2)
        At = At2[b % 2]; Bt = Bt2[b % 2]; Kt = Kt2[b % 2]; Rt = Rt2[b % 2]; Bht = Bht2[b % 2]; Kht = Kht2[b % 2]; us = us2[b % 2]
        k.tt('pool', tsum6[:], Ub[:, :, 0:512], Ub[:, :, 2:514], ALU.add, reads=[un], writes=['tsum6'])
        k.tt('dve', tsum6[:], tsum6[:], hmu[:, 0:6].unsqueeze(2).to_broadcast([128, 6, 512]), ALU.mult, reads=['tsum6', 'hmu'], writes=['tsum6'])
        k.tt('pool', us[:], Ub[:, :, 1:513], omu[:, 0:6].unsqueeze(2).to_broadcast([128, 6, 512]), ALU.mult, reads=[un, 'omu'], writes=['us' + sfx])
        k.tt('dve', us[:], us[:], tsum6[:], ALU.add, reads=['us' + sfx, 'tsum6'], writes=['us' + sfx])
        r2 = us[:, 0, :]; k2 = us[:, 1, :]; v2 = us[:, 2, :]
        k.act(tl[:], us[:, 3, :], AF.Tanh, reads=['us' + sfx], writes=['tl'])
        pb, pn = bank()
        for j in range(4):
            k.mm(pb[:, j * 128:(j + 1) * 128], lhsT=tl[:, j * 128:(j + 1) * 128], rhs=bd[:, 0, :], start=True, stop=False, reads=['tl', 'bd'], writes=[pn])
            k.mm(pb[:, j * 128:(j + 1) * 128], lhsT=ones_f[0:1, :], rhs=w0row[0:1, :], start=False, stop=True, reads=['ones_f', 'w0row'], writes=[pn])
        k.act(sg_tok[:].rearrange("p j n -> p (j n)"), pb[:, :], AF.Sigmoid, reads=[pn], writes=['sg_tok'])
        pI, pIn = bank(); pE, pEn = bank()
        for j in range(4):
            for d in range(2):
                P = slice(64 * d, 64 * d + 64)
                k.mm(pI[P, j * 128:(j + 1) * 128], lhsT=sg_tok[:, j, P], rhs=cst[:, 2 + 2 * d, :], reads=['sg_tok', 'cst'], writes=[pIn])
                k.mm(pE[P, j * 128:(j + 1) * 128], lhsT=sg_tok[:, j, P], rhs=cst[:, 1 + 2 * d, :], reads=['sg_tok', 'cst'], writes=[pEn])
        pb, pn = bank()
        k.mm(pb[:, :], lhsT=bd[:, 1, :], rhs=us[:, 4, :], reads=['bd', 'us' + sfx], writes=[pn])
        k.act(a_t[:], pb[:, :], AF.Sigmoid, bias=pp[:, 7:8], reads=[pn, 'pp'], writes=['a_t'])
        k.ts('dve', kk[:], k2, pp[:, 8:9], None, ALU.mult, reads=['us' + sfx, 'pp'], writes=['kk'])
        k.tt('pool', kk2[:], kk[:], kk[:], ALU.mult, reads=['kk'], writes=['kk2'])
        pb, pn = bank()
        k.mm(pb[:, :], lhsT=ones_bd[:], rhs=kk2[:], reads=['ones_bd', 'kk2'], writes=[pn])
        k.rsqrt(rn[:], pb[:, :], 1.0, 1e-24, reads=[pn], writes=['rn'])
        k.tt('dve', kkn[:], kk[:], rn[:], ALU.mult, reads=['kk', 'rn'], writes=['kkn'])
        k.ts('dve', t1[:], a_t[:], pp[:, 9:10], omka[:, 0:1], ALU.mult, ALU.add, reads=['a_t', 'pp', 'omka', 'rn', 'kkn'], writes=['rn'])
        k.tt('pool', kdir[:], k2, t1[:], ALU.mult, reads=['us' + sfx, 'rn', 'kkn'], writes=['kk'])
        k.tt('pool', bvec[:], kkn[:], a_t[:], ALU.mult, reads=['kkn', 'a_t', 'rn'], writes=['a_t'])
        k.stt('dve', rk[:], r2, pp[:, 10:11], kdir[:], ALU.mult, ALU.mult, reads=['us' + sfx, 'pp', 'kk'], writes=['rk'])
        pb, pn = bank()
        k.mm(pb[0:64, :], lhsT=ones_f[:, 0:64], rhs=rk[:], reads=['ones_f', 'rk'], writes=[pn])
        k.tt('dve', bon[:], pb[0:64, :], us[0:64, 2, :], ALU.mult, reads=[pn, 'us' + sfx], writes=['bon'])
        S.dma('sp', dr['bon_d'][:, tok0:tok0 + 512], bon[:], reads=['bon'], writes=['bon_d'], key='bon')
        k.act(sl[:], us[:, 5, :], AF.Sigmoid, reads=['us' + sfx], writes=['sl'])
        pb, pn = bank()
        k.mm(pb[0:64, :], lhsT=g2h[:], rhs=sl[:], reads=['g2h', 'sl'], writes=[pn])
        k.cp('act', g_t[:], pb[0:64, :], reads=[pn], writes=['g_t'])
        S.dma('sp', dr['g_d'][:, tok0:tok0 + 512], g_t[:], reads=['g_t'], writes=['g_d'], key='gd')
        k.act(Gi[:], pI[:, :], AF.Exp, scale=-CDEC, reads=[pIn], writes=['Gi'])
        k.act(Ginv[:], pI[:, :], AF.Exp, scale=CDEC, reads=[pIn], writes=['Ginv'])
        k.act(Ge[:], pE[:, :], AF.Exp, scale=-CDEC, reads=[pEn], writes=['Ge'])
        pI3 = pI[:, :].rearrange("p (j n) -> p j n", n=128)
        k.cp('dve', tot[0:64, :], pI3[0:64, :, 127], reads=[pIn], writes=['tot'])
        k.cp('dve', tot[64:128, :], pI3[64:128, :, 0], reads=[pIn], writes=['tot'])
        k.ts('dve', nct[:], tot[:], -CDEC, None, ALU.mult, reads=['tot'], writes=['nct'])
        k.act(gamL[:, b * 4:(b + 1) * 4], tot[:], AF.Exp, scale=-CDEC, reads=['tot'], writes=['gamL'])
        for j in range(4):
            k.act(Gh[:, j * 128:(j + 1) * 128], pI[:, j * 128:(j + 1) * 128], AF.Exp, bias=nct[:, j:j + 1], scale=CDEC, reads=[pIn, 'nct'], writes=['Gh'])
        k.stt('dve', At[:], kkn[:], -1.0, Ge[:], ALU.mult, ALU.mult, reads=['kkn', 'Ge'], writes=['At' + sfx])
        k.tt('pool', Bt[:], bvec[:], Ginv[:], ALU.mult, reads=['a_t', 'Ginv'], writes=['Bt' + sfx])
        k.tt('dve', Kt[:], kdir[:], Ginv[:], ALU.mult, reads=['kk', 'Ginv'], writes=['Kt' + sfx])
        k.tt('pool', Rt[:], r2, Gi[:], ALU.mult, reads=['us' + sfx, 'Gi'], writes=['Rt' + sfx])
        k.tt('dve', Bht[:], bvec[:], Gh[:], ALU.mult, reads=['a_t', 'Gh'], writes=['Bht' + sfx])
        k.tt('pool', Kht[:], kdir[:], Gh[:], ALU.mult, reads=['kk', 'Gh'], writes=['Kht' + sfx])

    def stages(b):
        Ub = U[b % 3]; un = 'U%d' % (b % 3)
        tok0 = b * 512
        sfx = str(b % 2)
        At = At2[b % 2]; Bt = Bt2[b % 2]; Kt = Kt2[b % 2]; Rt = Rt2[b % 2]; Bht = Bht2[b % 2]; Kht = Kht2[b % 2]; us = us2[b % 2]
        def scores(dst, dstn, L, Ln, R, Rn, mask_of_dir, ts_layout=False):
            for d in range(2):
                P = slice(64 * d, 64 * d + 64)
                pb, pn = bank()
                for j in range(4):
                    C = slice(j * 128, (j + 1) * 128)
                    k.mm(pb[:, C], lhsT=L[P, C], rhs=R[P, C], reads=[Ln, Rn], writes=[pn])
                mi = mask_of_dir[d]
                k.tt('dve', dst[:, 4 * d:4 * d + 4, :].rearrange("p j n -> p (j n)"), pb[:, :], m4[:, mi, :, :].rearrange("p j n -> p (j n)"), ALU.mult, reads=[pn, 'm4'], writes=[dstn + '_%d' % d])
        scores(SmT[0], 'SmT0', Bt, 'Bt' + sfx, At, 'At' + sfx, {0: 0, 1: 2})
        scores(Sm[0], 'Sm0', At, 'At' + sfx, Bt, 'Bt' + sfx, {0: 2, 1: 0})
        scores(AakT, 'AakT', Kt, 'Kt' + sfx, At, 'At' + sfx, {0: 0, 1: 2})
        scores(TrbT, 'TrbT', Bt, 'Bt' + sfx, Rt, 'Rt' + sfx, {0: 1, 1: 3})
        scores(TrkT, 'TrkT', Kt, 'Kt' + sfx, Rt, 'Rt' + sfx, {0: 1, 1: 3})
        for d in range(2):
            k.tt('pool', Qm[0][:, 4 * d:4 * d + 4, :], SmT[0][:, 4 * d:4 * d + 4, :], id4[:], ALU.add, reads=['SmT0_%d' % d, 'id4'], writes=['Qm0_%d' % d])
        if lvl < 4:
            return
        cur = 0
        for dl in range(1, dr.get('_ndl', 7)):
            nxt = 1 - cur
            sc, scn = Sm[cur], 'Sm%d' % cur
            stc, stcn = SmT[cur], 'SmT%d' % cur
            sn, snn = Sm[nxt], 'Sm%d' % nxt
            stn, stnn = SmT[nxt], 'SmT%d' % nxt
            for d in range(2):
                pb, pn = bank()
                for j in range(4):
                    c8 = 4 * d + j
                    k.mm(pb[:, j * 128:(j + 1) * 128], lhsT=stc[:, c8, :], rhs=sc[:, c8, :], reads=[stcn + '_%d' % d, scn + '_%d' % d], writes=[pn])
                k.cp(dr.get('_e1', 'act'), sn[:, 4 * d:4 * d + 4, :].rearrange("p j n -> p (j n)"), pb[:, :], reads=[pn], writes=[snn + '_%d' % d])
            if dr.get('_sub', 9) < 1:
                break
            if dl < 6:
                for d in range(2):
                    pb, pn = bank()
                    for j in range(4):
                        c8 = 4 * d + j
                        k.mm(pb[:, j * 128:(j + 1) * 128], lhsT=sc[:, c8, :], rhs=stc[:, c8, :], reads=[stcn + '_%d' % d, scn + '_%d' % d], writes=[pn])
                    k.cp('dve', stn[:, 4 * d:4 * d + 4, :].rearrange("p j n -> p (j n)"), pb[:, :], reads=[pn], writes=[stnn + '_%d' % d])
            qc, qcn = Qm[cur], 'Qm%d' % cur
            qn, qnn = Qm[nxt], 'Qm%d' % nxt
            if dr.get('_sub', 9) < 2:
                break
            for d in range(2):
                pb, pn = bank()
                for j in range(4):
                    c8 = 4 * d + j
                    k.mm(pb[:, j * 128:(j + 1) * 128], lhsT=sn[:, c8, :], rhs=qc[:, c8, :], reads=[snn + '_%d' % d, qcn + '_%d' % d], writes=[pn])
                k.tt('dve', qn[:, 4 * d:4 * d + 4, :].rearrange("p j n -> p (j n)"), pb[:, :], qc[:, 4 * d:4 * d + 4, :].rearrange("p j n -> p (j n)"), ALU.add, reads=[pn, qcn + '_%d' % d], writes=[qnn + '_%d' % d])
            cur = nxt
        Qf, Qfn = Qm[cur], 'Qm%d' % cur
        if lvl < 6:
            return
        def tokmajor(dst, dstn, src, srcn, col0, eng):
            pb, pn = bank()
            for j in range(4):
                k.mm(pb[:, j * 128:(j + 1) * 128], lhsT=src[:, j * 128:(j + 1) * 128], rhs=ident, reads=[srcn, 'cst'], writes=[pn])
            pv = pb[:, :].rearrange("p (j d n) -> p j d n", j=4, d=2, n=64)
            for d in range(2):
                k.cp(eng, dst[:, 4 * d:4 * d + 4, col0:col0 + 64], pv[:, :, d, :], reads=[pn], writes=[dstn + '_%d' % d])
        sub = dr.get('_sub', 9)
        if sub in (0, 9):
            tokmajor(AXm, 'AXm', At, 'At' + sfx, 0, 'act' if sub == 9 else 'dve')
        if sub in (1, 9):
            tokmajor(Bh_tok, 'Bh_tok', Bht, 'Bht' + sfx, 0, 'dve')
        if sub in (2, 9):
            tokmajor(Kh_tok, 'Kh_tok', Kht, 'Kht' + sfx, 0, 'act')
        if sub < 9:
            return
        pb, pn = bank()
        for j in range(4):
            k.mm(pb[:, j * 64:(j + 1) * 64], lhsT=us[0:64, 2, j * 128:(j + 1) * 128], rhs=cst[0:64, 0, 0:64], reads=['us' + sfx, 'cst'], writes=[pn])
        k.cp('dve', V_tok[:], pb[:, 0:256].rearrange("p (c n) -> p c n", n=64), reads=[pn], writes=['V_tok'])
        if lvl < 7:
            return
        pb, pn = bank()
        for d in range(2):
            for j in range(4):
                c8 = 4 * d + j
                k.mm(pb[:, c8 * 64:(c8 + 1) * 64], lhsT=AakT[:, c8, :], rhs=V_tok[:, j, :], reads=['AakT_%d' % d, 'V_tok'], writes=[pn])
        k.cp('act', AXm[:, :, 64:128], pb[:, :].rearrange("p (c n) -> p c n", n=64), reads=[pn], writes=['AXm_0', 'AXm_1'])
        if lvl < 8:
            return
        for d in range(2):
            pb, pn = bank()
            for j in range(4):
                c8 = 4 * d + j
                k.mm(pb[:, j * 128:(j + 1) * 128], lhsT=Qf[:, c8, :], rhs=AXm[:, c8, :], reads=[Qfn + '_%d' % d, 'AXm_%d' % d], writes=[pn])
            k.cp('act' if d == 0 else 'dve', WU[:, 4 * d:4 * d + 4, :].rearrange("p j n -> p (j n)"), pb[:, :], reads=[pn], writes=['WU_%d' % d])
        if lvl < 9:
            return
        pb, pn = bank()
        for d in range(2):
            P = slice(64 * d, 64 * d + 64)
            for j in range(4):
                c8 = 4 * d + j
                k.mm(pb[P, j * 128:(j + 1) * 128], lhsT=WU[:, c8, 0:64], rhs=TrbT[:, c8, :], reads=['WU_%d' % d, 'TrbT_%d' % d], writes=[pn])
        k.tt('dve', QhT[:], pb[:, :], Rt[:], ALU.add, reads=[pn, 'Rt' + sfx], writes=['QhT'])
        S.dma('sp', dr['qh_d'][:, tok0:tok0 + 512], QhT[:], reads=['QhT'], writes=['qh_d'], key='qh')
        pb, pn = bank()
        for j in range(4):
            for d in range(2):
                c8 = 4 * d + j
                k.mm(pb[0:64, j * 128:(j + 1) * 128], lhsT=WU[:, c8, 64:128], rhs=TrbT[:, c8, :], start=(d == 0), stop=False, reads=['WU_%d' % d, 'TrbT_%d' % d], writes=[pn])
                k.mm(pb[0:64, j * 128:(j + 1) * 128], lhsT=V_tok[:, j, :], rhs=TrkT[:, c8, :], start=False, stop=(d == 1), reads=['V_tok', 'TrkT_%d' % d], writes=[pn])
        k.cp('act', Oloc[:], pb[0:64, :], reads=[pn], writes=['Oloc'])
        S.dma('sp', dr['ol_d'][:, tok0:tok0 + 512], Oloc[:], reads=['Oloc'], writes=['ol_d'], key='ol')
        pb, pn = bank()
        for d in range(2):
            P = slice(64 * d, 64 * d + 64)
            for j in range(4):
                c8 = 4 * d + j
                k.mm(pb[P, j * 64:(j + 1) * 64], lhsT=WU[:, c8, 0:64], rhs=Bh_tok[:, c8, :], reads=['WU_%d' % d, 'Bh_tok_%d' % d], writes=[pn])
        k.cp('dve', MT_all[0:64, b * 4:(b + 1) * 4, 0:64], pb[0:64, 0:256].rearrange("p (c n) -> p c n", n=64), reads=[pn], writes=['MT_all'])
        for j in range(4):
            st1 = nblk * 4 - 1 - (b * 4 + j)
            k.cp('dve', MT_all[64:128, st1, 64:128], pb[64:128, j * 64:(j + 1) * 64], reads=[pn], writes=['MT_all'])
        pb, pn = bank()
        for d in range(2):
            P = slice(64 * d, 64 * d + 64)
            for j in range(4):
                c8 = 4 * d + j
                k.mm(pb[P, j * 64:(j + 1) * 64], lhsT=Bh_tok[:, c8, :], rhs=WU[:, c8, 64:128], start=True, stop=False, reads=['WU_%d' % d, 'Bh_tok_%d' % d], writes=[pn])
                k.mm(pb[P, j * 64:(j + 1) * 64], lhsT=Kh_tok[:, c8, :], rhs=V_tok[:, j, :], start=False, stop=True, reads=['Kh_tok_%d' % d, 'V_tok'], writes=[pn])
        k.cp('act', N_all[:, b * 4:(b + 1) * 4, :], pb[:, 0:256].rearrange("p (c n) -> p c n", n=64), reads=[pn], writes=['N_all'])

    def run_direct(fn, b, lo, hi):
        st['lo'], st['hi'] = lo, hi
        fn(b)
        st['lo'], st['hi'] = 0, 8

    def cap(fn, b, lo, hi):
        st['lo'], st['hi'] = lo, hi
        lst = S.capture(fn, b)
        st['lo'], st['hi'] = 0, 8
        return lst
    run_direct(project, 0, 0, 3)
    if nblk > 1:
        run_direct(project, 1, 0, 3)
    run_direct(prep, 0, 0, 8)
    for b in range(nblk):
        if b + 2 < nblk:
            run_direct(project, b + 2, 0, 3)
        A = cap(stages, b, *dr.get('_rgA', (0, 8)))
        Bp = cap(prep, b + 1, *dr.get('_rgB', (0, 8))) if b + 1 < nblk else []
        if not dr.get('_int'):
            S.emit_interleaved(A, []); S.emit_interleaved(Bp, [])
        else:
            S.emit_interleaved(A, Bp)
    if lvl < 10:
        return

    nch = nblk * 4
    S.barrier()
    Hf = Gi[:, 0:64]
    T1 = Gi[:, 64:128]
    k.memset('pool', Hf[:], 0.0, writes=['Hf'])
    k.memset('pool', Hh[:, 0, :], 0.0, writes=['Hh0'])
    def t1_for(s_):
        c0 = s_; c1 = nch - 1 - s_
        k.stt('dve', T1[0:64, :], Hf[0:64, :], gamL[0:64, c0:c0 + 1], N_all[0:64, c0, :], ALU.mult, ALU.add, reads=['Hf', 'gamL', 'N_all'], writes=['T1'])
        k.stt('dve', T1[64:128, :], Hf[64:128, :], gamL[64:128, c1:c1 + 1], N_all[64:128, c1, :], ALU.mult, ALU.add, reads=['Hf', 'gamL', 'N_all'], writes=['T1'])
    t1_for(0)
    for s in range(nch):
        pb, pn = bank()
        k.mm(pb[:, 0:64], lhsT=MT_all[:, s, :], rhs=Hh[:, s, :], reads=['MT_all', 'Hh%d' % s], writes=[pn])
        k.tt('dve', Hh[:, s + 1, :], pb[:, 0:64], T1[:], ALU.add, reads=[pn, 'T1'], writes=['Hh%d' % (s + 1)])
        k.tt('dve', Hf[:], pb[:, 0:64], T1[:], ALU.add, reads=[pn, 'T1'], writes=['Hf'])
        if s + 1 < nch:
            t1_for(s + 1)
    if lvl < 11:
        return
    S.barrier()
    qh = [At, Bt]; ol = [Kt[0:64, :], Rt[0:64, :]]; bo = [Bht[0:64, :], Kht[0:64, :]]; gg = [rk[0:64, :], kk2[0:64, :]]
    of = kkn[0:64, :]; ob = tl[0:64, :]; dd = Ginv[0:64, :]; d2 = sl[0:64, :]; rs = Ge[0:64, :]; yy = Gh[0:64, :]
    yo = [bon, g_t]
    mean_m = AakT[0:64, 0, 0:64]
    k.memset('pool', mean_m[:], 1.0 / 64.0, writes=['mean_m'])
    ridx = S.sb("ridx", [128, 2], I32)
    S.dma('sp', ridx[:], dr['ridx'], writes=['ridx'], key='rix')
    S.dma('sp', dr['hh_d'], Hh[:, 0:NCH, :], reads=['Hh%d' % i for i in range(NCH)], writes=['hh_d'], key='shh')
    rows2k = lambda ap: ap.rearrange("p (b n) -> (p b) n", n=TO)
    qh_o = us[:, 0:4, :].rearrange("p a n -> p (a n)")
    ol_o = hT[0][0:64, 0:4, :].rearrange("p a n -> p (a n)"); bo_o = hT[0][0:64, 4:8, :].rearrange("p a n -> p (a n)")
    gg_o = hT[1][0:64, 0:4, :].rearrange("p a n -> p (a n)")
    HhA = hT[1][:, 4:6, :].rearrange("p a n -> p (a n)"); HhB = hT[1][:, 6:8, :].rearrange("p a n -> p (a n)")
    S.gather(qh_o, rows2k(dr['qh_d']), ridx[:, 0:1], reads=['ridx', 'qh_d'], writes=['qh_o'], key='g1')
    S.gather(ol_o, rows2k(dr['ol_d']), ridx[0:64, 0:1], reads=['ridx', 'ol_d'], writes=['ol_o'], key='g2')
    S.gather(bo_o, rows2k(dr['bon_d']), ridx[0:64, 0:1], reads=['ridx', 'bon_d'], writes=['bo_o'], key='g3')
    S.gather(gg_o, rows2k(dr['g_d']), ridx[0:64, 0:1], reads=['ridx', 'g_d'], writes=['gg_o'], key='g4')
    hrows = dr['hh_d'].rearrange("p (b c) n -> (p b) (c n)", c=16)
    S.gather(HhA, hrows, ridx[:, 0:1], reads=['ridx', 'hh_d'], writes=['HhA'], key='g5')
    S.gather(HhB, hrows, ridx[:, 1:2], reads=['ridx', 'hh_d'], writes=['HhB'], key='g6')
    HhA3 = HhA.rearrange("p (c n) -> p c n", n=64); HhB3 = HhB.rearrange("p (c n) -> p c n", n=64)
    for b in range(4):
        i2 = b % 2
        TS = slice(b * 512, (b + 1) * 512)
        pb, pn = bank()
        for j in range(4):
            cl = b * 4 + j
            C = slice(j * 128, (j + 1) * 128)
            k.cp('dve', TrbT[0:64, j, 0:64], HhA3[0:64, cl, :], reads=['HhA'], writes=['Hc'])
            k.cp('pool', TrbT[64:128, j, 0:64], HhB3[64:128, 15 - cl, :], reads=['HhB'], writes=['Hc'])
            k.mm(pb[0:64, C], lhsT=TrbT[:, j, 0:64], rhs=qh_o[:, b * 512 + j * 128:b * 512 + (j + 1) * 128], reads=['Hc', 'qh_o'], writes=[pn])
        k.tt('dve', of[:], pb[0:64, :], ol_o[:, TS], ALU.add, reads=[pn, 'ol_o'], writes=['of'])
        k.cp('act', ob[:], of[:], reads=['of'], writes=['ob'])
        pb, pn = bank()
        k.mm(pb[0:64, :], lhsT=mean_m[:], rhs=ob[:], reads=['mean_m', 'ob'], writes=[pn])
        k.tt('dve', dd[:], of[:], pb[0:64, :], ALU.subtract, reads=['of', pn], writes=['dd'])
        k.act(d2[:], dd[:], AF.Square, reads=['dd'], writes=['d2'])
        pb, pn = bank()
        k.mm(pb[0:64, :], lhsT=mean_m[:], rhs=d2[:], reads=['mean_m', 'd2'], writes=[pn])
        k.rsqrt(rs[:], pb[0:64, :], 1.0, 64e-5, reads=[pn], writes=['rs'])
        k.tt('dve', yy[:], dd[:], rs[:], ALU.mult, reads=['dd', 'rs'], writes=['yy'])
        k.ts('dve', yy[:], yy[:], pp[0:64, 11:12], pp[0:64, 12:13], ALU.mult, ALU.add, reads=['yy', 'pp'], writes=['yy'])
        k.tt('pool', yy[:], yy[:], bo_o[:, TS], ALU.add, reads=['yy', 'bo_o'], writes=['yy'])
        k.tt('pool', yo[i2][:], yy[:], gg_o[:, TS], ALU.mult, reads=['yy', 'gg_o'], writes=['yo%d' % i2])
        S.dma('sp', dr['yrw_own'][:, TS], yo[i2][:], reads=['yo%d' % i2], writes=['yrw_own'], key='sy%d' % i2)


def host_consts():
    p = np.arange(128)[:, None]; f = np.arange(128)[None, :]
    cst = np.stack([(p == f), (p < f), (p <= f), (p > f), (p >= f)]).astype(np.float32)
    return cst


def prep_core(inp, hd):
    hc = slice(hd * 64, (hd + 1) * 64)
    w_in = inp['w_in'][0]
    o = {}
    r_c = np.arange(hd * 64, (hd + 1) * 64)
    cols = np.concatenate([r_c, r_c, 512 + r_c, 512 + r_c, 1024 + r_c, 1024 + r_c,
                           np.arange(1536, 1920),
                           np.arange(1920 + 256, 1920 + 384),
                           1920 + 384 + np.arange(16), 1920 + 384 + np.arange(16),
                           1920 + 400 + np.arange(16), 1920 + 400 + np.arange(16)])
    o['wa'] = np.ascontiguousarray(w_in[:, cols])
    mu = inp['rw_mu'][0]
    pp = np.zeros((128, NPP), np.float32)
    mucols = cols[:768]
    for tI in range(6):
        pp[:, tI] = mu[mucols[tI * 128:(tI + 1) * 128]]
    st2 = lambda v: np.concatenate([v[hc], v[hc]])
    pp[:, 6] = np.concatenate([inp['rw_w0'][0, 0, hc], inp['rw_w0'][0, 1, hc]])
    pp[:, 7] = np.concatenate([inp['rw_a0'][0, 0, hc], inp['rw_a0'][0, 1, hc]])
    pp[:, 8] = st2(inp['rw_k_k'][0]); pp[:, 9] = st2(inp['rw_k_a'][0]); pp[:, 10] = st2(inp['rw_r_k'][0])
    pp[:, 11] = st2(inp['rw_gn_w'][0]); pp[:, 12] = st2(inp['rw_gn_b'][0])
    pp[:, 13:21] = inp['g_mix'][0].reshape(8, 128).T
    o['pp'] = pp
    o['w2s'] = np.ascontiguousarray(np.concatenate([inp['rw_w2'][0, 0][:, hc], inp['rw_w2'][0, 1][:, hc]], 0))
    o['a2s'] = np.ascontiguousarray(np.concatenate([inp['rw_a2'][0, 0][:, hc], inp['rw_a2'][0, 1][:, hc]], 0))
    o['g2h'] = np.ascontiguousarray(inp['rw_g2'][0][:, hc])
    o['w0row'] = np.ascontiguousarray(pp[:, 6][None, :])
    o['cst'] = host_consts()
    return o


TO = 2048
TWO_PI = 6.283185307179586
ATT_SCALE = 96.0 ** -0.5


def make_banks(S, n=8):
    banks = [(S.ps("pb%d" % i, [128, 512], F32), "pb%d" % i) for i in range(n)]
    st = {'b': 0}

    def bank(lo=0, hi=n):
        b = banks[lo + st['b'] % (hi - lo)]
        st['b'] += 1
        return b
    return banks, bank


def norm_T(S, k, bank, x_rows, gcol, bufs, eps=1e-6):
    xt, sq, ss, rstd, xb, h = bufs['xt'], bufs['sq'], bufs['ss'], bufs['rstd'], bufs['xb'], bufs['hT']
    ident = bufs['ident']
    if x_rows is not None:
        S.dma('sp', xt[:], x_rows.rearrange("(j p) d -> p j d", p=128), writes=['xt'], key='xt')
    for j in range(4):
        k.act(sq[:], xt[:, j, :], AF.Square, reads=['xt'], writes=['sq'])
        S.op('dve', lambda e, j=j: e.reduce_sum(out=ss[:, j:j + 1], in_=sq[:], axis=AX.X), reads=['sq'], writes=['ss'])
    k.rsqrt(rstd[:], ss[:], 1.0 / D, eps, reads=['ss'], writes=['rstd'])
    for j in range(4):
        k.ts('dve' if j % 2 else 'pool', xb[:, j, :], xt[:, j, :], rstd[:, j:j + 1], None, ALU.mult, reads=['xt', 'rstd'], writes=['xb'])
    for c in range(8):
        pb, pn = bank()
        for j in range(4):
            k.mm(pb[:, j * 128:(j + 1) * 128], lhsT=xb[:, j, c * 128:(c + 1) * 128], rhs=ident, reads=['xb', 'cst'], writes=[pn])
        k.ts('dve', h[:, c, :], pb[:, :], gcol[:, c:c + 1], None, ALU.mult, reads=[pn, 'pp2'], writes=[bufs.get('hTn', 'hT')])


def load_w_bf16(S, k, dst, dstn, src_ap, stage, stagen, nk, ncols, key):
    for kt in range(nk):
        c0 = 0
        while c0 < ncols:
            w = min(stage.shape[-1], ncols - c0)
            S.dma('sp', stage[:, 0:w], src_ap[kt * 128:(kt + 1) * 128, c0:c0 + w], writes=[stagen], key=key)
            k.cp('dve', dst[:, kt, c0:c0 + w], stage[:, 0:w], reads=[stagen], writes=[dstn])
            c0 += w


def phase_attn(nc, S, k, dr, bank, cm):
    ident = cm['ident']; ones_f = cm['ones_f']; pp2 = cm['pp2']
    S.push()
    bufs = dict(xt=S.sb("xt", [128, 4, 1024], F32), sq=S.sb("sq", [128, 1024], BF16), ss=S.sb("ss", [128, 4], F32),
                rstd=S.sb("rstd", [128, 4], F32), xb=S.sb("xb", [128, 4, 1024], BF16), hT=S.sb("hT", [128, 8, 512], BF16), ident=ident)
    stage = S.sb("stage", [128, 1024], F32)
    wcq = S.sb("wcq", [128, 8, 256], BF16)
    load_w_bf16(S, k, wcq, 'wcq', dr['w_cq'], stage, 'stage', 8, 256, 'wst')
    wq = S.sb("wq", [128, 2, 768], BF16)
    load_w_bf16(S, k, wq, 'wq', dr['w_q'], stage, 'stage', 2, 768, 'wst')
    posi = S.sb("posi", [128, TO], I32)
    S.dma('sp', posi[:], dr['pos'].partition_broadcast(128), writes=['posi'], key='pos')
    ang = S.sb("ang", [128, TO], F32)
    k.cp('dve', ang[:], posi[:], reads=['posi'], writes=['ang'])
    cosT = S.sb("cosT", [128, TO], F32); sinT = S.sb("sinT", [128, TO], F32)
    tnf = S.sb("tnf", [128, TO], F32)
    PI = 3.141592653589793
    k.ts('dve', ang[:], ang[:], pp2[:, 16:17], None, ALU.mult, reads=['ang', 'pp2'], writes=['ang'])
    for (dst, dn, shift) in ((sinT, 'sinT', 0.0), (cosT, 'cosT', PI / 2)):
        k.ts('dve', dst[:], ang[:], shift, 1.0 / TWO_PI, ALU.add, ALU.mult, reads=['ang'], writes=[dn])
        k.cp('dve', posi[:], dst[:], reads=[dn], writes=['posi'])
        k.cp('dve', tnf[:], posi[:], reads=['posi'], writes=['tnf'])
        k.ts('dve', dst[:], ang[:], shift, None, ALU.add, reads=['ang'], writes=[dn])
        k.stt('dve', dst[:], tnf[:], -TWO_PI, dst[:], ALU.mult, ALU.add, reads=['tnf', dn], writes=[dn])
        k.ts('dve', dst[:], dst[:], -PI, PI, ALU.max, ALU.min, reads=[dn], writes=[dn])
        k.act(dst[:], dst[:], AF.Sin, reads=[dn], writes=[dn])
    QT = cm['QT']
    cq = S.sb("cq", [128, 2, 512], F32); cqs = S.sb("cqs", [128, 2, 512], BF16); cqn = S.sb("cqn", [128, 2, 512], BF16)
    rq = S.sb("rq", [128, 512], F32)
    x1s = S.sb("x1s", [128, 512], F32); x2s = S.sb("x2s", [128, 512], F32)
    ta = S.sb("ta", [128, 512], F32); tb = S.sb("tb", [128, 512], F32)
    x1p = S.sb("x1p", [128, 512], BF16); x2p = S.sb("x2p", [128, 512], BF16)
    for blk in range(4):
        T0 = blk * 512
        norm_T(S, k, bank, dr['x'][T0:T0 + 512, :], pp2[:, 0:8], bufs)
        hT = bufs['hT']
        pbs = []
        for t in range(2):
            pb, pn = bank()
            for c in range(8):
                k.mm(pb[:, :], lhsT=wcq[:, c, t * 128:(t + 1) * 128], rhs=hT[:, c, :], start=(c == 0), stop=(c == 7), reads=['wcq', 'hT'], writes=[pn])
            k.cp('act', cq[:, t, :], pb[:, :], reads=[pn], writes=['cq'])
        k.act(cqs[:], cq[:], AF.Square, reads=['cq'], writes=['cqs'])
        pb, pn = bank()
        for t in range(2):
            k.mm(pb[:, :], lhsT=ones_f[:], rhs=cqs[:, t, :], start=(t == 0), stop=(t == 1), reads=['ones_f', 'cqs'], writes=[pn])
        k.rsqrt(rq[:], pb[:, :], 1.0 / 256, 1e-6, reads=[pn], writes=['rq'])
        for t in range(2):
            k.stt('dve', cqn[:, t, :], cq[:, t, :], pp2[:, 8 + t:9 + t], rq[:], ALU.mult, ALU.mult, reads=['cq', 'pp2', 'rq'], writes=['cqn'])
        for hp in range(4):
            pb, pn = bank()
            for t in range(2):
                k.mm(pb[:, :], lhsT=wq[:, t, hp * 128:(hp + 1) * 128], rhs=cqn[:, t, :], start=(t == 0), stop=(t == 1), reads=['wq', 'cqn'], writes=[pn])
            k.cp('act', QT[0:64, 2 * hp, T0:T0 + 512], pb[0:64, :], reads=[pn], writes=['QT'])
            k.cp('dve', QT[0:64, 2 * hp + 1, T0:T0 + 512], pb[64:128, :], reads=[pn], writes=['QT'])
        for (dst, c0) in ((x1s, 512), (x2s, 640)):
            pb, pn = bank()
            for t in range(2):
                k.mm(pb[:, :], lhsT=wq[:, t, c0:c0 + 128], rhs=cqn[:, t, :], start=(t == 0), stop=(t == 1), reads=['wq', 'cqn'], writes=[pn])
            k.cp('act', dst[:], pb[:, :], reads=[pn], writes=['x1s' if c0 == 512 else 'x2s'])
        nc_ = cosT[:, T0:T0 + 512]; ns_ = sinT[:, T0:T0 + 512]
        k.tt('dve', ta[:], x1s[:], nc_, ALU.mult, reads=['x1s', 'cosT'], writes=['ta'])
        k.tt('pool', tb[:], x2s[:], ns_, ALU.mult, reads=['x2s', 'sinT'], writes=['tb'])
        k.tt('dve', x1p[:], ta[:], tb[:], ALU.subtract, reads=['ta', 'tb'], writes=['x1p'])
        k.tt('dve', ta[:], x2s[:], nc_, ALU.mult, reads=['x2s', 'cosT', 'x1p'], writes=['ta'])
        k.tt('pool', tb[:], x1s[:], ns_, ALU.mult, reads=['x1s', 'sinT', 'x1p'], writes=['tb'])
        k.tt('dve', x2p[:], ta[:], tb[:], ALU.add, reads=['ta', 'tb'], writes=['x2p'])
        for h in range(8):
            S.dma('sp', QT[64:80, h, T0:T0 + 512], x1p[h * 16:(h + 1) * 16, :], reads=['x1p'], writes=['QT'], key='qr')
            S.dma('sp', QT[80:96, h, T0:T0 + 512], x2p[h * 16:(h + 1) * 16, :], reads=['x2p'], writes=['QT'], key='qr')
    S.pop()

    S.push()
    ymla = cm['ymlaT']
    Kh = S.sb("Kh", [96, T], BF16)
    Vh = S.sb("Vh", [128, 128, 128], BF16)
    k.memset('pool', Vh[:, :, 64:128], 1.0, writes=['Vh'])
    PT = [S.sb("PT%d" % i, [128, 512], BF16) for i in range(3)]
    osb = S.sb("osb", [128, 512], F32); rden = S.sb("rden", [64, 512], F32)
    nh = dr.get('_nh', 8)
    it = 0
    S.dma('sp', Kh[64:96, :], dr['kr_d'], reads=['kr_d'], writes=['Kh'], key='kh')
    for h in range(nh):
        S.dma('sp', Kh[0:64, :], dr['kTn_d'][h], reads=['kTn_d'], writes=['Kh'], key='kh')
        S.dma('sp', Vh[:, :, 0:64], dr['vtok_d'][h], reads=['vtok_d'], writes=['Vh'], key='vh')
        for qg in range(4):
            acc, accn = cm['banks'][6 + (qg % 2)]
            def tail(pb, pn, kt):
                nonlocal it
                pt = PT[it % 3]; ptn = 'PT%d' % (it % 3); it += 1
                k.act(pt[:], pb[:, :], AF.Exp, scale=ATT_SCALE, reads=[pn], writes=[ptn])
                k.mm(acc[:, :], lhsT=Vh[:, kt, :], rhs=pt[:], start=(kt == 0), stop=(kt == 127), reads=['Vh', ptn], writes=[accn])
            pend = []
            for kt in range(128):
                pb, pn = bank(0, 6)
                k.mm(pb[:, :], lhsT=Kh[:, kt * 128:(kt + 1) * 128], rhs=QT[:, h, qg * 512:(qg + 1) * 512], reads=['Kh', 'QT'], writes=[pn])
                pend.append((pb, pn, kt))
                if len(pend) > 2:
                    tail(*pend.pop(0))
            while pend:
                tail(*pend.pop(0))
            k.cp('dve', osb[:], acc[:, :], reads=[accn], writes=['osb'])
            S.op('dve', lambda e: e.reciprocal(out=osb[64:128, :], in_=osb[64:128, :]), reads=['osb'], writes=['osb'])
            k.cp('dve', rden[:], osb[64:128, :], reads=['osb'], writes=['rden'])
            k.tt('dve', ymla[(h % 2) * 64:(h % 2) * 64 + 64, h // 2, qg * 512:(qg + 1) * 512], osb[0:64, :], rden[:], ALU.mult, reads=['osb', 'rden'], writes=['ymlaT'])
    S.pop()


NPP2 = 48


def common2(nc, S, k, dr):
    cm = {}
    banks, bank = make_banks(S)
    cm['banks'] = banks
    cst_st = S.sb("cst_st", [128, 128], F32)
    S.dma('sp', cst_st[:], dr['cst'][0], writes=['cst_st'], key='c2')
    ident = S.sb("ident", [128, 128], BF16)
    k.cp('dve', ident[:], cst_st[:], reads=['cst_st'], writes=['cst'])
    cm['ident'] = ident[:]
    ones_f = S.sb("ones_f", [128, 128], BF16)
    k.memset('pool', ones_f[:], 1.0, writes=['ones_f'])
    cm['ones_f'] = ones_f
    pp2 = S.sb("pp2", [128, NPP2], F32)
    S.dma('sp', pp2[:], dr['pp2'], writes=['pp2'], key='c1')
    cm['pp2'] = pp2
    epst = S.sb("epst", [128, 4], F32)
    k.epsc = {}
    for i_, ev in enumerate([1e-6, 1e-24, 64e-5]):
        k.memset('pool', epst[:, i_:i_ + 1], ev, writes=['epsc'])
        k.epsc[ev] = epst[:, i_:i_ + 1]
    cm['epsc'] = k.epsc
    negpi = S.sb("negpi", [128, 1], F32)
    k.memset('pool', negpi[:], -3.141592653589793, writes=['negpi'])
    cm['negpi'] = negpi
    return cm, bank


def alloc_attn(S, cm):
    cm['QT'] = S.sb("QT", [96, 8, TO], BF16)
    cm['ymlaT'] = S.sb("ymlaT", [128, 4, TO], BF16)


def prep2_core(inp, c):
    o = {}
    tok = slice(c * TO, (c + 1) * TO)
    w_in = inp['w_in'][0]
    o['x'] = np.ascontiguousarray(inp['x'][0, tok])
    o['pos'] = np.ascontiguousarray(inp['positions'][0, tok]).astype(np.int32)
    o['w_cq'] = np.ascontiguousarray(w_in[:, 1920:2176])
    wq = inp['mla_w_qup'][0].reshape(256, 8, 96)
    o['w_q'] = np.ascontiguousarray(np.concatenate([wq[:, :, 0:64].reshape(256, 512), wq[:, :, 64:80].reshape(256, 128), wq[:, :, 80:96].reshape(256, 128)], 1))
    pp2 = np.zeros((128, NPP2), np.float32)
    pp2[:, 0:8] = inp['g_mix'][0].reshape(8, 128).T
    pp2[:, 8:10] = inp['mla_g_qa'][0].reshape(2, 128).T
    inv = (10000.0 ** (-np.arange(0, 32, 2, dtype=np.float32) / 32)).astype(np.float32)
    pp2[:, 16] = np.tile(inv, 8)
    pp2[:, 17:25] = inp['g_ffn'][0].reshape(8, 128).T
    pp2[:, 25:33] = inp['g_ple'][0].reshape(8, 128).T
    pp2[:, 33:41] = inp['g_final'].reshape(8, 128).T
    pp2[0:64, 41] = np.tile(inv, 4)
    pp2[0:32, 42] = -1.0; pp2[32:64, 42] = 1.0
    pp2[:, 43] = inp['mla_g_kva'][0]
    o['pp2'] = pp2
    o['cst'] = host_consts()
    o['w_gate'] = np.ascontiguousarray(w_in[:, 2336:4384])
    o['w_a'] = np.ascontiguousarray(inp['w_br_rwkv'][0]); o['w_b'] = np.ascontiguousarray(inp['w_br_mla'][0])
    o['w_o'] = np.ascontiguousarray(inp['w_out'][0])
    o['w_pq'] = np.ascontiguousarray(inp['peer_w_q'][0])
    o['sk'] = np.ascontiguousarray(inp['peer_sub_keys'][0].reshape(16, 128, 128))
    o['w_pg'] = np.ascontiguousarray(inp['w_ple_gate'][0]); o['w_pp'] = np.ascontiguousarray(inp['w_ple_proj'][0])
    o['g_fin'] = np.ascontiguousarray(inp['g_final'])
    o['p'] = np.ascontiguousarray(inp['p'][0, 0, tok])
    kr = w_in[:, 1920 + 384:1920 + 416]
    x1c, x2c = kr[:, 0:16], kr[:, 16:32]
    o['w_kvin'] = np.ascontiguousarray(np.concatenate([w_in[:, 1920 + 256:1920 + 384], x1c, x1c, x2c, x2c, x2c, x2c, x1c, x1c], 1))
    wk = inp['mla_w_kvup'][0].reshape(128, 8, 128)
    o['w_kvup'] = np.ascontiguousarray(np.concatenate([wk[:, :, 0:64].reshape(128, 512), wk[:, :, 64:128].reshape(128, 512)], 1))
    o['u_sh'] = np.ascontiguousarray(inp['peer_u'][0, c * 2048:(c + 1) * 2048])
    o['v_sh'] = np.ascontiguousarray(inp['peer_v'][0, c * 2048:(c + 1) * 2048])
    return o


def phase_merge(nc, S, k, dr, bank, cm):
    ident = cm['ident']; pp2 = cm['pp2']
    S.push()
    bufs = dict(xt=S.sb("xt", [128, 4, 1024], F32), sq=S.sb("sq", [128, 1024], BF16), ss=S.sb("ss", [128, 4], F32),
                rstd=S.sb("rstd", [128, 4], F32), xb=S.sb("xb", [128, 4, 1024], BF16), hT=S.sb("hT", [128, 8, 512], BF16), ident=ident)
    stage = S.sb("stage", [128, 1024], F32)
    wg = S.sb("wg", [128, 8, 2048], BF16)
    load_w_bf16(S, k, wg, 'wg', dr['w_gate'], stage, 'stage', 8, 2048, 'wst')
    WA = S.sb("WA", [128, 4, 1024], BF16); WB = S.sb("WB", [128, 4, 1024], BF16); WO = S.sb("WO", [128, 8, 1024], BF16)
    load_w_bf16(S, k, WA, 'WA', dr['w_a'], stage, 'stage', 4, 1024, 'wst')
    load_w_bf16(S, k, WB, 'WB', dr['w_b'], stage, 'stage', 4, 1024, 'wst')
    load_w_bf16(S, k, WO, 'WO', dr['w_o'], stage, 'stage', 8, 1024, 'wst')
    yrw = S.sb("yrw", [128, 4, TO], BF16)
    S.dma('sp', yrw[:], dr['yrw_own'].rearrange("(c p) n -> p c n", p=128), reads=['yrw_own'], writes=['yrw'], key='yrw')
    ymla = cm['ymlaT']
    mT = S.sb("mT", [128, 8, 512], BF16)
    sgA = S.sb("sgA", [128, 512], F32); sgB = S.sb("sgB", [128, 512], F32)
    m1 = S.sb("m1", [128, 512], F32); m2 = S.sb("m2", [128, 512], F32)
    xt = bufs['xt']
    for blk in range(4):
        T0 = blk * 512
        TS = slice(T0, T0 + 512)
        norm_T(S, k, bank, dr['x'][T0:T0 + 512, :], pp2[:, 0:8], bufs)
        hT = bufs['hT']
        for dt in range(8):
            pA, pAn = bank(); pB, pBn = bank(); qA, qAn = bank(); qB, qBn = bank()
            for c in range(8):
                k.mm(pA[:, :], lhsT=wg[:, c, dt * 128:(dt + 1) * 128], rhs=hT[:, c, :], start=(c == 0), stop=(c == 7), reads=['wg', 'hT'], writes=[pAn])
            for c in range(8):
                k.mm(pB[:, :], lhsT=wg[:, c, 1024 + dt * 128:1024 + (dt + 1) * 128], rhs=hT[:, c, :], start=(c == 0), stop=(c == 7), reads=['wg', 'hT'], writes=[pBn])
            for c in range(4):
                k.mm(qA[:, :], lhsT=WA[:, c, dt * 128:(dt + 1) * 128], rhs=yrw[:, c, TS], start=(c == 0), stop=(c == 3), reads=['WA', 'yrw'], writes=[qAn])
            for c in range(4):
                k.mm(qB[:, :], lhsT=WB[:, c, dt * 128:(dt + 1) * 128], rhs=ymla[:, c, TS], start=(c == 0), stop=(c == 3), reads=['WB', 'ymlaT'], writes=[qBn])
            k.act(sgA[:], pA[:, :], AF.Sigmoid, reads=[pAn], writes=['sgA'])
            k.act(sgB[:], pB[:, :], AF.Sigmoid, reads=[pBn], writes=['sgB'])
            k.tt('dve', m1[:], qA[:, :], sgA[:], ALU.mult, reads=[qAn, 'sgA'], writes=['m1'])
            k.tt('dve', m2[:], qB[:, :], sgB[:], ALU.mult, reads=[qBn, 'sgB'], writes=['m2'])
            k.tt('pool', mT[:, dt, :], m1[:], m2[:], ALU.add, reads=['m1', 'm2'], writes=['mT'])
        for j in range(4):
            for hf in range(2):
                pb, pn = bank()
                for m in range(8):
                    k.mm(pb[:, :], lhsT=mT[:, m, j * 128:(j + 1) * 128], rhs=WO[:, m, hf * 512:(hf + 1) * 512], start=(m == 0), stop=(m == 7), reads=['mT', 'WO'], writes=[pn])
                k.tt('dve', xt[:, j, hf * 512:(hf + 1) * 512], pb[:, :], xt[:, j, hf * 512:(hf + 1) * 512], ALU.add, reads=[pn, 'xt'], writes=['xt'])
        S.dma('sp', dr['x1_d'][T0:T0 + 512, :].rearrange("(j p) d -> p j d", p=128), xt[:], reads=['xt'], writes=['x1_d'], key='x1s')
    S.pop()


def phase_peer(nc, S, k, dr, bank, cm):
    ident = cm['ident']; pp2 = cm['pp2']
    banks = cm['banks']
    S.push()
    h2T = S.sb("h2T", [128, 8, TO], BF16)
    S.push()
    bufs = dict(xt=S.sb("xt", [128, 4, 1024], F32), sq=S.sb("sq", [128, 1024], BF16), ss=S.sb("ss", [128, 4], F32),
                rstd=S.sb("rstd", [128, 4], F32), xb=S.sb("xb", [128, 4, 1024], BF16), hT=None, ident=ident)
    stage = S.sb("stage", [128, 1024], F32)
    wpq = S.sb("wpq", [128, 8, 2048], BF16)
    load_w_bf16(S, k, wpq, 'wpq', dr['w_pq'], stage, 'stage', 8, 2048, 'wst')
    skb = S.sb("skb", [128, 16, 128], BF16)
    skT = S.sb("skT", [128, 16, 128], BF16)
    for g4 in range(4):
        S.dma('sp', stage[:, 0:512].rearrange("p (a n) -> p a n", n=128), dr['sk'][g4 * 4:(g4 + 1) * 4].rearrange("a p n -> p a n"), writes=['stage'], key='wst')
        k.cp('dve', skb[:, g4 * 4:(g4 + 1) * 4, :], stage[:, 0:512].rearrange("p (a n) -> p a n", n=128), reads=['stage'], writes=['skb'])
    for g4 in range(4):
        pb, pn = bank()
        for a in range(4):
            k.mm(pb[:, a * 128:(a + 1) * 128], lhsT=skb[:, g4 * 4 + a, :], rhs=ident, reads=['skb', 'cst'], writes=[pn])
        k.cp('dve', skT[:, g4 * 4:(g4 + 1) * 4, :].rearrange("p a n -> p (a n)"), pb[:, :], reads=[pn], writes=['skT'])
    qpT = [S.sb("qpT%d" % i, [128, 512], BF16) for i in range(2)]
    s_sb = S.sb("s_sb", [128, 4, 16, 128], F32)
    for blk in range(4):
        T0 = blk * 512
        bufs['hT'] = h2T[:, :, T0:T0 + 512]
        norm_T(S, k, (lambda: bank(0, 4)), dr['x1_d'][T0:T0 + 512, :], pp2[:, 17:25], bufs)
        hT = bufs['hT']
        for hc in range(16):
            pb, pn = bank(0, 4)
            for c in range(8):
                k.mm(pb[:, :], lhsT=wpq[:, c, hc * 128:(hc + 1) * 128], rhs=hT[:, c, :], start=(c == 0), stop=(c == 7), reads=['wpq', 'hT'], writes=[pn])
            qp = qpT[hc % 2]; qpn = 'qpT%d' % (hc % 2)
            k.cp('act', qp[:], pb[:, :], reads=[pn], writes=[qpn])
            for j in range(4):
                sb_, sn_ = banks[4 + j]
                k.mm(sb_[:, (hc % 4) * 128:(hc % 4 + 1) * 128], lhsT=qp[:, j * 128:(j + 1) * 128], rhs=skT[:, hc, :], reads=[qpn, 'skT'], writes=[sn_])
            if hc % 4 == 3:
                for j in range(4):
                    sb_, sn_ = banks[4 + j]
                    k.cp('dve' if j % 2 else 'act', s_sb[:, j, hc - 3:hc + 1, :].rearrange("p a n -> p (a n)"), sb_[:, :], reads=[sn_], writes=['s_sb'])
        S.dma('sp', dr['s_d'][T0:T0 + 512].rearrange("(j p) a n -> p j a n", p=128), s_sb[:], reads=['s_sb'], writes=['s_d'], key='ssd')
    S.pop()

    S.push()
    st = S.sb("st", [128, 16, 128], F32)
    m16 = S.sb("m16", [128, 16, 16], F32)
    tmp = S.sb("tmp", [128, 256], F32)
    cand = S.sb("cand", [128, 8, 256], F32)
    top16 = S.sb("top16", [128, 8, 16], F32)
    thr = S.sb("thr", [128, 8], F32); mx = S.sb("mx", [128, 8], F32); negm = S.sb("negm", [128, 8], F32)
    e16 = S.sb("e16", [128, 8, 16], F32); Zs = S.sb("Zs", [128, 8], F32); rZ = S.sb("rZ", [128, 8], F32)
    Gb = [S.sb("G%d" % i, [128, 16384], BF16) for i in range(2)]
    RC = 16
    Cb = [S.sb("Cb%d" % i, [128, RC, 128], F32) for i in range(2)]
    Eb = [S.sb("Eb%d" % i, [128, RC, 128], BF16) for i in range(2)]
    Mb = [S.sb("Mb%d" % i, [128, RC, 128], BF16) for i in range(2)]
    UTg = [S.sb("UTg%d" % i, [128, 8, 512], BF16) for i in range(2)]
    Vg = [S.sb("Vg%d" % i, [128, 4, 1024], BF16) for i in range(3)]
    a_sb = [S.sb("a_sb%d" % i, [128, 512], F32) for i in range(2)]
    ga = [S.sb("ga%d" % i, [128, 512], BF16) for i in range(2)]
    gaT = [S.sb("gaT%d" % i, [128, 4, 128], BF16) for i in range(2)]
    x1t = S.sb("x1t", [128, 1024], F32)
    ntile = dr.get('_ntile', 16)
    cnt = {'c': 0}

    def topk(nt):
        N0 = nt * 128
        S.dma('sp', st[:], dr['s_d'][N0:N0 + 128], reads=['s_d'], writes=['st'], key='lst')
        for hc in range(16):
            S.op('dve', lambda e, hc=hc: e.max(out=m16[:, hc, 0:8], in_=st[:, hc, :]), reads=['st'], writes=['m16'])
            S.op('dve', lambda e, hc=hc: e.match_replace(out=tmp[:, 0:128], in_to_replace=m16[:, hc, 0:8], in_values=st[:, hc, :], imm_value=-1e30), reads=['st', 'm16'], writes=['tmp'])
            S.op('dve', lambda e, hc=hc: e.max(out=m16[:, hc, 8:16], in_=tmp[:, 0:128]), reads=['tmp'], writes=['m16'])
        for h in range(8):
            k.tt('pool', cand[:, h, :].rearrange("p (a b) -> p a b", b=16),
                 m16[:, 2 * h, :].unsqueeze(2).to_broadcast([128, 16, 16]),
                 m16[:, 2 * h + 1, :].unsqueeze(1).to_broadcast([128, 16, 16]), ALU.add, reads=['m16'], writes=['cand'])
        for h in range(8):
            S.op('dve', lambda e, h=h: e.max(out=top16[:, h, 0:8], in_=cand[:, h, :]), reads=['cand'], writes=['top16'])
            S.op('dve', lambda e, h=h: e.match_replace(out=tmp[:, :], in_to_replace=top16[:, h, 0:8], in_values=cand[:, h, :], imm_value=-1e30), reads=['cand', 'top16'], writes=['tmp'])
            S.op('dve', lambda e, h=h: e.max(out=top16[:, h, 8:16], in_=tmp[:, :]), reads=['tmp'], writes=['top16'])
        S.op('dve', lambda e: e.tensor_reduce(out=thr[:], in_=top16[:], axis=AX.X, op=ALU.min), reads=['top16'], writes=['thr'])
        S.op('dve', lambda e: e.tensor_reduce(out=mx[:], in_=top16[:], axis=AX.X, op=ALU.max), reads=['top16'], writes=['mx'])
        k.ts('dve', negm[:], mx[:], -1.0, None, ALU.mult, reads=['mx'], writes=['negm'])
        for h in range(8):
            k.act(e16[:, h, :], top16[:, h, :], AF.Exp, bias=negm[:, h:h + 1], reads=['top16', 'negm'], writes=['e16'])
        S.op('dve', lambda e: e.reduce_sum(out=Zs[:], in_=e16[:], axis=AX.X), reads=['e16'], writes=['Zs'])
        S.op('dve', lambda e: e.reciprocal(out=rZ[:], in_=Zs[:]), reads=['Zs'], writes=['rZ'])

    def gbuild(nt):
        G = Gb[nt % 2]; gn = 'G%d' % (nt % 2)
        k.memset('pool', G[:], 0.0, writes=[gn])
        yield
        for h in range(8):
            for ic in range(128 // RC):
                b2 = cnt['c'] % 2; cnt['c'] += 1
                C = Cb[b2]; E = Eb[b2]; M = Mb[b2]
                cn, en, mn = 'Cb%d' % b2, 'Eb%d' % b2, 'Mb%d' % b2
                k.tt('pool', C[:], st[:, 2 * h, ic * RC:(ic + 1) * RC].unsqueeze(2).to_broadcast([128, RC, 128]),
                     st[:, 2 * h + 1, :].unsqueeze(1).to_broadcast([128, RC, 128]), ALU.add, reads=['st'], writes=[cn])
                k.act(E[:], C[:], AF.Exp, bias=negm[:, h:h + 1], reads=[cn, 'negm'], writes=[en])
                k.stt('dve', M[:], C[:], thr[:, h:h + 1], E[:], ALU.is_ge, ALU.mult, reads=[cn, en, 'thr'], writes=[mn])
                Gs = G[:, ic * RC * 128:(ic + 1) * RC * 128].rearrange("p (a b) -> p a b", b=128)
                k.stt('dve', Gs, M[:], rZ[:, h:h + 1], Gs, ALU.mult, ALU.add, reads=[mn, 'rZ', gn], writes=[gn])
                yield

    def dense(nt):
        N0 = nt * 128
        G = Gb[nt % 2]; gn = 'G%d' % (nt % 2)
        acc = [banks[6], banks[7]]
        pre = {}; tr = {}

        def stA(eg):
            b2 = eg % 2; v3 = eg % 3
            if not (dr.get('_nodma') and (nt > 0 or eg > 2)):
                S.dma('sp', UTg[b2][:], dr['UT'][:, :, eg * 512:(eg + 1) * 512], reads=['UT'], writes=['UTg%d' % b2], key='ut%d' % b2)
                S.dma('sp', Vg[v3][:], dr['Vb'][eg * 512:(eg + 1) * 512, :].rearrange("(q p) d -> p q d", p=128), reads=['Vb'], writes=['Vg%d' % v3], key='vg%d' % v3)
            pb, pn = bank(0, 3)
            for c in range(8):
                k.mm(pb[:, :], lhsT=h2T[:, c, N0:N0 + 128], rhs=UTg[b2][:, c, :], start=(c == 0), stop=(c == 7), reads=['h2T', 'UTg%d' % b2], writes=[pn])
            k.act(a_sb[b2][:], pb[:, :], AF.Gelu, reads=[pn], writes=['a_sb%d' % b2])
            k.tt('dve', ga[b2][:], a_sb[b2][:], G[:, eg * 512:(eg + 1) * 512], ALU.mult, reads=['a_sb%d' % b2, gn], writes=['ga%d' % b2])

        def stB(eg):
            b2 = eg % 2
            pt, ptn = bank(3, 6)
            for q in range(4):
                k.mm(pt[:, q * 128:(q + 1) * 128], lhsT=ga[b2][:, q * 128:(q + 1) * 128], rhs=ident, reads=['ga%d' % b2, 'cst'], writes=[ptn])
            k.cp('act', gaT[b2][:].rearrange("p q n -> p (q n)"), pt[:, :], reads=[ptn], writes=['gaT%d' % b2])

        def stC(eg):
            b2 = eg % 2; v3 = eg % 3
            for q in range(4):
                for hf in range(2):
                    k.mm(acc[hf][0][:, :], lhsT=gaT[b2][:, q, :], rhs=Vg[v3][:, q, hf * 512:(hf + 1) * 512],
                         start=(eg == 0 and q == 0), stop=(eg == 31 and q == 3), reads=['gaT%d' % b2, 'Vg%d' % v3], writes=[acc[hf][1]])
        for g in range(34):
            if g < 32:
                stA(g)
            if 0 <= g - 1 < 32:
                stB(g - 1)
            if 0 <= g - 2 < 32:
                stC(g - 2)
            yield
        S.dma('sp', x1t[:], dr['x1_d'][N0:N0 + 128, :], reads=['x1_d'], writes=['x1t'], key='lx1')
        for hf in range(2):
            k.tt('dve', x1t[:, hf * 512:(hf + 1) * 512], acc[hf][0][:, :], x1t[:, hf * 512:(hf + 1) * 512], ALU.add, reads=[acc[hf][1], 'x1t'], writes=['x1t'])
        S.dma('sp', dr['x2_d'][N0:N0 + 128, :], x1t[:], reads=['x1t'], writes=['x2_d'], key='sx2')
        yield

    topk(0)
    if dr.get('_nog'):
        def gbuild(nt):
            yield
    for _ in gbuild(0):
        pass
    for nt in range(ntile):
        gb = None
        if nt + 1 < ntile:
            topk(nt + 1)
            gb = gbuild(nt + 1)
        for _ in dense(nt):
            if gb is not None:
                for _r in range(2):
                    try:
                        next(gb)
                    except StopIteration:
                        gb = None
                        break
        if gb is not None:
            for _ in gb:
                pass
    S.pop()
    S.pop()


def phase_final(nc, S, k, dr, bank, cm):
    ident = cm['ident']; pp2 = cm['pp2']
    S.push()
    bufs = dict(xt=S.sb("xt", [128, 4, 1024], F32), sq=S.sb("sq", [128, 1024], BF16), ss=S.sb("ss", [128, 4], F32),
                rstd=S.sb("rstd", [128, 4], F32), xb=S.sb("xb", [128, 4, 1024], BF16), hT=S.sb("hT", [128, 8, 512], BF16), ident=ident)
    stage = S.sb("stage", [128, 1024], F32)
    Wpg = S.sb("Wpg", [128, 8, 1024], BF16); Wpp = S.sb("Wpp", [128, 2, 1024], BF16)
    load_w_bf16(S, k, Wpg, 'Wpg', dr['w_pg'], stage, 'stage', 8, 1024, 'wst')
    load_w_bf16(S, k, Wpp, 'Wpp', dr['w_pp'], stage, 'stage', 2, 1024, 'wst')
    gfin = S.sb("gfin", [128, 1024], F32)
    S.dma('sp', gfin[:], dr['g_fin'].partition_broadcast(128), writes=['gfin'], key='gf')
    pt = S.sb("pt", [128, 4, 256], F32); pb16 = S.sb("pb16", [128, 4, 256], BF16); pT = S.sb("pT", [128, 2, 512], BF16)
    sg = S.sb("sg", [128, 512], F32); tq = S.sb("tq", [128, 512], F32)
    sq2 = S.sb("sq2", [128, 1024], F32); ss2 = S.sb("ss2", [128, 4], F32); rs2 = S.sb("rs2", [128, 4], F32)
    ot = S.sb("ot", [128, 4, 1024], F32)
    xt = bufs['xt']
    for blk in range(4):
        T0 = blk * 512
        norm_T(S, k, bank, dr['x2_d'][T0:T0 + 512, :], pp2[:, 25:33], bufs)
        hT = bufs['hT']
        S.dma('sp', pt[:], dr['p'][T0:T0 + 512, :].rearrange("(j p) d -> p j d", p=128), writes=['pt'], key='lp')
        k.cp('pool', pb16[:], pt[:], reads=['pt'], writes=['pb16'])
        for kt in range(2):
            pb, pn = bank()
            for j in range(4):
                k.mm(pb[:, j * 128:(j + 1) * 128], lhsT=pb16[:, j, kt * 128:(kt + 1) * 128], rhs=ident, reads=['pb16', 'cst'], writes=[pn])
            k.cp('act', pT[:, kt, :], pb[:, :], reads=[pn], writes=['pT'])
        for j in range(4):
            for hf in range(2):
                HS = slice(hf * 512, (hf + 1) * 512)
                pg, pgn = bank(); pq, pqn = bank()
                for c in range(8):
                    k.mm(pg[:, :], lhsT=hT[:, c, j * 128:(j + 1) * 128], rhs=Wpg[:, c, HS], start=(c == 0), stop=(c == 7), reads=['hT', 'Wpg'], writes=[pgn])
                for kt in range(2):
                    k.mm(pq[:, :], lhsT=pT[:, kt, j * 128:(j + 1) * 128], rhs=Wpp[:, kt, HS], start=(kt == 0), stop=(kt == 1), reads=['pT', 'Wpp'], writes=[pqn])
                k.act(sg[:], pg[:, :], AF.Sigmoid, reads=[pgn], writes=['sg'])
                k.tt('dve', tq[:], pq[:, :], sg[:], ALU.mult, reads=[pqn, 'sg'], writes=['tq'])
                k.tt('pool', xt[:, j, HS], xt[:, j, HS], tq[:], ALU.add, reads=['xt', 'tq'], writes=['xt'])
        for j in range(4):
            k.act(sq2[:], xt[:, j, :], AF.Square, reads=['xt'], writes=['sq2'])
            S.op('dve', lambda e, j=j: e.reduce_sum(out=ss2[:, j:j + 1], in_=sq2[:], axis=AX.X), reads=['sq2'], writes=['ss2'])
        k.rsqrt(rs2[:], ss2[:], 1.0 / D, 1e-6, reads=['ss2'], writes=['rs2'])
        for j in range(4):
            k.stt('dve', ot[:, j, :], xt[:, j, :], rs2[:, j:j + 1], gfin[:], ALU.mult, ALU.mult, reads=['xt', 'rs2', 'gfin'], writes=['ot'])
        S.dma('sp', dr['out'][T0:T0 + 512, :].rearrange("(j p) d -> p j d", p=128), ot[:], reads=['ot'], writes=['out'], key='so')
    S.pop()


def phase_h(nc, S, k, dr, bank, cm):
    ident = cm['ident']; pp2 = cm['pp2']
    S.push()
    bufs = dict(xt=S.sb("xt", [128, 4, 1024], F32), sq=S.sb("sq", [128, 1024], BF16), ss=S.sb("ss", [128, 4], F32),
                rstd=S.sb("rstd", [128, 4], F32), xb=S.sb("xb", [128, 4, 1024], BF16), hT=None, ident=ident)
    hTb = [S.sb("hTb%d" % i, [128, 8, 512], BF16) for i in range(2)]
    stage = S.sb("stage", [128, 1024], F32)
    wsh = S.sb("wsh", [128, 8, 384], BF16)
    load_w_bf16(S, k, wsh, 'wsh', dr['w_sh'], stage, 'stage', 8, 384, 'wst')
    ush = [S.sb("ush%d" % i, [128, 3, 512], BF16) for i in range(2)]
    for blk in range(NB):
        T0 = blk * 512
        b2 = blk % 2
        bufs['hT'] = hTb[b2]; bufs['hTn'] = 'hTb%d' % b2
        norm_T(S, k, bank, dr['x'][T0:T0 + 512, :], pp2[:, 0:8], bufs)
        S.dma('sp', dr['hT_d'][:, :, T0:T0 + 512], hTb[b2][:], reads=['hTb%d' % b2], writes=['hT_d'], key='sh%d' % b2)
        for tI in range(3):
            pb, pn = bank()
            for c in range(8):
                k.mm(pb[:, :], lhsT=wsh[:, c, tI * 128:(tI + 1) * 128], rhs=hTb[b2][:, c, :], start=(c == 0), stop=(c == 7), reads=['wsh', 'hTb%d' % b2], writes=[pn])
            k.cp('act' if tI % 2 else 'dve', ush[b2][:, tI, :], pb[:, :], reads=[pn], writes=['ush%d' % b2])
        S.dma('sp', dr['ush_d'][:, :, T0:T0 + 512].rearrange("a p n -> p a n"), ush[b2][:], reads=['ush%d' % b2], writes=['ush_d'], key='su%d' % b2)
    S.pop()


def phase_kv(nc, S, k, dr, bank, cm):
    ident = cm['ident']; pp2 = cm['pp2']; ones_f = cm['ones_f']
    S.push()
    hTk = [S.sb("hTk%d" % i, [128, 8, 512], BF16) for i in range(2)]
    stage = S.sb("stage", [128, 1024], F32)
    wki = S.sb("wki", [128, 8, 256], BF16)
    load_w_bf16(S, k, wki, 'wki', dr['w_kvin'], stage, 'stage', 8, 256, 'wst')
    wku = S.sb("wku", [128, 1, 1024], BF16)
    load_w_bf16(S, k, wku, 'wku', dr['w_kvup'], stage, 'stage', 1, 1024, 'wst')
    posi = S.sb("posi", [64, 512], I32); ang = S.sb("ang", [64, 512], F32)
    cosT = S.sb("cosT", [64, 512], F32); sinT = S.sb("sinT", [64, 512], F32); tnf = S.sb("tnf", [64, 512], F32)
    PI = 3.141592653589793
    cks = S.sb("cks", [128, 512], F32); ck2 = S.sb("ck2", [128, 512], BF16); rk_ = S.sb("rk_", [128, 512], F32); ckn = S.sb("ckn", [128, 512], BF16)
    krA = S.sb("krA", [64, 512], F32); krB = S.sb("krB", [64, 512], F32); krR = S.sb("krR", [64, 512], BF16)
    kTs = [S.sb("kTs%d" % i, [128, 512], BF16) for i in range(2)]
    vts = [S.sb("vts%d" % i, [128, 512], BF16) for i in range(2)]
    for blk in range(NB):
        T0 = blk * 512
        S.dma('sp', posi[:], dr['pos_all'][T0:T0 + 512].partition_broadcast(64), writes=['posi'], key='pos')
        k.cp('dve', ang[:], posi[:], reads=['posi'], writes=['ang'])
        k.ts('dve', ang[:], ang[:], pp2[0:64, 41:42], None, ALU.mult, reads=['ang', 'pp2'], writes=['ang'])
        for (dst, dn, shift) in ((sinT, 'sinT', 0.0), (cosT, 'cosT', PI / 2)):
            k.ts('dve', dst[:], ang[:], shift, 1.0 / TWO_PI, ALU.add, ALU.mult, reads=['ang'], writes=[dn])
            k.cp('dve', posi[:], dst[:], reads=[dn], writes=['posi'])
            k.cp('dve', tnf[:], posi[:], reads=['posi'], writes=['tnf'])
            k.ts('dve', dst[:], ang[:], shift, None, ALU.add, reads=['ang'], writes=[dn])
            k.stt('dve', dst[:], tnf[:], -TWO_PI, dst[:], ALU.mult, ALU.add, reads=['tnf', dn], writes=[dn])
            k.ts('dve', dst[:], dst[:], -PI, PI, ALU.max, ALU.min, reads=[dn], writes=[dn])
            k.act(dst[:], dst[:], AF.Sin, reads=[dn], writes=[dn])
        k.ts('dve', sinT[:], sinT[:], pp2[0:64, 42:43], None, ALU.mult, reads=['sinT', 'pp2'], writes=['sinT'])
        hT = hTk[blk % 2]; hTn = 'hTk%d' % (blk % 2)
        S.dma('sp', hT[:], dr['hT_d'][:, :, T0:T0 + 512], reads=['hT_d'], writes=[hTn], key='lh%d' % (blk % 2))
        pb, pn = bank()
        for c in range(8):
            k.mm(pb[:, :], lhsT=wki[:, c, 0:128], rhs=hT[:, c, :], start=(c == 0), stop=(c == 7), reads=['wki', hTn], writes=[pn])
        k.cp('act', cks[:], pb[:, :], reads=[pn], writes=['cks'])
        for (dst, dn, c0) in ((krA, 'krA', 128), (krB, 'krB', 192)):
            pb, pn = bank()
            for c in range(8):
                k.mm(pb[0:64, :], lhsT=wki[:, c, c0:c0 + 64], rhs=hT[:, c, :], start=(c == 0), stop=(c == 7), reads=['wki', hTn], writes=[pn])
            k.cp('act', dst[:], pb[0:64, :], reads=[pn], writes=[dn])
        k.act(ck2[:], cks[:], AF.Square, reads=['cks'], writes=['ck2'])
        pb, pn = bank()
        k.mm(pb[:, :], lhsT=ones_f[:], rhs=ck2[:], reads=['ones_f', 'ck2'], writes=[pn])
        k.rsqrt(rk_[:], pb[:, :], 1.0 / 128, 1e-6, reads=[pn], writes=['rk_'])
        k.stt('dve', ckn[:], cks[:], pp2[:, 43:44], rk_[:], ALU.mult, ALU.mult, reads=['cks', 'pp2', 'rk_'], writes=['ckn'])
        for hp in range(4):
            pb, pn = bank()
            k.mm(pb[:, :], lhsT=wku[:, 0, hp * 128:(hp + 1) * 128], rhs=ckn[:], reads=['wku', 'ckn'], writes=[pn])
            kt_ = kTs[hp % 2]; ktn = 'kTs%d' % (hp % 2)
            k.cp('act' if hp % 2 else 'dve', kt_[:], pb[:, :], reads=[pn], writes=[ktn])
            S.dma('sp', dr['kTn_d'][2 * hp, :, T0:T0 + 512], kt_[0:64, :], reads=[ktn], writes=['kTn_d'], key='sk%d' % (hp % 2))
            S.dma('sp', dr['kTn_d'][2 * hp + 1, :, T0:T0 + 512], kt_[64:128, :], reads=[ktn], writes=['kTn_d'], key='sk%d' % (hp % 2))
        for j in range(4):
            pb, pn = bank()
            k.mm(pb[:, :], lhsT=ckn[:, j * 128:(j + 1) * 128], rhs=wku[:, 0, 512:1024], reads=['wku', 'ckn'], writes=[pn])
            vt_ = vts[j % 2]; vtn = 'vts%d' % (j % 2)
            k.cp('act' if j % 2 else 'dve', vt_[:], pb[:, :], reads=[pn], writes=[vtn])
            S.dma('sp', dr['vtok_d'][:, :, blk * 4 + j, :].rearrange("h p d -> p h d"), vt_[:].rearrange("p (h d) -> p h d", d=64), reads=[vtn], writes=['vtok_d'], key='sv%d' % (j % 2))
        k.tt('dve', krA[:], krA[:], cosT[:], ALU.mult, reads=['krA', 'cosT'], writes=['krA'])
        k.tt('pool', krB[:], krB[:], sinT[:], ALU.mult, reads=['krB', 'sinT'], writes=['krB'])
        k.tt('dve', krR[:], krA[:], krB[:], ALU.add, reads=['krA', 'krB'], writes=['krR'])
        S.dma('sp', dr['kr_d'][0:16, T0:T0 + 512], krR[0:16, :], reads=['krR'], writes=['kr_d'], key='skr')
        S.dma('sp', dr['kr_d'][16:32, T0:T0 + 512], krR[32:48, :], reads=['krR'], writes=['kr_d'], key='skr')
    S.pop()


def phase_experts(nc, S, k, dr, bank, cm):
    ident = cm['ident']
    S.push()
    uf = [S.sb("uf%d" % i, [128, 1024], F32) for i in range(2)]
    ub = S.sb("ub", [128, 1024], BF16)
    utt = [S.sb("utt%d" % i, [128, 8, 128], BF16) for i in range(2)]
    vf = [S.sb("vf%d" % i, [128, 1024], F32) for i in range(2)]
    vb = [S.sb("vb%d" % i, [128, 1024], BF16) for i in range(2)]
    for et in range(128):
        b2 = et % 2
        S.dma('sp', uf[b2][:], dr['u_sh'][et * 128:(et + 1) * 128, :], writes=['uf%d' % b2], key='lu%d' % b2)
        k.cp('dve', ub[:], uf[b2][:], reads=['uf%d' % b2], writes=['ub'])
        for g in range(2):
            pb, pn = bank()
            for c4 in range(4):
                c = g * 4 + c4
                k.mm(pb[:, c4 * 128:(c4 + 1) * 128], lhsT=ub[:, c * 128:(c + 1) * 128], rhs=ident, reads=['ub', 'cst'], writes=[pn])
            k.cp('act', utt[b2][:, g * 4:(g + 1) * 4, :].rearrange("p a n -> p (a n)"), pb[:, :], reads=[pn], writes=['utt%d' % b2])
        S.dma('sp', dr['UT'][:, :, et * 128:(et + 1) * 128], utt[b2][:], reads=['utt%d' % b2], writes=['UT'], key='su%d' % b2)
        S.dma('sp', vf[b2][:], dr['v_sh'][et * 128:(et + 1) * 128, :], writes=['vf%d' % b2], key='lv%d' % b2)
        k.cp('pool', vb[b2][:], vf[b2][:], reads=['vf%d' % b2], writes=['vb%d' % b2])
        S.dma('sp', dr['Vb'][et * 128:(et + 1) * 128, :], vb[b2][:], reads=['vb%d' % b2], writes=['Vb'], key='svb%d' % b2)
    S.pop()


F_IN = [('x', [T, D], F32), ('pos_all', [T], I32),
        ('wa_all', [8, 1024, 384], F32), ('w_sh', [1024, 384], F32), ('pp_all', [8, 128, NPP], F32), ('w2s_all', [8, 128, 64], F32), ('a2s_all', [8, 128, 64], F32),
        ('g2h_all', [8, 128, 64], F32), ('w0row_all', [8, 1, 128], F32), ('cst', [5, 128, 128], F32),
        ('pp2', [128, NPP2], F32), ('w_kvin', [1024, 256], F32), ('w_kvup', [128, 1024], F32), ('u_sh', [16384, 1024], F32), ('v_sh', [16384, 1024], F32),
        ('x_own', [TO, D], F32), ('pos', [TO], I32), ('p', [TO, 256], F32), ('ridx', [128, 2], I32),
        ('w_cq', [1024, 256], F32), ('w_q', [256, 768], F32), ('w_gate', [1024, 2048], F32), ('w_a', [512, 1024], F32), ('w_b', [512, 1024], F32),
        ('w_o', [1024, 1024], F32), ('w_pq', [1024, 2048], F32), ('sk', [16, 128, 128], F32), ('w_pg', [1024, 1024], F32), ('w_pp', [256, 1024], F32),
        ('g_fin', [1024], F32)]


def build_nc():
    nc = bass.Bass("TRN2", target_bir_lowering=False)
    dr = {}
    for nm, sh, dt in F_IN:
        dr[nm] = nc.dram_tensor(nm, list(sh), dt, kind="ExternalInput").ap()
    for nm, sh in [('bon_d', [64, T]), ('g_d', [64, T]), ('qh_d', [128, T]), ('ol_d', [64, T]), ('yrw_own', [512, TO]), ('hh_d', [128, NCH, 64]), ('hT_d', [128, 8, T]), ('ush_d', [3, 128, T]),
                   ('kTn_d', [8, 64, T]), ('kr_d', [32, T]), ('vtok_d', [8, 128, 128, 64]), ('UT', [128, 8, 16384]), ('Vb', [16384, 1024])]:
        dr[nm] = nc.dram_tensor(nm, sh, BF16, kind="Internal").ap()
    dr['x1_d'] = nc.dram_tensor('x1_d', [TO, D], F32, kind="Internal").ap()
    dr['x2_d'] = nc.dram_tensor('x2_d', [TO, D], F32, kind="Internal").ap()
    dr['s_d'] = nc.dram_tensor('s_d', [TO, 16, 128], F32, kind="Internal").ap()
    dr['out'] = nc.dram_tensor('out', [TO, D], F32, kind="ExternalOutput").ap()
    S = Sched(nc)
    with S:
        k = K(S)
        cm, bank = common2(nc, S, k, dr)
        phase_h(nc, S, k, dr, bank, cm)
        for hd in range(8):
            d2 = dict(dr)
            d2['wa'] = dr['wa_all'][hd]; d2['pp'] = dr['pp_all'][hd]; d2['w2s'] = dr['w2s_all'][hd]; d2['a2s'] = dr['a2s_all'][hd]
            d2['g2h'] = dr['g2h_all'][hd]; d2['w0row'] = dr['w0row_all'][hd]
            d2['yrw_own'] = dr['yrw_own'][hd * 64:(hd + 1) * 64, :]
            d2['_banks'] = cm['banks']
            S.push()
            rwkv_phase(nc, S, k, d2)
            S.pop()
            k.epsc = cm['epsc']
        phase_kv(nc, S, k, dr, bank, cm)
        phase_experts(nc, S, k, dr, bank, cm)
        dr2 = dict(dr); dr2['x'] = dr['x_own']
        S.push()
        alloc_attn(S, cm)
        phase_attn(nc, S, k, dr2, bank, cm)
        phase_merge(nc, S, k, dr2, bank, cm)
        S.pop()
        phase_peer(nc, S, k, dr2, bank, cm)
        phase_final(nc, S, k, dr2, bank, cm)
        S.finish()
    return nc


def kernel(**inputs):
    inp = {k_: np.asarray(v) for k_, v in inputs.items()}
    x2d = np.ascontiguousarray(inp['x'][0])
    p1 = [prep_core(inp, h) for h in range(8)]
    shared = {'x': x2d, 'pos_all': np.ascontiguousarray(inp['positions'][0]).astype(np.int32),
              'wa_all': np.stack([np.ascontiguousarray(p['wa'][:, 0:384]) for p in p1]), 'w_sh': np.ascontiguousarray(inp['w_in'][0][:, 1536:1920]), 'pp_all': np.stack([p['pp'] for p in p1]),
              'w2s_all': np.stack([p['w2s'] for p in p1]), 'a2s_all': np.stack([p['a2s'] for p in p1]),
              'g2h_all': np.stack([p['g2h'] for p in p1]), 'w0row_all': np.stack([p['w0row'] for p in p1]),
              'u_sh': np.ascontiguousarray(inp['peer_u'][0]), 'v_sh': np.ascontiguousarray(inp['peer_v'][0])}
    nc = build_nc()
    in_maps = []
    for c in range(8):
        p2 = prep2_core(inp, c)
        m = dict(shared)
        for nm, _, _ in F_IN:
            if nm in m:
                continue
            if nm == 'x_own':
                m[nm] = p2['x']
            elif nm == 'ridx':
                m[nm] = np.stack([np.arange(128) * 8 + c, np.arange(128) * 8 + 7 - c], 1).astype(np.int32)
            else:
                m[nm] = p2[nm]
        in_maps.append(m)
    res = run_bass_kernel_spmd(nc, in_maps, core_ids=list(range(8))).results
    out = np.concatenate([np.asarray(r['out']) for r in res], axis=0)
    return out.reshape(1, T, D).astype(np.float32)
```

```python
import contextlib
import numpy as np
import concourse.bass as bass
import concourse.mybir as mybir
from concourse.bass_utils import run_bass_kernel_spmd

F32 = mybir.dt.float32
BF16 = mybir.dt.bfloat16
I32 = mybir.dt.int32
AF = mybir.ActivationFunctionType
ALU = mybir.AluOpType
AX = mybir.AxisListType

ENGS = ['pe', 'act', 'dve', 'pool', 'sp']


class Sched:
    def __init__(self, nc, n_dma_sems=48):
        self.nc = nc
        self.stack = contextlib.ExitStack()
        self.streams = {e: [] for e in ENGS}
        self.cnt = {}
        self.seen = {e: {} for e in ENGS}
        self.last_write = {}
        self.readers = {}
        self.n_dma_sems = n_dma_sems
        self.dma_keys = {}
        self.sems = {}
        self.scopes = [self.stack]
        self.cap = None

    def __enter__(self):
        self.stack.__enter__()
        for e in ENGS:
            self.sems[e] = self.stack.enter_context(self.nc.semaphore("s_" + e))
            self.cnt[e] = 0
        self.dma_pool = [self.stack.enter_context(self.nc.semaphore("d%d" % i)) for i in range(self.n_dma_sems)]
        self.sw_pool = [self.stack.enter_context(self.nc.semaphore("w%d" % i)) for i in range(8)]
        self.sw_keys = {}
        return self

    def __exit__(self, *a):
        return self.stack.__exit__(*a)

    def sb(self, name, shape, dt):
        self.uid = getattr(self, 'uid', 0) + 1
        return self.scopes[-1].enter_context(self.nc.sbuf_tensor("sb%d_%s" % (self.uid, name), list(shape), dt))

    def ps(self, name, shape, dt):
        self.uid = getattr(self, 'uid', 0) + 1
        return self.scopes[-1].enter_context(self.nc.psum_tensor("ps%d_%s" % (self.uid, name), list(shape), dt))

    def push(self):
        st = contextlib.ExitStack()
        st.__enter__()
        self.scopes.append(st)

    def pop(self):
        self.barrier()
        st = self.scopes.pop()
        st.__exit__(None, None, None)

    def _deps(self, eng, reads, writes):
        deps = {}
        def add(tok):
            if tok is None:
                return
            k, v = tok
            if deps.get(k, 0) < v:
                deps[k] = v
        for r in reads:
            add(self.last_write.get(r))
        for w in writes:
            add(self.last_write.get(w))
            for t in self.readers.get(w, ()):
                add(t)
        waits = []
        seen = self.seen[eng]
        for k, v in deps.items():
            if k == 'pe' and eng == 'pe':
                continue
            if seen.get(k, 0) >= v:
                continue
            seen[k] = v
            waits.append((k, v))
        return waits

    def _commit(self, tok, reads, writes):
        for w in writes:
            self.last_write[w] = tok
            self.readers[w] = []
        for r in reads:
            if r in writes:
                continue
            self.readers.setdefault(r, []).append(tok)

    @staticmethod
    def _excl(reads, writes):
        pr = [r for r in reads if isinstance(r, str) and r.startswith('pb')]
        if pr:
            reads = [r for r in reads if r not in pr]
            writes = list(writes) + [r for r in pr if r not in writes]
        return reads, writes

    def op(self, eng, fn, reads=(), writes=()):
        if self.cap is not None:
            self.cap.append(('op', (eng, fn, tuple(reads), tuple(writes)), {}))
            return
        reads, writes = self._excl(reads, writes)
        waits = self._deps(eng, reads, writes)
        self.cnt[eng] += 1
        tok = (eng, self.cnt[eng])
        self.streams[eng].append((waits, fn, (eng, 1)))
        self._commit(tok, reads, writes)

    def capture(self, fn, *a):
        self.cap = []
        fn(*a)
        lst, self.cap = self.cap, None
        return lst

    def emit_interleaved(self, A, B):
        ia = ib = 0
        na, nb = len(A), len(B)
        while ia < na or ib < nb:
            if ib >= nb or (ia < na and ia * max(nb, 1) <= ib * max(na, 1)):
                kind, args, kw = A[ia]; ia += 1
            else:
                kind, args, kw = B[ib]; ib += 1
            getattr(self, kind)(*args, **kw)

    def _dkey(self, key):
        if key not in self.dma_keys:
            idx = len(self.dma_keys)
            assert idx < self.n_dma_sems, "out of dma semaphores"
            self.dma_keys[key] = ('dma', idx)
            self.cnt.setdefault(('dma', idx), 0)
        return self.dma_keys[key]

    def dma(self, eng, out, in_, reads=(), writes=(), key=None, **kw):
        if self.cap is not None:
            self.cap.append(('dma', (eng, out, in_), dict(reads=tuple(reads), writes=tuple(writes), key=key, **kw)))
            return
        k = self._dkey(key)
        waits = self._deps(eng, reads, writes)
        self.cnt[k] += 16
        tok = (k, self.cnt[k])
        self.streams[eng].append((waits, (lambda e, o=out, i=in_, kw=kw: e.dma_start(out=o, in_=i, **kw)), (k, 16)))
        self._commit(tok, reads, writes)

    def gather(self, out, in_rows, idx_ap, reads=(), writes=(), key=None):
        if key not in self.sw_keys:
            assert len(self.sw_keys) < len(self.sw_pool)
            self.sw_keys[key] = ('swd', len(self.sw_keys))
            self.cnt.setdefault(self.sw_keys[key], 0)
        kk_ = self.sw_keys[key]
        waits = self._deps('pool', reads, writes)
        self.cnt[kk_] += 16
        tok = (kk_, self.cnt[kk_])
        self.streams['pool'].append((waits, (lambda e: e.indirect_dma_start(out=out, out_offset=None, in_=in_rows, in_offset=bass.IndirectOffsetOnAxis(ap=idx_ap, axis=0))), (kk_, 16)))
        self._commit(tok, reads, writes)

    def coll(self, kind, op, groups, ins, outs, reads=(), writes=()):
        key = 'coll'
        kk_ = self._dkey(key)
        waits = self._deps('pool', reads, writes)
        self.cnt[kk_] += 16
        tok = (kk_, self.cnt[kk_])
        self.streams['pool'].append((waits, (lambda e: e.collective_compute(kind, op, replica_groups=groups, ins=ins, outs=outs)), (kk_, 16)))
        self._commit(tok, reads, writes)

    def barrier(self):
        for e in ENGS:
            waits = []
            for k, v in self.cnt.items():
                if v == 0 or k == e:
                    continue
                if self.seen[e].get(k, 0) >= v:
                    continue
                self.seen[e][k] = v
                waits.append((k, v))
            if waits:
                self.streams[e].append((waits, None, None))
        for e in ENGS:
            if self.cnt[e] and self.seen[e].get(e, 0) < self.cnt[e]:
                self.seen[e][e] = self.cnt[e]
                self.streams[e].append(([(e, self.cnt[e])], None, None))
        self.last_write.clear()
        self.readers.clear()
        self.dma_keys = {}

    def _sem(self, k):
        if isinstance(k, tuple) and k[0] == 'swd':
            return self.sw_pool[k[1]]
        if isinstance(k, tuple):
            return self.dma_pool[k[1]]
        return self.sems[k]

    def finish(self):
        self.barrier()
        nc = self.nc
        streams = self.streams
        sem = self._sem

        def replay(engname):
            def run(eng):
                for waits, fn, inc in streams[engname]:
                    for k, v in waits:
                        eng.wait_ge(sem(k), v)
                    if fn is not None:
                        ins = fn(eng)
                        ins.then_inc(sem(inc[0]), inc[1])
            return run

        with nc.Block() as block:
            block.tensor(replay('pe'))
            block.scalar(replay('act'))
            block.vector(replay('dve'))
            block.gpsimd(replay('pool'))
            block.sync(replay('sp'))


T = 16384
D = 1024
NB = 32
CL = 128
NCH = T // CL
CDEC = 0.6065306597126334
NPP = 32


class K:
    def __init__(self, S):
        self.S = S
        self.nbank = 0

    def mm(self, out, lhsT, rhs, start=True, stop=True, reads=(), writes=()):
        self.S.op('pe', lambda e: e.matmul(out, lhsT=lhsT, rhs=rhs, start=start, stop=stop), reads=reads, writes=writes)

    def act(self, out, in_, func, bias=None, scale=None, reads=(), writes=()):
        kw = {}
        if bias is not None:
            kw['bias'] = bias
        if scale is not None:
            kw['scale'] = scale
        self.S.op('act', lambda e: e.activation(out=out, in_=in_, func=func, **kw), reads=reads, writes=writes)

    def tt(self, eng, out, in0, in1, op, reads=(), writes=()):
        self.S.op(eng, lambda e: e.tensor_tensor(out=out, in0=in0, in1=in1, op=op), reads=reads, writes=writes)

    def ts(self, eng, out, in0, s1, s2, op0, op1=None, reads=(), writes=()):
        if op1 is None and op0 == ALU.pow:
            self.S.op(eng, lambda e: e.tensor_scalar(out=out, in0=in0, scalar1=1.0, scalar2=s1, op0=ALU.mult, op1=ALU.pow), reads=reads, writes=writes)
        elif op1 is None:
            self.S.op(eng, lambda e: e.tensor_scalar(out=out, in0=in0, scalar1=s1, scalar2=0.0, op0=op0, op1=ALU.add), reads=reads, writes=writes)
        else:
            self.S.op(eng, lambda e: e.tensor_scalar(out=out, in0=in0, scalar1=s1, scalar2=s2, op0=op0, op1=op1), reads=reads, writes=writes)

    def rsqrt(self, out, in_, scale, eps, reads=(), writes=()):
        self.S.op('act', lambda e: e.activation(out=out, in_=in_, func=AF.Sqrt, bias=self.eps_ap(eps, out), scale=scale), reads=list(reads) + ['epsc'], writes=writes)
        self.S.op('dve', lambda e: e.reciprocal(out=out, in_=out), reads=writes, writes=writes)

    def eps_ap(self, eps, out):
        n = out.shape[0]
        return self.epsc[eps][0:n, 0:1]

    def stt(self, eng, out, in0, scalar, in1, op0, op1, reads=(), writes=()):
        self.S.op(eng, lambda e: e.scalar_tensor_tensor(out=out, in0=in0, scalar=scalar, in1=in1, op0=op0, op1=op1), reads=reads, writes=writes)

    def cp(self, eng, out, in_, reads=(), writes=()):
        if eng == 'act':
            self.S.op('act', lambda e: e.activation(out=out, in_=in_, func=AF.Copy), reads=reads, writes=writes)
        else:
            self.S.op(eng, lambda e: e.tensor_copy(out=out, in_=in_), reads=reads, writes=writes)

    def memset(self, eng, ap, val, writes=()):
        self.S.op(eng, lambda e: e.memset(ap, val), reads=(), writes=writes)


def rwkv_phase(nc, S, k, dr, core_dbg=None):
    x_d = dr['x']
    nblk = dr.get('_nblk', NB)
    lvl = dr.get('_lvl', 99)
    banks = dr.get('_banks') or [(S.ps("pb%d" % i, [128, 512], F32), "pb%d" % i) for i in range(8)]
    st = {'b': 0, 'lo': 0, 'hi': 8}

    def bank():
        b = banks[st['lo'] + st['b'] % (st['hi'] - st['lo'])]
        st['b'] += 1
        return b

    wa = S.sb("wa", [128, 8, 384], BF16)
    wst_ = S.sb("wst_", [128, 384], F32)
    for c in range(8):
        S.dma('sp', wst_[:], dr['wa'][c * 128:(c + 1) * 128, 0:384], writes=['wst_'], key='c0')
        k.cp('dve', wa[:, c, :], wst_[:], reads=['wst_'], writes=['wa'])
    pp = S.sb("pp", [128, NPP], F32)
    S.dma('sp', pp[:], dr['pp'], writes=['pp'], key='c1')
    cst_st = S.sb("cst_st", [128, 5, 128], F32)
    S.dma('sp', cst_st[:], dr['cst'].rearrange("m p n -> p m n"), writes=['cst_st'], key='c2')
    cst = S.sb("cst", [128, 5, 128], BF16)
    k.cp('dve', cst[:], cst_st[:], reads=['cst_st'], writes=['cst'])
    ident = cst[:, 0, :]
    m4 = S.sb("m4", [128, 4, 4, 128], BF16)
    for mi in range(4):
        for j in range(4):
            k.cp('pool', m4[:, mi, j, :], cst[:, 1 + mi, :], reads=['cst'], writes=['m4'])
    id4 = S.sb("id4", [128, 4, 128], BF16)
    for j in range(4):
        k.cp('pool', id4[:, j, :], cst[:, 0, :], reads=['cst'], writes=['id4'])
    ones_bd = S.sb("ones_bd", [128, 128], BF16)
    k.memset('pool', ones_bd[:], 0.0, writes=['ones_bd'])
    k.memset('pool', ones_bd[0:64, 0:64], 1.0, writes=['ones_bd'])
    k.memset('pool', ones_bd[64:128, 64:128], 1.0, writes=['ones_bd'])
    ones_f = S.sb("ones_f", [128, 128], BF16)
    k.memset('pool', ones_f[:], 1.0, writes=['ones_f'])
    bd_st = S.sb("bd_st", [128, 2, 128], F32)
    k.memset('pool', bd_st[:], 0.0, writes=['bd_st'])
    S.dma('sp', bd_st[0:64, 0, 0:64], dr['w2s'][0:64, :], reads=['bd_st'], writes=['bd_st'], key='c3')
    S.dma('sp', bd_st[64:128, 0, 64:128], dr['w2s'][64:128, :], reads=['bd_st'], writes=['bd_st'], key='c3')
    S.dma('sp', bd_st[0:64, 1, 0:64], dr['a2s'][0:64, :], reads=['bd_st'], writes=['bd_st'], key='c3')
    S.dma('sp', bd_st[64:128, 1, 64:128], dr['a2s'][64:128, :], reads=['bd_st'], writes=['bd_st'], key='c3')
    bd = S.sb("bd", [128, 2, 128], BF16)
    k.cp('dve', bd[:], bd_st[:], reads=['bd_st'], writes=['bd'])
    g2_st = S.sb("g2_st", [128, 64], F32)
    S.dma('sp', g2_st[:], dr['g2h'], writes=['g2_st'], key='c4')
    g2h = S.sb("g2h", [128, 64], BF16)
    k.cp('dve', g2h[:], g2_st[:], reads=['g2_st'], writes=['g2h'])
    w0r_st = S.sb("w0r_st", [1, 128], F32)
    S.dma('sp', w0r_st[:], dr['w0row'], writes=['w0r_st'], key='c5')
    w0row = S.sb("w0row", [1, 128], BF16)
    k.cp('dve', w0row[:], w0r_st[:], reads=['w0r_st'], writes=['w0row'])
    epst = S.sb("epst", [128, 4], F32)
    k.epsc = {}
    for i_, ev in enumerate([1e-6, 1e-24, 64e-5]):
        k.memset('pool', epst[:, i_:i_ + 1], ev, writes=['epsc'])
        k.epsc[ev] = epst[:, i_:i_ + 1]
    omka = S.sb("omka", [128, 1], F32)
    k.ts('dve', omka[:], pp[:, 9:10], -1.0, 1.0, ALU.mult, ALU.add, reads=['pp'], writes=['omka'])

    MT_all = S.sb("MT_all", [128, NCH, 128], BF16)
    k.memset('pool', MT_all[:], 0.0, writes=['MT_all'])
    N_all = S.sb("N_all", [128, NCH, 64], BF16)
    gamL = S.sb("gamL", [128, NCH], F32)
    Hh = S.sb("Hh", [128, NCH + 1, 64], BF16)

    hT = [S.sb("hT%d" % i, [128, 8, 512], BF16) for i in range(2)]
    U = [S.sb("U%d" % i, [128, 6, 514], BF16) for i in range(3)]
    for i in range(3):
        k.memset('pool', U[i][:], 0.0, writes=['U%d' % i])

    def w(name, shape, dt=BF16):
        return S.sb(name, shape, dt)
    tsum6 = w("tsum6", [128, 6, 512], BF16)
    us2 = [w("us%d" % i, [128, 6, 512], BF16) for i in range(2)]
    us = us2[0]
    hmu = w("hmu", [128, 6], F32); omu = w("omu", [128, 6], F32)
    k.ts('dve', hmu[:], pp[:, 0:6], 0.5, None, ALU.mult, reads=['pp'], writes=['hmu'])
    k.ts('dve', omu[:], pp[:, 0:6], -1.0, 1.0, ALU.mult, ALU.add, reads=['pp'], writes=['omu'])
    tl = w("tl", [128, 512]); sl = w("sl", [128, 512])
    sg_tok = w("sg_tok", [128, 4, 128])
    Gi = w("Gi", [128, 512], F32); Ginv = w("Ginv", [128, 512], F32); Ge = w("Ge", [128, 512], F32); Gh = w("Gh", [128, 512], F32)
    tot = w("tot", [128, 4], F32); nct = w("nct", [128, 4], F32)
    a_t = w("a_t", [128, 512], F32)
    kk = w("kk", [128, 512], F32); kk2 = w("kk2", [128, 512]); rn = w("rn", [128, 512], F32); kkn = w("kkn", [128, 512], F32)
    t1 = rn; kdir = kk; bvec = a_t
    At2 = [w("At%d" % i, [128, 512]) for i in range(2)]; Bt2 = [w("Bt%d" % i, [128, 512]) for i in range(2)]
    Kt2 = [w("Kt%d" % i, [128, 512]) for i in range(2)]; Rt2 = [w("Rt%d" % i, [128, 512]) for i in range(2)]
    Bht2 = [w("Bht%d" % i, [128, 512]) for i in range(2)]; Kht2 = [w("Kht%d" % i, [128, 512]) for i in range(2)]
    At, Bt, Kt, Rt, Bht, Kht = At2[0], Bt2[0], Kt2[0], Rt2[0], Bht2[0], Kht2[0]
    rk = w("rk", [128, 512]); bon = w("bon", [64, 512]); g_t = w("g_t", [64, 512])
    Sm = [w("Sm%d" % i, [128, 8, 128]) for i in range(2)]
    SmT = [w("SmT%d" % i, [128, 8, 128]) for i in range(2)]
    Qm = [w("Qm%d" % i, [128, 8, 128]) for i in range(2)]
    AakT = w("AakT", [128, 8, 128]); TrbT = w("TrbT", [128, 8, 128]); TrkT = w("TrkT", [128, 8, 128])
    AXm = w("AXm", [128, 8, 128]); WU = w("WU", [128, 8, 128])
    Bh_tok = w("Bh_tok", [128, 8, 64]); Kh_tok = w("Kh_tok", [128, 8, 64]); V_tok = w("V_tok", [128, 4, 64])
    QhT = w("QhT", [128, 512]); Oloc = w("Oloc", [64, 512])

    MASK = {0: {'ss': 0, 'si': 1}, 1: {'ss': 2, 'si': 3}}
    MASK_TS = {0: 2, 1: 0}

    def load_h(b):
        S.dma('sp', hT[b % 2][:], dr['hT_d'][:, :, b * 512:(b + 1) * 512], reads=['hT_d'], writes=['hT%d' % (b % 2)], key='lh%d' % (b % 2))

    def project(b):
        if b == 0:
            load_h(0)
        if b + 1 < nblk:
            load_h(b + 1)
        h = hT[b % 2]; hn = 'hT%d' % (b % 2)
        Ub = U[b % 3]; un = 'U%d' % (b % 3)
        S.dma('sp', Ub[:, 3:6, 1:513], dr['ush_d'][:, :, b * 512:(b + 1) * 512].rearrange("a p n -> p a n"), reads=['ush_d'], writes=[un], key='lu%d' % (b % 3))
        for tI in range(3):
            pb, pn = bank()
            for c in range(8):
                k.mm(pb[:, :], lhsT=wa[:, c, tI * 128:(tI + 1) * 128], rhs=h[:, c, :], start=(c == 0), stop=(c == 7), reads=['wa', hn], writes=[pn])
            if tI % 2 == 0:
                k.cp('act', Ub[:, tI, 1:513], pb[:, :], reads=[pn], writes=[un])
            else:
                k.cp('dve', Ub[:, tI, 1:513], pb[:, :], reads=[pn], writes=[un])
        if b > 0:
            pu = U[(b - 1) % 3]; pun = 'U%d' % ((b - 1) % 3)
            k.cp('pool', pu[:, :, 513:514], Ub[:, :, 1:2], reads=[un], writes=[pun])
            k.cp('pool', Ub[:, :, 0:1], pu[:, :, 512:513], reads=[pun], writes=[un])
        else:
            k.memset('pool', Ub[:, :, 0:1], 0.0, writes=[un])
        if b == NB - 1:
            k.memset('pool', Ub[:, :, 513:514], 0.0, writes=[un])

    def prep(b):
        Ub = U[b % 3]; un = 'U%d' % (b % 3)
        tok0 = b * 512
        sfx = str(b % 2)
        At = At2[b % 2]; Bt = Bt2[b % 2]; Kt = Kt2[b % 2]; Rt = Rt2[b % 2]; Bht = Bht2[b % 2]; Kht = Kht2[b % 2]; us = us2[b % 2]
        k.tt('pool', tsum6[:], Ub[:, :, 0:512], Ub[:, :, 2:514], ALU.add, reads=[un], writes=['tsum6'])
        k.tt('dve', tsum6[:], tsum6[:], hmu[:, 0:6].unsqueeze(2).to_broadcast([128, 6, 512]), ALU.mult, reads=['tsum6', 'hmu'], writes=['tsum6'])
        k.tt('pool', us[:], Ub[:, :, 1:513], omu[:, 0:6].unsqueeze(2).to_broadcast([128, 6, 512]), ALU.mult, reads=[un, 'omu'], writes=['us' + sfx])
        k.tt('dve', us[:], us[:], tsum6[:], ALU.add, reads=['us' + sfx, 'tsum6'], writes=['us' + sfx])
        r2 = us[:, 0, :]; k2 = us[:, 1, :]; v2 = us[:, 2, :]
        k.act(tl[:], us[:, 3, :], AF.Tanh, reads=['us' + sfx], writes=['tl'])
        pb, pn = bank()
        for j in range(4):
            k.mm(pb[:, j * 128:(j + 1) * 128], lhsT=tl[:, j * 128:(j + 1) * 128], rhs=bd[:, 0, :], start=True, stop=False, reads=['tl', 'bd'], writes=[pn])
            k.mm(pb[:, j * 128:(j + 1) * 128], lhsT=ones_f[0:1, :], rhs=w0row[0:1, :], start=False, stop=True, reads=['ones_f', 'w0row'], writes=[pn])
        k.act(sg_tok[:].rearrange("p j n -> p (j n)"), pb[:, :], AF.Sigmoid, reads=[pn], writes=['sg_tok'])
        pI, pIn = bank(); pE, pEn = bank()
        for j in range(4):
            for d in range(2):
                P = slice(64 * d, 64 * d + 64)
                k.mm(pI[P, j * 128:(j + 1) * 128], lhsT=sg_tok[:, j, P], rhs=cst[:, 2 + 2 * d, :], reads=['sg_tok', 'cst'], writes=[pIn])
                k.mm(pE[P, j * 128:(j + 1) * 128], lhsT=sg_tok[:, j, P], rhs=cst[:, 1 + 2 * d, :], reads=['sg_tok', 'cst'], writes=[pEn])
        pb, pn = bank()
        k.mm(pb[:, :], lhsT=bd[:, 1, :], rhs=us[:, 4, :], reads=['bd', 'us' + sfx], writes=[pn])
        k.act(a_t[:], pb[:, :], AF.Sigmoid, bias=pp[:, 7:8], reads=[pn, 'pp'], writes=['a_t'])
        k.ts('dve', kk[:], k2, pp[:, 8:9], None, ALU.mult, reads=['us' + sfx, 'pp'], writes=['kk'])
        k.tt('pool', kk2[:], kk[:], kk[:], ALU.mult, reads=['kk'], writes=['kk2'])
        pb, pn = bank()
        k.mm(pb[:, :], lhsT=ones_bd[:], rhs=kk2[:], reads=['ones_bd', 'kk2'], writes=[pn])
        k.rsqrt(rn[:], pb[:, :], 1.0, 1e-24, reads=[pn], writes=['rn'])
        k.tt('dve', kkn[:], kk[:], rn[:], ALU.mult, reads=['kk', 'rn'], writes=['kkn'])
        k.ts('dve', t1[:], a_t[:], pp[:, 9:10], omka[:, 0:1], ALU.mult, ALU.add, reads=['a_t', 'pp', 'omka', 'rn', 'kkn'], writes=['rn'])
        k.tt('pool', kdir[:], k2, t1[:], ALU.mult, reads=['us' + sfx, 'rn', 'kkn'], writes=['kk'])
        k.tt('pool', bvec[:], kkn[:], a_t[:], ALU.mult, reads=['kkn', 'a_t', 'rn'], writes=['a_t'])
        k.stt('dve', rk[:], r2, pp[:, 10:11], kdir[:], ALU.mult, ALU.mult, reads=['us' + sfx, 'pp', 'kk'], writes=['rk'])
        pb, pn = bank()
        k.mm(pb[0:64, :], lhsT=ones_f[:, 0:64], rhs=rk[:], reads=['ones_f', 'rk'], writes=[pn])
        k.tt('dve', bon[:], pb[0:64, :], us[0:64, 2, :], ALU.mult, reads=[pn, 'us' + sfx], writes=['bon'])
        S.dma('sp', dr['bon_d'][:, tok0:tok0 + 512], bon[:], reads=['bon'], writes=['bon_d'], key='bon')
        k.act(sl[:], us[:, 5, :], AF.Sigmoid, reads=['us' + sfx], writes=['sl'])
        pb, pn = bank()
        k.mm(pb[0:64, :], lhsT=g2h[:], rhs=sl[:], reads=['g2h', 'sl'], writes=[pn])
        k.cp('act', g_t[:], pb[0:64, :], reads=[pn], writes=['g_t'])
        S.dma('sp', dr['g_d'][:, tok0:tok0 + 512], g_t[:], reads=['g_t'], writes=['g_d'], key='gd')
        k.act(Gi[:], pI[:, :], AF.Exp, scale=-CDEC, reads=[pIn], writes=['Gi'])
        k.act(Ginv[:], pI[:, :], AF.Exp, scale=CDEC, reads=[pIn], writes=['Ginv'])
        k.act(Ge[:], pE[:, :], AF.Exp, scale=-CDEC, reads=[pEn], writes=['Ge'])
        pI3 = pI[:, :].rearrange("p (j n) -> p j n", n=128)
        k.cp('dve', tot[0:64, :], pI3[0:64, :, 127], reads=[pIn], writes=['tot'])
        k.cp('dve', tot[64:128, :], pI3[64:128, :, 0], reads=[pIn], writes=['tot'])
        k.ts('dve', nct[:], tot[:], -CDEC, None, ALU.mult, reads=['tot'], writes=['nct'])
        k.act(gamL[:, b * 4:(b + 1) * 4], tot[:], AF.Exp, scale=-CDEC, reads=['tot'], writes=['gamL'])
        for j in range(4):
            k.act(Gh[:, j * 128:(j + 1) * 128], pI[:, j * 128:(j + 1) * 128], AF.Exp, bias=nct[:, j:j + 1], scale=CDEC, reads=[pIn, 'nct'], writes=['Gh'])
        k.stt('dve', At[:], kkn[:], -1.0, Ge[:], ALU.mult, ALU.mult, reads=['kkn', 'Ge'], writes=['At' + sfx])
        k.tt('pool', Bt[:], bvec[:], Ginv[:], ALU.mult, reads=['a_t', 'Ginv'], writes=['Bt' + sfx])
        k.tt('dve', Kt[:], kdir[:], Ginv[:], ALU.mult, reads=['kk', 'Ginv'], writes=['Kt' + sfx])
        k.tt('pool', Rt[:], r2, Gi[:], ALU.mult, reads=['us' + sfx, 'Gi'], writes=['Rt' + sfx])
        k.tt('dve', Bht[:], bvec[:], Gh[:], ALU.mult, reads=['a_t', 'Gh'], writes=['Bht' + sfx])
        k.tt('pool', Kht[:], kdir[:], Gh[:], ALU.mult, reads=['kk', 'Gh'], writes=['Kht' + sfx])

    def stages(b):
        Ub = U[b % 3]; un = 'U%d' % (b % 3)
        tok0 = b * 512
        sfx = str(b % 2)
        At = At2[b % 2]; Bt = Bt2[b % 2]; Kt = Kt2[b % 2]; Rt = Rt2[b % 2]; Bht = Bht2[b % 2]; Kht = Kht2[b % 2]; us = us2[b % 2]
        def scores(dst, dstn, L, Ln, R, Rn, mask_of_dir, ts_layout=False):
            for d in range(2):
                P = slice(64 * d, 64 * d + 64)
                pb, pn = bank()
                for j in range(4):
                    C = slice(j * 128, (j + 1) * 128)
                    k.mm(pb[:, C], lhsT=L[P, C], rhs=R[P, C], reads=[Ln, Rn], writes=[pn])
                mi = mask_of_dir[d]
                k.tt('dve', dst[:, 4 * d:4 * d + 4, :].rearrange("p j n -> p (j n)"), pb[:, :], m4[:, mi, :, :].rearrange("p j n -> p (j n)"), ALU.mult, reads=[pn, 'm4'], writes=[dstn + '_%d' % d])
        scores(SmT[0], 'SmT0', Bt, 'Bt' + sfx, At, 'At' + sfx, {0: 0, 1: 2})
        scores(Sm[0], 'Sm0', At, 'At' + sfx, Bt, 'Bt' + sfx, {0: 2, 1: 0})
        scores(AakT, 'AakT', Kt, 'Kt' + sfx, At, 'At' + sfx, {0: 0, 1: 2})
        scores(TrbT, 'TrbT', Bt, 'Bt' + sfx, Rt, 'Rt' + sfx, {0: 1, 1: 3})
        scores(TrkT, 'TrkT', Kt, 'Kt' + sfx, Rt, 'Rt' + sfx, {0: 1, 1: 3})
        for d in range(2):
            k.tt('pool', Qm[0][:, 4 * d:4 * d + 4, :], SmT[0][:, 4 * d:4 * d + 4, :], id4[:], ALU.add, reads=['SmT0_%d' % d, 'id4'], writes=['Qm0_%d' % d])
        if lvl < 4:
            return
        cur = 0
        for dl in range(1, dr.get('_ndl', 7)):
            nxt = 1 - cur
            sc, scn = Sm[cur], 'Sm%d' % cur
            stc, stcn = SmT[cur], 'SmT%d' % cur
            sn, snn = Sm[nxt], 'Sm%d' % nxt
            stn, stnn = SmT[nxt], 'SmT%d' % nxt
            for d in range(2):
                pb, pn = bank()
                for j in range(4):
                    c8 = 4 * d + j
                    k.mm(pb[:, j * 128:(j + 1) * 128], lhsT=stc[:, c8, :], rhs=sc[:, c8, :], reads=[stcn + '_%d' % d, scn + '_%d' % d], writes=[pn])
                k.cp(dr.get('_e1', 'act'), sn[:, 4 * d:4 * d + 4, :].rearrange("p j n -> p (j n)"), pb[:, :], reads=[pn], writes=[snn + '_%d' % d])
            if dr.get('_sub', 9) < 1:
                break
            if dl < 6:
                for d in range(2):
                    pb, pn = bank()
                    for j in range(4):
                        c8 = 4 * d + j
                        k.mm(pb[:, j * 128:(j + 1) * 128], lhsT=sc[:, c8, :], rhs=stc[:, c8, :], reads=[stcn + '_%d' % d, scn + '_%d' % d], writes=[pn])
                    k.cp('dve', stn[:, 4 * d:4 * d + 4, :].rearrange("p j n -> p (j n)"), pb[:, :], reads=[pn], writes=[stnn + '_%d' % d])
            qc, qcn = Qm[cur], 'Qm%d' % cur
            qn, qnn = Qm[nxt], 'Qm%d' % nxt
            if dr.get('_sub', 9) < 2:
                break
            for d in range(2):
                pb, pn = bank()
                for j in range(4):
                    c8 = 4 * d + j
                    k.mm(pb[:, j * 128:(j + 1) * 128], lhsT=sn[:, c8, :], rhs=qc[:, c8, :], reads=[snn + '_%d' % d, qcn + '_%d' % d], writes=[pn])
                k.tt('dve', qn[:, 4 * d:4 * d + 4, :].rearrange("p j n -> p (j n)"), pb[:, :], qc[:, 4 * d:4 * d + 4, :].rearrange("p j n -> p (j n)"), ALU.add, reads=[pn, qcn + '_%d' % d], writes=[qnn + '_%d' % d])
            cur = nxt
        Qf, Qfn = Qm[cur], 'Qm%d' % cur
        if lvl < 6:
            return
        def tokmajor(dst, dstn, src, srcn, col0, eng):
            pb, pn = bank()
            for j in range(4):
                k.mm(pb[:, j * 128:(j + 1) * 128], lhsT=src[:, j * 128:(j + 1) * 128], rhs=ident, reads=[srcn, 'cst'], writes=[pn])
            pv = pb[:, :].rearrange("p (j d n) -> p j d n", j=4, d=2, n=64)
            for d in range(2):
                k.cp(eng, dst[:, 4 * d:4 * d + 4, col0:col0 + 64], pv[:, :, d, :], reads=[pn], writes=[dstn + '_%d' % d])
        sub = dr.get('_sub', 9)
        if sub in (0, 9):
            tokmajor(AXm, 'AXm', At, 'At' + sfx, 0, 'act' if sub == 9 else 'dve')
        if sub in (1, 9):
            tokmajor(Bh_tok, 'Bh_tok', Bht, 'Bht' + sfx, 0, 'dve')
        if sub in (2, 9):
            tokmajor(Kh_tok, 'Kh_tok', Kht, 'Kht' + sfx, 0, 'act')
        if sub < 9:
            return
        pb, pn = bank()
        for j in range(4):
            k.mm(pb[:, j * 64:(j + 1) * 64], lhsT=us[0:64, 2, j * 128:(j + 1) * 128], rhs=cst[0:64, 0, 0:64], reads=['us' + sfx, 'cst'], writes=[pn])
        k.cp('dve', V_tok[:], pb[:, 0:256].rearrange("p (c n) -> p c n", n=64), reads=[pn], writes=['V_tok'])
        if lvl < 7:
            return
        pb, pn = bank()
        for d in range(2):
            for j in range(4):
                c8 = 4 * d + j
                k.mm(pb[:, c8 * 64:(c8 + 1) * 64], lhsT=AakT[:, c8, :], rhs=V_tok[:, j, :], reads=['AakT_%d' % d, 'V_tok'], writes=[pn])
        k.cp('act', AXm[:, :, 64:128], pb[:, :].rearrange("p (c n) -> p c n", n=64), reads=[pn], writes=['AXm_0', 'AXm_1'])
        if lvl < 8:
            return
        for d in range(2):
            pb, pn = bank()
            for j in range(4):
                c8 = 4 * d + j
                k.mm(pb[:, j * 128:(j + 1) * 128], lhsT=Qf[:, c8, :], rhs=AXm[:, c8, :], reads=[Qfn + '_%d' % d, 'AXm_%d' % d], writes=[pn])
            k.cp('act' if d == 0 else 'dve', WU[:, 4 * d:4 * d + 4, :].rearrange("p j n -> p (j n)"), pb[:, :], reads=[pn], writes=['WU_%d' % d])
        if lvl < 9:
            return
        pb, pn = bank()
        for d in range(2):
            P = slice(64 * d, 64 * d + 64)
            for j in range(4):
                c8 = 4 * d + j
                k.mm(pb[P, j * 128:(j + 1) * 128], lhsT=WU[:, c8, 0:64], rhs=TrbT[:, c8, :], reads=['WU_%d' % d, 'TrbT_%d' % d], writes=[pn])
        k.tt('dve', QhT[:], pb[:, :], Rt[:], ALU.add, reads=[pn, 'Rt' + sfx], writes=['QhT'])
        S.dma('sp', dr['qh_d'][:, tok0:tok0 + 512], QhT[:], reads=['QhT'], writes=['qh_d'], key='qh')
        pb, pn = bank()
        for j in range(4):
            for d in range(2):
                c8 = 4 * d + j
                k.mm(pb[0:64, j * 128:(j + 1) * 128], lhsT=WU[:, c8, 64:128], rhs=TrbT[:, c8, :], start=(d == 0), stop=False, reads=['WU_%d' % d, 'TrbT_%d' % d], writes=[pn])
                k.mm(pb[0:64, j * 128:(j + 1) * 128], lhsT=V_tok[:, j, :], rhs=TrkT[:, c8, :], start=False, stop=(d == 1), reads=['V_tok', 'TrkT_%d' % d], writes=[pn])
        k.cp('act', Oloc[:], pb[0:64, :], reads=[pn], writes=['Oloc'])
        S.dma('sp', dr['ol_d'][:, tok0:tok0 + 512], Oloc[:], reads=['Oloc'], writes=['ol_d'], key='ol')
        pb, pn = bank()
        for d in range(2):
            P = slice(64 * d, 64 * d + 64)
            for j in range(4):
                c8 = 4 * d + j
                k.mm(pb[P, j * 64:(j + 1) * 64], lhsT=WU[:, c8, 0:64], rhs=Bh_tok[:, c8, :], reads=['WU_%d' % d, 'Bh_tok_%d' % d], writes=[pn])
        k.cp('dve', MT_all[0:64, b * 4:(b + 1) * 4, 0:64], pb[0:64, 0:256].rearrange("p (c n) -> p c n", n=64), reads=[pn], writes=['MT_all'])
        for j in range(4):
            st1 = nblk * 4 - 1 - (b * 4 + j)
            k.cp('dve', MT_all[64:128, st1, 64:128], pb[64:128, j * 64:(j + 1) * 64], reads=[pn], writes=['MT_all'])
        pb, pn = bank()
        for d in range(2):
            P = slice(64 * d, 64 * d + 64)
            for j in range(4):
                c8 = 4 * d + j
                k.mm(pb[P, j * 64:(j + 1) * 64], lhsT=Bh_tok[:, c8, :], rhs=WU[:, c8, 64:128], start=True, stop=False, reads=['WU_%d' % d, 'Bh_tok_%d' % d], writes=[pn])
                k.mm(pb[P, j * 64:(j + 1) * 64], lhsT=Kh_tok[:, c8, :], rhs=V_tok[:, j, :], start=False, stop=True, reads=['Kh_tok_%d' % d, 'V_tok'], writes=[pn])
        k.cp('act', N_all[:, b * 4:(b + 1) * 4, :], pb[:, 0:256].rearrange("p (c n) -> p c n", n=64), reads=[pn], writes=['N_all'])

    def run_direct(fn, b, lo, hi):
        st['lo'], st['hi'] = lo, hi
        fn(b)
        st['lo'], st['hi'] = 0, 8

    def cap(fn, b, lo, hi):
        st['lo'], st['hi'] = lo, hi
        lst = S.capture(fn, b)
        st['lo'], st['hi'] = 0, 8
        return lst
    run_direct(project, 0, 0, 3)
    if nblk > 1:
        run_direct(project, 1, 0, 3)
    run_direct(prep, 0, 0, 8)
    for b in range(nblk):
        if b + 2 < nblk:
            run_direct(project, b + 2, 0, 3)
        A = cap(stages, b, *dr.get('_rgA', (0, 8)))
        Bp = cap(prep, b + 1, *dr.get('_rgB', (0, 8))) if b + 1 < nblk else []
        if not dr.get('_int'):
            S.emit_interleaved(A, []); S.emit_interleaved(Bp, [])
        else:
            S.emit_interleaved(A, Bp)
    if lvl < 10:
        return

    nch = nblk * 4
    S.barrier()
    Hf = Gi[:, 0:64]
    T1 = Gi[:, 64:128]
    k.memset('pool', Hf[:], 0.0, writes=['Hf'])
    k.memset('pool', Hh[:, 0, :], 0.0, writes=['Hh0'])
    def t1_for(s_):
        c0 = s_; c1 = nch - 1 - s_
        k.stt('dve', T1[0:64, :], Hf[0:64, :], gamL[0:64, c0:c0 + 1], N_all[0:64, c0, :], ALU.mult, ALU.add, reads=['Hf', 'gamL', 'N_all'], writes=['T1'])
        k.stt('dve', T1[64:128, :], Hf[64:128, :], gamL[64:128, c1:c1 + 1], N_all[64:128, c1, :], ALU.mult, ALU.add, reads=['Hf', 'gamL', 'N_all'], writes=['T1'])
    t1_for(0)
    for s in range(nch):
        pb, pn = bank()
        k.mm(pb[:, 0:64], lhsT=MT_all[:, s, :], rhs=Hh[:, s, :], reads=['MT_all', 'Hh%d' % s], writes=[pn])
        k.tt('dve', Hh[:, s + 1, :], pb[:, 0:64], T1[:], ALU.add, reads=[pn, 'T1'], writes=['Hh%d' % (s + 1)])
        k.tt('dve', Hf[:], pb[:, 0:64], T1[:], ALU.add, reads=[pn, 'T1'], writes=['Hf'])
        if s + 1 < nch:
            t1_for(s + 1)
    if lvl < 11:
        return
    S.barrier()
    qh = [At, Bt]; ol = [Kt[0:64, :], Rt[0:64, :]]; bo = [Bht[0:64, :], Kht[0:64, :]]; gg = [rk[0:64, :], kk2[0:64, :]]
    of = kkn[0:64, :]; ob = tl[0:64, :]; dd = Ginv[0:64, :]; d2 = sl[0:64, :]; rs = Ge[0:64, :]; yy = Gh[0:64, :]
    yo = [bon, g_t]
    mean_m = AakT[0:64, 0, 0:64]
    k.memset('pool', mean_m[:], 1.0 / 64.0, writes=['mean_m'])
    ridx = S.sb("ridx", [128, 2], I32)
    S.dma('sp', ridx[:], dr['ridx'], writes=['ridx'], key='rix')
    S.dma('sp', dr['hh_d'], Hh[:, 0:NCH, :], reads=['Hh%d' % i for i in range(NCH)], writes=['hh_d'], key='shh')
    rows2k = lambda ap: ap.rearrange("p (b n) -> (p b) n", n=TO)
    qh_o = us[:, 0:4, :].rearrange("p a n -> p (a n)")
    ol_o = hT[0][0:64, 0:4, :].rearrange("p a n -> p (a n)"); bo_o = hT[0][0:64, 4:8, :].rearrange("p a n -> p (a n)")
    gg_o = hT[1][0:64, 0:4, :].rearrange("p a n -> p (a n)")
    HhA = hT[1][:, 4:6, :].rearrange("p a n -> p (a n)"); HhB = hT[1][:, 6:8, :].rearrange("p a n -> p (a n)")
    S.gather(qh_o, rows2k(dr['qh_d']), ridx[:, 0:1], reads=['ridx', 'qh_d'], writes=['qh_o'], key='g1')
    S.gather(ol_o, rows2k(dr['ol_d']), ridx[0:64, 0:1], reads=['ridx', 'ol_d'], writes=['ol_o'], key='g2')
    S.gather(bo_o, rows2k(dr['bon_d']), ridx[0:64, 0:1], reads=['ridx', 'bon_d'], writes=['bo_o'], key='g3')
    S.gather(gg_o, rows2k(dr['g_d']), ridx[0:64, 0:1], reads=['ridx', 'g_d'], writes=['gg_o'], key='g4')
    hrows = dr['hh_d'].rearrange("p (b c) n -> (p b) (c n)", c=16)
    S.gather(HhA, hrows, ridx[:, 0:1], reads=['ridx', 'hh_d'], writes=['HhA'], key='g5')
    S.gather(HhB, hrows, ridx[:, 1:2], reads=['ridx', 'hh_d'], writes=['HhB'], key='g6')
    HhA3 = HhA.rearrange("p (c n) -> p c n", n=64); HhB3 = HhB.rearrange("p (c n) -> p c n", n=64)
    for b in range(4):
        i2 = b % 2
        TS = slice(b * 512, (b + 1) * 512)
        pb, pn = bank()
        for j in range(4):
            cl = b * 4 + j
            C = slice(j * 128, (j + 1) * 128)
            k.cp('dve', TrbT[0:64, j, 0:64], HhA3[0:64, cl, :], reads=['HhA'], writes=['Hc'])
            k.cp('pool', TrbT[64:128, j, 0:64], HhB3[64:128, 15 - cl, :], reads=['HhB'], writes=['Hc'])
            k.mm(pb[0:64, C], lhsT=TrbT[:, j, 0:64], rhs=qh_o[:, b * 512 + j * 128:b * 512 + (j + 1) * 128], reads=['Hc', 'qh_o'], writes=[pn])
        k.tt('dve', of[:], pb[0:64, :], ol_o[:, TS], ALU.add, reads=[pn, 'ol_o'], writes=['of'])
        k.cp('act', ob[:], of[:], reads=['of'], writes=['ob'])
        pb, pn = bank()
        k.mm(pb[0:64, :], lhsT=mean_m[:], rhs=ob[:], reads=['mean_m', 'ob'], writes=[pn])
        k.tt('dve', dd[:], of[:], pb[0:64, :], ALU.subtract, reads=['of', pn], writes=['dd'])
        k.act(d2[:], dd[:], AF.Square, reads=['dd'], writes=['d2'])
        pb, pn = bank()
        k.mm(pb[0:64, :], lhsT=mean_m[:], rhs=d2[:], reads=['mean_m', 'd2'], writes=[pn])
        k.rsqrt(rs[:], pb[0:64, :], 1.0, 64e-5, reads=[pn], writes=['rs'])
        k.tt('dve', yy[:], dd[:], rs[:], ALU.mult, reads=['dd', 'rs'], writes=['yy'])
        k.ts('dve', yy[:], yy[:], pp[0:64, 11:12], pp[0:64, 12:13], ALU.mult, ALU.add, reads=['yy', 'pp'], writes=['yy'])
        k.tt('pool', yy[:], yy[:], bo_o[:, TS], ALU.add, reads=['yy', 'bo_o'], writes=['yy'])
        k.tt('pool', yo[i2][:], yy[:], gg_o[:, TS], ALU.mult, reads=['yy', 'gg_o'], writes=['yo%d' % i2])
        S.dma('sp', dr['yrw_own'][:, TS], yo[i2][:], reads=['yo%d' % i2], writes=['yrw_own'], key='sy%d' % i2)


def host_consts():
    p = np.arange(128)[:, None]; f = np.arange(128)[None, :]
    cst = np.stack([(p == f), (p < f), (p <= f), (p > f), (p >= f)]).astype(np.float32)
    return cst


def prep_core(inp, hd):
    hc = slice(hd * 64, (hd + 1) * 64)
    w_in = inp['w_in'][0]
    o = {}
    r_c = np.arange(hd * 64, (hd + 1) * 64)
    cols = np.concatenate([r_c, r_c, 512 + r_c, 512 + r_c, 1024 + r_c, 1024 + r_c,
                           np.arange(1536, 1920),
                           np.arange(1920 + 256, 1920 + 384),
                           1920 + 384 + np.arange(16), 1920 + 384 + np.arange(16),
                           1920 + 400 + np.arange(16), 1920 + 400 + np.arange(16)])
    o['wa'] = np.ascontiguousarray(w_in[:, cols])
    mu = inp['rw_mu'][0]
    pp = np.zeros((128, NPP), np.float32)
    mucols = cols[:768]
    for tI in range(6):
        pp[:, tI] = mu[mucols[tI * 128:(tI + 1) * 128]]
    st2 = lambda v: np.concatenate([v[hc], v[hc]])
    pp[:, 6] = np.concatenate([inp['rw_w0'][0, 0, hc], inp['rw_w0'][0, 1, hc]])
    pp[:, 7] = np.concatenate([inp['rw_a0'][0, 0, hc], inp['rw_a0'][0, 1, hc]])
    pp[:, 8] = st2(inp['rw_k_k'][0]); pp[:, 9] = st2(inp['rw_k_a'][0]); pp[:, 10] = st2(inp['rw_r_k'][0])
    pp[:, 11] = st2(inp['rw_gn_w'][0]); pp[:, 12] = st2(inp['rw_gn_b'][0])
    pp[:, 13:21] = inp['g_mix'][0].reshape(8, 128).T
    o['pp'] = pp
    o['w2s'] = np.ascontiguousarray(np.concatenate([inp['rw_w2'][0, 0][:, hc], inp['rw_w2'][0, 1][:, hc]], 0))
    o['a2s'] = np.ascontiguousarray(np.concatenate([inp['rw_a2'][0, 0][:, hc], inp['rw_a2'][0, 1][:, hc]], 0))
    o['g2h'] = np.ascontiguousarray(inp['rw_g2'][0][:, hc])
    o['w0row'] = np.ascontiguousarray(pp[:, 6][None, :])
    o['cst'] = host_consts()
    return o


TO = 2048
TWO_PI = 6.283185307179586
ATT_SCALE = 96.0 ** -0.5


def make_banks(S, n=8):
    banks = [(S.ps("pb%d" % i, [128, 512], F32), "pb%d" % i) for i in range(n)]
    st = {'b': 0}

    def bank(lo=0, hi=n):
        b = banks[lo + st['b'] % (hi - lo)]
        st['b'] += 1
        return b
    return banks, bank


def norm_T(S, k, bank, x_rows, gcol, bufs, eps=1e-6):
    xt, sq, ss, rstd, xb, h = bufs['xt'], bufs['sq'], bufs['ss'], bufs['rstd'], bufs['xb'], bufs['hT']
    ident = bufs['ident']
    if x_rows is not None:
        S.dma('sp', xt[:], x_rows.rearrange("(j p) d -> p j d", p=128), writes=['xt'], key='xt')
    for j in range(4):
        k.act(sq[:], xt[:, j, :], AF.Square, reads=['xt'], writes=['sq'])
        S.op('dve', lambda e, j=j: e.reduce_sum(out=ss[:, j:j + 1], in_=sq[:], axis=AX.X), reads=['sq'], writes=['ss'])
    k.rsqrt(rstd[:], ss[:], 1.0 / D, eps, reads=['ss'], writes=['rstd'])
    for j in range(4):
        k.ts('dve' if j % 2 else 'pool', xb[:, j, :], xt[:, j, :], rstd[:, j:j + 1], None, ALU.mult, reads=['xt', 'rstd'], writes=['xb'])
    for c in range(8):
        pb, pn = bank()
        for j in range(4):
            k.mm(pb[:, j * 128:(j + 1) * 128], lhsT=xb[:, j, c * 128:(c + 1) * 128], rhs=ident, reads=['xb', 'cst'], writes=[pn])
        k.ts('dve', h[:, c, :], pb[:, :], gcol[:, c:c + 1], None, ALU.mult, reads=[pn, 'pp2'], writes=[bufs.get('hTn', 'hT')])


def load_w_bf16(S, k, dst, dstn, src_ap, stage, stagen, nk, ncols, key):
    for kt in range(nk):
        c0 = 0
        while c0 < ncols:
            w = min(stage.shape[-1], ncols - c0)
            S.dma('sp', stage[:, 0:w], src_ap[kt * 128:(kt + 1) * 128, c0:c0 + w], writes=[stagen], key=key)
            k.cp('dve', dst[:, kt, c0:c0 + w], stage[:, 0:w], reads=[stagen], writes=[dstn])
            c0 += w


def phase_attn(nc, S, k, dr, bank, cm):
    ident = cm['ident']; ones_f = cm['ones_f']; pp2 = cm['pp2']
    S.push()
    bufs = dict(xt=S.sb("xt", [128, 4, 1024], F32), sq=S.sb("sq", [128, 1024], BF16), ss=S.sb("ss", [128, 4], F32),
                rstd=S.sb("rstd", [128, 4], F32), xb=S.sb("xb", [128, 4, 1024], BF16), hT=S.sb("hT", [128, 8, 512], BF16), ident=ident)
    stage = S.sb("stage", [128, 1024], F32)
    wcq = S.sb("wcq", [128, 8, 256], BF16)
    load_w_bf16(S, k, wcq, 'wcq', dr['w_cq'], stage, 'stage', 8, 256, 'wst')
    wq = S.sb("wq", [128, 2, 768], BF16)
    load_w_bf16(S, k, wq, 'wq', dr['w_q'], stage, 'stage', 2, 768, 'wst')
    posi = S.sb("posi", [128, TO], I32)
    S.dma('sp', posi[:], dr['pos'].partition_broadcast(128), writes=['posi'], key='pos')
    ang = S.sb("ang", [128, TO], F32)
    k.cp('dve', ang[:], posi[:], reads=['posi'], writes=['ang'])
    cosT = S.sb("cosT", [128, TO], F32); sinT = S.sb("sinT", [128, TO], F32)
    tnf = S.sb("tnf", [128, TO], F32)
    PI = 3.141592653589793
    k.ts('dve', ang[:], ang[:], pp2[:, 16:17], None, ALU.mult, reads=['ang', 'pp2'], writes=['ang'])
    for (dst, dn, shift) in ((sinT, 'sinT', 0.0), (cosT, 'cosT', PI / 2)):
        k.ts('dve', dst[:], ang[:], shift, 1.0 / TWO_PI, ALU.add, ALU.mult, reads=['ang'], writes=[dn])
        k.cp('dve', posi[:], dst[:], reads=[dn], writes=['posi'])
        k.cp('dve', tnf[:], posi[:], reads=['posi'], writes=['tnf'])
        k.ts('dve', dst[:], ang[:], shift, None, ALU.add, reads=['ang'], writes=[dn])
        k.stt('dve', dst[:], tnf[:], -TWO_PI, dst[:], ALU.mult, ALU.add, reads=['tnf', dn], writes=[dn])
        k.ts('dve', dst[:], dst[:], -PI, PI, ALU.max, ALU.min, reads=[dn], writes=[dn])
        k.act(dst[:], dst[:], AF.Sin, reads=[dn], writes=[dn])
    QT = cm['QT']
    cq = S.sb("cq", [128, 2, 512], F32); cqs = S.sb("cqs", [128, 2, 512], BF16); cqn = S.sb("cqn", [128, 2, 512], BF16)
    rq = S.sb("rq", [128, 512], F32)
    x1s = S.sb("x1s", [128, 512], F32); x2s = S.sb("x2s", [128, 512], F32)
    ta = S.sb("ta", [128, 512], F32); tb = S.sb("tb", [128, 512], F32)
    x1p = S.sb("x1p", [128, 512], BF16); x2p = S.sb("x2p", [128, 512], BF16)
    for blk in range(4):
        T0 = blk * 512
        norm_T(S, k, bank, dr['x'][T0:T0 + 512, :], pp2[:, 0:8], bufs)
        hT = bufs['hT']
        pbs = []
        for t in range(2):
            pb, pn = bank()
            for c in range(8):
                k.mm(pb[:, :], lhsT=wcq[:, c, t * 128:(t + 1) * 128], rhs=hT[:, c, :], start=(c == 0), stop=(c == 7), reads=['wcq', 'hT'], writes=[pn])
            k.cp('act', cq[:, t, :], pb[:, :], reads=[pn], writes=['cq'])
        k.act(cqs[:], cq[:], AF.Square, reads=['cq'], writes=['cqs'])
        pb, pn = bank()
        for t in range(2):
            k.mm(pb[:, :], lhsT=ones_f[:], rhs=cqs[:, t, :], start=(t == 0), stop=(t == 1), reads=['ones_f', 'cqs'], writes=[pn])
        k.rsqrt(rq[:], pb[:, :], 1.0 / 256, 1e-6, reads=[pn], writes=['rq'])
        for t in range(2):
            k.stt('dve', cqn[:, t, :], cq[:, t, :], pp2[:, 8 + t:9 + t], rq[:], ALU.mult, ALU.mult, reads=['cq', 'pp2', 'rq'], writes=['cqn'])
        for hp in range(4):
            pb, pn = bank()
            for t in range(2):
                k.mm(pb[:, :], lhsT=wq[:, t, hp * 128:(hp + 1) * 128], rhs=cqn[:, t, :], start=(t == 0), stop=(t == 1), reads=['wq', 'cqn'], writes=[pn])
            k.cp('act', QT[0:64, 2 * hp, T0:T0 + 512], pb[0:64, :], reads=[pn], writes=['QT'])
            k.cp('dve', QT[0:64, 2 * hp + 1, T0:T0 + 512], pb[64:128, :], reads=[pn], writes=['QT'])
        for (dst, c0) in ((x1s, 512), (x2s, 640)):
            pb, pn = bank()
            for t in range(2):
                k.mm(pb[:, :], lhsT=wq[:, t, c0:c0 + 128], rhs=cqn[:, t, :], start=(t == 0), stop=(t == 1), reads=['wq', 'cqn'], writes=[pn])
            k.cp('act', dst[:], pb[:, :], reads=[pn], writes=['x1s' if c0 == 512 else 'x2s'])
        nc_ = cosT[:, T0:T0 + 512]; ns_ = sinT[:, T0:T0 + 512]
        k.tt('dve', ta[:], x1s[:], nc_, ALU.mult, reads=['x1s', 'cosT'], writes=['ta'])
        k.tt('pool', tb[:], x2s[:], ns_, ALU.mult, reads=['x2s', 'sinT'], writes=['tb'])
        k.tt('dve', x1p[:], ta[:], tb[:], ALU.subtract, reads=['ta', 'tb'], writes=['x1p'])
        k.tt('dve', ta[:], x2s[:], nc_, ALU.mult, reads=['x2s', 'cosT', 'x1p'], writes=['ta'])
        k.tt('pool', tb[:], x1s[:], ns_, ALU.mult, reads=['x1s', 'sinT', 'x1p'], writes=['tb'])
        k.tt('dve', x2p[:], ta[:], tb[:], ALU.add, reads=['ta', 'tb'], writes=['x2p'])
        for h in range(8):
            S.dma('sp', QT[64:80, h, T0:T0 + 512], x1p[h * 16:(h + 1) * 16, :], reads=['x1p'], writes=['QT'], key='qr')
            S.dma('sp', QT[80:96, h, T0:T0 + 512], x2p[h * 16:(h + 1) * 16, :], reads=['x2p'], writes=['QT'], key='qr')
    S.pop()

    S.push()
    ymla = cm['ymlaT']
    Kh = S.sb("Kh", [96, T], BF16)
    Vh = S.sb("Vh", [128, 128, 128], BF16)
    k.memset('pool', Vh[:, :, 64:128], 1.0, writes=['Vh'])
    PT = [S.sb("PT%d" % i, [128, 512], BF16) for i in range(3)]
    osb = S.sb("osb", [128, 512], F32); rden = S.sb("rden", [64, 512], F32)
    nh = dr.get('_nh', 8)
    it = 0
    S.dma('sp', Kh[64:96, :], dr['kr_d'], reads=['kr_d'], writes=['Kh'], key='kh')
    for h in range(nh):
        S.dma('sp', Kh[0:64, :], dr['kTn_d'][h], reads=['kTn_d'], writes=['Kh'], key='kh')
        S.dma('sp', Vh[:, :, 0:64], dr['vtok_d'][h], reads=['vtok_d'], writes=['Vh'], key='vh')
        for qg in range(4):
            acc, accn = cm['banks'][6 + (qg % 2)]
            def tail(pb, pn, kt):
                nonlocal it
                pt = PT[it % 3]; ptn = 'PT%d' % (it % 3); it += 1
                k.act(pt[:], pb[:, :], AF.Exp, scale=ATT_SCALE, reads=[pn], writes=[ptn])
                k.mm(acc[:, :], lhsT=Vh[:, kt, :], rhs=pt[:], start=(kt == 0), stop=(kt == 127), reads=['Vh', ptn], writes=[accn])
            pend = []
            for kt in range(128):
                pb, pn = bank(0, 6)
                k.mm(pb[:, :], lhsT=Kh[:, kt * 128:(kt + 1) * 128], rhs=QT[:, h, qg * 512:(qg + 1) * 512], reads=['Kh', 'QT'], writes=[pn])
                pend.append((pb, pn, kt))
                if len(pend) > 2:
                    tail(*pend.pop(0))
            while pend:
                tail(*pend.pop(0))
            k.cp('dve', osb[:], acc[:, :], reads=[accn], writes=['osb'])
            S.op('dve', lambda e: e.reciprocal(out=osb[64:128, :], in_=osb[64:128, :]), reads=['osb'], writes=['osb'])
            k.cp('dve', rden[:], osb[64:128, :], reads=['osb'], writes=['rden'])
            k.tt('dve', ymla[(h % 2) * 64:(h % 2) * 64 + 64, h // 2, qg * 512:(qg + 1) * 512], osb[0:64, :], rden[:], ALU.mult, reads=['osb', 'rden'], writes=['ymlaT'])
    S.pop()


NPP2 = 48


def common2(nc, S, k, dr):
    cm = {}
    banks, bank = make_banks(S)
    cm['banks'] = banks
    cst_st = S.sb("cst_st", [128, 128], F32)
    S.dma('sp', cst_st[:], dr['cst'][0], writes=['cst_st'], key='c2')
    ident = S.sb("ident", [128, 128], BF16)
    k.cp('dve', ident[:], cst_st[:], reads=['cst_st'], writes=['cst'])
    cm['ident'] = ident[:]
    ones_f = S.sb("ones_f", [128, 128], BF16)
    k.memset('pool', ones_f[:], 1.0, writes=['ones_f'])
    cm['ones_f'] = ones_f
    pp2 = S.sb("pp2", [128, NPP2], F32)
    S.dma('sp', pp2[:], dr['pp2'], writes=['pp2'], key='c1')
    cm['pp2'] = pp2
    epst = S.sb("epst", [128, 4], F32)
    k.epsc = {}
    for i_, ev in enumerate([1e-6, 1e-24, 64e-5]):
        k.memset('pool', epst[:, i_:i_ + 1], ev, writes=['epsc'])
        k.epsc[ev] = epst[:, i_:i_ + 1]
    cm['epsc'] = k.epsc
    negpi = S.sb("negpi", [128, 1], F32)
    k.memset('pool', negpi[:], -3.141592653589793, writes=['negpi'])
    cm['negpi'] = negpi
    return cm, bank


def alloc_attn(S, cm):
    cm['QT'] = S.sb("QT", [96, 8, TO], BF16)
    cm['ymlaT'] = S.sb("ymlaT", [128, 4, TO], BF16)


def prep2_core(inp, c):
    o = {}
    tok = slice(c * TO, (c + 1) * TO)
    w_in = inp['w_in'][0]
    o['x'] = np.ascontiguousarray(inp['x'][0, tok])
    o['pos'] = np.ascontiguousarray(inp['positions'][0, tok]).astype(np.int32)
    o['w_cq'] = np.ascontiguousarray(w_in[:, 1920:2176])
    wq = inp['mla_w_qup'][0].reshape(256, 8, 96)
    o['w_q'] = np.ascontiguousarray(np.concatenate([wq[:, :, 0:64].reshape(256, 512), wq[:, :, 64:80].reshape(256, 128), wq[:, :, 80:96].reshape(256, 128)], 1))
    pp2 = np.zeros((128, NPP2), np.float32)
    pp2[:, 0:8] = inp['g_mix'][0].reshape(8, 128).T
    pp2[:, 8:10] = inp['mla_g_qa'][0].reshape(2, 128).T
    inv = (10000.0 ** (-np.arange(0, 32, 2, dtype=np.float32) / 32)).astype(np.float32)
    pp2[:, 16] = np.tile(inv, 8)
    pp2[:, 17:25] = inp['g_ffn'][0].reshape(8, 128).T
    pp2[:, 25:33] = inp['g_ple'][0].reshape(8, 128).T
    pp2[:, 33:41] = inp['g_final'].reshape(8, 128).T
    pp2[0:64, 41] = np.tile(inv, 4)
    pp2[0:32, 42] = -1.0; pp2[32:64, 42] = 1.0
    pp2[:, 43] = inp['mla_g_kva'][0]
    o['pp2'] = pp2
    o['cst'] = host_consts()
    o['w_gate'] = np.ascontiguousarray(w_in[:, 2336:4384])
    o['w_a'] = np.ascontiguousarray(inp['w_br_rwkv'][0]); o['w_b'] = np.ascontiguousarray(inp['w_br_mla'][0])
    o['w_o'] = np.ascontiguousarray(inp['w_out'][0])
    o['w_pq'] = np.ascontiguousarray(inp['peer_w_q'][0])
    o['sk'] = np.ascontiguousarray(inp['peer_sub_keys'][0].reshape(16, 128, 128))
    o['w_pg'] = np.ascontiguousarray(inp['w_ple_gate'][0]); o['w_pp'] = np.ascontiguousarray(inp['w_ple_proj'][0])
    o['g_fin'] = np.ascontiguousarray(inp['g_final'])
    o['p'] = np.ascontiguousarray(inp['p'][0, 0, tok])
    kr = w_in[:, 1920 + 384:1920 + 416]
    x1c, x2c = kr[:, 0:16], kr[:, 16:32]
    o['w_kvin'] = np.ascontiguousarray(np.concatenate([w_in[:, 1920 + 256:1920 + 384], x1c, x1c, x2c, x2c, x2c, x2c, x1c, x1c], 1))
    wk = inp['mla_w_kvup'][0].reshape(128, 8, 128)
    o['w_kvup'] = np.ascontiguousarray(np.concatenate([wk[:, :, 0:64].reshape(128, 512), wk[:, :, 64:128].reshape(128, 512)], 1))
    o['u_sh'] = np.ascontiguousarray(inp['peer_u'][0, c * 2048:(c + 1) * 2048])
    o['v_sh'] = np.ascontiguousarray(inp['peer_v'][0, c * 2048:(c + 1) * 2048])
    return o


def phase_merge(nc, S, k, dr, bank, cm):
    ident = cm['ident']; pp2 = cm['pp2']
    S.push()
    bufs = dict(xt=S.sb("xt", [128, 4, 1024], F32), sq=S.sb("sq", [128, 1024], BF16), ss=S.sb("ss", [128, 4], F32),
                rstd=S.sb("rstd", [128, 4], F32), xb=S.sb("xb", [128, 4, 1024], BF16), hT=S.sb("hT", [128, 8, 512], BF16), ident=ident)
    stage = S.sb("stage", [128, 1024], F32)
    wg = S.sb("wg", [128, 8, 2048], BF16)
    load_w_bf16(S, k, wg, 'wg', dr['w_gate'], stage, 'stage', 8, 2048, 'wst')
    WA = S.sb("WA", [128, 4, 1024], BF16); WB = S.sb("WB", [128, 4, 1024], BF16); WO = S.sb("WO", [128, 8, 1024], BF16)
    load_w_bf16(S, k, WA, 'WA', dr['w_a'], stage, 'stage', 4, 1024, 'wst')
    load_w_bf16(S, k, WB, 'WB', dr['w_b'], stage, 'stage', 4, 1024, 'wst')
    load_w_bf16(S, k, WO, 'WO', dr['w_o'], stage, 'stage', 8, 1024, 'wst')
    yrw = S.sb("yrw", [128, 4, TO], BF16)
    S.dma('sp', yrw[:], dr['yrw_own'].rearrange("(c p) n -> p c n", p=128), reads=['yrw_own'], writes=['yrw'], key='yrw')
    ymla = cm['ymlaT']
    mT = S.sb("mT", [128, 8, 512], BF16)
    sgA = S.sb("sgA", [128, 512], F32); sgB = S.sb("sgB", [128, 512], F32)
    m1 = S.sb("m1", [128, 512], F32); m2 = S.sb("m2", [128, 512], F32)
    xt = bufs['xt']
    for blk in range(4):
        T0 = blk * 512
        TS = slice(T0, T0 + 512)
        norm_T(S, k, bank, dr['x'][T0:T0 + 512, :], pp2[:, 0:8], bufs)
        hT = bufs['hT']
        for dt in range(8):
            pA, pAn = bank(); pB, pBn = bank(); qA, qAn = bank(); qB, qBn = bank()
            for c in range(8):
                k.mm(pA[:, :], lhsT=wg[:, c, dt * 128:(dt + 1) * 128], rhs=hT[:, c, :], start=(c == 0), stop=(c == 7), reads=['wg', 'hT'], writes=[pAn])
            for c in range(8):
                k.mm(pB[:, :], lhsT=wg[:, c, 1024 + dt * 128:1024 + (dt + 1) * 128], rhs=hT[:, c, :], start=(c == 0), stop=(c == 7), reads=['wg', 'hT'], writes=[pBn])
            for c in range(4):
                k.mm(qA[:, :], lhsT=WA[:, c, dt * 128:(dt + 1) * 128], rhs=yrw[:, c, TS], start=(c == 0), stop=(c == 3), reads=['WA', 'yrw'], writes=[qAn])
            for c in range(4):
                k.mm(qB[:, :], lhsT=WB[:, c, dt * 128:(dt + 1) * 128], rhs=ymla[:, c, TS], start=(c == 0), stop=(c == 3), reads=['WB', 'ymlaT'], writes=[qBn])
            k.act(sgA[:], pA[:, :], AF.Sigmoid, reads=[pAn], writes=['sgA'])
            k.act(sgB[:], pB[:, :], AF.Sigmoid, reads=[pBn], writes=['sgB'])
            k.tt('dve', m1[:], qA[:, :], sgA[:], ALU.mult, reads=[qAn, 'sgA'], writes=['m1'])
            k.tt('dve', m2[:], qB[:, :], sgB[:], ALU.mult, reads=[qBn, 'sgB'], writes=['m2'])
            k.tt('pool', mT[:, dt, :], m1[:], m2[:], ALU.add, reads=['m1', 'm2'], writes=['mT'])
        for j in range(4):
            for hf in range(2):
                pb, pn = bank()
                for m in range(8):
                    k.mm(pb[:, :], lhsT=mT[:, m, j * 128:(j + 1) * 128], rhs=WO[:, m, hf * 512:(hf + 1) * 512], start=(m == 0), stop=(m == 7), reads=['mT', 'WO'], writes=[pn])
                k.tt('dve', xt[:, j, hf * 512:(hf + 1) * 512], pb[:, :], xt[:, j, hf * 512:(hf + 1) * 512], ALU.add, reads=[pn, 'xt'], writes=['xt'])
        S.dma('sp', dr['x1_d'][T0:T0 + 512, :].rearrange("(j p) d -> p j d", p=128), xt[:], reads=['xt'], writes=['x1_d'], key='x1s')
    S.pop()


def phase_peer(nc, S, k, dr, bank, cm):
    ident = cm['ident']; pp2 = cm['pp2']
    banks = cm['banks']
    S.push()
    h2T = S.sb("h2T", [128, 8, TO], BF16)
    S.push()
    bufs = dict(xt=S.sb("xt", [128, 4, 1024], F32), sq=S.sb("sq", [128, 1024], BF16), ss=S.sb("ss", [128, 4], F32),
                rstd=S.sb("rstd", [128, 4], F32), xb=S.sb("xb", [128, 4, 1024], BF16), hT=None, ident=ident)
    stage = S.sb("stage", [128, 1024], F32)
    wpq = S.sb("wpq", [128, 8, 2048], BF16)
    load_w_bf16(S, k, wpq, 'wpq', dr['w_pq'], stage, 'stage', 8, 2048, 'wst')
    skb = S.sb("skb", [128, 16, 128], BF16)
    skT = S.sb("skT", [128, 16, 128], BF16)
    for g4 in range(4):
        S.dma('sp', stage[:, 0:512].rearrange("p (a n) -> p a n", n=128), dr['sk'][g4 * 4:(g4 + 1) * 4].rearrange("a p n -> p a n"), writes=['stage'], key='wst')
        k.cp('dve', skb[:, g4 * 4:(g4 + 1) * 4, :], stage[:, 0:512].rearrange("p (a n) -> p a n", n=128), reads=['stage'], writes=['skb'])
    for g4 in range(4):
        pb, pn = bank()
        for a in range(4):
            k.mm(pb[:, a * 128:(a + 1) * 128], lhsT=skb[:, g4 * 4 + a, :], rhs=ident, reads=['skb', 'cst'], writes=[pn])
        k.cp('dve', skT[:, g4 * 4:(g4 + 1) * 4, :].rearrange("p a n -> p (a n)"), pb[:, :], reads=[pn], writes=['skT'])
    qpT = [S.sb("qpT%d" % i, [128, 512], BF16) for i in range(2)]
    s_sb = S.sb("s_sb", [128, 4, 16, 128], F32)
    for blk in range(4):
        T0 = blk * 512
        bufs['hT'] = h2T[:, :, T0:T0 + 512]
        norm_T(S, k, (lambda: bank(0, 4)), dr['x1_d'][T0:T0 + 512, :], pp2[:, 17:25], bufs)
        hT = bufs['hT']
        for hc in range(16):
            pb, pn = bank(0, 4)
            for c in range(8):
                k.mm(pb[:, :], lhsT=wpq[:, c, hc * 128:(hc + 1) * 128], rhs=hT[:, c, :], start=(c == 0), stop=(c == 7), reads=['wpq', 'hT'], writes=[pn])
            qp = qpT[hc % 2]; qpn = 'qpT%d' % (hc % 2)
            k.cp('act', qp[:], pb[:, :], reads=[pn], writes=[qpn])
            for j in range(4):
                sb_, sn_ = banks[4 + j]
                k.mm(sb_[:, (hc % 4) * 128:(hc % 4 + 1) * 128], lhsT=qp[:, j * 128:(j + 1) * 128], rhs=skT[:, hc, :], reads=[qpn, 'skT'], writes=[sn_])
            if hc % 4 == 3:
                for j in range(4):
                    sb_, sn_ = banks[4 + j]
                    k.cp('dve' if j % 2 else 'act', s_sb[:, j, hc - 3:hc + 1, :].rearrange("p a n -> p (a n)"), sb_[:, :], reads=[sn_], writes=['s_sb'])
        S.dma('sp', dr['s_d'][T0:T0 + 512].rearrange("(j p) a n -> p j a n", p=128), s_sb[:], reads=['s_sb'], writes=['s_d'], key='ssd')
    S.pop()

    S.push()
    st = S.sb("st", [128, 16, 128], F32)
    m16 = S.sb("m16", [128, 16, 16], F32)
    tmp = S.sb("tmp", [128, 256], F32)
    cand = S.sb("cand", [128, 8, 256], F32)
    top16 = S.sb("top16", [128, 8, 16], F32)
    thr = S.sb("thr", [128, 8], F32); mx = S.sb("mx", [128, 8], F32); negm = S.sb("negm", [128, 8], F32)
    e16 = S.sb("e16", [128, 8, 16], F32); Zs = S.sb("Zs", [128, 8], F32); rZ = S.sb("rZ", [128, 8], F32)
    Gb = [S.sb("G%d" % i, [128, 16384], BF16) for i in range(2)]
    RC = 16
    Cb = [S.sb("Cb%d" % i, [128, RC, 128], F32) for i in range(2)]
    Eb = [S.sb("Eb%d" % i, [128, RC, 128], BF16) for i in range(2)]
    Mb = [S.sb("Mb%d" % i, [128, RC, 128], BF16) for i in range(2)]
    UTg = [S.sb("UTg%d" % i, [128, 8, 512], BF16) for i in range(2)]
    Vg = [S.sb("Vg%d" % i, [128, 4, 1024], BF16) for i in range(3)]
    a_sb = [S.sb("a_sb%d" % i, [128, 512], F32) for i in range(2)]
    ga = [S.sb("ga%d" % i, [128, 512], BF16) for i in range(2)]
    gaT = [S.sb("gaT%d" % i, [128, 4, 128], BF16) for i in range(2)]
    x1t = S.sb("x1t", [128, 1024], F32)
    ntile = dr.get('_ntile', 16)
    cnt = {'c': 0}

    def topk(nt):
        N0 = nt * 128
        S.dma('sp', st[:], dr['s_d'][N0:N0 + 128], reads=['s_d'], writes=['st'], key='lst')
        for hc in range(16):
            S.op('dve', lambda e, hc=hc: e.max(out=m16[:, hc, 0:8], in_=st[:, hc, :]), reads=['st'], writes=['m16'])
            S.op('dve', lambda e, hc=hc: e.match_replace(out=tmp[:, 0:128], in_to_replace=m16[:, hc, 0:8], in_values=st[:, hc, :], imm_value=-1e30), reads=['st', 'm16'], writes=['tmp'])
            S.op('dve', lambda e, hc=hc: e.max(out=m16[:, hc, 8:16], in_=tmp[:, 0:128]), reads=['tmp'], writes=['m16'])
        for h in range(8):
            k.tt('pool', cand[:, h, :].rearrange("p (a b) -> p a b", b=16),
                 m16[:, 2 * h, :].unsqueeze(2).to_broadcast([128, 16, 16]),
                 m16[:, 2 * h + 1, :].unsqueeze(1).to_broadcast([128, 16, 16]), ALU.add, reads=['m16'], writes=['cand'])
        for h in range(8):
            S.op('dve', lambda e, h=h: e.max(out=top16[:, h, 0:8], in_=cand[:, h, :]), reads=['cand'], writes=['top16'])
            S.op('dve', lambda e, h=h: e.match_replace(out=tmp[:, :], in_to_replace=top16[:, h, 0:8], in_values=cand[:, h, :], imm_value=-1e30), reads=['cand', 'top16'], writes=['tmp'])
            S.op('dve', lambda e, h=h: e.max(out=top16[:, h, 8:16], in_=tmp[:, :]), reads=['tmp'], writes=['top16'])
        S.op('dve', lambda e: e.tensor_reduce(out=thr[:], in_=top16[:], axis=AX.X, op=ALU.min), reads=['top16'], writes=['thr'])
        S.op('dve', lambda e: e.tensor_reduce(out=mx[:], in_=top16[:], axis=AX.X, op=ALU.max), reads=['top16'], writes=['mx'])
        k.ts('dve', negm[:], mx[:], -1.0, None, ALU.mult, reads=['mx'], writes=['negm'])
        for h in range(8):
            k.act(e16[:, h, :], top16[:, h, :], AF.Exp, bias=negm[:, h:h + 1], reads=['top16', 'negm'], writes=['e16'])
        S.op('dve', lambda e: e.reduce_sum(out=Zs[:], in_=e16[:], axis=AX.X), reads=['e16'], writes=['Zs'])
        S.op('dve', lambda e: e.reciprocal(out=rZ[:], in_=Zs[:]), reads=['Zs'], writes=['rZ'])

    def gbuild(nt):
        G = Gb[nt % 2]; gn = 'G%d' % (nt % 2)
        k.memset('pool', G[:], 0.0, writes=[gn])
        yield
        for h in range(8):
            for ic in range(128 // RC):
                b2 = cnt['c'] % 2; cnt['c'] += 1
                C = Cb[b2]; E = Eb[b2]; M = Mb[b2]
                cn, en, mn = 'Cb%d' % b2, 'Eb%d' % b2, 'Mb%d' % b2
                k.tt('pool', C[:], st[:, 2 * h, ic * RC:(ic + 1) * RC].unsqueeze(2).to_broadcast([128, RC, 128]),
                     st[:, 2 * h + 1, :].unsqueeze(1).to_broadcast([128, RC, 128]), ALU.add, reads=['st'], writes=[cn])
                k.act(E[:], C[:], AF.Exp, bias=negm[:, h:h + 1], reads=[cn, 'negm'], writes=[en])
                k.stt('dve', M[:], C[:], thr[:, h:h + 1], E[:], ALU.is_ge, ALU.mult, reads=[cn, en, 'thr'], writes=[mn])
                Gs = G[:, ic * RC * 128:(ic + 1) * RC * 128].rearrange("p (a b) -> p a b", b=128)
                k.stt('dve', Gs, M[:], rZ[:, h:h + 1], Gs, ALU.mult, ALU.add, reads=[mn, 'rZ', gn], writes=[gn])
                yield

    def dense(nt):
        N0 = nt * 128
        G = Gb[nt % 2]; gn = 'G%d' % (nt % 2)
        acc = [banks[6], banks[7]]
        pre = {}; tr = {}

        def stA(eg):
            b2 = eg % 2; v3 = eg % 3
            if not (dr.get('_nodma') and (nt > 0 or eg > 2)):
                S.dma('sp', UTg[b2][:], dr['UT'][:, :, eg * 512:(eg + 1) * 512], reads=['UT'], writes=['UTg%d' % b2], key='ut%d' % b2)
                S.dma('sp', Vg[v3][:], dr['Vb'][eg * 512:(eg + 1) * 512, :].rearrange("(q p) d -> p q d", p=128), reads=['Vb'], writes=['Vg%d' % v3], key='vg%d' % v3)
            pb, pn = bank(0, 3)
            for c in range(8):
                k.mm(pb[:, :], lhsT=h2T[:, c, N0:N0 + 128], rhs=UTg[b2][:, c, :], start=(c == 0), stop=(c == 7), reads=['h2T', 'UTg%d' % b2], writes=[pn])
            k.act(a_sb[b2][:], pb[:, :], AF.Gelu, reads=[pn], writes=['a_sb%d' % b2])
            k.tt('dve', ga[b2][:], a_sb[b2][:], G[:, eg * 512:(eg + 1) * 512], ALU.mult, reads=['a_sb%d' % b2, gn], writes=['ga%d' % b2])

        def stB(eg):
            b2 = eg % 2
            pt, ptn = bank(3, 6)
            for q in range(4):
                k.mm(pt[:, q * 128:(q + 1) * 128], lhsT=ga[b2][:, q * 128:(q + 1) * 128], rhs=ident, reads=['ga%d' % b2, 'cst'], writes=[ptn])
            k.cp('act', gaT[b2][:].rearrange("p q n -> p (q n)"), pt[:, :], reads=[ptn], writes=['gaT%d' % b2])

        def stC(eg):
            b2 = eg % 2; v3 = eg % 3
            for q in range(4):
                for hf in range(2):
                    k.mm(acc[hf][0][:, :], lhsT=gaT[b2][:, q, :], rhs=Vg[v3][:, q, hf * 512:(hf + 1) * 512],
                         start=(eg == 0 and q == 0), stop=(eg == 31 and q == 3), reads=['gaT%d' % b2, 'Vg%d' % v3], writes=[acc[hf][1]])
        for g in range(34):
            if g < 32:
                stA(g)
            if 0 <= g - 1 < 32:
                stB(g - 1)
            if 0 <= g - 2 < 32:
                stC(g - 2)
            yield
        S.dma('sp', x1t[:], dr['x1_d'][N0:N0 + 128, :], reads=['x1_d'], writes=['x1t'], key='lx1')
        for hf in range(2):
            k.tt('dve', x1t[:, hf * 512:(hf + 1) * 512], acc[hf][0][:, :], x1t[:, hf * 512:(hf + 1) * 512], ALU.add, reads=[acc[hf][1], 'x1t'], writes=['x1t'])
        S.dma('sp', dr['x2_d'][N0:N0 + 128, :], x1t[:], reads=['x1t'], writes=['x2_d'], key='sx2')
        yield

    topk(0)
    if dr.get('_nog'):
        def gbuild(nt):
            yield
    for _ in gbuild(0):
        pass
    for nt in range(ntile):
        gb = None
        if nt + 1 < ntile:
            topk(nt + 1)
            gb = gbuild(nt + 1)
        for _ in dense(nt):
            if gb is not None:
                for _r in range(2):
                    try:
                        next(gb)
                    except StopIteration:
                        gb = None
                        break
        if gb is not None:
            for _ in gb:
                pass
    S.pop()
    S.pop()


def phase_final(nc, S, k, dr, bank, cm):
    ident = cm['ident']; pp2 = cm['pp2']
    S.push()
    bufs = dict(xt=S.sb("xt", [128, 4, 1024], F32), sq=S.sb("sq", [128, 1024], BF16), ss=S.sb("ss", [128, 4], F32),
                rstd=S.sb("rstd", [128, 4], F32), xb=S.sb("xb", [128, 4, 1024], BF16), hT=S.sb("hT", [128, 8, 512], BF16), ident=ident)
    stage = S.sb("stage", [128, 1024], F32)
    Wpg = S.sb("Wpg", [128, 8, 1024], BF16); Wpp = S.sb("Wpp", [128, 2, 1024], BF16)
    load_w_bf16(S, k, Wpg, 'Wpg', dr['w_pg'], stage, 'stage', 8, 1024, 'wst')
    load_w_bf16(S, k, Wpp, 'Wpp', dr['w_pp'], stage, 'stage', 2, 1024, 'wst')
    gfin = S.sb("gfin", [128, 1024], F32)
    S.dma('sp', gfin[:], dr['g_fin'].partition_broadcast(128), writes=['gfin'], key='gf')
    pt = S.sb("pt", [128, 4, 256], F32); pb16 = S.sb("pb16", [128, 4, 256], BF16); pT = S.sb("pT", [128, 2, 512], BF16)
    sg = S.sb("sg", [128, 512], F32); tq = S.sb("tq", [128, 512], F32)
    sq2 = S.sb("sq2", [128, 1024], F32); ss2 = S.sb("ss2", [128, 4], F32); rs2 = S.sb("rs2", [128, 4], F32)
    ot = S.sb("ot", [128, 4, 1024], F32)
    xt = bufs['xt']
    for blk in range(4):
        T0 = blk * 512
        norm_T(S, k, bank, dr['x2_d'][T0:T0 + 512, :], pp2[:, 25:33], bufs)
        hT = bufs['hT']
        S.dma('sp', pt[:], dr['p'][T0:T0 + 512, :].rearrange("(j p) d -> p j d", p=128), writes=['pt'], key='lp')
        k.cp('pool', pb16[:], pt[:], reads=['pt'], writes=['pb16'])
        for kt in range(2):
            pb, pn = bank()
            for j in range(4):
                k.mm(pb[:, j * 128:(j + 1) * 128], lhsT=pb16[:, j, kt * 128:(kt + 1) * 128], rhs=ident, reads=['pb16', 'cst'], writes=[pn])
            k.cp('act', pT[:, kt, :], pb[:, :], reads=[pn], writes=['pT'])
        for j in range(4):
            for hf in range(2):
                HS = slice(hf * 512, (hf + 1) * 512)
                pg, pgn = bank(); pq, pqn = bank()
                for c in range(8):
                    k.mm(pg[:, :], lhsT=hT[:, c, j * 128:(j + 1) * 128], rhs=Wpg[:, c, HS], start=(c == 0), stop=(c == 7), reads=['hT', 'Wpg'], writes=[pgn])
                for kt in range(2):
                    k.mm(pq[:, :], lhsT=pT[:, kt, j * 128:(j + 1) * 128], rhs=Wpp[:, kt, HS], start=(kt == 0), stop=(kt == 1), reads=['pT', 'Wpp'], writes=[pqn])
                k.act(sg[:], pg[:, :], AF.Sigmoid, reads=[pgn], writes=['sg'])
                k.tt('dve', tq[:], pq[:, :], sg[:], ALU.mult, reads=[pqn, 'sg'], writes=['tq'])
                k.tt('pool', xt[:, j, HS], xt[:, j, HS], tq[:], ALU.add, reads=['xt', 'tq'], writes=['xt'])
        for j in range(4):
            k.act(sq2[:], xt[:, j, :], AF.Square, reads=['xt'], writes=['sq2'])
            S.op('dve', lambda e, j=j: e.reduce_sum(out=ss2[:, j:j + 1], in_=sq2[:], axis=AX.X), reads=['sq2'], writes=['ss2'])
        k.rsqrt(rs2[:], ss2[:], 1.0 / D, 1e-6, reads=['ss2'], writes=['rs2'])
        for j in range(4):
            k.stt('dve', ot[:, j, :], xt[:, j, :], rs2[:, j:j + 1], gfin[:], ALU.mult, ALU.mult, reads=['xt', 'rs2', 'gfin'], writes=['ot'])
        S.dma('sp', dr['out'][T0:T0 + 512, :].rearrange("(j p) d -> p j d", p=128), ot[:], reads=['ot'], writes=['out'], key='so')
    S.pop()


def phase_h(nc, S, k, dr, bank, cm):
    ident = cm['ident']; pp2 = cm['pp2']
    S.push()
    bufs = dict(xt=S.sb("xt", [128, 4, 1024], F32), sq=S.sb("sq", [128, 1024], BF16), ss=S.sb("ss", [128, 4], F32),
                rstd=S.sb("rstd", [128, 4], F32), xb=S.sb("xb", [128, 4, 1024], BF16), hT=None, ident=ident)
    hTb = [S.sb("hTb%d" % i, [128, 8, 512], BF16) for i in range(2)]
    stage = S.sb("stage", [128, 1024], F32)
    wsh = S.sb("wsh", [128, 8, 384], BF16)
    load_w_bf16(S, k, wsh, 'wsh', dr['w_sh'], stage, 'stage', 8, 384, 'wst')
    ush = [S.sb("ush%d" % i, [128, 3, 512], BF16) for i in range(2)]
    for blk in range(NB):
        T0 = blk * 512
        b2 = blk % 2
        bufs['hT'] = hTb[b2]; bufs['hTn'] = 'hTb%d' % b2
        norm_T(S, k, bank, dr['x'][T0:T0 + 512, :], pp2[:, 0:8], bufs)
        S.dma('sp', dr['hT_d'][:, :, T0:T0 + 512], hTb[b2][:], reads=['hTb%d' % b2], writes=['hT_d'], key='sh%d' % b2)
        for tI in range(3):
            pb, pn = bank()
            for c in range(8):
                k.mm(pb[:, :], lhsT=wsh[:, c, tI * 128:(tI + 1) * 128], rhs=hTb[b2][:, c, :], start=(c == 0), stop=(c == 7), reads=['wsh', 'hTb%d' % b2], writes=[pn])
            k.cp('act' if tI % 2 else 'dve', ush[b2][:, tI, :], pb[:, :], reads=[pn], writes=['ush%d' % b2])
        S.dma('sp', dr['ush_d'][:, :, T0:T0 + 512].rearrange("a p n -> p a n"), ush[b2][:], reads=['ush%d' % b2], writes=['ush_d'], key='su%d' % b2)
    S.pop()


def phase_kv(nc, S, k, dr, bank, cm):
    ident = cm['ident']; pp2 = cm['pp2']; ones_f = cm['ones_f']
    S.push()
    hTk = [S.sb("hTk%d" % i, [128, 8, 512], BF16) for i in range(2)]
    stage = S.sb("stage", [128, 1024], F32)
    wki = S.sb("wki", [128, 8, 256], BF16)
    load_w_bf16(S, k, wki, 'wki', dr['w_kvin'], stage, 'stage', 8, 256, 'wst')
    wku = S.sb("wku", [128, 1, 1024], BF16)
    load_w_bf16(S, k, wku, 'wku', dr['w_kvup'], stage, 'stage', 1, 1024, 'wst')
    posi = S.sb("posi", [64, 512], I32); ang = S.sb("ang", [64, 512], F32)
    cosT = S.sb("cosT", [64, 512], F32); sinT = S.sb("sinT", [64, 512], F32); tnf = S.sb("tnf", [64, 512], F32)
    PI = 3.141592653589793
    cks = S.sb("cks", [128, 512], F32); ck2 = S.sb("ck2", [128, 512], BF16); rk_ = S.sb("rk_", [128, 512], F32); ckn = S.sb("ckn", [128, 512], BF16)
    krA = S.sb("krA", [64, 512], F32); krB = S.sb("krB", [64, 512], F32); krR = S.sb("krR", [64, 512], BF16)
    kTs = [S.sb("kTs%d" % i, [128, 512], BF16) for i in range(2)]
    vts = [S.sb("vts%d" % i, [128, 512], BF16) for i in range(2)]
    for blk in range(NB):
        T0 = blk * 512
        S.dma('sp', posi[:], dr['pos_all'][T0:T0 + 512].partition_broadcast(64), writes=['posi'], key='pos')
        k.cp('dve', ang[:], posi[:], reads=['posi'], writes=['ang'])
        k.ts('dve', ang[:], ang[:], pp2[0:64, 41:42], None, ALU.mult, reads=['ang', 'pp2'], writes=['ang'])
        for (dst, dn, shift) in ((sinT, 'sinT', 0.0), (cosT, 'cosT', PI / 2)):
            k.ts('dve', dst[:], ang[:], shift, 1.0 / TWO_PI, ALU.add, ALU.mult, reads=['ang'], writes=[dn])
            k.cp('dve', posi[:], dst[:], reads=[dn], writes=['posi'])
            k.cp('dve', tnf[:], posi[:], reads=['posi'], writes=['tnf'])
            k.ts('dve', dst[:], ang[:], shift, None, ALU.add, reads=['ang'], writes=[dn])
            k.stt('dve', dst[:], tnf[:], -TWO_PI, dst[:], ALU.mult, ALU.add, reads=['tnf', dn], writes=[dn])
            k.ts('dve', dst[:], dst[:], -PI, PI, ALU.max, ALU.min, reads=[dn], writes=[dn])
            k.act(dst[:], dst[:], AF.Sin, reads=[dn], writes=[dn])
        k.ts('dve', sinT[:], sinT[:], pp2[0:64, 42:43], None, ALU.mult, reads=['sinT', 'pp2'], writes=['sinT'])
        hT = hTk[blk % 2]; hTn = 'hTk%d' % (blk % 2)
        S.dma('sp', hT[:], dr['hT_d'][:, :, T0:T0 + 512], reads=['hT_d'], writes=[hTn], key='lh%d' % (blk % 2))
        pb, pn = bank()
        for c in range(8):
            k.mm(pb[:, :], lhsT=wki[:, c, 0:128], rhs=hT[:, c, :], start=(c == 0), stop=(c == 7), reads=['wki', hTn], writes=[pn])
        k.cp('act', cks[:], pb[:, :], reads=[pn], writes=['cks'])
        for (dst, dn, c0) in ((krA, 'krA', 128), (krB, 'krB', 192)):
            pb, pn = bank()
            for c in range(8):
                k.mm(pb[0:64, :], lhsT=wki[:, c, c0:c0 + 64], rhs=hT[:, c, :], start=(c == 0), stop=(c == 7), reads=['wki', hTn], writes=[pn])
            k.cp('act', dst[:], pb[0:64, :], reads=[pn], writes=[dn])
        k.act(ck2[:], cks[:], AF.Square, reads=['cks'], writes=['ck2'])
        pb, pn = bank()
        k.mm(pb[:, :], lhsT=ones_f[:], rhs=ck2[:], reads=['ones_f', 'ck2'], writes=[pn])
        k.rsqrt(rk_[:], pb[:, :], 1.0 / 128, 1e-6, reads=[pn], writes=['rk_'])
        k.stt('dve', ckn[:], cks[:], pp2[:, 43:44], rk_[:], ALU.mult, ALU.mult, reads=['cks', 'pp2', 'rk_'], writes=['ckn'])
        for hp in range(4):
            pb, pn = bank()
            k.mm(pb[:, :], lhsT=wku[:, 0, hp * 128:(hp + 1) * 128], rhs=ckn[:], reads=['wku', 'ckn'], writes=[pn])
            kt_ = kTs[hp % 2]; ktn = 'kTs%d' % (hp % 2)
            k.cp('act' if hp % 2 else 'dve', kt_[:], pb[:, :], reads=[pn], writes=[ktn])
            S.dma('sp', dr['kTn_d'][2 * hp, :, T0:T0 + 512], kt_[0:64, :], reads=[ktn], writes=['kTn_d'], key='sk%d' % (hp % 2))
            S.dma('sp', dr['kTn_d'][2 * hp + 1, :, T0:T0 + 512], kt_[64:128, :], reads=[ktn], writes=['kTn_d'], key='sk%d' % (hp % 2))
        for j in range(4):
            pb, pn = bank()
            k.mm(pb[:, :], lhsT=ckn[:, j * 128:(j + 1) * 128], rhs=wku[:, 0, 512:1024], reads=['wku', 'ckn'], writes=[pn])
            vt_ = vts[j % 2]; vtn = 'vts%d' % (j % 2)
            k.cp('act' if j % 2 else 'dve', vt_[:], pb[:, :], reads=[pn], writes=[vtn])
            S.dma('sp', dr['vtok_d'][:, :, blk * 4 + j, :].rearrange("h p d -> p h d"), vt_[:].rearrange("p (h d) -> p h d", d=64), reads=[vtn], writes=['vtok_d'], key='sv%d' % (j % 2))
        k.tt('dve', krA[:], krA[:], cosT[:], ALU.mult, reads=['krA', 'cosT'], writes=['krA'])
        k.tt('pool', krB[:], krB[:], sinT[:], ALU.mult, reads=['krB', 'sinT'], writes=['krB'])
        k.tt('dve', krR[:], krA[:], krB[:], ALU.add, reads=['krA', 'krB'], writes=['krR'])
        S.dma('sp', dr['kr_d'][0:16, T0:T0 + 512], krR[0:16, :], reads=['krR'], writes=['kr_d'], key='skr')
        S.dma('sp', dr['kr_d'][16:32, T0:T0 + 512], krR[32:48, :], reads=['krR'], writes=['kr_d'], key='skr')
    S.pop()


def phase_experts(nc, S, k, dr, bank, cm):
    ident = cm['ident']
    S.push()
    uf = [S.sb("uf%d" % i, [128, 1024], F32) for i in range(2)]
    ub = S.sb("ub", [128, 1024], BF16)
    utt = [S.sb("utt%d" % i, [128, 8, 128], BF16) for i in range(2)]
    vf = [S.sb("vf%d" % i, [128, 1024], F32) for i in range(2)]
    vb = [S.sb("vb%d" % i, [128, 1024], BF16) for i in range(2)]
    for et in range(128):
        b2 = et % 2
        S.dma('sp', uf[b2][:], dr['u_sh'][et * 128:(et + 1) * 128, :], writes=['uf%d' % b2], key='lu%d' % b2)
        k.cp('dve', ub[:], uf[b2][:], reads=['uf%d' % b2], writes=['ub'])
        for g in range(2):
            pb, pn = bank()
            for c4 in range(4):
                c = g * 4 + c4
                k.mm(pb[:, c4 * 128:(c4 + 1) * 128], lhsT=ub[:, c * 128:(c + 1) * 128], rhs=ident, reads=['ub', 'cst'], writes=[pn])
            k.cp('act', utt[b2][:, g * 4:(g + 1) * 4, :].rearrange("p a n -> p (a n)"), pb[:, :], reads=[pn], writes=['utt%d' % b2])
        S.dma('sp', dr['UT'][:, :, et * 128:(et + 1) * 128], utt[b2][:], reads=['utt%d' % b2], writes=['UT'], key='su%d' % b2)
        S.dma('sp', vf[b2][:], dr['v_sh'][et * 128:(et + 1) * 128, :], writes=['vf%d' % b2], key='lv%d' % b2)
        k.cp('pool', vb[b2][:], vf[b2][:], reads=['vf%d' % b2], writes=['vb%d' % b2])
        S.dma('sp', dr['Vb'][et * 128:(et + 1) * 128, :], vb[b2][:], reads=['vb%d' % b2], writes=['Vb'], key='svb%d' % b2)
    S.pop()


F_IN = [('x', [T, D], F32), ('pos_all', [T], I32),
        ('wa_all', [8, 1024, 384], F32), ('w_sh', [1024, 384], F32), ('pp_all', [8, 128, NPP], F32), ('w2s_all', [8, 128, 64], F32), ('a2s_all', [8, 128, 64], F32),
        ('g2h_all', [8, 128, 64], F32), ('w0row_all', [8, 1, 128], F32), ('cst', [5, 128, 128], F32),
        ('pp2', [128, NPP2], F32), ('w_kvin', [1024, 256], F32), ('w_kvup', [128, 1024], F32), ('u_sh', [16384, 1024], F32), ('v_sh', [16384, 1024], F32),
        ('x_own', [TO, D], F32), ('pos', [TO], I32), ('p', [TO, 256], F32), ('ridx', [128, 2], I32),
        ('w_cq', [1024, 256], F32), ('w_q', [256, 768], F32), ('w_gate', [1024, 2048], F32), ('w_a', [512, 1024], F32), ('w_b', [512, 1024], F32),
        ('w_o', [1024, 1024], F32), ('w_pq', [1024, 2048], F32), ('sk', [16, 128, 128], F32), ('w_pg', [1024, 1024], F32), ('w_pp', [256, 1024], F32),
        ('g_fin', [1024], F32)]


def build_nc():
    nc = bass.Bass("TRN2", target_bir_lowering=False)
    dr = {}
    for nm, sh, dt in F_IN:
        dr[nm] = nc.dram_tensor(nm, list(sh), dt, kind="ExternalInput").ap()
    for nm, sh in [('bon_d', [64, T]), ('g_d', [64, T]), ('qh_d', [128, T]), ('ol_d', [64, T]), ('yrw_own', [512, TO]), ('hh_d', [128, NCH, 64]), ('hT_d', [128, 8, T]), ('ush_d', [3, 128, T]),
                   ('kTn_d', [8, 64, T]), ('kr_d', [32, T]), ('vtok_d', [8, 128, 128, 64]), ('UT', [128, 8, 16384]), ('Vb', [16384, 1024])]:
        dr[nm] = nc.dram_tensor(nm, sh, BF16, kind="Internal").ap()
    dr['x1_d'] = nc.dram_tensor('x1_d', [TO, D], F32, kind="Internal").ap()
    dr['x2_d'] = nc.dram_tensor('x2_d', [TO, D], F32, kind="Internal").ap()
    dr['s_d'] = nc.dram_tensor('s_d', [TO, 16, 128], F32, kind="Internal").ap()
    dr['out'] = nc.dram_tensor('out', [TO, D], F32, kind="ExternalOutput").ap()
    S = Sched(nc)
    with S:
        k = K(S)
        cm, bank = common2(nc, S, k, dr)
        phase_h(nc, S, k, dr, bank, cm)
        for hd in range(8):
            d2 = dict(dr)
            d2['wa'] = dr['wa_all'][hd]; d2['pp'] = dr['pp_all'][hd]; d2['w2s'] = dr['w2s_all'][hd]; d2['a2s'] = dr['a2s_all'][hd]
            d2['g2h'] = dr['g2h_all'][hd]; d2['w0row'] = dr['w0row_all'][hd]
            d2['yrw_own'] = dr['yrw_own'][hd * 64:(hd + 1) * 64, :]
            d2['_banks'] = cm['banks']
            S.push()
            rwkv_phase(nc, S, k, d2)
            S.pop()
            k.epsc = cm['epsc']
        phase_kv(nc, S, k, dr, bank, cm)
        phase_experts(nc, S, k, dr, bank, cm)
        dr2 = dict(dr); dr2['x'] = dr['x_own']
        S.push()
        alloc_attn(S, cm)
        phase_attn(nc, S, k, dr2, bank, cm)
        phase_merge(nc, S, k, dr2, bank, cm)
        S.pop()
        phase_peer(nc, S, k, dr2, bank, cm)
        phase_final(nc, S, k, dr2, bank, cm)
        S.finish()
    return nc


def kernel(**inputs):
    inp = {k_: np.asarray(v) for k_, v in inputs.items()}
    x2d = np.ascontiguousarray(inp['x'][0])
    p1 = [prep_core(inp, h) for h in range(8)]
    shared = {'x': x2d, 'pos_all': np.ascontiguousarray(inp['positions'][0]).astype(np.int32),
              'wa_all': np.stack([np.ascontiguousarray(p['wa'][:, 0:384]) for p in p1]), 'w_sh': np.ascontiguousarray(inp['w_in'][0][:, 1536:1920]), 'pp_all': np.stack([p['pp'] for p in p1]),
              'w2s_all': np.stack([p['w2s'] for p in p1]), 'a2s_all': np.stack([p['a2s'] for p in p1]),
              'g2h_all': np.stack([p['g2h'] for p in p1]), 'w0row_all': np.stack([p['w0row'] for p in p1]),
              'u_sh': np.ascontiguousarray(inp['peer_u'][0]), 'v_sh': np.ascontiguousarray(inp['peer_v'][0])}
    nc = build_nc()
    in_maps = []
    for c in range(8):
        p2 = prep2_core(inp, c)
        m = dict(shared)
        for nm, _, _ in F_IN:
            if nm in m:
                continue
            if nm == 'x_own':
                m[nm] = p2['x']
            elif nm == 'ridx':
                m[nm] = np.stack([np.arange(128) * 8 + c, np.arange(128) * 8 + 7 - c], 1).astype(np.int32)
            else:
                m[nm] = p2[nm]
        in_maps.append(m)
    res = run_bass_kernel_spmd(nc, in_maps, core_ids=list(range(8))).results
    out = np.concatenate([np.asarray(r['out']) for r in res], axis=0)
    return out.reshape(1, T, D).astype(np.float32)
```

```python
import contextlib
import numpy as np
import concourse.bass as bass
import concourse.mybir as mybir
from concourse.bass_utils import run_bass_kernel_spmd

F32 = mybir.dt.float32
BF16 = mybir.dt.bfloat16
I32 = mybir.dt.int32
AF = mybir.ActivationFunctionType
ALU = mybir.AluOpType
AX = mybir.AxisListType

ENGS = ['pe', 'act', 'dve', 'pool', 'sp']


class Sched:
    def __init__(self, nc, n_dma_sems=48):
        self.nc = nc
        self.stack = contextlib.ExitStack()
        self.streams = {e: [] for e in ENGS}
        self.cnt = {}
        self.seen = {e: {} for e in ENGS}
        self.last_write = {}
        self.readers = {}
        self.n_dma_sems = n_dma_sems
        self.dma_keys = {}
        self.sems = {}
        self.scopes = [self.stack]
        self.cap = None

    def __enter__(self):
        self.stack.__enter__()
        for e in ENGS:
            self.sems[e] = self.stack.enter_context(self.nc.semaphore("s_" + e))
            self.cnt[e] = 0
        self.dma_pool = [self.stack.enter_context(self.nc.semaphore("d%d" % i)) for i in range(self.n_dma_sems)]
        self.sw_pool = [self.stack.enter_context(self.nc.semaphore("w%d" % i)) for i in range(8)]
        self.sw_keys = {}
        return self

    def __exit__(self, *a):
        return self.stack.__exit__(*a)

    def sb(self, name, shape, dt):
        self.uid = getattr(self, 'uid', 0) + 1
        return self.scopes[-1].enter_context(self.nc.sbuf_tensor("sb%d_%s" % (self.uid, name), list(shape), dt))

    def ps(self, name, shape, dt):
        self.uid = getattr(self, 'uid', 0) + 1
        return self.scopes[-1].enter_context(self.nc.psum_tensor("ps%d_%s" % (self.uid, name), list(shape), dt))

    def push(self):
        st = contextlib.ExitStack()
        st.__enter__()
        self.scopes.append(st)

    def pop(self):
        self.barrier()
        st = self.scopes.pop()
        st.__exit__(None, None, None)

    def _deps(self, eng, reads, writes):
        deps = {}
        def add(tok):
            if tok is None:
                return
            k, v = tok
            if deps.get(k, 0) < v:
                deps[k] = v
        for r in reads:
            add(self.last_write.get(r))
        for w in writes:
            add(self.last_write.get(w))
            for t in self.readers.get(w, ()):
                add(t)
        waits = []
        seen = self.seen[eng]
        for k, v in deps.items():
            if k == 'pe' and eng == 'pe':
                continue
            if seen.get(k, 0) >= v:
                continue
            seen[k] = v
            waits.append((k, v))
        return waits

    def _commit(self, tok, reads, writes):
        for w in writes:
            self.last_write[w] = tok
            self.readers[w] = []
        for r in reads:
            if r in writes:
                continue
            self.readers.setdefault(r, []).append(tok)

    @staticmethod
    def _excl(reads, writes):
        pr = [r for r in reads if isinstance(r, str) and r.startswith('pb')]
        if pr:
            reads = [r for r in reads if r not in pr]
            writes = list(writes) + [r for r in pr if r not in writes]
        return reads, writes

    def op(self, eng, fn, reads=(), writes=()):
        if self.cap is not None:
            self.cap.append(('op', (eng, fn, tuple(reads), tuple(writes)), {}))
            return
        reads, writes = self._excl(reads, writes)
        waits = self._deps(eng, reads, writes)
        self.cnt[eng] += 1
        tok = (eng, self.cnt[eng])
        self.streams[eng].append((waits, fn, (eng, 1)))
        self._commit(tok, reads, writes)

    def capture(self, fn, *a):
        self.cap = []
        fn(*a)
        lst, self.cap = self.cap, None
        return lst

    def emit_interleaved(self, A, B):
        ia = ib = 0
        na, nb = len(A), len(B)
        while ia < na or ib < nb:
            if ib >= nb or (ia < na and ia * max(nb, 1) <= ib * max(na, 1)):
                kind, args, kw = A[ia]; ia += 1
            else:
                kind, args, kw = B[ib]; ib += 1
            getattr(self, kind)(*args, **kw)

    def _dkey(self, key):
        if key not in self.dma_keys:
            idx = len(self.dma_keys)
            assert idx < self.n_dma_sems, "out of dma semaphores"
            self.dma_keys[key] = ('dma', idx)
            self.cnt.setdefault(('dma', idx), 0)
        return self.dma_keys[key]

    def dma(self, eng, out, in_, reads=(), writes=(), key=None, **kw):
        if self.cap is not None:
            self.cap.append(('dma', (eng, out, in_), dict(reads=tuple(reads), writes=tuple(writes), key=key, **kw)))
            return
        k = self._dkey(key)
        waits = self._deps(eng, reads, writes)
        self.cnt[k] += 16
        tok = (k, self.cnt[k])
        self.streams[eng].append((waits, (lambda e, o=out, i=in_, kw=kw: e.dma_start(out=o, in_=i, **kw)), (k, 16)))
        self._commit(tok, reads, writes)

    def gather(self, out, in_rows, idx_ap, reads=(), writes=(), key=None):
        if key not in self.sw_keys:
            assert len(self.sw_keys) < len(self.sw_pool)
            self.sw_keys[key] = ('swd', len(self.sw_keys))
            self.cnt.setdefault(self.sw_keys[key], 0)
        kk_ = self.sw_keys[key]
        waits = self._deps('pool', reads, writes)
        self.cnt[kk_] += 16
        tok = (kk_, self.cnt[kk_])
        self.streams['pool'].append((waits, (lambda e: e.indirect_dma_start(out=out, out_offset=None, in_=in_rows, in_offset=bass.IndirectOffsetOnAxis(ap=idx_ap, axis=0))), (kk_, 16)))
        self._commit(tok, reads, writes)

    def coll(self, kind, op, groups, ins, outs, reads=(), writes=()):
        key = 'coll'
        kk_ = self._dkey(key)
        waits = self._deps('pool', reads, writes)
        self.cnt[kk_] += 16
        tok = (kk_, self.cnt[kk_])
        self.streams['pool'].append((waits, (lambda e: e.collective_compute(kind, op, replica_groups=groups, ins=ins, outs=outs)), (kk_, 16)))
        self._commit(tok, reads, writes)

    def barrier(self):
        for e in ENGS:
            waits = []
            for k, v in self.cnt.items():
                if v == 0 or k == e:
                    continue
                if self.seen[e].get(k, 0) >= v:
                    continue
                self.seen[e][k] = v
                waits.append((k, v))
            if waits:
                self.streams[e].append((waits, None, None))
        for e in ENGS:
            if self.cnt[e] and self.seen[e].get(e, 0) < self.cnt[e]:
                self.seen[e][e] = self.cnt[e]
                self.streams[e].append(([(e, self.cnt[e])], None, None))
        self.last_write.clear()
        self.readers.clear()
        self.dma_keys = {}

    def _sem(self, k):
        if isinstance(k, tuple) and k[0] == 'swd':
            return self.sw_pool[k[1]]
        if isinstance(k, tuple):
            return self.dma_pool[k[1]]
        return self.sems[k]

    def finish(self):
        self.barrier()
        nc = self.nc
        streams = self.streams
        sem = self._sem

        def replay(engname):
            def run(eng):
                for waits, fn, inc in streams[engname]:
                    for k, v in waits:
                        eng.wait_ge(sem(k), v)
                    if fn is not None:
                        ins = fn(eng)
                        ins.then_inc(sem(inc[0]), inc[1])
            return run

        with nc.Block() as block:
            block.tensor(replay('pe'))
            block.scalar(replay('act'))
            block.vector(replay('dve'))
            block.gpsimd(replay('pool'))
            block.sync(replay('sp'))


T = 16384
D = 1024
NB = 32
CL = 128
NCH = T // CL
CDEC = 0.6065306597126334
NPP = 32


class K:
    def __init__(self, S):
        self.S = S
        self.nbank = 0

    def mm(self, out, lhsT, rhs, start=True, stop=True, reads=(), writes=()):
        self.S.op('pe', lambda e: e.matmul(out, lhsT=lhsT, rhs=rhs, start=start, stop=stop), reads=reads, writes=writes)

    def act(self, out, in_, func, bias=None, scale=None, reads=(), writes=()):
        kw = {}
        if bias is not None:
            kw['bias'] = bias
        if scale is not None:
            kw['scale'] = scale
        self.S.op('act', lambda e: e.activation(out=out, in_=in_, func=func, **kw), reads=reads, writes=writes)

    def tt(self, eng, out, in0, in1, op, reads=(), writes=()):
        self.S.op(eng, lambda e: e.tensor_tensor(out=out, in0=in0, in1=in1, op=op), reads=reads, writes=writes)

    def ts(self, eng, out, in0, s1, s2, op0, op1=None, reads=(), writes=()):
        if op1 is None and op0 == ALU.pow:
            self.S.op(eng, lambda e: e.tensor_scalar(out=out, in0=in0, scalar1=1.0, scalar2=s1, op0=ALU.mult, op1=ALU.pow), reads=reads, writes=writes)
        elif op1 is None:
            self.S.op(eng, lambda e: e.tensor_scalar(out=out, in0=in0, scalar1=s1, scalar2=0.0, op0=op0, op1=ALU.add), reads=reads, writes=writes)
        else:
            self.S.op(eng, lambda e: e.tensor_scalar(out=out, in0=in0, scalar1=s1, scalar2=s2, op0=op0, op1=op1), reads=reads, writes=writes)

    def rsqrt(self, out, in_, scale, eps, reads=(), writes=()):
        self.S.op('act', lambda e: e.activation(out=out, in_=in_, func=AF.Sqrt, bias=self.eps_ap(eps, out), scale=scale), reads=list(reads) + ['epsc'], writes=writes)
        self.S.op('dve', lambda e: e.reciprocal(out=out, in_=out), reads=writes, writes=writes)

    def eps_ap(self, eps, out):
        n = out.shape[0]
        return self.epsc[eps][0:n, 0:1]

    def stt(self, eng, out, in0, scalar, in1, op0, op1, reads=(), writes=()):
        self.S.op(eng, lambda e: e.scalar_tensor_tensor(out=out, in0=in0, scalar=scalar, in1=in1, op0=op0, op1=op1), reads=reads, writes=writes)

    def cp(self, eng, out, in_, reads=(), writes=()):
        if eng == 'act':
            self.S.op('act', lambda e: e.activation(out=out, in_=in_, func=AF.Copy), reads=reads, writes=writes)
        else:
            self.S.op(eng, lambda e: e.tensor_copy(out=out, in_=in_), reads=reads, writes=writes)

    def memset(self, eng, ap, val, writes=()):
        self.S.op(eng, lambda e: e.memset(ap, val), reads=(), writes=writes)


def rwkv_phase(nc, S, k, dr, core_dbg=None):
    x_d = dr['x']
    nblk = dr.get('_nblk', NB)
    lvl = dr.get('_lvl', 99)
    banks = dr.get('_banks') or [(S.ps("pb%d" % i, [128, 512], F32), "pb%d" % i) for i in range(8)]
    st = {'b': 0, 'lo': 0, 'hi': 8}

    def bank():
        b = banks[st['lo'] + st['b'] % (st['hi'] - st['lo'])]
        st['b'] += 1
        return b

    wa = S.sb("wa", [128, 8, 384], BF16)
    wst_ = S.sb("wst_", [128, 384], F32)
    for c in range(8):
        S.dma('sp', wst_[:], dr['wa'][c * 128:(c + 1) * 128, 0:384], writes=['wst_'], key='c0')
        k.cp('dve', wa[:, c, :], wst_[:], reads=['wst_'], writes=['wa'])
    pp = S.sb("pp", [128, NPP], F32)
    S.dma('sp', pp[:], dr['pp'], writes=['pp'], key='c1')
    cst_st = S.sb("cst_st", [128, 5, 128], F32)
    S.dma('sp', cst_st[:], dr['cst'].rearrange("m p n -> p m n"), writes=['cst_st'], key='c2')
    cst = S.sb("cst", [128, 5, 128], BF16)
    k.cp('dve', cst[:], cst_st[:], reads=['cst_st'], writes=['cst'])
    ident = cst[:, 0, :]
    m4 = S.sb("m4", [128, 4, 4, 128], BF16)
    for mi in range(4):
        for j in range(4):
            k.cp('pool', m4[:, mi, j, :], cst[:, 1 + mi, :], reads=['cst'], writes=['m4'])
    id4 = S.sb("id4", [128, 4, 128], BF16)
    for j in range(4):
        k.cp('pool', id4[:, j, :], cst[:, 0, :], reads=['cst'], writes=['id4'])
    ones_bd = S.sb("ones_bd", [128, 128], BF16)
    k.memset('pool', ones_bd[:], 0.0, writes=['ones_bd'])
    k.memset('pool', ones_bd[0:64, 0:64], 1.0, writes=['ones_bd'])
    k.memset('pool', ones_bd[64:128, 64:128], 1.0, writes=['ones_bd'])
    ones_f = S.sb("ones_f", [128, 128], BF16)
    k.memset('pool', ones_f[:], 1.0, writes=['ones_f'])
    bd_st = S.sb("bd_st", [128, 2, 128], F32)
    k.memset('pool', bd_st[:], 0.0, writes=['bd_st'])
    S.dma('sp', bd_st[0:64, 0, 0:64], dr['w2s'][0:64, :], reads=['bd_st'], writes=['bd_st'], key='c3')
    S.dma('sp', bd_st[64:128, 0, 64:128], dr['w2s'][64:128, :], reads=['bd_st'], writes=['bd_st'], key='c3')
    S.dma('sp', bd_st[0:64, 1, 0:64], dr['a2s'][0:64, :], reads=['bd_st'], writes=['bd_st'], key='c3')
    S.dma('sp', bd_st[64:128, 1, 64:128], dr['a2s'][64:128, :], reads=['bd_st'], writes=['bd_st'], key='c3')
    bd = S.sb("bd", [128, 2, 128], BF16)
    k.cp('dve', bd[:], bd_st[:], reads=['bd_st'], writes=['bd'])
    g2_st = S.sb("g2_st", [128, 64], F32)
    S.dma('sp', g2_st[:], dr['g2h'], writes=['g2_st'], key='c4')
    g2h = S.sb("g2h", [128, 64], BF16)
    k.cp('dve', g2h[:], g2_st[:], reads=['g2_st'], writes=['g2h'])
    w0r_st = S.sb("w0r_st", [1, 128], F32)
    S.dma('sp', w0r_st[:], dr['w0row'], writes=['w0r_st'], key='c5')
    w0row = S.sb("w0row", [1, 128], BF16)
    k.cp('dve', w0row[:], w0r_st[:], reads=['w0r_st'], writes=['w0row'])
    epst = S.sb("epst", [128, 4], F32)
    k.epsc = {}
    for i_, ev in enumerate([1e-6, 1e-24, 64e-5]):
        k.memset('pool', epst[:, i_:i_ + 1], ev, writes=['epsc'])
        k.epsc[ev] = epst[:, i_:i_ + 1]
    omka = S.sb("omka", [128, 1], F32)
    k.ts('dve', omka[:], pp[:, 9:10], -1.0, 1.0, ALU.mult, ALU.add, reads=['pp'], writes=['omka'])

    MT_all = S.sb("MT_all", [128, NCH, 128], BF16)
    k.memset('pool', MT_all[:], 0.0, writes=['MT_all'])
    N_all = S.sb("N_all", [128, NCH, 64], BF16)
    gamL = S.sb("gamL", [128, NCH], F32)
    Hh = S.sb("Hh", [128, NCH + 1, 64], BF16)

    hT = [S.sb("hT%d" % i, [128, 8, 512], BF16) for i in range(2)]
    U = [S.sb("U%d" % i, [128, 6, 514], BF16) for i in range(3)]
    for i in range(3):
        k.memset('pool', U[i][:], 0.0, writes=['U%d' % i])

    def w(name, shape, dt=BF16):
        return S.sb(name, shape, dt)
    tsum6 = w("tsum6", [128, 6, 512], BF16)
    us2 = [w("us%d" % i, [128, 6, 512], BF16) for i in range(2)]
    us = us2[0]
    hmu = w("hmu", [128, 6], F32); omu = w("omu", [128, 6], F32)
    k.ts('dve', hmu[:], pp[:, 0:6], 0.5, None, ALU.mult, reads=['pp'], writes=['hmu'])
    k.ts('dve', omu[:], pp[:, 0:6], -1.0, 1.0, ALU.mult, ALU.add, reads=['pp'], writes=['omu'])
    tl = w("tl", [128, 512]); sl = w("sl", [128, 512])
    sg_tok = w("sg_tok", [128, 4, 128])
    Gi = w("Gi", [128, 512], F32); Ginv = w("Ginv", [128, 512], F32); Ge = w("Ge", [128, 512], F32); Gh = w("Gh", [128, 512], F32)
    tot = w("tot", [128, 4], F32); nct = w("nct", [128, 4], F32)
    a_t = w("a_t", [128, 512], F32)
    kk = w("kk", [128, 512], F32); kk2 = w("kk2", [128, 512]); rn = w("rn", [128, 512], F32); kkn = w("kkn", [128, 512], F32)
    t1 = rn; kdir = kk; bvec = a_t
    At2 = [w("At%d" % i, [128, 512]) for i in range(2)]; Bt2 = [w("Bt%d" % i, [128, 512]) for i in range(2)]
    Kt2 = [w("Kt%d" % i, [128, 512]) for i in range(2)]; Rt2 = [w("Rt%d" % i, [128, 512]) for i in range(2)]
    Bht2 = [w("Bht%d" % i, [128, 512]) for i in range(2)]; Kht2 = [w("Kht%d" % i, [128, 512]) for i in range(2)]
    At, Bt, Kt, Rt, Bht, Kht = At2[0], Bt2[0], Kt2[0], Rt2[0], Bht2[0], Kht2[0]
    rk = w("rk", [128, 512]); bon = w("bon", [64, 512]); g_t = w("g_t", [64, 512])
    Sm = [w("Sm%d" % i, [128, 8, 128]) for i in range(2)]
    SmT = [w("SmT%d" % i, [128, 8, 128]) for i in range(2)]
    Qm = [w("Qm%d" % i, [128, 8, 128]) for i in range(2)]
    AakT = w("AakT", [128, 8, 128]); TrbT = w("TrbT", [128, 8, 128]); TrkT = w("TrkT", [128, 8, 128])
    AXm = w("AXm", [128, 8, 128]); WU = w("WU", [128, 8, 128])
    Bh_tok = w("Bh_tok", [128, 8, 64]); Kh_tok = w("Kh_tok", [128, 8, 64]); V_tok = w("V_tok", [128, 4, 64])
    QhT = w("QhT", [128, 512]); Oloc = w("Oloc", [64, 512])

    MASK = {0: {'ss': 0, 'si': 1}, 1: {'ss': 2, 'si': 3}}
    MASK_TS = {0: 2, 1: 0}

    def load_h(b):
        S.dma('sp', hT[b % 2][:], dr['hT_d'][:, :, b * 512:(b + 1) * 512], reads=['hT_d'], writes=['hT%d' % (b % 2)], key='lh%d' % (b % 2))

    def project(b):
        if b == 0:
            load_h(0)
        if b + 1 < nblk:
            load_h(b + 1)
        h = hT[b % 2]; hn = 'hT%d' % (b % 2)
        Ub = U[b % 3]; un = 'U%d' % (b % 3)
        S.dma('sp', Ub[:, 3:6, 1:513], dr['ush_d'][:, :, b * 512:(b + 1) * 512].rearrange("a p n -> p a n"), reads=['ush_d'], writes=[un], key='lu%d' % (b % 3))
        for tI in range(3):
            pb, pn = bank()
            for c in range(8):
                k.mm(pb[:, :], lhsT=wa[:, c, tI * 128:(tI + 1) * 128], rhs=h[:, c, :], start=(c == 0), stop=(c == 7), reads=['wa', hn], writes=[pn])
            if tI % 2 == 0:
                k.cp('act', Ub[:, tI, 1:513], pb[:, :], reads=[pn], writes=[un])
            else:
                k.cp('dve', Ub[:, tI, 1:513], pb[:, :], reads=[pn], writes=[un])
        if b > 0:
            pu = U[(b - 1) % 3]; pun = 'U%d' % ((b - 1) % 3)
            k.cp('pool', pu[:, :, 513:514], Ub[:, :, 1:2], reads=[un], writes=[pun])
            k.cp('pool', Ub[:, :, 0:1], pu[:, :, 512:513], reads=[pun], writes=[un])
        else:
            k.memset('pool', Ub[:, :, 0:1], 0.0, writes=[un])
        if b == NB - 1:
            k.memset('pool', Ub[:, :, 513:514], 0.0, writes=[un])

    def prep(b):
        Ub = U[b % 3]; un = 'U%d' % (b % 3)
        tok0 = b * 512
        sfx = str(b % 2)
        At = At2[b % 2]; Bt = Bt2[b % 2]; Kt = Kt2[b % 2]; Rt = Rt2[b % 2]; Bht = Bht2[b % 2]; Kht = Kht2[b % 2]; us = us2[b % 2]
        k.tt('pool', tsum6[:], Ub[:, :, 0:512], Ub[:, :, 2:514], ALU.add, reads=[un], writes=['tsum6'])
        k.tt('dve', tsum6[:], tsum6[:], hmu[:, 0:6].unsqueeze(2).to_broadcast([128, 6, 512]), ALU.mult, reads=['tsum6', 'hmu'], writes=['tsum6'])
        k.tt('pool', us[:], Ub[:, :, 1:513], omu[:, 0:6].unsqueeze(2).to_broadcast([128, 6, 512]), ALU.mult, reads=[un, 'omu'], writes=['us' + sfx])
        k.tt('dve', us[:], us[:], tsum6[:], ALU.add, reads=['us' + sfx, 'tsum6'], writes=['us' + sfx])
        r2 = us[:, 0, :]; k2 = us[:, 1, :]; v2 = us[:, 2, :]
        k.act(tl[:], us[:, 3, :], AF.Tanh, reads=['us' + sfx], writes=['tl'])
        pb, pn = bank()
        for j in range(4):
            k.mm(pb[:, j * 128:(j + 1) * 128], lhsT=tl[:, j * 128:(j + 1) * 128], rhs=bd[:, 0, :], start=True, stop=False, reads=['tl', 'bd'], writes=[pn])
            k.mm(pb[:, j * 128:(j + 1) * 128], lhsT=ones_f[0:1, :], rhs=w0row[0:1, :], start=False, stop=True, reads=['ones_f', 'w0row'], writes=[pn])
        k.act(sg_tok[:].rearrange("p j n -> p (j n)"), pb[:, :], AF.Sigmoid, reads=[pn], writes=['sg_tok'])
        pI, pIn = bank(); pE, pEn = bank()
        for j in range(4):
            for d in range(2):
                P = slice(64 * d, 64 * d + 64)
                k.mm(pI[P, j * 128:(j + 1) * 128], lhsT=sg_tok[:, j, P], rhs=cst[:, 2 + 2 * d, :], reads=['sg_tok', 'cst'], writes=[pIn])
                k.mm(pE[P, j * 128:(j + 1) * 128], lhsT=sg_tok[:, j, P], rhs=cst[:, 1 + 2 * d, :], reads=['sg_tok', 'cst'], writes=[pEn])
        pb, pn = bank()
        k.mm(pb[:, :], lhsT=bd[:, 1, :], rhs=us[:, 4, :], reads=['bd', 'us' + sfx], writes=[pn])
        k.act(a_t[:], pb[:, :], AF.Sigmoid, bias=pp[:, 7:8], reads=[pn, 'pp'], writes=['a_t'])
        k.ts('dve', kk[:], k2, pp[:, 8:9], None, ALU.mult, reads=['us' + sfx, 'pp'], writes=['kk'])
        k.tt('pool', kk2[:], kk[:], kk[:], ALU.mult, reads=['kk'], writes=['kk2'])
        pb, pn = bank()
        k.mm(pb[:, :], lhsT=ones_bd[:], rhs=kk2[:], reads=['ones_bd', 'kk2'], writes=[pn])
        k.rsqrt(rn[:], pb[:, :], 1.0, 1e-24, reads=[pn], writes=['rn'])
        k.tt('dve', kkn[:], kk[:], rn[:], ALU.mult, reads=['kk', 'rn'], writes=['kkn'])
        k.ts('dve', t1[:], a_t[:], pp[:, 9:10], omka[:, 0:1], ALU.mult, ALU.add, reads=['a_t', 'pp', 'omka', 'rn', 'kkn'], writes=['rn'])
        k.tt('pool', kdir[:], k2, t1[:], ALU.mult, reads=['us' + sfx, 'rn', 'kkn'], writes=['kk'])
        k.tt('pool', bvec[:], kkn[:], a_t[:], ALU.mult, reads=['kkn', 'a_t', 'rn'], writes=['a_t'])
        k.stt('dve', rk[:], r2, pp[:, 10:11], kdir[:], ALU.mult, ALU.mult, reads=['us' + sfx, 'pp', 'kk'], writes=['rk'])
        pb, pn = bank()
        k.mm(pb[0:64, :], lhsT=ones_f[:, 0:64], rhs=rk[:], reads=['ones_f', 'rk'], writes=[pn])
        k.tt('dve', bon[:], pb[0:64, :], us[0:64, 2, :], ALU.mult, reads=[pn, 'us' + sfx], writes=['bon'])
        S.dma('sp', dr['bon_d'][:, tok0:tok0 + 512], bon[:], reads=['bon'], writes=['bon_d'], key='bon')
        k.act(sl[:], us[:, 5, :], AF.Sigmoid, reads=['us' + sfx], writes=['sl'])
        pb, pn = bank()
        k.mm(pb[0:64, :], lhsT=g2h[:], rhs=sl[:], reads=['g2h', 'sl'], writes=[pn])
        k.cp('act', g_t[:], pb[0:64, :], reads=[pn], writes=['g_t'])
        S.dma('sp', dr['g_d'][:, tok0:tok0 + 512], g_t[:], reads=['g_t'], writes=['g_d'], key='gd')
        k.act(Gi[:], pI[:, :], AF.Exp, scale=-CDEC, reads=[pIn], writes=['Gi'])
        k.act(Ginv[:], pI[:, :], AF.Exp, scale=CDEC, reads=[pIn], writes=['Ginv'])
        k.act(Ge[:], pE[:, :], AF.Exp, scale=-CDEC, reads=[pEn], writes=['Ge'])
        pI3 = pI[:, :].rearrange("p (j n) -> p j n", n=128)
        k.cp('dve', tot[0:64, :], pI3[0:64, :, 127], reads=[pIn], writes=['tot'])
        k.cp('dve', tot[64:128, :], pI3[64:128, :, 0], reads=[pIn], writes=['tot'])
        k.ts('dve', nct[:], tot[:], -CDEC, None, ALU.mult, reads=['tot'], writes=['nct'])
        k.act(gamL[:, b * 4:(b + 1) * 4], tot[:], AF.Exp, scale=-CDEC, reads=['tot'], writes=['gamL'])
        for j in range(4):
            k.act(Gh[:, j * 128:(j + 1) * 128], pI[:, j * 128:(j + 1) * 128], AF.Exp, bias=nct[:, j:j + 1], scale=CDEC, reads=[pIn, 'nct'], writes=['Gh'])
        k.stt('dve', At[:], kkn[:], -1.0, Ge[:], ALU.mult, ALU.mult, reads=['kkn', 'Ge'], writes=['At' + sfx])
        k.tt('pool', Bt[:], bvec[:], Ginv[:], ALU.mult, reads=['a_t', 'Ginv'], writes=['Bt' + sfx])
        k.tt('dve', Kt[:], kdir[:], Ginv[:], ALU.mult, reads=['kk', 'Ginv'], writes=['Kt' + sfx])
        k.tt('pool', Rt[:], r2, Gi[:], ALU.mult, reads=['us' + sfx, 'Gi'], writes=['Rt' + sfx])
        k.tt('dve', Bht[:], bvec[:], Gh[:], ALU.mult, reads=['a_t', 'Gh'], writes=['Bht' + sfx])
        k.tt('pool', Kht[:], kdir[:], Gh[:], ALU.mult, reads=['kk', 'Gh'], writes=['Kht' + sfx])

    def stages(b):
        Ub = U[b % 3]; un = 'U%d' % (b % 3)
        tok0 = b * 512
        sfx = str(b % 2)
        At = At2[b % 2]; Bt = Bt2[b % 2]; Kt = Kt2[b % 2]; Rt = Rt2[b % 2]; Bht = Bht2[b % 2]; Kht = Kht2[b % 2]; us = us2[b % 2]
        def scores(dst, dstn, L, Ln, R, Rn, mask_of_dir, ts_layout=False):
            for d in range(2):
                P = slice(64 * d, 64 * d + 64)
                pb, pn = bank()
                for j in range(4):
                    C = slice(j * 128, (j + 1) * 128)
                    k.mm(pb[:, C], lhsT=L[P, C], rhs=R[P, C], reads=[Ln, Rn], writes=[pn])
                mi = mask_of_dir[d]
                k.tt('dve', dst[:, 4 * d:4 * d + 4, :].rearrange("p j n -> p (j n)"), pb[:, :], m4[:, mi, :, :].rearrange("p j n -> p (j n)"), ALU.mult, reads=[pn, 'm4'], writes=[dstn + '_%d' % d])
        scores(SmT[0], 'SmT0', Bt, 'Bt' + sfx, At, 'At' + sfx, {0: 0, 1: 2})
        scores(Sm[0], 'Sm0', At, 'At' + sfx, Bt, 'Bt' + sfx, {0: 2, 1: 0})
        scores(AakT, 'AakT', Kt, 'Kt' + sfx, At, 'At' + sfx, {0: 0, 1: 2})
        scores(TrbT, 'TrbT', Bt, 'Bt' + sfx, Rt, 'Rt' + sfx, {0: 1, 1: 3})
        scores(TrkT, 'TrkT', Kt, 'Kt' + sfx, Rt, 'Rt' + sfx, {0: 1, 1: 3})
        for d in range(2):
            k.tt('pool', Qm[0][:, 4 * d:4 * d + 4, :], SmT[0][:, 4 * d:4 * d + 4, :], id4[:], ALU.add, reads=['SmT0_%d' % d, 'id4'], writes=['Qm0_%d' % d])
        if lvl < 4:
            return
        cur = 0
        for dl in range(1, dr.get('_ndl', 7)):
            nxt = 1 - cur
            sc, scn = Sm[cur], 'Sm%d' % cur
            stc, stcn = SmT[cur], 'SmT%d' % cur
            sn, snn = Sm[nxt], 'Sm%d' % nxt
            stn, stnn = SmT[nxt], 'SmT%d' % nxt
            for d in range(2):
                pb, pn = bank()
                for j in range(4):
                    c8 = 4 * d + j
                    k.mm(pb[:, j * 128:(j + 1) * 128], lhsT=stc[:, c8, :], rhs=sc[:, c8, :], reads=[stcn + '_%d' % d, scn + '_%d' % d], writes=[pn])
                k.cp(dr.get('_e1', 'act'), sn[:, 4 * d:4 * d + 4, :].rearrange("p j n -> p (j n)"), pb[:, :], reads=[pn], writes=[snn + '_%d' % d])
            if dr.get('_sub', 9) < 1:
                break
            if dl < 6:
                for d in range(2):
                    pb, pn = bank()
                    for j in range(4):
                        c8 = 4 * d + j
                        k.mm(pb[:, j * 128:(j + 1) * 128], lhsT=sc[:, c8, :], rhs=stc[:, c8, :], reads=[stcn + '_%d' % d, scn + '_%d' % d], writes=[pn])
                    k.cp('dve', stn[:, 4 * d:4 * d + 4, :].rearrange("p j n -> p (j n)"), pb[:, :], reads=[pn], writes=[stnn + '_%d' % d])
            qc, qcn = Qm[cur], 'Qm%d' % cur
            qn, qnn = Qm[nxt], 'Qm%d' % nxt
            if dr.get('_sub', 9) < 2:
                break
            for d in range(2):
                pb, pn = bank()
                for j in range(4):
                    c8 = 4 * d + j
                    k.mm(pb[:, j * 128:(j + 1) * 128], lhsT=sn[:, c8, :], rhs=qc[:, c8, :], reads=[snn + '_%d' % d, qcn + '_%d' % d], writes=[pn])
                k.tt('dve', qn[:, 4 * d:4 * d + 4, :].rearrange("p j n -> p (j n)"), pb[:, :], qc[:, 4 * d:4 * d + 4, :].rearrange("p j n -> p (j n)"), ALU.add, reads=[pn, qcn + '_%d' % d], writes=[qnn + '_%d' % d])
            cur = nxt
        Qf, Qfn = Qm[cur], 'Qm%d' % cur
        if lvl < 6:
            return
        def tokmajor(dst, dstn, src, srcn, col0, eng):
            pb, pn = bank()
            for j in range(4):
                k.mm(pb[:, j * 128:(j + 1) * 128], lhsT=src[:, j * 128:(j + 1) * 128], rhs=ident, reads=[srcn, 'cst'], writes=[pn])
            pv = pb[:, :].rearrange("p (j d n) -> p j d n", j=4, d=2, n=64)
            for d in range(2):
                k.cp(eng, dst[:, 4 * d:4 * d + 4, col0:col0 + 64], pv[:, :, d, :], reads=[pn], writes=[dstn + '_%d' % d])
        sub = dr.get('_sub', 9)
        if sub in (0, 9):
            tokmajor(AXm, 'AXm', At, 'At' + sfx, 0, 'act' if sub == 9 else 'dve')
        if sub in (1, 9):
            tokmajor(Bh_tok, 'Bh_tok', Bht, 'Bht' + sfx, 0, 'dve')
        if sub in (2, 9):
            tokmajor(Kh_tok, 'Kh_tok', Kht, 'Kht' + sfx, 0, 'act')
        if sub < 9:
            return
        pb, pn = bank()
        for j in range(4):
            k.mm(pb[:, j * 64:(j + 1) * 64], lhsT=us[0:64, 2, j * 128:(j + 1) * 128], rhs=cst[0:64, 0, 0:64], reads=['us' + sfx, 'cst'], writes=[pn])
        k.cp('dve', V_tok[:], pb[:, 0:256].rearrange("p (c n) -> p c n", n=64), reads=[pn], writes=['V_tok'])
        if lvl < 7:
            return
        pb, pn = bank()
        for d in range(2):
            for j in range(4):
                c8 = 4 * d + j
                k.mm(pb[:, c8 * 64:(c8 + 1) * 64], lhsT=AakT[:, c8, :], rhs=V_tok[:, j, :], reads=['AakT_%d' % d, 'V_tok'], writes=[pn])
        k.cp('act', AXm[:, :, 64:128], pb[:, :].rearrange("p (c n) -> p c n", n=64), reads=[pn], writes=['AXm_0', 'AXm_1'])
        if lvl < 8:
            return
        for d in range(2):
            pb, pn = bank()
            for j in range(4):
                c8 = 4 * d + j
                k.mm(pb[:, j * 128:(j + 1) * 128], lhsT=Qf[:, c8, :], rhs=AXm[:, c8, :], reads=[Qfn + '_%d' % d, 'AXm_%d' % d], writes=[pn])
            k.cp('act' if d == 0 else 'dve', WU[:, 4 * d:4 * d + 4, :].rearrange("p j n -> p (j n)"), pb[:, :], reads=[pn], writes=['WU_%d' % d])
        if lvl < 9:
            return
        pb, pn = bank()
        for d in range(2):
            P = slice(64 * d, 64 * d + 64)
            for j in range(4):
                c8 = 4 * d + j
                k.mm(pb[P, j * 128:(j + 1) * 128], lhsT=WU[:, c8, 0:64], rhs=TrbT[:, c8, :], reads=['WU_%d' % d, 'TrbT_%d' % d], writes=[pn])
        k.tt('dve', QhT[:], pb[:, :], Rt[:], ALU.add, reads=[pn, 'Rt' + sfx], writes=['QhT'])
        S.dma('sp', dr['qh_d'][:, tok0:tok0 + 512], QhT[:], reads=['QhT'], writes=['qh_d'], key='qh')
        pb, pn = bank()
        for j in range(4):
            for d in range(2):
                c8 = 4 * d + j
                k.mm(pb[0:64, j * 128:(j + 1) * 128], lhsT=WU[:, c8, 64:128], rhs=TrbT[:, c8, :], start=(d == 0), stop=False, reads=['WU_%d' % d, 'TrbT_%d' % d], writes=[pn])
                k.mm(pb[0:64, j * 128:(j + 1) * 128], lhsT=V_tok[:, j, :], rhs=TrkT[:, c8, :], start=False, stop=(d == 1), reads=['V_tok', 'TrkT_%d' % d], writes=[pn])
        k.cp('act', Oloc[:], pb[0:64, :], reads=[pn], writes=['Oloc'])
        S.dma('sp', dr['ol_d'][:, tok0:tok0 + 512], Oloc[:], reads=['Oloc'], writes=['ol_d'], key='ol')
        pb, pn = bank()
        for d in range(2):
            P = slice(64 * d, 64 * d + 64)
            for j in range(4):
                c8 = 4 * d + j
                k.mm(pb[P, j * 64:(j + 1) * 64], lhsT=WU[:, c8, 0:64], rhs=Bh_tok[:, c8, :], reads=['WU_%d' % d, 'Bh_tok_%d' % d], writes=[pn])
        k.cp('dve', MT_all[0:64, b * 4:(b + 1) * 4, 0:64], pb[0:64, 0:256].rearrange("p (c n) -> p c n", n=64), reads=[pn], writes=['MT_all'])
        for j in range(4):
            st1 = nblk * 4 - 1 - (b * 4 + j)
            k.cp('dve', MT_all[64:128, st1, 64:128], pb[64:128, j * 64:(j + 1) * 64], reads=[pn], writes=['MT_all'])
        pb, pn = bank()
        for d in range(2):
            P = slice(64 * d, 64 * d + 64)
            for j in range(4):
                c8 = 4 * d + j
                k.mm(pb[P, j * 64:(j + 1) * 64], lhsT=Bh_tok[:, c8, :], rhs=WU[:, c8, 64:128], start=True, stop=False, reads=['WU_%d' % d, 'Bh_tok_%d' % d], writes=[pn])
                k.mm(pb[P, j * 64:(j + 1) * 64], lhsT=Kh_tok[:, c8, :], rhs=V_tok[:, j, :], start=False, stop=True, reads=['Kh_tok_%d' % d, 'V_tok'], writes=[pn])
        k.cp('act', N_all[:, b * 4:(b + 1) * 4, :], pb[:, 0:256].rearrange("p (c n) -> p c n", n=64), reads=[pn], writes=['N_all'])

    def run_direct(fn, b, lo, hi):
        st['lo'], st['hi'] = lo, hi
        fn(b)
        st['lo'], st['hi'] = 0, 8

    def cap(fn, b, lo, hi):
        st['lo'], st['hi'] = lo, hi
        lst = S.capture(fn, b)
        st['lo'], st['hi'] = 0, 8
        return lst
    run_direct(project, 0, 0, 3)
    if nblk > 1:
        run_direct(project, 1, 0, 3)
    run_direct(prep, 0, 0, 8)
    for b in range(nblk):
        if b + 2 < nblk:
            run_direct(project, b + 2, 0, 3)
        A = cap(stages, b, *dr.get('_rgA', (0, 8)))
        Bp = cap(prep, b + 1, *dr.get('_rgB', (0, 8))) if b + 1 < nblk else []
        if not dr.get('_int'):
            S.emit_interleaved(A, []); S.emit_interleaved(Bp, [])
        else:
            S.emit_interleaved(A, Bp)
    if lvl < 10:
        return

    nch = nblk * 4
    S.barrier()
    Hf = Gi[:, 0:64]
    T1 = Gi[:, 64:128]
    k.memset('pool', Hf[:], 0.0, writes=['Hf'])
    k.memset('pool', Hh[:, 0, :], 0.0, writes=['Hh0'])
    def t1_for(s_):
        c0 = s_; c1 = nch - 1 - s_
        k.stt('dve', T1[0:64, :], Hf[0:64, :], gamL[0:64, c0:c0 + 1], N_all[0:64, c0, :], ALU.mult, ALU.add, reads=['Hf', 'gamL', 'N_all'], writes=['T1'])
        k.stt('dve', T1[64:128, :], Hf[64:128, :], gamL[64:128, c1:c1 + 1], N_all[64:128, c1, :], ALU.mult, ALU.add, reads=['Hf', 'gamL', 'N_all'], writes=['T1'])
    t1_for(0)
    for s in range(nch):
        pb, pn = bank()
        k.mm(pb[:, 0:64], lhsT=MT_all[:, s, :], rhs=Hh[:, s, :], reads=['MT_all', 'Hh%d' % s], writes=[pn])
        k.tt('dve', Hh[:, s + 1, :], pb[:, 0:64], T1[:], ALU.add, reads=[pn, 'T1'], writes=['Hh%d' % (s + 1)])
        k.tt('dve', Hf[:], pb[:, 0:64], T1[:], ALU.add, reads=[pn, 'T1'], writes=['Hf'])
        if s + 1 < nch:
            t1_for(s + 1)
    if lvl < 11:
        return
    S.barrier()
    qh = [At, Bt]; ol = [Kt[0:64, :], Rt[0:64, :]]; bo = [Bht[0:64, :], Kht[0:64, :]]; gg = [rk[0:64, :], kk2[0:64, :]]
    of = kkn[0:64, :]; ob = tl[0:64, :]; dd = Ginv[0:64, :]; d2 = sl[0:64, :]; rs = Ge[0:64, :]; yy = Gh[0:64, :]
    yo = [bon, g_t]
    mean_m = AakT[0:64, 0, 0:64]
    k.memset('pool', mean_m[:], 1.0 / 64.0, writes=['mean_m'])
    ridx = S.sb("ridx", [128, 2], I32)
    S.dma('sp', ridx[:], dr['ridx'], writes=['ridx'], key='rix')
    S.dma('sp', dr['hh_d'], Hh[:, 0:NCH, :], reads=['Hh%d' % i for i in range(NCH)], writes=['hh_d'], key='shh')
    rows2k = lambda ap: ap.rearrange("p (b n) -> (p b) n", n=TO)
    qh_o = us[:, 0:4, :].rearrange("p a n -> p (a n)")
    ol_o = hT[0][0:64, 0:4, :].rearrange("p a n -> p (a n)"); bo_o = hT[0][0:64, 4:8, :].rearrange("p a n -> p (a n)")
    gg_o = hT[1][0:64, 0:4, :].rearrange("p a n -> p (a n)")
    HhA = hT[1][:, 4:6, :].rearrange("p a n -> p (a n)"); HhB = hT[1][:, 6:8, :].rearrange("p a n -> p (a n)")
    S.gather(qh_o, rows2k(dr['qh_d']), ridx[:, 0:1], reads=['ridx', 'qh_d'], writes=['qh_o'], key='g1')
    S.gather(ol_o, rows2k(dr['ol_d']), ridx[0:64, 0:1], reads=['ridx', 'ol_d'], writes=['ol_o'], key='g2')
    S.gather(bo_o, rows2k(dr['bon_d']), ridx[0:64, 0:1], reads=['ridx', 'bon_d'], writes=['bo_o'], key='g3')
    S.gather(gg_o, rows2k(dr['g_d']), ridx[0:64, 0:1], reads=['ridx', 'g_d'], writes=['gg_o'], key='g4')
    hrows = dr['hh_d'].rearrange("p (b c) n -> (p b) (c n)", c=16)
    S.gather(HhA, hrows, ridx[:, 0:1], reads=['ridx', 'hh_d'], writes=['HhA'], key='g5')
    S.gather(HhB, hrows, ridx[:, 1:2], reads=['ridx', 'hh_d'], writes=['HhB'], key='g6')
    HhA3 = HhA.rearrange("p (c n) -> p c n", n=64); HhB3 = HhB.rearrange("p (c n) -> p c n", n=64)
    for b in range(4):
        i2 = b % 2
        TS = slice(b * 512, (b + 1) * 512)
        pb, pn = bank()
        for j in range(4):
            cl = b * 4 + j
            C = slice(j * 128, (j + 1) * 128)
            k.cp('dve', TrbT[0:64, j, 0:64], HhA3[0:64, cl, :], reads=['HhA'], writes=['Hc'])
            k.cp('pool', TrbT[64:128, j, 0:64], HhB3[64:128, 15 - cl, :], reads=['HhB'], writes=['Hc'])
            k.mm(pb[0:64, C], lhsT=TrbT[:, j, 0:64], rhs=qh_o[:, b * 512 + j * 128:b * 512 + (j + 1) * 128], reads=['Hc', 'qh_o'], writes=[pn])
        k.tt('dve', of[:], pb[0:64, :], ol_o[:, TS], ALU.add, reads=[pn, 'ol_o'], writes=['of'])
        k.cp('act', ob[:], of[:], reads=['of'], writes=['ob'])
        pb, pn = bank()
        k.mm(pb[0:64, :], lhsT=mean_m[:], rhs=ob[:], reads=['mean_m', 'ob'], writes=[pn])
        k.tt('dve', dd[:], of[:], pb[0:64, :], ALU.subtract, reads=['of', pn], writes=['dd'])
        k.act(d2[:], dd[:], AF.Square, reads=['dd'], writes=['d2'])
        pb, pn = bank()
        k.mm(pb[0:64, :], lhsT=mean_m[:], rhs=d2[:], reads=['mean_m', 'd2'], writes=[pn])
        k.rsqrt(rs[:], pb[0:64, :], 1.0, 64e-5, reads=[pn], writes=['rs'])
        k.tt('dve', yy[:], dd[:], rs[:], ALU.mult, reads=['dd', 'rs'], writes=['yy'])
        k.ts('dve', yy[:], yy[:], pp[0:64, 11:12], pp[0:64, 12:13], ALU.mult, ALU.add, reads=['yy', 'pp'], writes=['yy'])
        k.tt('pool', yy[:], yy[:], bo_o[:, TS], ALU.add, reads=['yy', 'bo_o'], writes=['yy'])
        k.tt('pool', yo[i2][:], yy[:], gg_o[:, TS], ALU.mult, reads=['yy', 'gg_o'], writes=['yo%d' % i2])
        S.dma('sp', dr['yrw_own'][:, TS], yo[i2][:], reads=['yo%d' % i2], writes=['yrw_own'], key='sy%d' % i2)


def host_consts():
    p = np.arange(128)[:, None]; f = np.arange(128)[None, :]
    cst = np.stack([(p == f), (p < f), (p <= f), (p > f), (p >= f)]).astype(np.float32)
    return cst


def prep_core(inp, hd):
    hc = slice(hd * 64, (hd + 1) * 64)
    w_in = inp['w_in'][0]
    o = {}
    r_c = np.arange(hd * 64, (hd + 1) * 64)
    cols = np.concatenate([r_c, r_c, 512 + r_c, 512 + r_c, 1024 + r_c, 1024 + r_c,
                           np.arange(1536, 1920),
                           np.arange(1920 + 256, 1920 + 384),
                           1920 + 384 + np.arange(16), 1920 + 384 + np.arange(16),
                           1920 + 400 + np.arange(16), 1920 + 400 + np.arange(16)])
    o['wa'] = np.ascontiguousarray(w_in[:, cols])
    mu = inp['rw_mu'][0]
    pp = np.zeros((128, NPP), np.float32)
    mucols = cols[:768]
    for tI in range(6):
        pp[:, tI] = mu[mucols[tI * 128:(tI + 1) * 128]]
    st2 = lambda v: np.concatenate([v[hc], v[hc]])
    pp[:, 6] = np.concatenate([inp['rw_w0'][0, 0, hc], inp['rw_w0'][0, 1, hc]])
    pp[:, 7] = np.concatenate([inp['rw_a0'][0, 0, hc], inp['rw_a0'][0, 1, hc]])
    pp[:, 8] = st2(inp['rw_k_k'][0]); pp[:, 9] = st2(inp['rw_k_a'][0]); pp[:, 10] = st2(inp['rw_r_k'][0])
    pp[:, 11] = st2(inp['rw_gn_w'][0]); pp[:, 12] = st2(inp['rw_gn_b'][0])
    pp[:, 13:21] = inp['g_mix'][0].reshape(8, 128).T
    o['pp'] = pp
    o['w2s'] = np.ascontiguousarray(np.concatenate([inp['rw_w2'][0, 0][:, hc], inp['rw_w2'][0, 1][:, hc]], 0))
    o['a2s'] = np.ascontiguousarray(np.concatenate([inp['rw_a2'][0, 0][:, hc], inp['rw_a2'][0, 1][:, hc]], 0))
    o['g2h'] = np.ascontiguousarray(inp['rw_g2'][0][:, hc])
    o['w0row'] = np.ascontiguousarray(pp[:, 6][None, :])
    o['cst'] = host_consts()
    return o


TO = 2048
TWO_PI = 6.283185307179586
ATT_SCALE = 96.0 ** -0.5


def make_banks(S, n=8):
    banks = [(S.ps("pb%d" % i, [128, 512], F32), "pb%d" % i) for i in range(n)]
    st = {'b': 0}

    def bank(lo=0, hi=n):
        b = banks[lo + st['b'] % (hi - lo)]
        st['b'] += 1
        return b
    return banks, bank


def norm_T(S, k, bank, x_rows, gcol, bufs, eps=1e-6):
    xt, sq, ss, rstd, xb, h = bufs['xt'], bufs['sq'], bufs['ss'], bufs['rstd'], bufs['xb'], bufs['hT']
    ident = bufs['ident']
    xtn = bufs.get('xtn', 'xt')
    if x_rows is not None:
        S.dma('sp', xt[:], x_rows.rearrange("(j p) d -> p j d", p=128), writes=[xtn], key='xt')
    for j in range(4):
        k.act(sq[:], xt[:, j, :], AF.Square, reads=[xtn], writes=['sq'])
        S.op('dve', lambda e, j=j: e.reduce_sum(out=ss[:, j:j + 1], in_=sq[:], axis=AX.X), reads=['sq'], writes=['ss'])
    k.rsqrt(rstd[:], ss[:], 1.0 / D, eps, reads=['ss'], writes=['rstd'])
    for j in range(4):
        k.ts('dve' if j % 2 else 'pool', xb[:, j, :], xt[:, j, :], rstd[:, j:j + 1], None, ALU.mult, reads=[xtn, 'rstd'], writes=['xb'])
    for c in range(8):
        pb, pn = bank()
        for j in range(4):
            k.mm(pb[:, j * 128:(j + 1) * 128], lhsT=xb[:, j, c * 128:(c + 1) * 128], rhs=ident, reads=['xb', 'cst'], writes=[pn])
        k.ts('dve', h[:, c, :], pb[:, :], gcol[:, c:c + 1], None, ALU.mult, reads=[pn, 'pp2'], writes=[bufs.get('hTn', 'hT')])


def load_w_bf16(S, k, dst, dstn, src_ap, stage, stagen, nk, ncols, key):
    for kt in range(nk):
        c0 = 0
        while c0 < ncols:
            w = min(stage.shape[-1], ncols - c0)
            S.dma('sp', stage[:, 0:w], src_ap[kt * 128:(kt + 1) * 128, c0:c0 + w], writes=[stagen], key=key)
            k.cp('dve', dst[:, kt, c0:c0 + w], stage[:, 0:w], reads=[stagen], writes=[dstn])
            c0 += w


def phase_attn(nc, S, k, dr, bank, cm):
    ident = cm['ident']; ones_f = cm['ones_f']; pp2 = cm['pp2']
    S.push()
    bufs = dict(xt=S.sb("xt", [128, 4, 1024], F32), sq=S.sb("sq", [128, 1024], BF16), ss=S.sb("ss", [128, 4], F32),
                rstd=S.sb("rstd", [128, 4], F32), xb=S.sb("xb", [128, 4, 1024], BF16), hT=S.sb("hT", [128, 8, 512], BF16), ident=ident)
    stage = S.sb("stage", [128, 1024], F32)
    wcq = S.sb("wcq", [128, 8, 256], BF16)
    load_w_bf16(S, k, wcq, 'wcq', dr['w_cq'], stage, 'stage', 8, 256, 'wst')
    wq = S.sb("wq", [128, 2, 768], BF16)
    load_w_bf16(S, k, wq, 'wq', dr['w_q'], stage, 'stage', 2, 768, 'wst')
    posi = S.sb("posi", [128, TO], I32)
    S.dma('sp', posi[:], dr['pos'].partition_broadcast(128), writes=['posi'], key='pos')
    ang = S.sb("ang", [128, TO], F32)
    k.cp('dve', ang[:], posi[:], reads=['posi'], writes=['ang'])
    cosT = S.sb("cosT", [128, TO], F32); sinT = S.sb("sinT", [128, TO], F32)
    tnf = S.sb("tnf", [128, TO], F32)
    PI = 3.141592653589793
    k.ts('dve', ang[:], ang[:], pp2[:, 16:17], None, ALU.mult, reads=['ang', 'pp2'], writes=['ang'])
    for (dst, dn, shift) in ((sinT, 'sinT', 0.0), (cosT, 'cosT', PI / 2)):
        k.ts('dve', dst[:], ang[:], shift, 1.0 / TWO_PI, ALU.add, ALU.mult, reads=['ang'], writes=[dn])
        k.cp('dve', posi[:], dst[:], reads=[dn], writes=['posi'])
        k.cp('dve', tnf[:], posi[:], reads=['posi'], writes=['tnf'])
        k.ts('dve', dst[:], ang[:], shift, None, ALU.add, reads=['ang'], writes=[dn])
        k.stt('dve', dst[:], tnf[:], -TWO_PI, dst[:], ALU.mult, ALU.add, reads=['tnf', dn], writes=[dn])
        k.ts('dve', dst[:], dst[:], -PI, PI, ALU.max, ALU.min, reads=[dn], writes=[dn])
        k.act(dst[:], dst[:], AF.Sin, reads=[dn], writes=[dn])
    QT = cm['QT']
    cq = S.sb("cq", [128, 2, 512], F32); cqs = S.sb("cqs", [128, 2, 512], BF16); cqn = S.sb("cqn", [128, 2, 512], BF16)
    rq = S.sb("rq", [128, 512], F32)
    x1s = S.sb("x1s", [128, 512], F32); x2s = S.sb("x2s", [128, 512], F32)
    ta = S.sb("ta", [128, 512], F32); tb = S.sb("tb", [128, 512], F32)
    x1p = S.sb("x1p", [128, 512], BF16); x2p = S.sb("x2p", [128, 512], BF16)
    for blk in range(4):
        T0 = blk * 512
        norm_T(S, k, bank, dr['x'][T0:T0 + 512, :], pp2[:, 0:8], bufs)
        hT = bufs['hT']
        pbs = []
        for t in range(2):
            pb, pn = bank()
            for c in range(8):
                k.mm(pb[:, :], lhsT=wcq[:, c, t * 128:(t + 1) * 128], rhs=hT[:, c, :], start=(c == 0), stop=(c == 7), reads=['wcq', 'hT'], writes=[pn])
            k.cp('act', cq[:, t, :], pb[:, :], reads=[pn], writes=['cq'])
        k.act(cqs[:], cq[:], AF.Square, reads=['cq'], writes=['cqs'])
        pb, pn = bank()
        for t in range(2):
            k.mm(pb[:, :], lhsT=ones_f[:], rhs=cqs[:, t, :], start=(t == 0), stop=(t == 1), reads=['ones_f', 'cqs'], writes=[pn])
        k.rsqrt(rq[:], pb[:, :], 1.0 / 256, 1e-6, reads=[pn], writes=['rq'])
        for t in range(2):
            k.stt('dve', cqn[:, t, :], cq[:, t, :], pp2[:, 8 + t:9 + t], rq[:], ALU.mult, ALU.mult, reads=['cq', 'pp2', 'rq'], writes=['cqn'])
        for hp in range(4):
            pb, pn = bank()
            for t in range(2):
                k.mm(pb[:, :], lhsT=wq[:, t, hp * 128:(hp + 1) * 128], rhs=cqn[:, t, :], start=(t == 0), stop=(t == 1), reads=['wq', 'cqn'], writes=[pn])
            k.cp('act', QT[0:64, 2 * hp, T0:T0 + 512], pb[0:64, :], reads=[pn], writes=['QT'])
            k.cp('dve', QT[0:64, 2 * hp + 1, T0:T0 + 512], pb[64:128, :], reads=[pn], writes=['QT'])
        for (dst, c0) in ((x1s, 512), (x2s, 640)):
            pb, pn = bank()
            for t in range(2):
                k.mm(pb[:, :], lhsT=wq[:, t, c0:c0 + 128], rhs=cqn[:, t, :], start=(t == 0), stop=(t == 1), reads=['wq', 'cqn'], writes=[pn])
            k.cp('act', dst[:], pb[:, :], reads=[pn], writes=['x1s' if c0 == 512 else 'x2s'])
        nc_ = cosT[:, T0:T0 + 512]; ns_ = sinT[:, T0:T0 + 512]
        k.tt('dve', ta[:], x1s[:], nc_, ALU.mult, reads=['x1s', 'cosT'], writes=['ta'])
        k.tt('pool', tb[:], x2s[:], ns_, ALU.mult, reads=['x2s', 'sinT'], writes=['tb'])
        k.tt('dve', x1p[:], ta[:], tb[:], ALU.subtract, reads=['ta', 'tb'], writes=['x1p'])
        k.tt('dve', ta[:], x2s[:], nc_, ALU.mult, reads=['x2s', 'cosT', 'x1p'], writes=['ta'])
        k.tt('pool', tb[:], x1s[:], ns_, ALU.mult, reads=['x1s', 'sinT', 'x1p'], writes=['tb'])
        k.tt('dve', x2p[:], ta[:], tb[:], ALU.add, reads=['ta', 'tb'], writes=['x2p'])
        for h in range(8):
            S.dma('sp', QT[64:80, h, T0:T0 + 512], x1p[h * 16:(h + 1) * 16, :], reads=['x1p'], writes=['QT'], key='qr')
            S.dma('sp', QT[80:96, h, T0:T0 + 512], x2p[h * 16:(h + 1) * 16, :], reads=['x2p'], writes=['QT'], key='qr')
    S.pop()

    S.push()
    ymla = cm['ymlaT']
    Kh = S.sb("Kh", [96, T], BF16)
    Vh = S.sb("Vh", [128, 128, 128], BF16)
    k.memset('pool', Vh[:, :, 64:128], 1.0, writes=['Vh'])
    PT = [S.sb("PT%d" % i, [128, 512], BF16) for i in range(3)]
    osb = S.sb("osb", [128, 512], F32); rden = S.sb("rden", [64, 512], F32)
    nh = dr.get('_nh', 8)
    it = 0
    S.dma('sp', Kh[64:96, :], dr['kr_d'], reads=['kr_d'], writes=['Kh'], key='kh')
    for h in range(nh):
        S.dma('sp', Kh[0:64, :], dr['kTn_d'][h], reads=['kTn_d'], writes=['Kh'], key='kh')
        S.dma('sp', Vh[:, :, 0:64], dr['vtok_d'][h], reads=['vtok_d'], writes=['Vh'], key='vh')
        for qg in range(4):
            acc, accn = cm['banks'][6 + (qg % 2)]
            def tail(pb, pn, kt):
                nonlocal it
                pt = PT[it % 3]; ptn = 'PT%d' % (it % 3); it += 1
                k.act(pt[:], pb[:, :], AF.Exp, scale=ATT_SCALE, reads=[pn], writes=[ptn])
                k.mm(acc[:, :], lhsT=Vh[:, kt, :], rhs=pt[:], start=(kt == 0), stop=(kt == 127), reads=['Vh', ptn], writes=[accn])
            pend = []
            for kt in range(128):
                pb, pn = bank(0, 6)
                k.mm(pb[:, :], lhsT=Kh[:, kt * 128:(kt + 1) * 128], rhs=QT[:, h, qg * 512:(qg + 1) * 512], reads=['Kh', 'QT'], writes=[pn])
                pend.append((pb, pn, kt))
                if len(pend) > 2:
                    tail(*pend.pop(0))
            while pend:
                tail(*pend.pop(0))
            k.cp('dve', osb[:], acc[:, :], reads=[accn], writes=['osb'])
            S.op('dve', lambda e: e.reciprocal(out=osb[64:128, :], in_=osb[64:128, :]), reads=['osb'], writes=['osb'])
            k.cp('dve', rden[:], osb[64:128, :], reads=['osb'], writes=['rden'])
            k.tt('dve', ymla[(h % 2) * 64:(h % 2) * 64 + 64, h // 2, qg * 512:(qg + 1) * 512], osb[0:64, :], rden[:], ALU.mult, reads=['osb', 'rden'], writes=['ymlaT'])
    S.pop()


NPP2 = 48


def common2(nc, S, k, dr):
    cm = {}
    banks, bank = make_banks(S)
    cm['banks'] = banks
    cst_st = S.sb("cst_st", [128, 128], F32)
    S.dma('sp', cst_st[:], dr['cst'][0], writes=['cst_st'], key='c2')
    ident = S.sb("ident", [128, 128], BF16)
    k.cp('dve', ident[:], cst_st[:], reads=['cst_st'], writes=['cst'])
    cm['ident'] = ident[:]
    ones_f = S.sb("ones_f", [128, 128], BF16)
    k.memset('pool', ones_f[:], 1.0, writes=['ones_f'])
    cm['ones_f'] = ones_f
    pp2 = S.sb("pp2", [128, NPP2], F32)
    S.dma('sp', pp2[:], dr['pp2'], writes=['pp2'], key='c1')
    cm['pp2'] = pp2
    epst = S.sb("epst", [128, 4], F32)
    k.epsc = {}
    for i_, ev in enumerate([1e-6, 1e-24, 64e-5]):
        k.memset('pool', epst[:, i_:i_ + 1], ev, writes=['epsc'])
        k.epsc[ev] = epst[:, i_:i_ + 1]
    cm['epsc'] = k.epsc
    negpi = S.sb("negpi", [128, 1], F32)
    k.memset('pool', negpi[:], -3.141592653589793, writes=['negpi'])
    cm['negpi'] = negpi
    return cm, bank


def alloc_attn(S, cm):
    cm['QT'] = S.sb("QT", [96, 8, TO], BF16)
    cm['ymlaT'] = S.sb("ymlaT", [128, 4, TO], BF16)


def prep2_core(inp, c):
    o = {}
    tok = slice(c * TO, (c + 1) * TO)
    w_in = inp['w_in'][0]
    o['x'] = np.ascontiguousarray(inp['x'][0, tok])
    o['pos'] = np.ascontiguousarray(inp['positions'][0, tok]).astype(np.int32)
    o['w_cq'] = np.ascontiguousarray(w_in[:, 1920:2176])
    wq = inp['mla_w_qup'][0].reshape(256, 8, 96)
    o['w_q'] = np.ascontiguousarray(np.concatenate([wq[:, :, 0:64].reshape(256, 512), wq[:, :, 64:80].reshape(256, 128), wq[:, :, 80:96].reshape(256, 128)], 1))
    pp2 = np.zeros((128, NPP2), np.float32)
    pp2[:, 0:8] = inp['g_mix'][0].reshape(8, 128).T
    pp2[:, 8:10] = inp['mla_g_qa'][0].reshape(2, 128).T
    inv = (10000.0 ** (-np.arange(0, 32, 2, dtype=np.float32) / 32)).astype(np.float32)
    pp2[:, 16] = np.tile(inv, 8)
    pp2[:, 17:25] = inp['g_ffn'][0].reshape(8, 128).T
    pp2[:, 25:33] = inp['g_ple'][0].reshape(8, 128).T
    pp2[:, 33:41] = inp['g_final'].reshape(8, 128).T
    pp2[0:64, 41] = np.tile(inv, 4)
    pp2[0:32, 42] = -1.0; pp2[32:64, 42] = 1.0
    pp2[:, 43] = inp['mla_g_kva'][0]
    o['pp2'] = pp2
    o['cst'] = host_consts()
    o['w_gate'] = np.ascontiguousarray(w_in[:, 2336:4384])
    o['w_a'] = np.ascontiguousarray(inp['w_br_rwkv'][0]); o['w_b'] = np.ascontiguousarray(inp['w_br_mla'][0])
    o['w_o'] = np.ascontiguousarray(inp['w_out'][0])
    o['w_pq'] = np.ascontiguousarray(inp['peer_w_q'][0])
    o['sk'] = np.ascontiguousarray(inp['peer_sub_keys'][0].reshape(16, 128, 128))
    o['w_pg'] = np.ascontiguousarray(inp['w_ple_gate'][0]); o['w_pp'] = np.ascontiguousarray(inp['w_ple_proj'][0])
    o['g_fin'] = np.ascontiguousarray(inp['g_final'])
    o['p'] = np.ascontiguousarray(inp['p'][0, 0, tok])
    kr = w_in[:, 1920 + 384:1920 + 416]
    x1c, x2c = kr[:, 0:16], kr[:, 16:32]
    o['w_kvin'] = np.ascontiguousarray(np.concatenate([w_in[:, 1920 + 256:1920 + 384], x1c, x1c, x2c, x2c, x2c, x2c, x1c, x1c], 1))
    wk = inp['mla_w_kvup'][0].reshape(128, 8, 128)
    o['w_kvup'] = np.ascontiguousarray(np.concatenate([wk[:, :, 0:64].reshape(128, 512), wk[:, :, 64:128].reshape(128, 512)], 1))
    o['u_sh'] = np.ascontiguousarray(inp['peer_u'][0, c * 2048:(c + 1) * 2048])
    o['v_sh'] = np.ascontiguousarray(inp['peer_v'][0, c * 2048:(c + 1) * 2048])
    return o


def phase_merge(nc, S, k, dr, bank, cm):
    ident = cm['ident']; pp2 = cm['pp2']
    S.push()
    bufs = dict(xt=S.sb("xt", [128, 4, 1024], F32), sq=S.sb("sq", [128, 1024], BF16), ss=S.sb("ss", [128, 4], F32),
                rstd=S.sb("rstd", [128, 4], F32), xb=S.sb("xb", [128, 4, 1024], BF16), hT=S.sb("hT", [128, 8, 512], BF16), ident=ident)
    stage = S.sb("stage", [128, 1024], F32)
    wg = S.sb("wg", [128, 8, 2048], BF16)
    load_w_bf16(S, k, wg, 'wg', dr['w_gate'], stage, 'stage', 8, 2048, 'wst')
    WA = S.sb("WA", [128, 4, 1024], BF16); WB = S.sb("WB", [128, 4, 1024], BF16); WO = S.sb("WO", [128, 8, 1024], BF16)
    load_w_bf16(S, k, WA, 'WA', dr['w_a'], stage, 'stage', 4, 1024, 'wst')
    load_w_bf16(S, k, WB, 'WB', dr['w_b'], stage, 'stage', 4, 1024, 'wst')
    load_w_bf16(S, k, WO, 'WO', dr['w_o'], stage, 'stage', 8, 1024, 'wst')
    yrw = S.sb("yrw", [128, 4, TO], BF16)
    S.dma('sp', yrw[:], dr['yrw_own'].rearrange("(c p) n -> p c n", p=128), reads=['yrw_own'], writes=['yrw'], key='yrw')
    ymla = cm['ymlaT']
    mT = S.sb("mT", [128, 8, 512], BF16)
    sgA = S.sb("sgA", [128, 512], F32); sgB = S.sb("sgB", [128, 512], F32)
    m1 = S.sb("m1", [128, 512], F32); m2 = S.sb("m2", [128, 512], F32)
    xt = bufs['xt']
    for blk in range(4):
        T0 = blk * 512
        TS = slice(T0, T0 + 512)
        norm_T(S, k, bank, dr['x'][T0:T0 + 512, :], pp2[:, 0:8], bufs)
        hT = bufs['hT']
        for dt in range(8):
            pA, pAn = bank(); pB, pBn = bank(); qA, qAn = bank(); qB, qBn = bank()
            for c in range(8):
                k.mm(pA[:, :], lhsT=wg[:, c, dt * 128:(dt + 1) * 128], rhs=hT[:, c, :], start=(c == 0), stop=(c == 7), reads=['wg', 'hT'], writes=[pAn])
            for c in range(8):
                k.mm(pB[:, :], lhsT=wg[:, c, 1024 + dt * 128:1024 + (dt + 1) * 128], rhs=hT[:, c, :], start=(c == 0), stop=(c == 7), reads=['wg', 'hT'], writes=[pBn])
            for c in range(4):
                k.mm(qA[:, :], lhsT=WA[:, c, dt * 128:(dt + 1) * 128], rhs=yrw[:, c, TS], start=(c == 0), stop=(c == 3), reads=['WA', 'yrw'], writes=[qAn])
            for c in range(4):
                k.mm(qB[:, :], lhsT=WB[:, c, dt * 128:(dt + 1) * 128], rhs=ymla[:, c, TS], start=(c == 0), stop=(c == 3), reads=['WB', 'ymlaT'], writes=[qBn])
            k.act(sgA[:], pA[:, :], AF.Sigmoid, reads=[pAn], writes=['sgA'])
            k.act(sgB[:], pB[:, :], AF.Sigmoid, reads=[pBn], writes=['sgB'])
            k.tt('dve', m1[:], qA[:, :], sgA[:], ALU.mult, reads=[qAn, 'sgA'], writes=['m1'])
            k.tt('dve', m2[:], qB[:, :], sgB[:], ALU.mult, reads=[qBn, 'sgB'], writes=['m2'])
            k.tt('pool', mT[:, dt, :], m1[:], m2[:], ALU.add, reads=['m1', 'm2'], writes=['mT'])
        for j in range(4):
            for hf in range(2):
                pb, pn = bank()
                for m in range(8):
                    k.mm(pb[:, :], lhsT=mT[:, m, j * 128:(j + 1) * 128], rhs=WO[:, m, hf * 512:(hf + 1) * 512], start=(m == 0), stop=(m == 7), reads=['mT', 'WO'], writes=[pn])
                k.tt('dve', xt[:, j, hf * 512:(hf + 1) * 512], pb[:, :], xt[:, j, hf * 512:(hf + 1) * 512], ALU.add, reads=[pn, 'xt'], writes=['xt'])
        S.dma('sp', dr['x1_d'][T0:T0 + 512, :].rearrange("(j p) d -> p j d", p=128), xt[:], reads=['xt'], writes=['x1_d'], key='x1s')
    S.pop()


def phase_peer(nc, S, k, dr, bank, cm):
    ident = cm['ident']; pp2 = cm['pp2']
    banks = cm['banks']
    S.push()
    h2T = S.sb("h2T", [128, 8, TO], BF16)
    S.push()
    bufs = dict(xt=S.sb("xt", [128, 4, 1024], F32), sq=S.sb("sq", [128, 1024], BF16), ss=S.sb("ss", [128, 4], F32),
                rstd=S.sb("rstd", [128, 4], F32), xb=S.sb("xb", [128, 4, 1024], BF16), hT=None, ident=ident)
    stage = S.sb("stage", [128, 1024], F32)
    wpq = S.sb("wpq", [128, 8, 2048], BF16)
    load_w_bf16(S, k, wpq, 'wpq', dr['w_pq'], stage, 'stage', 8, 2048, 'wst')
    skb = S.sb("skb", [128, 16, 128], BF16)
    skT = S.sb("skT", [128, 16, 128], BF16)
    for g4 in range(4):
        S.dma('sp', stage[:, 0:512].rearrange("p (a n) -> p a n", n=128), dr['sk'][g4 * 4:(g4 + 1) * 4].rearrange("a p n -> p a n"), writes=['stage'], key='wst')
        k.cp('dve', skb[:, g4 * 4:(g4 + 1) * 4, :], stage[:, 0:512].rearrange("p (a n) -> p a n", n=128), reads=['stage'], writes=['skb'])
    for g4 in range(4):
        pb, pn = bank()
        for a in range(4):
            k.mm(pb[:, a * 128:(a + 1) * 128], lhsT=skb[:, g4 * 4 + a, :], rhs=ident, reads=['skb', 'cst'], writes=[pn])
        k.cp('dve', skT[:, g4 * 4:(g4 + 1) * 4, :].rearrange("p a n -> p (a n)"), pb[:, :], reads=[pn], writes=['skT'])
    qpT = [S.sb("qpT%d" % i, [128, 512], BF16) for i in range(2)]
    s_sb = S.sb("s_sb", [128, 4, 16, 128], F32)
    for blk in range(4):
        T0 = blk * 512
        bufs['hT'] = h2T[:, :, T0:T0 + 512]
        norm_T(S, k, (lambda: bank(0, 4)), dr['x1_d'][T0:T0 + 512, :], pp2[:, 17:25], bufs)
        hT = bufs['hT']
        for hc in range(16):
            pb, pn = bank(0, 4)
            for c in range(8):
                k.mm(pb[:, :], lhsT=wpq[:, c, hc * 128:(hc + 1) * 128], rhs=hT[:, c, :], start=(c == 0), stop=(c == 7), reads=['wpq', 'hT'], writes=[pn])
            qp = qpT[hc % 2]; qpn = 'qpT%d' % (hc % 2)
            k.cp('act', qp[:], pb[:, :], reads=[pn], writes=[qpn])
            for j in range(4):
                sb_, sn_ = banks[4 + j]
                k.mm(sb_[:, (hc % 4) * 128:(hc % 4 + 1) * 128], lhsT=qp[:, j * 128:(j + 1) * 128], rhs=skT[:, hc, :], reads=[qpn, 'skT'], writes=[sn_])
            if hc % 4 == 3:
                for j in range(4):
                    sb_, sn_ = banks[4 + j]
                    k.cp('dve' if j % 2 else 'act', s_sb[:, j, hc - 3:hc + 1, :].rearrange("p a n -> p (a n)"), sb_[:, :], reads=[sn_], writes=['s_sb'])
        S.dma('sp', dr['s_d'][T0:T0 + 512].rearrange("(j p) a n -> p j a n", p=128), s_sb[:], reads=['s_sb'], writes=['s_d'], key='ssd')
    S.pop()

    S.push()
    st = S.sb("st", [128, 16, 128], F32)
    m16 = S.sb("m16", [128, 16, 16], F32)
    tmp = S.sb("tmp", [128, 256], F32)
    cand = S.sb("cand", [128, 8, 256], F32)
    top16 = S.sb("top16", [128, 8, 16], F32)
    thr = S.sb("thr", [128, 8], F32); mx = S.sb("mx", [128, 8], F32); negm = S.sb("negm", [128, 8], F32)
    e16 = S.sb("e16", [128, 8, 16], F32); Zs = S.sb("Zs", [128, 8], F32); rZ = S.sb("rZ", [128, 8], F32)
    Gb = [S.sb("G%d" % i, [128, 16384], BF16) for i in range(2)]
    RC = 16
    Cb = [S.sb("Cb%d" % i, [128, RC, 128], F32) for i in range(2)]
    Eb = [S.sb("Eb%d" % i, [128, RC, 128], BF16) for i in range(2)]
    Mb = [S.sb("Mb%d" % i, [128, RC, 128], BF16) for i in range(2)]
    UTg = [S.sb("UTg%d" % i, [128, 8, 512], BF16) for i in range(2)]
    Vg = [S.sb("Vg%d" % i, [128, 4, 1024], BF16) for i in range(3)]
    a_sb = [S.sb("a_sb%d" % i, [128, 512], F32) for i in range(2)]
    ga = [S.sb("ga%d" % i, [128, 512], BF16) for i in range(2)]
    gaT = [S.sb("gaT%d" % i, [128, 4, 128], BF16) for i in range(2)]
    x1t = S.sb("x1t", [128, 1024], F32)
    ntile = dr.get('_ntile', 16)
    cnt = {'c': 0}

    def topk(nt):
        N0 = nt * 128
        S.dma('sp', st[:], dr['s_d'][N0:N0 + 128], reads=['s_d'], writes=['st'], key='lst')
        for hc in range(16):
            S.op('dve', lambda e, hc=hc: e.max(out=m16[:, hc, 0:8], in_=st[:, hc, :]), reads=['st'], writes=['m16'])
            S.op('dve', lambda e, hc=hc: e.match_replace(out=tmp[:, 0:128], in_to_replace=m16[:, hc, 0:8], in_values=st[:, hc, :], imm_value=-1e30), reads=['st', 'm16'], writes=['tmp'])
            S.op('dve', lambda e, hc=hc: e.max(out=m16[:, hc, 8:16], in_=tmp[:, 0:128]), reads=['tmp'], writes=['m16'])
        for h in range(8):
            k.tt('pool', cand[:, h, :].rearrange("p (a b) -> p a b", b=16),
                 m16[:, 2 * h, :].unsqueeze(2).to_broadcast([128, 16, 16]),
                 m16[:, 2 * h + 1, :].unsqueeze(1).to_broadcast([128, 16, 16]), ALU.add, reads=['m16'], writes=['cand'])
        for h in range(8):
            S.op('dve', lambda e, h=h: e.max(out=top16[:, h, 0:8], in_=cand[:, h, :]), reads=['cand'], writes=['top16'])
            S.op('dve', lambda e, h=h: e.match_replace(out=tmp[:, :], in_to_replace=top16[:, h, 0:8], in_values=cand[:, h, :], imm_value=-1e30), reads=['cand', 'top16'], writes=['tmp'])
            S.op('dve', lambda e, h=h: e.max(out=top16[:, h, 8:16], in_=tmp[:, :]), reads=['tmp'], writes=['top16'])
        S.op('dve', lambda e: e.tensor_reduce(out=thr[:], in_=top16[:], axis=AX.X, op=ALU.min), reads=['top16'], writes=['thr'])
        S.op('dve', lambda e: e.tensor_reduce(out=mx[:], in_=top16[:], axis=AX.X, op=ALU.max), reads=['top16'], writes=['mx'])
        k.ts('dve', negm[:], mx[:], -1.0, None, ALU.mult, reads=['mx'], writes=['negm'])
        for h in range(8):
            k.act(e16[:, h, :], top16[:, h, :], AF.Exp, bias=negm[:, h:h + 1], reads=['top16', 'negm'], writes=['e16'])
        S.op('dve', lambda e: e.reduce_sum(out=Zs[:], in_=e16[:], axis=AX.X), reads=['e16'], writes=['Zs'])
        S.op('dve', lambda e: e.reciprocal(out=rZ[:], in_=Zs[:]), reads=['Zs'], writes=['rZ'])

    def gbuild(nt):
        G = Gb[nt % 2]; gn = 'G%d' % (nt % 2)
        k.memset('pool', G[:], 0.0, writes=[gn])
        yield
        for h in range(8):
            for ic in range(128 // RC):
                b2 = cnt['c'] % 2; cnt['c'] += 1
                C = Cb[b2]; E = Eb[b2]; M = Mb[b2]
                cn, en, mn = 'Cb%d' % b2, 'Eb%d' % b2, 'Mb%d' % b2
                k.tt('pool', C[:], st[:, 2 * h, ic * RC:(ic + 1) * RC].unsqueeze(2).to_broadcast([128, RC, 128]),
                     st[:, 2 * h + 1, :].unsqueeze(1).to_broadcast([128, RC, 128]), ALU.add, reads=['st'], writes=[cn])
                k.act(E[:], C[:], AF.Exp, bias=negm[:, h:h + 1], reads=[cn, 'negm'], writes=[en])
                k.stt('dve', M[:], C[:], thr[:, h:h + 1], E[:], ALU.is_ge, ALU.mult, reads=[cn, en, 'thr'], writes=[mn])
                Gs = G[:, ic * RC * 128:(ic + 1) * RC * 128].rearrange("p (a b) -> p a b", b=128)
                k.stt('dve', Gs, M[:], rZ[:, h:h + 1], Gs, ALU.mult, ALU.add, reads=[mn, 'rZ', gn], writes=[gn])
                yield

    def dense(nt):
        N0 = nt * 128
        G = Gb[nt % 2]; gn = 'G%d' % (nt % 2)
        acc = [banks[6], banks[7]]
        pre = {}; tr = {}

        def stA(eg):
            b2 = eg % 2; v3 = eg % 3
            if not (dr.get('_nodma') and (nt > 0 or eg > 2)):
                S.dma('sp', UTg[b2][:], dr['UT'][:, :, eg * 512:(eg + 1) * 512], reads=['UT'], writes=['UTg%d' % b2], key='ut%d' % b2)
                S.dma('sp', Vg[v3][:], dr['Vb'][eg * 512:(eg + 1) * 512, :].rearrange("(q p) d -> p q d", p=128), reads=['Vb'], writes=['Vg%d' % v3], key='vg%d' % v3)
            pb, pn = bank(0, 3)
            for c in range(8):
                k.mm(pb[:, :], lhsT=h2T[:, c, N0:N0 + 128], rhs=UTg[b2][:, c, :], start=(c == 0), stop=(c == 7), reads=['h2T', 'UTg%d' % b2], writes=[pn])
            k.act(a_sb[b2][:], pb[:, :], AF.Gelu, reads=[pn], writes=['a_sb%d' % b2])
            k.tt('dve', ga[b2][:], a_sb[b2][:], G[:, eg * 512:(eg + 1) * 512], ALU.mult, reads=['a_sb%d' % b2, gn], writes=['ga%d' % b2])

        def stB(eg):
            b2 = eg % 2
            pt, ptn = bank(3, 6)
            for q in range(4):
                k.mm(pt[:, q * 128:(q + 1) * 128], lhsT=ga[b2][:, q * 128:(q + 1) * 128], rhs=ident, reads=['ga%d' % b2, 'cst'], writes=[ptn])
            k.cp('act', gaT[b2][:].rearrange("p q n -> p (q n)"), pt[:, :], reads=[ptn], writes=['gaT%d' % b2])

        def stC(eg):
            b2 = eg % 2; v3 = eg % 3
            for q in range(4):
                for hf in range(2):
                    k.mm(acc[hf][0][:, :], lhsT=gaT[b2][:, q, :], rhs=Vg[v3][:, q, hf * 512:(hf + 1) * 512],
                         start=(eg == 0 and q == 0), stop=(eg == 31 and q == 3), reads=['gaT%d' % b2, 'Vg%d' % v3], writes=[acc[hf][1]])
        for g in range(34):
            if g < 32:
                stA(g)
            if 0 <= g - 1 < 32:
                stB(g - 1)
            if 0 <= g - 2 < 32:
                stC(g - 2)
            yield
        S.dma('sp', x1t[:], dr['x1_d'][N0:N0 + 128, :], reads=['x1_d'], writes=['x1t'], key='lx1')
        for hf in range(2):
            k.tt('dve', x1t[:, hf * 512:(hf + 1) * 512], acc[hf][0][:, :], x1t[:, hf * 512:(hf + 1) * 512], ALU.add, reads=[acc[hf][1], 'x1t'], writes=['x1t'])
        S.dma('sp', dr['x2_d'][N0:N0 + 128, :], x1t[:], reads=['x1t'], writes=['x2_d'], key='sx2')
        yield

    topk(0)
    if dr.get('_nog'):
        def gbuild(nt):
            yield
    for _ in gbuild(0):
        pass
    for nt in range(ntile):
        gb = None
        if nt + 1 < ntile:
            topk(nt + 1)
            gb = gbuild(nt + 1)
        for _ in dense(nt):
            if gb is not None:
                for _r in range(2):
                    try:
                        next(gb)
                    except StopIteration:
                        gb = None
                        break
        if gb is not None:
            for _ in gb:
                pass
    S.pop()
    S.pop()


def phase_final(nc, S, k, dr, bank, cm):
    ident = cm['ident']; pp2 = cm['pp2']
    S.push()
    bufs = dict(xt=S.sb("xt", [128, 4, 1024], F32), sq=S.sb("sq", [128, 1024], BF16), ss=S.sb("ss", [128, 4], F32),
                rstd=S.sb("rstd", [128, 4], F32), xb=S.sb("xb", [128, 4, 1024], BF16), hT=S.sb("hT", [128, 8, 512], BF16), ident=ident)
    stage = S.sb("stage", [128, 1024], F32)
    Wpg = S.sb("Wpg", [128, 8, 1024], BF16); Wpp = S.sb("Wpp", [128, 2, 1024], BF16)
    load_w_bf16(S, k, Wpg, 'Wpg', dr['w_pg'], stage, 'stage', 8, 1024, 'wst')
    load_w_bf16(S, k, Wpp, 'Wpp', dr['w_pp'], stage, 'stage', 2, 1024, 'wst')
    gfin = S.sb("gfin", [128, 1024], F32)
    S.dma('sp', gfin[:], dr['g_fin'].partition_broadcast(128), writes=['gfin'], key='gf')
    pt = S.sb("pt", [128, 4, 256], F32); pb16 = S.sb("pb16", [128, 4, 256], BF16); pT = S.sb("pT", [128, 2, 512], BF16)
    sg = S.sb("sg", [128, 512], F32); tq = S.sb("tq", [128, 512], F32)
    sq2 = S.sb("sq2", [128, 1024], F32); ss2 = S.sb("ss2", [128, 4], F32); rs2 = S.sb("rs2", [128, 4], F32)
    ot = S.sb("ot", [128, 4, 1024], F32)
    xt = bufs['xt']
    for blk in range(4):
        T0 = blk * 512
        norm_T(S, k, bank, dr['x2_d'][T0:T0 + 512, :], pp2[:, 25:33], bufs)
        hT = bufs['hT']
        S.dma('sp', pt[:], dr['p'][T0:T0 + 512, :].rearrange("(j p) d -> p j d", p=128), writes=['pt'], key='lp')
        k.cp('pool', pb16[:], pt[:], reads=['pt'], writes=['pb16'])
        for kt in range(2):
            pb, pn = bank()
            for j in range(4):
                k.mm(pb[:, j * 128:(j + 1) * 128], lhsT=pb16[:, j, kt * 128:(kt + 1) * 128], rhs=ident, reads=['pb16', 'cst'], writes=[pn])
            k.cp('act', pT[:, kt, :], pb[:, :], reads=[pn], writes=['pT'])
        for j in range(4):
            for hf in range(2):
                HS = slice(hf * 512, (hf + 1) * 512)
                pg, pgn = bank(); pq, pqn = bank()
                for c in range(8):
                    k.mm(pg[:, :], lhsT=hT[:, c, j * 128:(j + 1) * 128], rhs=Wpg[:, c, HS], start=(c == 0), stop=(c == 7), reads=['hT', 'Wpg'], writes=[pgn])
                for kt in range(2):
                    k.mm(pq[:, :], lhsT=pT[:, kt, j * 128:(j + 1) * 128], rhs=Wpp[:, kt, HS], start=(kt == 0), stop=(kt == 1), reads=['pT', 'Wpp'], writes=[pqn])
                k.act(sg[:], pg[:, :], AF.Sigmoid, reads=[pgn], writes=['sg'])
                k.tt('dve', tq[:], pq[:, :], sg[:], ALU.mult, reads=[pqn, 'sg'], writes=['tq'])
                k.tt('pool', xt[:, j, HS], xt[:, j, HS], tq[:], ALU.add, reads=['xt', 'tq'], writes=['xt'])
        for j in range(4):
            k.act(sq2[:], xt[:, j, :], AF.Square, reads=['xt'], writes=['sq2'])
            S.op('dve', lambda e, j=j: e.reduce_sum(out=ss2[:, j:j + 1], in_=sq2[:], axis=AX.X), reads=['sq2'], writes=['ss2'])
        k.rsqrt(rs2[:], ss2[:], 1.0 / D, 1e-6, reads=['ss2'], writes=['rs2'])
        for j in range(4):
            k.stt('dve', ot[:, j, :], xt[:, j, :], rs2[:, j:j + 1], gfin[:], ALU.mult, ALU.mult, reads=['xt', 'rs2', 'gfin'], writes=['ot'])
        S.dma('sp', dr['out'][T0:T0 + 512, :].rearrange("(j p) d -> p j d", p=128), ot[:], reads=['ot'], writes=['out'], key='so')
    S.pop()


def phase_h(nc, S, k, dr, bank, cm):
    ident = cm['ident']; pp2 = cm['pp2']
    S.push()
    xtb2 = [S.sb("xtb%d" % i, [128, 4, 1024], F32) for i in range(2)]
    bufs = dict(xt=None, sq=S.sb("sq", [128, 1024], BF16), ss=S.sb("ss", [128, 4], F32),
                rstd=S.sb("rstd", [128, 4], F32), xb=S.sb("xb", [128, 4, 1024], BF16), hT=None, ident=ident)
    hTb = [S.sb("hTb%d" % i, [128, 8, 512], BF16) for i in range(2)]

    def ldx(blk_):
        S.dma('sp', xtb2[blk_ % 2][:], dr['x'][blk_ * 512:(blk_ + 1) * 512, :].rearrange("(j p) d -> p j d", p=128), writes=['xtb%d' % (blk_ % 2)], key='lx%d' % (blk_ % 2))
    ldx(0)
    stage = S.sb("stage", [128, 1024], F32)
    wsh = S.sb("wsh", [128, 8, 384], BF16)
    load_w_bf16(S, k, wsh, 'wsh', dr['w_sh'], stage, 'stage', 8, 384, 'wst')
    ush = [S.sb("ush%d" % i, [128, 3, 512], BF16) for i in range(2)]
    for blk in range(NB):
        T0 = blk * 512
        b2 = blk % 2
        bufs['hT'] = hTb[b2]; bufs['hTn'] = 'hTb%d' % b2
        bufs['xt'] = xtb2[b2]; bufs['xtn'] = 'xtb%d' % b2
        if blk + 1 < NB:
            ldx(blk + 1)
        norm_T(S, k, bank, None, pp2[:, 0:8], bufs)
        S.dma('sp', dr['hT_d'][:, :, T0:T0 + 512], hTb[b2][:], reads=['hTb%d' % b2], writes=['hT_d'], key='sh%d' % b2)
        for tI in range(3):
            pb, pn = bank()
            for c in range(8):
                k.mm(pb[:, :], lhsT=wsh[:, c, tI * 128:(tI + 1) * 128], rhs=hTb[b2][:, c, :], start=(c == 0), stop=(c == 7), reads=['wsh', 'hTb%d' % b2], writes=[pn])
            k.cp('act' if tI % 2 else 'dve', ush[b2][:, tI, :], pb[:, :], reads=[pn], writes=['ush%d' % b2])
        S.dma('sp', dr['ush_d'][:, :, T0:T0 + 512].rearrange("a p n -> p a n"), ush[b2][:], reads=['ush%d' % b2], writes=['ush_d'], key='su%d' % b2)
    S.pop()


def phase_kv(nc, S, k, dr, bank, cm):
    ident = cm['ident']; pp2 = cm['pp2']; ones_f = cm['ones_f']
    S.push()
    hTk = [S.sb("hTk%d" % i, [128, 8, 512], BF16) for i in range(2)]
    stage = S.sb("stage", [128, 1024], F32)
    wki = S.sb("wki", [128, 8, 256], BF16)
    load_w_bf16(S, k, wki, 'wki', dr['w_kvin'], stage, 'stage', 8, 256, 'wst')
    wku = S.sb("wku", [128, 1, 1024], BF16)
    load_w_bf16(S, k, wku, 'wku', dr['w_kvup'], stage, 'stage', 1, 1024, 'wst')
    posi = S.sb("posi", [64, 512], I32); ang = S.sb("ang", [64, 512], F32)
    cosT = S.sb("cosT", [64, 512], F32); sinT = S.sb("sinT", [64, 512], F32); tnf = S.sb("tnf", [64, 512], F32)
    PI = 3.141592653589793
    cks = S.sb("cks", [128, 512], F32); ck2 = S.sb("ck2", [128, 512], BF16); rk_ = S.sb("rk_", [128, 512], F32); ckn = S.sb("ckn", [128, 512], BF16)
    krA = S.sb("krA", [64, 512], F32); krB = S.sb("krB", [64, 512], F32); krR = S.sb("krR", [64, 512], BF16)
    kTs = [S.sb("kTs%d" % i, [128, 512], BF16) for i in range(2)]
    vts = [S.sb("vts%d" % i, [128, 512], BF16) for i in range(2)]
    for blk in range(NB):
        T0 = blk * 512
        S.dma('sp', posi[:], dr['pos_all'][T0:T0 + 512].partition_broadcast(64), writes=['posi'], key='pos')
        k.cp('dve', ang[:], posi[:], reads=['posi'], writes=['ang'])
        k.ts('dve', ang[:], ang[:], pp2[0:64, 41:42], None, ALU.mult, reads=['ang', 'pp2'], writes=['ang'])
        for (dst, dn, shift) in ((sinT, 'sinT', 0.0), (cosT, 'cosT', PI / 2)):
            k.ts('dve', dst[:], ang[:], shift, 1.0 / TWO_PI, ALU.add, ALU.mult, reads=['ang'], writes=[dn])
            k.cp('dve', posi[:], dst[:], reads=[dn], writes=['posi'])
            k.cp('dve', tnf[:], posi[:], reads=['posi'], writes=['tnf'])
            k.ts('dve', dst[:], ang[:], shift, None, ALU.add, reads=['ang'], writes=[dn])
            k.stt('dve', dst[:], tnf[:], -TWO_PI, dst[:], ALU.mult, ALU.add, reads=['tnf', dn], writes=[dn])
            k.ts('dve', dst[:], dst[:], -PI, PI, ALU.max, ALU.min, reads=[dn], writes=[dn])
            k.act(dst[:], dst[:], AF.Sin, reads=[dn], writes=[dn])
        k.ts('dve', sinT[:], sinT[:], pp2[0:64, 42:43], None, ALU.mult, reads=['sinT', 'pp2'], writes=['sinT'])
        hT = hTk[blk % 2]; hTn = 'hTk%d' % (blk % 2)
        if blk == 0:
            S.dma('sp', hTk[0][:], dr['hT_d'][:, :, 0:512], reads=['hT_d'], writes=['hTk0'], key='lh0')
        if blk + 1 < NB:
            nb_ = (blk + 1) % 2
            S.dma('sp', hTk[nb_][:], dr['hT_d'][:, :, T0 + 512:T0 + 1024], reads=['hT_d'], writes=['hTk%d' % nb_], key='lh%d' % nb_)
        pb, pn = bank()
        for c in range(8):
            k.mm(pb[:, :], lhsT=wki[:, c, 0:128], rhs=hT[:, c, :], start=(c == 0), stop=(c == 7), reads=['wki', hTn], writes=[pn])
        k.cp('act', cks[:], pb[:, :], reads=[pn], writes=['cks'])
        for (dst, dn, c0) in ((krA, 'krA', 128), (krB, 'krB', 192)):
            pb, pn = bank()
            for c in range(8):
                k.mm(pb[0:64, :], lhsT=wki[:, c, c0:c0 + 64], rhs=hT[:, c, :], start=(c == 0), stop=(c == 7), reads=['wki', hTn], writes=[pn])
            k.cp('act', dst[:], pb[0:64, :], reads=[pn], writes=[dn])
        k.act(ck2[:], cks[:], AF.Square, reads=['cks'], writes=['ck2'])
        pb, pn = bank()
        k.mm(pb[:, :], lhsT=ones_f[:], rhs=ck2[:], reads=['ones_f', 'ck2'], writes=[pn])
        k.rsqrt(rk_[:], pb[:, :], 1.0 / 128, 1e-6, reads=[pn], writes=['rk_'])
        k.stt('dve', ckn[:], cks[:], pp2[:, 43:44], rk_[:], ALU.mult, ALU.mult, reads=['cks', 'pp2', 'rk_'], writes=['ckn'])
        for hp in range(4):
            pb, pn = bank()
            k.mm(pb[:, :], lhsT=wku[:, 0, hp * 128:(hp + 1) * 128], rhs=ckn[:], reads=['wku', 'ckn'], writes=[pn])
            kt_ = kTs[hp % 2]; ktn = 'kTs%d' % (hp % 2)
            k.cp('act' if hp % 2 else 'dve', kt_[:], pb[:, :], reads=[pn], writes=[ktn])
            S.dma('sp', dr['kTn_d'][2 * hp, :, T0:T0 + 512], kt_[0:64, :], reads=[ktn], writes=['kTn_d'], key='sk%d' % (hp % 2))
            S.dma('sp', dr['kTn_d'][2 * hp + 1, :, T0:T0 + 512], kt_[64:128, :], reads=[ktn], writes=['kTn_d'], key='sk%d' % (hp % 2))
        for j in range(4):
            pb, pn = bank()
            k.mm(pb[:, :], lhsT=ckn[:, j * 128:(j + 1) * 128], rhs=wku[:, 0, 512:1024], reads=['wku', 'ckn'], writes=[pn])
            vt_ = vts[j % 2]; vtn = 'vts%d' % (j % 2)
            k.cp('act' if j % 2 else 'dve', vt_[:], pb[:, :], reads=[pn], writes=[vtn])
            S.dma('sp', dr['vtok_d'][:, :, blk * 4 + j, :].rearrange("h p d -> p h d"), vt_[:].rearrange("p (h d) -> p h d", d=64), reads=[vtn], writes=['vtok_d'], key='sv%d' % (j % 2))
        k.tt('dve', krA[:], krA[:], cosT[:], ALU.mult, reads=['krA', 'cosT'], writes=['krA'])
        k.tt('pool', krB[:], krB[:], sinT[:], ALU.mult, reads=['krB', 'sinT'], writes=['krB'])
        k.tt('dve', krR[:], krA[:], krB[:], ALU.add, reads=['krA', 'krB'], writes=['krR'])
        S.dma('sp', dr['kr_d'][0:16, T0:T0 + 512], krR[0:16, :], reads=['krR'], writes=['kr_d'], key='skr')
        S.dma('sp', dr['kr_d'][16:32, T0:T0 + 512], krR[32:48, :], reads=['krR'], writes=['kr_d'], key='skr')
    S.pop()


def phase_experts(nc, S, k, dr, bank, cm):
    ident = cm['ident']
    S.push()
    uf = [S.sb("uf%d" % i, [128, 1024], F32) for i in range(2)]
    ub = S.sb("ub", [128, 1024], BF16)
    utt = [S.sb("utt%d" % i, [128, 8, 128], BF16) for i in range(2)]
    vf = [S.sb("vf%d" % i, [128, 1024], F32) for i in range(2)]
    vb = [S.sb("vb%d" % i, [128, 1024], BF16) for i in range(2)]
    def ld(et):
        b2 = et % 2
        S.dma('sp', uf[b2][:], dr['u_sh'][et * 128:(et + 1) * 128, :], writes=['uf%d' % b2], key='lu%d' % b2)
        S.dma('sp', vf[b2][:], dr['v_sh'][et * 128:(et + 1) * 128, :], writes=['vf%d' % b2], key='lv%d' % b2)
    ld(0)
    for et in range(128):
        b2 = et % 2
        if et + 1 < 128:
            ld(et + 1)
        k.cp('dve', ub[:], uf[b2][:], reads=['uf%d' % b2], writes=['ub'])
        for g in range(2):
            pb, pn = bank()
            for c4 in range(4):
                c = g * 4 + c4
                k.mm(pb[:, c4 * 128:(c4 + 1) * 128], lhsT=ub[:, c * 128:(c + 1) * 128], rhs=ident, reads=['ub', 'cst'], writes=[pn])
            k.cp('act', utt[b2][:, g * 4:(g + 1) * 4, :].rearrange("p a n -> p (a n)"), pb[:, :], reads=[pn], writes=['utt%d' % b2])
        S.dma('sp', dr['UT'][:, :, et * 128:(et + 1) * 128], utt[b2][:], reads=['utt%d' % b2], writes=['UT'], key='su%d' % b2)
        k.cp('pool', vb[b2][:], vf[b2][:], reads=['vf%d' % b2], writes=['vb%d' % b2])
        S.dma('sp', dr['Vb'][et * 128:(et + 1) * 128, :], vb[b2][:], reads=['vb%d' % b2], writes=['Vb'], key='svb%d' % b2)
    S.pop()


F_IN = [('x', [T, D], F32), ('pos_all', [T], I32),
        ('wa_all', [8, 1024, 384], F32), ('w_sh', [1024, 384], F32), ('pp_all', [8, 128, NPP], F32), ('w2s_all', [8, 128, 64], F32), ('a2s_all', [8, 128, 64], F32),
        ('g2h_all', [8, 128, 64], F32), ('w0row_all', [8, 1, 128], F32), ('cst', [5, 128, 128], F32),
        ('pp2', [128, NPP2], F32), ('w_kvin', [1024, 256], F32), ('w_kvup', [128, 1024], F32), ('u_sh', [16384, 1024], F32), ('v_sh', [16384, 1024], F32),
        ('x_own', [TO, D], F32), ('pos', [TO], I32), ('p', [TO, 256], F32), ('ridx', [128, 2], I32),
        ('w_cq', [1024, 256], F32), ('w_q', [256, 768], F32), ('w_gate', [1024, 2048], F32), ('w_a', [512, 1024], F32), ('w_b', [512, 1024], F32),
        ('w_o', [1024, 1024], F32), ('w_pq', [1024, 2048], F32), ('sk', [16, 128, 128], F32), ('w_pg', [1024, 1024], F32), ('w_pp', [256, 1024], F32),
        ('g_fin', [1024], F32)]


def build_nc():
    nc = bass.Bass("TRN2", target_bir_lowering=False)
    dr = {}
    for nm, sh, dt in F_IN:
        dr[nm] = nc.dram_tensor(nm, list(sh), dt, kind="ExternalInput").ap()
    for nm, sh in [('bon_d', [64, T]), ('g_d', [64, T]), ('qh_d', [128, T]), ('ol_d', [64, T]), ('yrw_own', [512, TO]), ('hh_d', [128, NCH, 64]), ('hT_d', [128, 8, T]), ('ush_d', [3, 128, T]),
                   ('kTn_d', [8, 64, T]), ('kr_d', [32, T]), ('vtok_d', [8, 128, 128, 64]), ('UT', [128, 8, 16384]), ('Vb', [16384, 1024])]:
        dr[nm] = nc.dram_tensor(nm, sh, BF16, kind="Internal").ap()
    dr['x1_d'] = nc.dram_tensor('x1_d', [TO, D], F32, kind="Internal").ap()
    dr['x2_d'] = nc.dram_tensor('x2_d', [TO, D], F32, kind="Internal").ap()
    dr['s_d'] = nc.dram_tensor('s_d', [TO, 16, 128], F32, kind="Internal").ap()
    dr['out'] = nc.dram_tensor('out', [TO, D], F32, kind="ExternalOutput").ap()
    S = Sched(nc)
    with S:
        k = K(S)
        cm, bank = common2(nc, S, k, dr)
        phase_h(nc, S, k, dr, bank, cm)
        for hd in range(8):
            d2 = dict(dr)
            d2['wa'] = dr['wa_all'][hd]; d2['pp'] = dr['pp_all'][hd]; d2['w2s'] = dr['w2s_all'][hd]; d2['a2s'] = dr['a2s_all'][hd]
            d2['g2h'] = dr['g2h_all'][hd]; d2['w0row'] = dr['w0row_all'][hd]
            d2['yrw_own'] = dr['yrw_own'][hd * 64:(hd + 1) * 64, :]
            d2['_banks'] = cm['banks']
            S.push()
            rwkv_phase(nc, S, k, d2)
            S.pop()
            k.epsc = cm['epsc']
        phase_kv(nc, S, k, dr, bank, cm)
        phase_experts(nc, S, k, dr, bank, cm)
        dr2 = dict(dr); dr2['x'] = dr['x_own']
        S.push()
        alloc_attn(S, cm)
        phase_attn(nc, S, k, dr2, bank, cm)
        phase_merge(nc, S, k, dr2, bank, cm)
        S.pop()
        phase_peer(nc, S, k, dr2, bank, cm)
        phase_final(nc, S, k, dr2, bank, cm)
        S.finish()
    return nc


def kernel(**inputs):
    inp = {k_: np.asarray(v) for k_, v in inputs.items()}
    x2d = np.ascontiguousarray(inp['x'][0])
    p1 = [prep_core(inp, h) for h in range(8)]
    shared = {'x': x2d, 'pos_all': np.ascontiguousarray(inp['positions'][0]).astype(np.int32),
              'wa_all': np.stack([np.ascontiguousarray(p['wa'][:, 0:384]) for p in p1]), 'w_sh': np.ascontiguousarray(inp['w_in'][0][:, 1536:1920]), 'pp_all': np.stack([p['pp'] for p in p1]),
              'w2s_all': np.stack([p['w2s'] for p in p1]), 'a2s_all': np.stack([p['a2s'] for p in p1]),
              'g2h_all': np.stack([p['g2h'] for p in p1]), 'w0row_all': np.stack([p['w0row'] for p in p1]),
              'u_sh': np.ascontiguousarray(inp['peer_u'][0]), 'v_sh': np.ascontiguousarray(inp['peer_v'][0])}
    nc = build_nc()
    in_maps = []
    for c in range(8):
        p2 = prep2_core(inp, c)
        m = dict(shared)
        for nm, _, _ in F_IN:
            if nm in m:
                continue
            if nm == 'x_own':
                m[nm] = p2['x']
            elif nm == 'ridx':
                m[nm] = np.stack([np.arange(128) * 8 + c, np.arange(128) * 8 + 7 - c], 1).astype(np.int32)
            else:
                m[nm] = p2[nm]
        in_maps.append(m)
    res = run_bass_kernel_spmd(nc, in_maps, core_ids=list(range(8))).results
    out = np.concatenate([np.asarray(r['out']) for r in res], axis=0)
    return out.reshape(1, T, D).astype(np.float32)
```

```python
import contextlib
import numpy as np
import concourse.bass as bass
import concourse.mybir as mybir
from concourse.bass_utils import run_bass_kernel_spmd

F32 = mybir.dt.float32
BF16 = mybir.dt.bfloat16
I32 = mybir.dt.int32
AF = mybir.ActivationFunctionType
ALU = mybir.AluOpType
AX = mybir.AxisListType

ENGS = ['pe', 'act', 'dve', 'pool', 'sp']


class Sched:
    def __init__(self, nc, n_dma_sems=48):
        self.nc = nc
        self.stack = contextlib.ExitStack()
        self.streams = {e: [] for e in ENGS}
        self.cnt = {}
        self.seen = {e: {} for e in ENGS}
        self.last_write = {}
        self.readers = {}
        self.n_dma_sems = n_dma_sems
        self.dma_keys = {}
        self.sems = {}
        self.scopes = [self.stack]
        self.cap = None

    def __enter__(self):
        self.stack.__enter__()
        for e in ENGS:
            self.sems[e] = self.stack.enter_context(self.nc.semaphore("s_" + e))
            self.cnt[e] = 0
        self.dma_pool = [self.stack.enter_context(self.nc.semaphore("d%d" % i)) for i in range(self.n_dma_sems)]
        self.sw_pool = [self.stack.enter_context(self.nc.semaphore("w%d" % i)) for i in range(8)]
        self.sw_keys = {}
        return self

    def __exit__(self, *a):
        return self.stack.__exit__(*a)

    def sb(self, name, shape, dt):
        self.uid = getattr(self, 'uid', 0) + 1
        return self.scopes[-1].enter_context(self.nc.sbuf_tensor("sb%d_%s" % (self.uid, name), list(shape), dt))

    def ps(self, name, shape, dt):
        self.uid = getattr(self, 'uid', 0) + 1
        return self.scopes[-1].enter_context(self.nc.psum_tensor("ps%d_%s" % (self.uid, name), list(shape), dt))

    def push(self):
        st = contextlib.ExitStack()
        st.__enter__()
        self.scopes.append(st)

    def pop(self):
        self.barrier()
        st = self.scopes.pop()
        st.__exit__(None, None, None)

    def _deps(self, eng, reads, writes):
        deps = {}
        def add(tok):
            if tok is None:
                return
            k, v = tok
            if deps.get(k, 0) < v:
                deps[k] = v
        for r in reads:
            add(self.last_write.get(r))
        for w in writes:
            add(self.last_write.get(w))
            for t in self.readers.get(w, ()):
                add(t)
        waits = []
        seen = self.seen[eng]
        for k, v in deps.items():
            if k == 'pe' and eng == 'pe':
                continue
            if seen.get(k, 0) >= v:
                continue
            seen[k] = v
            waits.append((k, v))
        return waits

    def _commit(self, tok, reads, writes):
        for w in writes:
            self.last_write[w] = tok
            self.readers[w] = []
        for r in reads:
            if r in writes:
                continue
            self.readers.setdefault(r, []).append(tok)

    @staticmethod
    def _excl(reads, writes):
        pr = [r for r in reads if isinstance(r, str) and r.startswith('pb')]
        if pr:
            reads = [r for r in reads if r not in pr]
            writes = list(writes) + [r for r in pr if r not in writes]
        return reads, writes

    def op(self, eng, fn, reads=(), writes=()):
        if self.cap is not None:
            self.cap.append(('op', (eng, fn, tuple(reads), tuple(writes)), {}))
            return
        reads, writes = self._excl(reads, writes)
        waits = self._deps(eng, reads, writes)
        self.cnt[eng] += 1
        tok = (eng, self.cnt[eng])
        self.streams[eng].append((waits, fn, (eng, 1)))
        self._commit(tok, reads, writes)

    def capture(self, fn, *a):
        self.cap = []
        fn(*a)
        lst, self.cap = self.cap, None
        return lst

    def emit_interleaved(self, A, B):
        ia = ib = 0
        na, nb = len(A), len(B)
        while ia < na or ib < nb:
            if ib >= nb or (ia < na and ia * max(nb, 1) <= ib * max(na, 1)):
                kind, args, kw = A[ia]; ia += 1
            else:
                kind, args, kw = B[ib]; ib += 1
            getattr(self, kind)(*args, **kw)

    def _dkey(self, key):
        if key not in self.dma_keys:
            idx = len(self.dma_keys)
            assert idx < self.n_dma_sems, "out of dma semaphores"
            self.dma_keys[key] = ('dma', idx)
            self.cnt.setdefault(('dma', idx), 0)
        return self.dma_keys[key]

    def dma(self, eng, out, in_, reads=(), writes=(), key=None, **kw):
        if self.cap is not None:
            self.cap.append(('dma', (eng, out, in_), dict(reads=tuple(reads), writes=tuple(writes), key=key, **kw)))
            return
        k = self._dkey(key)
        waits = self._deps(eng, reads, writes)
        self.cnt[k] += 16
        tok = (k, self.cnt[k])
        self.streams[eng].append((waits, (lambda e, o=out, i=in_, kw=kw: e.dma_start(out=o, in_=i, **kw)), (k, 16)))
        self._commit(tok, reads, writes)

    def gather(self, out, in_rows, idx_ap, reads=(), writes=(), key=None):
        if key not in self.sw_keys:
            assert len(self.sw_keys) < len(self.sw_pool)
            self.sw_keys[key] = ('swd', len(self.sw_keys))
            self.cnt.setdefault(self.sw_keys[key], 0)
        kk_ = self.sw_keys[key]
        waits = self._deps('pool', reads, writes)
        self.cnt[kk_] += 16
        tok = (kk_, self.cnt[kk_])
        self.streams['pool'].append((waits, (lambda e: e.indirect_dma_start(out=out, out_offset=None, in_=in_rows, in_offset=bass.IndirectOffsetOnAxis(ap=idx_ap, axis=0))), (kk_, 16)))
        self._commit(tok, reads, writes)

    def coll(self, kind, op, groups, ins, outs, reads=(), writes=()):
        key = 'coll'
        kk_ = self._dkey(key)
        waits = self._deps('pool', reads, writes)
        self.cnt[kk_] += 16
        tok = (kk_, self.cnt[kk_])
        self.streams['pool'].append((waits, (lambda e: e.collective_compute(kind, op, replica_groups=groups, ins=ins, outs=outs)), (kk_, 16)))
        self._commit(tok, reads, writes)

    def barrier(self):
        for e in ENGS:
            waits = []
            for k, v in self.cnt.items():
                if v == 0 or k == e:
                    continue
                if self.seen[e].get(k, 0) >= v:
                    continue
                self.seen[e][k] = v
                waits.append((k, v))
            if waits:
                self.streams[e].append((waits, None, None))
        for e in ENGS:
            if self.cnt[e] and self.seen[e].get(e, 0) < self.cnt[e]:
                self.seen[e][e] = self.cnt[e]
                self.streams[e].append(([(e, self.cnt[e])], None, None))
        self.last_write.clear()
        self.readers.clear()
        self.dma_keys = {}

    def _sem(self, k):
        if isinstance(k, tuple) and k[0] == 'swd':
            return self.sw_pool[k[1]]
        if isinstance(k, tuple):
            return self.dma_pool[k[1]]
        return self.sems[k]

    def finish(self):
        self.barrier()
        nc = self.nc
        streams = self.streams
        sem = self._sem

        def replay(engname):
            def run(eng):
                for waits, fn, inc in streams[engname]:
                    for k, v in waits:
                        eng.wait_ge(sem(k), v)
                    if fn is not None:
                        ins = fn(eng)
                        ins.then_inc(sem(inc[0]), inc[1])
            return run

        with nc.Block() as block:
            block.tensor(replay('pe'))
            block.scalar(replay('act'))
            block.vector(replay('dve'))
            block.gpsimd(replay('pool'))
            block.sync(replay('sp'))


T = 16384
D = 1024
NB = 32
CL = 128
NCH = T // CL
CDEC = 0.6065306597126334
NPP = 32


class K:
    def __init__(self, S):
        self.S = S
        self.nbank = 0

    def mm(self, out, lhsT, rhs, start=True, stop=True, reads=(), writes=()):
        self.S.op('pe', lambda e: e.matmul(out, lhsT=lhsT, rhs=rhs, start=start, stop=stop), reads=reads, writes=writes)

    def act(self, out, in_, func, bias=None, scale=None, reads=(), writes=()):
        kw = {}
        if bias is not None:
            kw['bias'] = bias
        if scale is not None:
            kw['scale'] = scale
        self.S.op('act', lambda e: e.activation(out=out, in_=in_, func=func, **kw), reads=reads, writes=writes)

    def tt(self, eng, out, in0, in1, op, reads=(), writes=()):
        self.S.op(eng, lambda e: e.tensor_tensor(out=out, in0=in0, in1=in1, op=op), reads=reads, writes=writes)

    def ts(self, eng, out, in0, s1, s2, op0, op1=None, reads=(), writes=()):
        if op1 is None and op0 == ALU.pow:
            self.S.op(eng, lambda e: e.tensor_scalar(out=out, in0=in0, scalar1=1.0, scalar2=s1, op0=ALU.mult, op1=ALU.pow), reads=reads, writes=writes)
        elif op1 is None:
            self.S.op(eng, lambda e: e.tensor_scalar(out=out, in0=in0, scalar1=s1, scalar2=0.0, op0=op0, op1=ALU.add), reads=reads, writes=writes)
        else:
            self.S.op(eng, lambda e: e.tensor_scalar(out=out, in0=in0, scalar1=s1, scalar2=s2, op0=op0, op1=op1), reads=reads, writes=writes)

    def rsqrt(self, out, in_, scale, eps, reads=(), writes=()):
        self.S.op('act', lambda e: e.activation(out=out, in_=in_, func=AF.Sqrt, bias=self.eps_ap(eps, out), scale=scale), reads=list(reads) + ['epsc'], writes=writes)
        self.S.op('dve', lambda e: e.reciprocal(out=out, in_=out), reads=writes, writes=writes)

    def eps_ap(self, eps, out):
        n = out.shape[0]
        return self.epsc[eps][0:n, 0:1]

    def stt(self, eng, out, in0, scalar, in1, op0, op1, reads=(), writes=()):
        self.S.op(eng, lambda e: e.scalar_tensor_tensor(out=out, in0=in0, scalar=scalar, in1=in1, op0=op0, op1=op1), reads=reads, writes=writes)

    def cp(self, eng, out, in_, reads=(), writes=()):
        if eng == 'act':
            self.S.op('act', lambda e: e.activation(out=out, in_=in_, func=AF.Copy), reads=reads, writes=writes)
        else:
            self.S.op(eng, lambda e: e.tensor_copy(out=out, in_=in_), reads=reads, writes=writes)

    def memset(self, eng, ap, val, writes=()):
        self.S.op(eng, lambda e: e.memset(ap, val), reads=(), writes=writes)


def rwkv_phase(nc, S, k, dr, core_dbg=None):
    x_d = dr['x']
    nblk = dr.get('_nblk', NB)
    lvl = dr.get('_lvl', 99)
    banks = dr.get('_banks') or [(S.ps("pb%d" % i, [128, 512], F32), "pb%d" % i) for i in range(8)]
    st = {'b': 0, 'lo': 0, 'hi': 8}

    def bank():
        b = banks[st['lo'] + st['b'] % (st['hi'] - st['lo'])]
        st['b'] += 1
        return b

    wa = S.sb("wa", [128, 8, 384], BF16)
    wst_ = S.sb("wst_", [128, 384], F32)
    for c in range(8):
        S.dma('sp', wst_[:], dr['wa'][c * 128:(c + 1) * 128, 0:384], writes=['wst_'], key='c0')
        k.cp('dve', wa[:, c, :], wst_[:], reads=['wst_'], writes=['wa'])
    pp = S.sb("pp", [128, NPP], F32)
    S.dma('sp', pp[:], dr['pp'], writes=['pp'], key='c1')
    cst_st = S.sb("cst_st", [128, 5, 128], F32)
    S.dma('sp', cst_st[:], dr['cst'].rearrange("m p n -> p m n"), writes=['cst_st'], key='c2')
    cst = S.sb("cst", [128, 5, 128], BF16)
    k.cp('dve', cst[:], cst_st[:], reads=['cst_st'], writes=['cst'])
    ident = cst[:, 0, :]
    m4 = S.sb("m4", [128, 4, 4, 128], BF16)
    for mi in range(4):
        for j in range(4):
            k.cp('pool', m4[:, mi, j, :], cst[:, 1 + mi, :], reads=['cst'], writes=['m4'])
    id4 = S.sb("id4", [128, 4, 128], BF16)
    for j in range(4):
        k.cp('pool', id4[:, j, :], cst[:, 0, :], reads=['cst'], writes=['id4'])
    ones_bd = S.sb("ones_bd", [128, 128], BF16)
    k.memset('pool', ones_bd[:], 0.0, writes=['ones_bd'])
    k.memset('pool', ones_bd[0:64, 0:64], 1.0, writes=['ones_bd'])
    k.memset('pool', ones_bd[64:128, 64:128], 1.0, writes=['ones_bd'])
    ones_f = S.sb("ones_f", [128, 128], BF16)
    k.memset('pool', ones_f[:], 1.0, writes=['ones_f'])
    bd_st = S.sb("bd_st", [128, 2, 128], F32)
    k.memset('pool', bd_st[:], 0.0, writes=['bd_st'])
    S.dma('sp', bd_st[0:64, 0, 0:64], dr['w2s'][0:64, :], reads=['bd_st'], writes=['bd_st'], key='c3')
    S.dma('sp', bd_st[64:128, 0, 64:128], dr['w2s'][64:128, :], reads=['bd_st'], writes=['bd_st'], key='c3')
    S.dma('sp', bd_st[0:64, 1, 0:64], dr['a2s'][0:64, :], reads=['bd_st'], writes=['bd_st'], key='c3')
    S.dma('sp', bd_st[64:128, 1, 64:128], dr['a2s'][64:128, :], reads=['bd_st'], writes=['bd_st'], key='c3')
    bd = S.sb("bd", [128, 2, 128], BF16)
    k.cp('dve', bd[:], bd_st[:], reads=['bd_st'], writes=['bd'])
    g2_st = S.sb("g2_st", [128, 64], F32)
    S.dma('sp', g2_st[:], dr['g2h'], writes=['g2_st'], key='c4')
    g2h = S.sb("g2h", [128, 64], BF16)
    k.cp('dve', g2h[:], g2_st[:], reads=['g2_st'], writes=['g2h'])
    w0r_st = S.sb("w0r_st", [1, 128], F32)
    S.dma('sp', w0r_st[:], dr['w0row'], writes=['w0r_st'], key='c5')
    w0row = S.sb("w0row", [1, 128], BF16)
    k.cp('dve', w0row[:], w0r_st[:], reads=['w0r_st'], writes=['w0row'])
    epst = S.sb("epst", [128, 4], F32)
    k.epsc = {}
    for i_, ev in enumerate([1e-6, 1e-24, 64e-5]):
        k.memset('pool', epst[:, i_:i_ + 1], ev, writes=['epsc'])
        k.epsc[ev] = epst[:, i_:i_ + 1]
    omka = S.sb("omka", [128, 1], F32)
    k.ts('dve', omka[:], pp[:, 9:10], -1.0, 1.0, ALU.mult, ALU.add, reads=['pp'], writes=['omka'])

    MT_all = S.sb("MT_all", [128, NCH, 128], BF16)
    k.memset('pool', MT_all[:], 0.0, writes=['MT_all'])
    N_all = S.sb("N_all", [128, NCH, 64], BF16)
    gamL = S.sb("gamL", [128, NCH], F32)
    Hh = S.sb("Hh", [128, NCH + 1, 64], BF16)

    hT = [S.sb("hT%d" % i, [128, 8, 512], BF16) for i in range(2)]
    U = [S.sb("U%d" % i, [128, 6, 514], BF16) for i in range(3)]
    for i in range(3):
        k.memset('pool', U[i][:], 0.0, writes=['U%d' % i])

    def w(name, shape, dt=BF16):
        return S.sb(name, shape, dt)
    tsum6 = w("tsum6", [128, 6, 512], BF16)
    us2 = [w("us%d" % i, [128, 6, 512], BF16) for i in range(2)]
    us = us2[0]
    hmu = w("hmu", [128, 6], F32); omu = w("omu", [128, 6], F32)
    k.ts('dve', hmu[:], pp[:, 0:6], 0.5, None, ALU.mult, reads=['pp'], writes=['hmu'])
    k.ts('dve', omu[:], pp[:, 0:6], -1.0, 1.0, ALU.mult, ALU.add, reads=['pp'], writes=['omu'])
    tl = w("tl", [128, 512]); sl = w("sl", [128, 512])
    sg_tok = w("sg_tok", [128, 4, 128])
    Gi = w("Gi", [128, 512], F32); Ginv = w("Ginv", [128, 512], F32); Ge = w("Ge", [128, 512], F32); Gh = w("Gh", [128, 512], F32)
    tot = w("tot", [128, 4], F32); nct = w("nct", [128, 4], F32)
    a_t = w("a_t", [128, 512], F32)
    kk = w("kk", [128, 512], F32); kk2 = w("kk2", [128, 512]); rn = w("rn", [128, 512], F32); kkn = w("kkn", [128, 512], F32)
    t1 = rn; kdir = kk; bvec = a_t
    At2 = [w("At%d" % i, [128, 512]) for i in range(2)]; Bt2 = [w("Bt%d" % i, [128, 512]) for i in range(2)]
    Kt2 = [w("Kt%d" % i, [128, 512]) for i in range(2)]; Rt2 = [w("Rt%d" % i, [128, 512]) for i in range(2)]
    Bht2 = [w("Bht%d" % i, [128, 512]) for i in range(2)]; Kht2 = [w("Kht%d" % i, [128, 512]) for i in range(2)]
    At, Bt, Kt, Rt, Bht, Kht = At2[0], Bt2[0], Kt2[0], Rt2[0], Bht2[0], Kht2[0]
    rk = w("rk", [128, 512]); bon = w("bon", [64, 512]); g_t = w("g_t", [64, 512])
    Sm = [w("Sm%d" % i, [128, 8, 128]) for i in range(2)]
    SmT = [w("SmT%d" % i, [128, 8, 128]) for i in range(2)]
    Qm = [w("Qm%d" % i, [128, 8, 128]) for i in range(2)]
    AakT = w("AakT", [128, 8, 128]); TrbT = w("TrbT", [128, 8, 128]); TrkT = w("TrkT", [128, 8, 128])
    AXm = w("AXm", [128, 8, 128]); WU = w("WU", [128, 8, 128])
    Bh_tok = w("Bh_tok", [128, 8, 64]); Kh_tok = w("Kh_tok", [128, 8, 64]); V_tok = w("V_tok", [128, 4, 64])
    QhT = w("QhT", [128, 512]); Oloc = w("Oloc", [64, 512])

    MASK = {0: {'ss': 0, 'si': 1}, 1: {'ss': 2, 'si': 3}}
    MASK_TS = {0: 2, 1: 0}

    def load_h(b):
        S.dma('sp', hT[b % 2][:], dr['hT_d'][:, :, b * 512:(b + 1) * 512], reads=['hT_d'], writes=['hT%d' % (b % 2)], key='lh%d' % (b % 2))

    def project(b):
        if b == 0:
            load_h(0)
        if b + 1 < nblk:
            load_h(b + 1)
        h = hT[b % 2]; hn = 'hT%d' % (b % 2)
        Ub = U[b % 3]; un = 'U%d' % (b % 3)
        S.dma('sp', Ub[:, 3:6, 1:513], dr['ush_d'][:, :, b * 512:(b + 1) * 512].rearrange("a p n -> p a n"), reads=['ush_d'], writes=[un], key='lu%d' % (b % 3))
        for tI in range(3):
            pb, pn = bank()
            for c in range(8):
                k.mm(pb[:, :], lhsT=wa[:, c, tI * 128:(tI + 1) * 128], rhs=h[:, c, :], start=(c == 0), stop=(c == 7), reads=['wa', hn], writes=[pn])
            if tI % 2 == 0:
                k.cp('act', Ub[:, tI, 1:513], pb[:, :], reads=[pn], writes=[un])
            else:
                k.cp('dve', Ub[:, tI, 1:513], pb[:, :], reads=[pn], writes=[un])
        if b > 0:
            pu = U[(b - 1) % 3]; pun = 'U%d' % ((b - 1) % 3)
            k.cp('pool', pu[:, :, 513:514], Ub[:, :, 1:2], reads=[un], writes=[pun])
            k.cp('pool', Ub[:, :, 0:1], pu[:, :, 512:513], reads=[pun], writes=[un])
        else:
            k.memset('pool', Ub[:, :, 0:1], 0.0, writes=[un])
        if b == NB - 1:
            k.memset('pool', Ub[:, :, 513:514], 0.0, writes=[un])

    def prep(b):
        Ub = U[b % 3]; un = 'U%d' % (b % 3)
        tok0 = b * 512
        sfx = str(b % 2)
        At = At2[b % 2]; Bt = Bt2[b % 2]; Kt = Kt2[b % 2]; Rt = Rt2[b % 2]; Bht = Bht2[b % 2]; Kht = Kht2[b % 2]; us = us2[b % 2]
        k.tt('pool', tsum6[:], Ub[:, :, 0:512], Ub[:, :, 2:514], ALU.add, reads=[un], writes=['tsum6'])
        k.tt('dve', tsum6[:], tsum6[:], hmu[:, 0:6].unsqueeze(2).to_broadcast([128, 6, 512]), ALU.mult, reads=['tsum6', 'hmu'], writes=['tsum6'])
        k.tt('pool', us[:], Ub[:, :, 1:513], omu[:, 0:6].unsqueeze(2).to_broadcast([128, 6, 512]), ALU.mult, reads=[un, 'omu'], writes=['us' + sfx])
        k.tt('dve', us[:], us[:], tsum6[:], ALU.add, reads=['us' + sfx, 'tsum6'], writes=['us' + sfx])
        r2 = us[:, 0, :]; k2 = us[:, 1, :]; v2 = us[:, 2, :]
        k.act(tl[:], us[:, 3, :], AF.Tanh, reads=['us' + sfx], writes=['tl'])
        pb, pn = bank()
        for j in range(4):
            k.mm(pb[:, j * 128:(j + 1) * 128], lhsT=tl[:, j * 128:(j + 1) * 128], rhs=bd[:, 0, :], start=True, stop=False, reads=['tl', 'bd'], writes=[pn])
            k.mm(pb[:, j * 128:(j + 1) * 128], lhsT=ones_f[0:1, :], rhs=w0row[0:1, :], start=False, stop=True, reads=['ones_f', 'w0row'], writes=[pn])
        k.act(sg_tok[:].rearrange("p j n -> p (j n)"), pb[:, :], AF.Sigmoid, reads=[pn], writes=['sg_tok'])
        pI, pIn = bank(); pE, pEn = bank()
        for j in range(4):
            for d in range(2):
                P = slice(64 * d, 64 * d + 64)
                k.mm(pI[P, j * 128:(j + 1) * 128], lhsT=sg_tok[:, j, P], rhs=cst[:, 2 + 2 * d, :], reads=['sg_tok', 'cst'], writes=[pIn])
                k.mm(pE[P, j * 128:(j + 1) * 128], lhsT=sg_tok[:, j, P], rhs=cst[:, 1 + 2 * d, :], reads=['sg_tok', 'cst'], writes=[pEn])
        pb, pn = bank()
        k.mm(pb[:, :], lhsT=bd[:, 1, :], rhs=us[:, 4, :], reads=['bd', 'us' + sfx], writes=[pn])
        k.act(a_t[:], pb[:, :], AF.Sigmoid, bias=pp[:, 7:8], reads=[pn, 'pp'], writes=['a_t'])
        k.ts('dve', kk[:], k2, pp[:, 8:9], None, ALU.mult, reads=['us' + sfx, 'pp'], writes=['kk'])
        k.tt('pool', kk2[:], kk[:], kk[:], ALU.mult, reads=['kk'], writes=['kk2'])
        pb, pn = bank()
        k.mm(pb[:, :], lhsT=ones_bd[:], rhs=kk2[:], reads=['ones_bd', 'kk2'], writes=[pn])
        k.rsqrt(rn[:], pb[:, :], 1.0, 1e-24, reads=[pn], writes=['rn'])
        k.tt('dve', kkn[:], kk[:], rn[:], ALU.mult, reads=['kk', 'rn'], writes=['kkn'])
        k.ts('dve', t1[:], a_t[:], pp[:, 9:10], omka[:, 0:1], ALU.mult, ALU.add, reads=['a_t', 'pp', 'omka', 'rn', 'kkn'], writes=['rn'])
        k.tt('pool', kdir[:], k2, t1[:], ALU.mult, reads=['us' + sfx, 'rn', 'kkn'], writes=['kk'])
        k.tt('pool', bvec[:], kkn[:], a_t[:], ALU.mult, reads=['kkn', 'a_t', 'rn'], writes=['a_t'])
        k.stt('dve', rk[:], r2, pp[:, 10:11], kdir[:], ALU.mult, ALU.mult, reads=['us' + sfx, 'pp', 'kk'], writes=['rk'])
        pb, pn = bank()
        k.mm(pb[0:64, :], lhsT=ones_f[:, 0:64], rhs=rk[:], reads=['ones_f', 'rk'], writes=[pn])
        k.tt('dve', bon[:], pb[0:64, :], us[0:64, 2, :], ALU.mult, reads=[pn, 'us' + sfx], writes=['bon'])
        S.dma('sp', dr['bon_d'][:, tok0:tok0 + 512], bon[:], reads=['bon'], writes=['bon_d'], key='bon')
        k.act(sl[:], us[:, 5, :], AF.Sigmoid, reads=['us' + sfx], writes=['sl'])
        pb, pn = bank()
        k.mm(pb[0:64, :], lhsT=g2h[:], rhs=sl[:], reads=['g2h', 'sl'], writes=[pn])
        k.cp('act', g_t[:], pb[0:64, :], reads=[pn], writes=['g_t'])
        S.dma('sp', dr['g_d'][:, tok0:tok0 + 512], g_t[:], reads=['g_t'], writes=['g_d'], key='gd')
        k.act(Gi[:], pI[:, :], AF.Exp, scale=-CDEC, reads=[pIn], writes=['Gi'])
        k.act(Ginv[:], pI[:, :], AF.Exp, scale=CDEC, reads=[pIn], writes=['Ginv'])
        k.act(Ge[:], pE[:, :], AF.Exp, scale=-CDEC, reads=[pEn], writes=['Ge'])
        pI3 = pI[:, :].rearrange("p (j n) -> p j n", n=128)
        k.cp('dve', tot[0:64, :], pI3[0:64, :, 127], reads=[pIn], writes=['tot'])
        k.cp('dve', tot[64:128, :], pI3[64:128, :, 0], reads=[pIn], writes=['tot'])
        k.ts('dve', nct[:], tot[:], -CDEC, None, ALU.mult, reads=['tot'], writes=['nct'])
        k.act(gamL[:, b * 4:(b + 1) * 4], tot[:], AF.Exp, scale=-CDEC, reads=['tot'], writes=['gamL'])
        for j in range(4):
            k.act(Gh[:, j * 128:(j + 1) * 128], pI[:, j * 128:(j + 1) * 128], AF.Exp, bias=nct[:, j:j + 1], scale=CDEC, reads=[pIn, 'nct'], writes=['Gh'])
        k.stt('dve', At[:], kkn[:], -1.0, Ge[:], ALU.mult, ALU.mult, reads=['kkn', 'Ge'], writes=['At' + sfx])
        k.tt('pool', Bt[:], bvec[:], Ginv[:], ALU.mult, reads=['a_t', 'Ginv'], writes=['Bt' + sfx])
        k.tt('dve', Kt[:], kdir[:], Ginv[:], ALU.mult, reads=['kk', 'Ginv'], writes=['Kt' + sfx])
        k.tt('pool', Rt[:], r2, Gi[:], ALU.mult, reads=['us' + sfx, 'Gi'], writes=['Rt' + sfx])
        k.tt('dve', Bht[:], bvec[:], Gh[:], ALU.mult, reads=['a_t', 'Gh'], writes=['Bht' + sfx])
        k.tt('pool', Kht[:], kdir[:], Gh[:], ALU.mult, reads=['kk', 'Gh'], writes=['Kht' + sfx])

    def stages(b):
        Ub = U[b % 3]; un = 'U%d' % (b % 3)
        tok0 = b * 512
        sfx = str(b % 2)
        At = At2[b % 2]; Bt = Bt2[b % 2]; Kt = Kt2[b % 2]; Rt = Rt2[b % 2]; Bht = Bht2[b % 2]; Kht = Kht2[b % 2]; us = us2[b % 2]
        def scores(dst, dstn, L, Ln, R, Rn, mask_of_dir, ts_layout=False):
            for d in range(2):
                P = slice(64 * d, 64 * d + 64)
                pb, pn = bank()
                for j in range(4):
                    C = slice(j * 128, (j + 1) * 128)
                    k.mm(pb[:, C], lhsT=L[P, C], rhs=R[P, C], reads=[Ln, Rn], writes=[pn])
                mi = mask_of_dir[d]
                k.tt('dve', dst[:, 4 * d:4 * d + 4, :].rearrange("p j n -> p (j n)"), pb[:, :], m4[:, mi, :, :].rearrange("p j n -> p (j n)"), ALU.mult, reads=[pn, 'm4'], writes=[dstn + '_%d' % d])
        scores(SmT[0], 'SmT0', Bt, 'Bt' + sfx, At, 'At' + sfx, {0: 0, 1: 2})
        scores(Sm[0], 'Sm0', At, 'At' + sfx, Bt, 'Bt' + sfx, {0: 2, 1: 0})
        scores(AakT, 'AakT', Kt, 'Kt' + sfx, At, 'At' + sfx, {0: 0, 1: 2})
        scores(TrbT, 'TrbT', Bt, 'Bt' + sfx, Rt, 'Rt' + sfx, {0: 1, 1: 3})
        scores(TrkT, 'TrkT', Kt, 'Kt' + sfx, Rt, 'Rt' + sfx, {0: 1, 1: 3})
        for d in range(2):
            k.tt('pool', Qm[0][:, 4 * d:4 * d + 4, :], SmT[0][:, 4 * d:4 * d + 4, :], id4[:], ALU.add, reads=['SmT0_%d' % d, 'id4'], writes=['Qm0_%d' % d])
        if lvl < 4:
            return
        cur = 0
        for dl in range(1, dr.get('_ndl', 7)):
            nxt = 1 - cur
            sc, scn = Sm[cur], 'Sm%d' % cur
            stc, stcn = SmT[cur], 'SmT%d' % cur
            sn, snn = Sm[nxt], 'Sm%d' % nxt
            stn, stnn = SmT[nxt], 'SmT%d' % nxt
            for d in range(2):
                pb, pn = bank()
                for j in range(4):
                    c8 = 4 * d + j
                    k.mm(pb[:, j * 128:(j + 1) * 128], lhsT=stc[:, c8, :], rhs=sc[:, c8, :], reads=[stcn + '_%d' % d, scn + '_%d' % d], writes=[pn])
                k.cp(dr.get('_e1', 'act'), sn[:, 4 * d:4 * d + 4, :].rearrange("p j n -> p (j n)"), pb[:, :], reads=[pn], writes=[snn + '_%d' % d])
            if dr.get('_sub', 9) < 1:
                break
            if dl < 6:
                for d in range(2):
                    pb, pn = bank()
                    for j in range(4):
                        c8 = 4 * d + j
                        k.mm(pb[:, j * 128:(j + 1) * 128], lhsT=sc[:, c8, :], rhs=stc[:, c8, :], reads=[stcn + '_%d' % d, scn + '_%d' % d], writes=[pn])
                    k.cp('dve', stn[:, 4 * d:4 * d + 4, :].rearrange("p j n -> p (j n)"), pb[:, :], reads=[pn], writes=[stnn + '_%d' % d])
            qc, qcn = Qm[cur], 'Qm%d' % cur
            qn, qnn = Qm[nxt], 'Qm%d' % nxt
            if dr.get('_sub', 9) < 2:
                break
            for d in range(2):
                pb, pn = bank()
                for j in range(4):
                    c8 = 4 * d + j
                    k.mm(pb[:, j * 128:(j + 1) * 128], lhsT=sn[:, c8, :], rhs=qc[:, c8, :], reads=[snn + '_%d' % d, qcn + '_%d' % d], writes=[pn])
                k.tt('dve', qn[:, 4 * d:4 * d + 4, :].rearrange("p j n -> p (j n)"), pb[:, :], qc[:, 4 * d:4 * d + 4, :].rearrange("p j n -> p (j n)"), ALU.add, reads=[pn, qcn + '_%d' % d], writes=[qnn + '_%d' % d])
            cur = nxt
        Qf, Qfn = Qm[cur], 'Qm%d' % cur
        if lvl < 6:
            return
        def tokmajor(dst, dstn, src, srcn, col0, eng):
            pb, pn = bank()
            for j in range(4):
                k.mm(pb[:, j * 128:(j + 1) * 128], lhsT=src[:, j * 128:(j + 1) * 128], rhs=ident, reads=[srcn, 'cst'], writes=[pn])
            pv = pb[:, :].rearrange("p (j d n) -> p j d n", j=4, d=2, n=64)
            for d in range(2):
                k.cp(eng, dst[:, 4 * d:4 * d + 4, col0:col0 + 64], pv[:, :, d, :], reads=[pn], writes=[dstn + '_%d' % d])
        sub = dr.get('_sub', 9)
        if sub in (0, 9):
            tokmajor(AXm, 'AXm', At, 'At' + sfx, 0, 'act' if sub == 9 else 'dve')
        if sub in (1, 9):
            tokmajor(Bh_tok, 'Bh_tok', Bht, 'Bht' + sfx, 0, 'dve')
        if sub in (2, 9):
            tokmajor(Kh_tok, 'Kh_tok', Kht, 'Kht' + sfx, 0, 'act')
        if sub < 9:
            return
        pb, pn = bank()
        for j in range(4):
            k.mm(pb[:, j * 64:(j + 1) * 64], lhsT=us[0:64, 2, j * 128:(j + 1) * 128], rhs=cst[0:64, 0, 0:64], reads=['us' + sfx, 'cst'], writes=[pn])
        k.cp('dve', V_tok[:], pb[:, 0:256].rearrange("p (c n) -> p c n", n=64), reads=[pn], writes=['V_tok'])
        if lvl < 7:
            return
        pb, pn = bank()
        for d in range(2):
            for j in range(4):
                c8 = 4 * d + j
                k.mm(pb[:, c8 * 64:(c8 + 1) * 64], lhsT=AakT[:, c8, :], rhs=V_tok[:, j, :], reads=['AakT_%d' % d, 'V_tok'], writes=[pn])
        k.cp('act', AXm[:, :, 64:128], pb[:, :].rearrange("p (c n) -> p c n", n=64), reads=[pn], writes=['AXm_0', 'AXm_1'])
        if lvl < 8:
            return
        for d in range(2):
            pb, pn = bank()
            for j in range(4):
                c8 = 4 * d + j
                k.mm(pb[:, j * 128:(j + 1) * 128], lhsT=Qf[:, c8, :], rhs=AXm[:, c8, :], reads=[Qfn + '_%d' % d, 'AXm_%d' % d], writes=[pn])
            k.cp('act' if d == 0 else 'dve', WU[:, 4 * d:4 * d + 4, :].rearrange("p j n -> p (j n)"), pb[:, :], reads=[pn], writes=['WU_%d' % d])
        if lvl < 9:
            return
        pb, pn = bank()
        for d in range(2):
            P = slice(64 * d, 64 * d + 64)
            for j in range(4):
                c8 = 4 * d + j
                k.mm(pb[P, j * 128:(j + 1) * 128], lhsT=WU[:, c8, 0:64], rhs=TrbT[:, c8, :], reads=['WU_%d' % d, 'TrbT_%d' % d], writes=[pn])
        k.tt('dve', QhT[:], pb[:, :], Rt[:], ALU.add, reads=[pn, 'Rt' + sfx], writes=['QhT'])
        S.dma('sp', dr['qh_d'][:, tok0:tok0 + 512], QhT[:], reads=['QhT'], writes=['qh_d'], key='qh')
        pb, pn = bank()
        for j in range(4):
            for d in range(2):
                c8 = 4 * d + j
                k.mm(pb[0:64, j * 128:(j + 1) * 128], lhsT=WU[:, c8, 64:128], rhs=TrbT[:, c8, :], start=(d == 0), stop=False, reads=['WU_%d' % d, 'TrbT_%d' % d], writes=[pn])
                k.mm(pb[0:64, j * 128:(j + 1) * 128], lhsT=V_tok[:, j, :], rhs=TrkT[:, c8, :], start=False, stop=(d == 1), reads=['V_tok', 'TrkT_%d' % d], writes=[pn])
        k.cp('act', Oloc[:], pb[0:64, :], reads=[pn], writes=['Oloc'])
        S.dma('sp', dr['ol_d'][:, tok0:tok0 + 512], Oloc[:], reads=['Oloc'], writes=['ol_d'], key='ol')
        pb, pn = bank()
        for d in range(2):
            P = slice(64 * d, 64 * d + 64)
            for j in range(4):
                c8 = 4 * d + j
                k.mm(pb[P, j * 64:(j + 1) * 64], lhsT=WU[:, c8, 0:64], rhs=Bh_tok[:, c8, :], reads=['WU_%d' % d, 'Bh_tok_%d' % d], writes=[pn])
        k.cp('dve', MT_all[0:64, b * 4:(b + 1) * 4, 0:64], pb[0:64, 0:256].rearrange("p (c n) -> p c n", n=64), reads=[pn], writes=['MT_all'])
        for j in range(4):
            st1 = nblk * 4 - 1 - (b * 4 + j)
            k.cp('dve', MT_all[64:128, st1, 64:128], pb[64:128, j * 64:(j + 1) * 64], reads=[pn], writes=['MT_all'])
        pb, pn = bank()
        for d in range(2):
            P = slice(64 * d, 64 * d + 64)
            for j in range(4):
                c8 = 4 * d + j
                k.mm(pb[P, j * 64:(j + 1) * 64], lhsT=Bh_tok[:, c8, :], rhs=WU[:, c8, 64:128], start=True, stop=False, reads=['WU_%d' % d, 'Bh_tok_%d' % d], writes=[pn])
                k.mm(pb[P, j * 64:(j + 1) * 64], lhsT=Kh_tok[:, c8, :], rhs=V_tok[:, j, :], start=False, stop=True, reads=['Kh_tok_%d' % d, 'V_tok'], writes=[pn])
        k.cp('act', N_all[:, b * 4:(b + 1) * 4, :], pb[:, 0:256].rearrange("p (c n) -> p c n", n=64), reads=[pn], writes=['N_all'])

    def run_direct(fn, b, lo, hi):
        st['lo'], st['hi'] = lo, hi
        fn(b)
        st['lo'], st['hi'] = 0, 8

    def cap(fn, b, lo, hi):
        st['lo'], st['hi'] = lo, hi
        lst = S.capture(fn, b)
        st['lo'], st['hi'] = 0, 8
        return lst
    run_direct(project, 0, 0, 3)
    if nblk > 1:
        run_direct(project, 1, 0, 3)
    run_direct(prep, 0, 0, 8)
    for b in range(nblk):
        if b + 2 < nblk:
            run_direct(project, b + 2, 0, 3)
        A = cap(stages, b, *dr.get('_rgA', (0, 8)))
        Bp = cap(prep, b + 1, *dr.get('_rgB', (0, 8))) if b + 1 < nblk else []
        if not dr.get('_int'):
            S.emit_interleaved(A, []); S.emit_interleaved(Bp, [])
        else:
            S.emit_interleaved(A, Bp)
    if lvl < 10:
        return

    nch = nblk * 4
    S.barrier()
    Hf = Gi[:, 0:64]
    T1 = Gi[:, 64:128]
    k.memset('pool', Hf[:], 0.0, writes=['Hf'])
    k.memset('pool', Hh[:, 0, :], 0.0, writes=['Hh0'])
    def t1_for(s_):
        c0 = s_; c1 = nch - 1 - s_
        k.stt('dve', T1[0:64, :], Hf[0:64, :], gamL[0:64, c0:c0 + 1], N_all[0:64, c0, :], ALU.mult, ALU.add, reads=['Hf', 'gamL', 'N_all'], writes=['T1'])
        k.stt('dve', T1[64:128, :], Hf[64:128, :], gamL[64:128, c1:c1 + 1], N_all[64:128, c1, :], ALU.mult, ALU.add, reads=['Hf', 'gamL', 'N_all'], writes=['T1'])
    t1_for(0)
    for s in range(nch):
        pb, pn = bank()
        k.mm(pb[:, 0:64], lhsT=MT_all[:, s, :], rhs=Hh[:, s, :], reads=['MT_all', 'Hh%d' % s], writes=[pn])
        k.tt('dve', Hh[:, s + 1, :], pb[:, 0:64], T1[:], ALU.add, reads=[pn, 'T1'], writes=['Hh%d' % (s + 1)])
        k.tt('dve', Hf[:], pb[:, 0:64], T1[:], ALU.add, reads=[pn, 'T1'], writes=['Hf'])
        if s + 1 < nch:
            t1_for(s + 1)
    if lvl < 11:
        return
    S.barrier()
    qh = [At, Bt]; ol = [Kt[0:64, :], Rt[0:64, :]]; bo = [Bht[0:64, :], Kht[0:64, :]]; gg = [rk[0:64, :], kk2[0:64, :]]
    of = kkn[0:64, :]; ob = tl[0:64, :]; dd = Ginv[0:64, :]; d2 = sl[0:64, :]; rs = Ge[0:64, :]; yy = Gh[0:64, :]
    yo = [bon, g_t]
    mean_m = AakT[0:64, 0, 0:64]
    k.memset('pool', mean_m[:], 1.0 / 64.0, writes=['mean_m'])
    ridx = S.sb("ridx", [128, 2], I32)
    S.dma('sp', ridx[:], dr['ridx'], writes=['ridx'], key='rix')
    S.dma('sp', dr['hh_d'], Hh[:, 0:NCH, :], reads=['Hh%d' % i for i in range(NCH)], writes=['hh_d'], key='shh')
    rows2k = lambda ap: ap.rearrange("p (b n) -> (p b) n", n=TO)
    qh_o = us[:, 0:4, :].rearrange("p a n -> p (a n)")
    ol_o = hT[0][0:64, 0:4, :].rearrange("p a n -> p (a n)"); bo_o = hT[0][0:64, 4:8, :].rearrange("p a n -> p (a n)")
    gg_o = hT[1][0:64, 0:4, :].rearrange("p a n -> p (a n)")
    HhA = hT[1][:, 4:6, :].rearrange("p a n -> p (a n)"); HhB = hT[1][:, 6:8, :].rearrange("p a n -> p (a n)")
    S.gather(qh_o, rows2k(dr['qh_d']), ridx[:, 0:1], reads=['ridx', 'qh_d'], writes=['qh_o'], key='g1')
    S.gather(ol_o, rows2k(dr['ol_d']), ridx[0:64, 0:1], reads=['ridx', 'ol_d'], writes=['ol_o'], key='g2')
    S.gather(bo_o, rows2k(dr['bon_d']), ridx[0:64, 0:1], reads=['ridx', 'bon_d'], writes=['bo_o'], key='g3')
    S.gather(gg_o, rows2k(dr['g_d']), ridx[0:64, 0:1], reads=['ridx', 'g_d'], writes=['gg_o'], key='g4')
    hrows = dr['hh_d'].rearrange("p (b c) n -> (p b) (c n)", c=16)
    S.gather(HhA, hrows, ridx[:, 0:1], reads=['ridx', 'hh_d'], writes=['HhA'], key='g5')
    S.gather(HhB, hrows, ridx[:, 1:2], reads=['ridx', 'hh_d'], writes=['HhB'], key='g6')
    HhA3 = HhA.rearrange("p (c n) -> p c n", n=64); HhB3 = HhB.rearrange("p (c n) -> p c n", n=64)
    for b in range(4):
        i2 = b % 2
        TS = slice(b * 512, (b + 1) * 512)
        pb, pn = bank()
        for j in range(4):
            cl = b * 4 + j
            C = slice(j * 128, (j + 1) * 128)
            k.cp('dve', TrbT[0:64, j, 0:64], HhA3[0:64, cl, :], reads=['HhA'], writes=['Hc'])
            k.cp('pool', TrbT[64:128, j, 0:64], HhB3[64:128, 15 - cl, :], reads=['HhB'], writes=['Hc'])
            k.mm(pb[0:64, C], lhsT=TrbT[:, j, 0:64], rhs=qh_o[:, b * 512 + j * 128:b * 512 + (j + 1) * 128], reads=['Hc', 'qh_o'], writes=[pn])
        k.tt('dve', of[:], pb[0:64, :], ol_o[:, TS], ALU.add, reads=[pn, 'ol_o'], writes=['of'])
        k.cp('act', ob[:], of[:], reads=['of'], writes=['ob'])
        pb, pn = bank()
        k.mm(pb[0:64, :], lhsT=mean_m[:], rhs=ob[:], reads=['mean_m', 'ob'], writes=[pn])
        k.tt('dve', dd[:], of[:], pb[0:64, :], ALU.subtract, reads=['of', pn], writes=['dd'])
        k.act(d2[:], dd[:], AF.Square, reads=['dd'], writes=['d2'])
        pb, pn = bank()
        k.mm(pb[0:64, :], lhsT=mean_m[:], rhs=d2[:], reads=['mean_m', 'd2'], writes=[pn])
        k.rsqrt(rs[:], pb[0:64, :], 1.0, 64e-5, reads=[pn], writes=['rs'])
        k.tt('dve', yy[:], dd[:], rs[:], ALU.mult, reads=['dd', 'rs'], writes=['yy'])
        k.ts('dve', yy[:], yy[:], pp[0:64, 11:12], pp[0:64, 12:13], ALU.mult, ALU.add, reads=['yy', 'pp'], writes=['yy'])
        k.tt('pool', yy[:], yy[:], bo_o[:, TS], ALU.add, reads=['yy', 'bo_o'], writes=['yy'])
        k.tt('pool', yo[i2][:], yy[:], gg_o[:, TS], ALU.mult, reads=['yy', 'gg_o'], writes=['yo%d' % i2])
        S.dma('sp', dr['yrw_own'][:, TS], yo[i2][:], reads=['yo%d' % i2], writes=['yrw_own'], key='sy%d' % i2)


def host_consts():
    p = np.arange(128)[:, None]; f = np.arange(128)[None, :]
    cst = np.stack([(p == f), (p < f), (p <= f), (p > f), (p >= f)]).astype(np.float32)
    return cst


def prep_core(inp, hd):
    hc = slice(hd * 64, (hd + 1) * 64)
    w_in = inp['w_in'][0]
    o = {}
    r_c = np.arange(hd * 64, (hd + 1) * 64)
    cols = np.concatenate([r_c, r_c, 512 + r_c, 512 + r_c, 1024 + r_c, 1024 + r_c,
                           np.arange(1536, 1920),
                           np.arange(1920 + 256, 1920 + 384),
                           1920 + 384 + np.arange(16), 1920 + 384 + np.arange(16),
                           1920 + 400 + np.arange(16), 1920 + 400 + np.arange(16)])
    o['wa'] = np.ascontiguousarray(w_in[:, cols])
    mu = inp['rw_mu'][0]
    pp = np.zeros((128, NPP), np.float32)
    mucols = cols[:768]
    for tI in range(6):
        pp[:, tI] = mu[mucols[tI * 128:(tI + 1) * 128]]
    st2 = lambda v: np.concatenate([v[hc], v[hc]])
    pp[:, 6] = np.concatenate([inp['rw_w0'][0, 0, hc], inp['rw_w0'][0, 1, hc]])
    pp[:, 7] = np.concatenate([inp['rw_a0'][0, 0, hc], inp['rw_a0'][0, 1, hc]])
    pp[:, 8] = st2(inp['rw_k_k'][0]); pp[:, 9] = st2(inp['rw_k_a'][0]); pp[:, 10] = st2(inp['rw_r_k'][0])
    pp[:, 11] = st2(inp['rw_gn_w'][0]); pp[:, 12] = st2(inp['rw_gn_b'][0])
    pp[:, 13:21] = inp['g_mix'][0].reshape(8, 128).T
    o['pp'] = pp
    o['w2s'] = np.ascontiguousarray(np.concatenate([inp['rw_w2'][0, 0][:, hc], inp['rw_w2'][0, 1][:, hc]], 0))
    o['a2s'] = np.ascontiguousarray(np.concatenate([inp['rw_a2'][0, 0][:, hc], inp['rw_a2'][0, 1][:, hc]], 0))
    o['g2h'] = np.ascontiguousarray(inp['rw_g2'][0][:, hc])
    o['w0row'] = np.ascontiguousarray(pp[:, 6][None, :])
    o['cst'] = host_consts()
    return o


TO = 2048
TWO_PI = 6.283185307179586
ATT_SCALE = 96.0 ** -0.5


def make_banks(S, n=8):
    banks = [(S.ps("pb%d" % i, [128, 512], F32), "pb%d" % i) for i in range(n)]
    st = {'b': 0}

    def bank(lo=0, hi=n):
        b = banks[lo + st['b'] % (hi - lo)]
        st['b'] += 1
        return b
    return banks, bank


def norm_T(S, k, bank, x_rows, gcol, bufs, eps=1e-6):
    xt, sq, ss, rstd, xb, h = bufs['xt'], bufs['sq'], bufs['ss'], bufs['rstd'], bufs['xb'], bufs['hT']
    ident = bufs['ident']
    xtn = bufs.get('xtn', 'xt')
    if x_rows is not None:
        S.dma('sp', xt[:], x_rows.rearrange("(j p) d -> p j d", p=128), writes=[xtn], key='xt')
    for j in range(4):
        k.act(sq[:], xt[:, j, :], AF.Square, reads=[xtn], writes=['sq'])
        S.op('dve', lambda e, j=j: e.reduce_sum(out=ss[:, j:j + 1], in_=sq[:], axis=AX.X), reads=['sq'], writes=['ss'])
    k.rsqrt(rstd[:], ss[:], 1.0 / D, eps, reads=['ss'], writes=['rstd'])
    for j in range(4):
        k.ts('dve' if j % 2 else 'pool', xb[:, j, :], xt[:, j, :], rstd[:, j:j + 1], None, ALU.mult, reads=[xtn, 'rstd'], writes=['xb'])
    for c in range(8):
        pb, pn = bank()
        for j in range(4):
            k.mm(pb[:, j * 128:(j + 1) * 128], lhsT=xb[:, j, c * 128:(c + 1) * 128], rhs=ident, reads=['xb', 'cst'], writes=[pn])
        k.ts('dve', h[:, c, :], pb[:, :], gcol[:, c:c + 1], None, ALU.mult, reads=[pn, 'pp2'], writes=[bufs.get('hTn', 'hT')])


def load_w_bf16(S, k, dst, dstn, src_ap, stage, stagen, nk, ncols, key):
    for kt in range(nk):
        c0 = 0
        while c0 < ncols:
            w = min(stage.shape[-1], ncols - c0)
            S.dma('sp', stage[:, 0:w], src_ap[kt * 128:(kt + 1) * 128, c0:c0 + w], writes=[stagen], key=key)
            k.cp('dve', dst[:, kt, c0:c0 + w], stage[:, 0:w], reads=[stagen], writes=[dstn])
            c0 += w


def phase_attn(nc, S, k, dr, bank, cm):
    ident = cm['ident']; ones_f = cm['ones_f']; pp2 = cm['pp2']
    S.push()
    bufs = dict(xt=S.sb("xt", [128, 4, 1024], F32), sq=S.sb("sq", [128, 1024], BF16), ss=S.sb("ss", [128, 4], F32),
                rstd=S.sb("rstd", [128, 4], F32), xb=S.sb("xb", [128, 4, 1024], BF16), hT=S.sb("hT", [128, 8, 512], BF16), ident=ident)
    stage = S.sb("stage", [128, 1024], F32)
    wcq = S.sb("wcq", [128, 8, 256], BF16)
    load_w_bf16(S, k, wcq, 'wcq', dr['w_cq'], stage, 'stage', 8, 256, 'wst')
    wq = S.sb("wq", [128, 2, 768], BF16)
    load_w_bf16(S, k, wq, 'wq', dr['w_q'], stage, 'stage', 2, 768, 'wst')
    posi = S.sb("posi", [128, TO], I32)
    S.dma('sp', posi[:], dr['pos'].partition_broadcast(128), writes=['posi'], key='pos')
    ang = S.sb("ang", [128, TO], F32)
    k.cp('dve', ang[:], posi[:], reads=['posi'], writes=['ang'])
    cosT = S.sb("cosT", [128, TO], F32); sinT = S.sb("sinT", [128, TO], F32)
    tnf = S.sb("tnf", [128, TO], F32)
    PI = 3.141592653589793
    k.ts('dve', ang[:], ang[:], pp2[:, 16:17], None, ALU.mult, reads=['ang', 'pp2'], writes=['ang'])
    for (dst, dn, shift) in ((sinT, 'sinT', 0.0), (cosT, 'cosT', PI / 2)):
        k.ts('dve', dst[:], ang[:], shift, 1.0 / TWO_PI, ALU.add, ALU.mult, reads=['ang'], writes=[dn])
        k.cp('dve', posi[:], dst[:], reads=[dn], writes=['posi'])
        k.cp('dve', tnf[:], posi[:], reads=['posi'], writes=['tnf'])
        k.ts('dve', dst[:], ang[:], shift, None, ALU.add, reads=['ang'], writes=[dn])
        k.stt('dve', dst[:], tnf[:], -TWO_PI, dst[:], ALU.mult, ALU.add, reads=['tnf', dn], writes=[dn])
        k.ts('dve', dst[:], dst[:], -PI, PI, ALU.max, ALU.min, reads=[dn], writes=[dn])
        k.act(dst[:], dst[:], AF.Sin, reads=[dn], writes=[dn])
    QT = cm['QT']
    cq = S.sb("cq", [128, 2, 512], F32); cqs = S.sb("cqs", [128, 2, 512], BF16); cqn = S.sb("cqn", [128, 2, 512], BF16)
    rq = S.sb("rq", [128, 512], F32)
    x1s = S.sb("x1s", [128, 512], F32); x2s = S.sb("x2s", [128, 512], F32)
    ta = S.sb("ta", [128, 512], F32); tb = S.sb("tb", [128, 512], F32)
    x1p = S.sb("x1p", [128, 512], BF16); x2p = S.sb("x2p", [128, 512], BF16)
    for blk in range(4):
        T0 = blk * 512
        norm_T(S, k, bank, dr['x'][T0:T0 + 512, :], pp2[:, 0:8], bufs)
        hT = bufs['hT']
        pbs = []
        for t in range(2):
            pb, pn = bank()
            for c in range(8):
                k.mm(pb[:, :], lhsT=wcq[:, c, t * 128:(t + 1) * 128], rhs=hT[:, c, :], start=(c == 0), stop=(c == 7), reads=['wcq', 'hT'], writes=[pn])
            k.cp('act', cq[:, t, :], pb[:, :], reads=[pn], writes=['cq'])
        k.act(cqs[:], cq[:], AF.Square, reads=['cq'], writes=['cqs'])
        pb, pn = bank()
        for t in range(2):
            k.mm(pb[:, :], lhsT=ones_f[:], rhs=cqs[:, t, :], start=(t == 0), stop=(t == 1), reads=['ones_f', 'cqs'], writes=[pn])
        k.rsqrt(rq[:], pb[:, :], 1.0 / 256, 1e-6, reads=[pn], writes=['rq'])
        for t in range(2):
            k.stt('dve', cqn[:, t, :], cq[:, t, :], pp2[:, 8 + t:9 + t], rq[:], ALU.mult, ALU.mult, reads=['cq', 'pp2', 'rq'], writes=['cqn'])
        for hp in range(4):
            pb, pn = bank()
            for t in range(2):
                k.mm(pb[:, :], lhsT=wq[:, t, hp * 128:(hp + 1) * 128], rhs=cqn[:, t, :], start=(t == 0), stop=(t == 1), reads=['wq', 'cqn'], writes=[pn])
            k.cp('act', QT[0:64, 2 * hp, T0:T0 + 512], pb[0:64, :], reads=[pn], writes=['QT'])
            k.cp('dve', QT[0:64, 2 * hp + 1, T0:T0 + 512], pb[64:128, :], reads=[pn], writes=['QT'])
        for (dst, c0) in ((x1s, 512), (x2s, 640)):
            pb, pn = bank()
            for t in range(2):
                k.mm(pb[:, :], lhsT=wq[:, t, c0:c0 + 128], rhs=cqn[:, t, :], start=(t == 0), stop=(t == 1), reads=['wq', 'cqn'], writes=[pn])
            k.cp('act', dst[:], pb[:, :], reads=[pn], writes=['x1s' if c0 == 512 else 'x2s'])
        nc_ = cosT[:, T0:T0 + 512]; ns_ = sinT[:, T0:T0 + 512]
        k.tt('dve', ta[:], x1s[:], nc_, ALU.mult, reads=['x1s', 'cosT'], writes=['ta'])
        k.tt('pool', tb[:], x2s[:], ns_, ALU.mult, reads=['x2s', 'sinT'], writes=['tb'])
        k.tt('dve', x1p[:], ta[:], tb[:], ALU.subtract, reads=['ta', 'tb'], writes=['x1p'])
        k.tt('dve', ta[:], x2s[:], nc_, ALU.mult, reads=['x2s', 'cosT', 'x1p'], writes=['ta'])
        k.tt('pool', tb[:], x1s[:], ns_, ALU.mult, reads=['x1s', 'sinT', 'x1p'], writes=['tb'])
        k.tt('dve', x2p[:], ta[:], tb[:], ALU.add, reads=['ta', 'tb'], writes=['x2p'])
        for h in range(8):
            S.dma('sp', QT[64:80, h, T0:T0 + 512], x1p[h * 16:(h + 1) * 16, :], reads=['x1p'], writes=['QT'], key='qr')
            S.dma('sp', QT[80:96, h, T0:T0 + 512], x2p[h * 16:(h + 1) * 16, :], reads=['x2p'], writes=['QT'], key='qr')
    S.pop()

    S.push()
    ymla = cm['ymlaT']
    Kh2 = [S.sb("Kh%d" % i, [96, T], BF16) for i in range(2)]
    Vh2 = [S.sb("Vh%d" % i, [128, 128, 128], BF16) for i in range(2)]
    for i in range(2):
        k.memset('pool', Vh2[i][:, :, 64:128], 1.0, writes=['Vh%d' % i])
        S.dma('sp', Kh2[i][64:96, :], dr['kr_d'], reads=['kr_d'], writes=['Kh%d' % i], key='kr%d' % i)
    PT = [S.sb("PT%d" % i, [128, 512], BF16) for i in range(3)]
    osb = S.sb("osb", [128, 512], F32); rden = S.sb("rden", [64, 512], F32)
    nh = dr.get('_nh', 8)
    it = 0
    def ldh(h_):
        i_ = h_ % 2
        S.dma('sp', Kh2[i_][0:64, :], dr['kTn_d'][h_], reads=['kTn_d'], writes=['Kh%d' % i_], key='kh%d' % i_)
        S.dma('sp', Vh2[i_][:, :, 0:64], dr['vtok_d'][h_], reads=['vtok_d'], writes=['Vh%d' % i_], key='vh%d' % i_)
    ldh(0)
    for h in range(nh):
        if h + 1 < nh:
            ldh(h + 1)
        Kh = Kh2[h % 2]; Vh = Vh2[h % 2]; khn = 'Kh%d' % (h % 2); vhn = 'Vh%d' % (h % 2)
        for qg in range(4):
            acc, accn = cm['banks'][6 + (qg % 2)]
            def tail(pb, pn, kt):
                nonlocal it
                pt = PT[it % 3]; ptn = 'PT%d' % (it % 3); it += 1
                k.act(pt[:], pb[:, :], AF.Exp, scale=ATT_SCALE, reads=[pn], writes=[ptn])
                k.mm(acc[:, :], lhsT=Vh[:, kt, :], rhs=pt[:], start=(kt == 0), stop=(kt == 127), reads=[vhn, ptn], writes=[accn])
            pend = []
            for kt in range(128):
                pb, pn = bank(0, 6)
                k.mm(pb[:, :], lhsT=Kh[:, kt * 128:(kt + 1) * 128], rhs=QT[:, h, qg * 512:(qg + 1) * 512], reads=[khn, 'QT'], writes=[pn])
                pend.append((pb, pn, kt))
                if len(pend) > 2:
                    tail(*pend.pop(0))
            while pend:
                tail(*pend.pop(0))
            k.cp('dve', osb[:], acc[:, :], reads=[accn], writes=['osb'])
            S.op('dve', lambda e: e.reciprocal(out=osb[64:128, :], in_=osb[64:128, :]), reads=['osb'], writes=['osb'])
            k.cp('dve', rden[:], osb[64:128, :], reads=['osb'], writes=['rden'])
            k.tt('dve', ymla[(h % 2) * 64:(h % 2) * 64 + 64, h // 2, qg * 512:(qg + 1) * 512], osb[0:64, :], rden[:], ALU.mult, reads=['osb', 'rden'], writes=['ymlaT'])
    S.pop()


NPP2 = 48


def common2(nc, S, k, dr):
    cm = {}
    banks, bank = make_banks(S)
    cm['banks'] = banks
    cst_st = S.sb("cst_st", [128, 128], F32)
    S.dma('sp', cst_st[:], dr['cst'][0], writes=['cst_st'], key='c2')
    ident = S.sb("ident", [128, 128], BF16)
    k.cp('dve', ident[:], cst_st[:], reads=['cst_st'], writes=['cst'])
    cm['ident'] = ident[:]
    ones_f = S.sb("ones_f", [128, 128], BF16)
    k.memset('pool', ones_f[:], 1.0, writes=['ones_f'])
    cm['ones_f'] = ones_f
    pp2 = S.sb("pp2", [128, NPP2], F32)
    S.dma('sp', pp2[:], dr['pp2'], writes=['pp2'], key='c1')
    cm['pp2'] = pp2
    epst = S.sb("epst", [128, 4], F32)
    k.epsc = {}
    for i_, ev in enumerate([1e-6, 1e-24, 64e-5]):
        k.memset('pool', epst[:, i_:i_ + 1], ev, writes=['epsc'])
        k.epsc[ev] = epst[:, i_:i_ + 1]
    cm['epsc'] = k.epsc
    negpi = S.sb("negpi", [128, 1], F32)
    k.memset('pool', negpi[:], -3.141592653589793, writes=['negpi'])
    cm['negpi'] = negpi
    return cm, bank


def alloc_attn(S, cm):
    cm['QT'] = S.sb("QT", [96, 8, TO], BF16)
    cm['ymlaT'] = S.sb("ymlaT", [128, 4, TO], BF16)


def prep2_core(inp, c):
    o = {}
    tok = slice(c * TO, (c + 1) * TO)
    w_in = inp['w_in'][0]
    o['x'] = np.ascontiguousarray(inp['x'][0, tok])
    o['pos'] = np.ascontiguousarray(inp['positions'][0, tok]).astype(np.int32)
    o['w_cq'] = np.ascontiguousarray(w_in[:, 1920:2176])
    wq = inp['mla_w_qup'][0].reshape(256, 8, 96)
    o['w_q'] = np.ascontiguousarray(np.concatenate([wq[:, :, 0:64].reshape(256, 512), wq[:, :, 64:80].reshape(256, 128), wq[:, :, 80:96].reshape(256, 128)], 1))
    pp2 = np.zeros((128, NPP2), np.float32)
    pp2[:, 0:8] = inp['g_mix'][0].reshape(8, 128).T
    pp2[:, 8:10] = inp['mla_g_qa'][0].reshape(2, 128).T
    inv = (10000.0 ** (-np.arange(0, 32, 2, dtype=np.float32) / 32)).astype(np.float32)
    pp2[:, 16] = np.tile(inv, 8)
    pp2[:, 17:25] = inp['g_ffn'][0].reshape(8, 128).T
    pp2[:, 25:33] = inp['g_ple'][0].reshape(8, 128).T
    pp2[:, 33:41] = inp['g_final'].reshape(8, 128).T
    pp2[0:64, 41] = np.tile(inv, 4)
    pp2[0:32, 42] = -1.0; pp2[32:64, 42] = 1.0
    pp2[:, 43] = inp['mla_g_kva'][0]
    o['pp2'] = pp2
    o['cst'] = host_consts()
    o['w_gate'] = np.ascontiguousarray(w_in[:, 2336:4384])
    o['w_a'] = np.ascontiguousarray(inp['w_br_rwkv'][0]); o['w_b'] = np.ascontiguousarray(inp['w_br_mla'][0])
    o['w_o'] = np.ascontiguousarray(inp['w_out'][0])
    o['w_pq'] = np.ascontiguousarray(inp['peer_w_q'][0])
    o['sk'] = np.ascontiguousarray(inp['peer_sub_keys'][0].reshape(16, 128, 128))
    o['w_pg'] = np.ascontiguousarray(inp['w_ple_gate'][0]); o['w_pp'] = np.ascontiguousarray(inp['w_ple_proj'][0])
    o['g_fin'] = np.ascontiguousarray(inp['g_final'])
    o['p'] = np.ascontiguousarray(inp['p'][0, 0, tok])
    kr = w_in[:, 1920 + 384:1920 + 416]
    x1c, x2c = kr[:, 0:16], kr[:, 16:32]
    o['w_kvin'] = np.ascontiguousarray(np.concatenate([w_in[:, 1920 + 256:1920 + 384], x1c, x1c, x2c, x2c, x2c, x2c, x1c, x1c], 1))
    wk = inp['mla_w_kvup'][0].reshape(128, 8, 128)
    o['w_kvup'] = np.ascontiguousarray(np.concatenate([wk[:, :, 0:64].reshape(128, 512), wk[:, :, 64:128].reshape(128, 512)], 1))
    o['u_sh'] = np.ascontiguousarray(inp['peer_u'][0, c * 2048:(c + 1) * 2048])
    o['v_sh'] = np.ascontiguousarray(inp['peer_v'][0, c * 2048:(c + 1) * 2048])
    return o


def phase_merge(nc, S, k, dr, bank, cm):
    ident = cm['ident']; pp2 = cm['pp2']
    S.push()
    bufs = dict(xt=S.sb("xt", [128, 4, 1024], F32), sq=S.sb("sq", [128, 1024], BF16), ss=S.sb("ss", [128, 4], F32),
                rstd=S.sb("rstd", [128, 4], F32), xb=S.sb("xb", [128, 4, 1024], BF16), hT=S.sb("hT", [128, 8, 512], BF16), ident=ident)
    stage = S.sb("stage", [128, 1024], F32)
    wg = S.sb("wg", [128, 8, 2048], BF16)
    load_w_bf16(S, k, wg, 'wg', dr['w_gate'], stage, 'stage', 8, 2048, 'wst')
    WA = S.sb("WA", [128, 4, 1024], BF16); WB = S.sb("WB", [128, 4, 1024], BF16); WO = S.sb("WO", [128, 8, 1024], BF16)
    load_w_bf16(S, k, WA, 'WA', dr['w_a'], stage, 'stage', 4, 1024, 'wst')
    load_w_bf16(S, k, WB, 'WB', dr['w_b'], stage, 'stage', 4, 1024, 'wst')
    load_w_bf16(S, k, WO, 'WO', dr['w_o'], stage, 'stage', 8, 1024, 'wst')
    yrw = S.sb("yrw", [128, 4, TO], BF16)
    S.dma('sp', yrw[:], dr['yrw_own'].rearrange("(c p) n -> p c n", p=128), reads=['yrw_own'], writes=['yrw'], key='yrw')
    ymla = cm['ymlaT']
    mT = S.sb("mT", [128, 8, 512], BF16)
    sgA = S.sb("sgA", [128, 512], F32); sgB = S.sb("sgB", [128, 512], F32)
    m1 = S.sb("m1", [128, 512], F32); m2 = S.sb("m2", [128, 512], F32)
    xt = bufs['xt']
    for blk in range(4):
        T0 = blk * 512
        TS = slice(T0, T0 + 512)
        norm_T(S, k, bank, dr['x'][T0:T0 + 512, :], pp2[:, 0:8], bufs)
        hT = bufs['hT']
        for dt in range(8):
            pA, pAn = bank(); pB, pBn = bank(); qA, qAn = bank(); qB, qBn = bank()
            for c in range(8):
                k.mm(pA[:, :], lhsT=wg[:, c, dt * 128:(dt + 1) * 128], rhs=hT[:, c, :], start=(c == 0), stop=(c == 7), reads=['wg', 'hT'], writes=[pAn])
            for c in range(8):
                k.mm(pB[:, :], lhsT=wg[:, c, 1024 + dt * 128:1024 + (dt + 1) * 128], rhs=hT[:, c, :], start=(c == 0), stop=(c == 7), reads=['wg', 'hT'], writes=[pBn])
            for c in range(4):
                k.mm(qA[:, :], lhsT=WA[:, c, dt * 128:(dt + 1) * 128], rhs=yrw[:, c, TS], start=(c == 0), stop=(c == 3), reads=['WA', 'yrw'], writes=[qAn])
            for c in range(4):
                k.mm(qB[:, :], lhsT=WB[:, c, dt * 128:(dt + 1) * 128], rhs=ymla[:, c, TS], start=(c == 0), stop=(c == 3), reads=['WB', 'ymlaT'], writes=[qBn])
            k.act(sgA[:], pA[:, :], AF.Sigmoid, reads=[pAn], writes=['sgA'])
            k.act(sgB[:], pB[:, :], AF.Sigmoid, reads=[pBn], writes=['sgB'])
            k.tt('dve', m1[:], qA[:, :], sgA[:], ALU.mult, reads=[qAn, 'sgA'], writes=['m1'])
            k.tt('dve', m2[:], qB[:, :], sgB[:], ALU.mult, reads=[qBn, 'sgB'], writes=['m2'])
            k.tt('pool', mT[:, dt, :], m1[:], m2[:], ALU.add, reads=['m1', 'm2'], writes=['mT'])
        for j in range(4):
            for hf in range(2):
                pb, pn = bank()
                for m in range(8):
                    k.mm(pb[:, :], lhsT=mT[:, m, j * 128:(j + 1) * 128], rhs=WO[:, m, hf * 512:(hf + 1) * 512], start=(m == 0), stop=(m == 7), reads=['mT', 'WO'], writes=[pn])
                k.tt('dve', xt[:, j, hf * 512:(hf + 1) * 512], pb[:, :], xt[:, j, hf * 512:(hf + 1) * 512], ALU.add, reads=[pn, 'xt'], writes=['xt'])
        S.dma('sp', dr['x1_d'][T0:T0 + 512, :].rearrange("(j p) d -> p j d", p=128), xt[:], reads=['xt'], writes=['x1_d'], key='x1s')
    S.pop()


def phase_peer(nc, S, k, dr, bank, cm):
    ident = cm['ident']; pp2 = cm['pp2']
    banks = cm['banks']
    S.push()
    h2T = S.sb("h2T", [128, 8, TO], BF16)
    S.push()
    bufs = dict(xt=S.sb("xt", [128, 4, 1024], F32), sq=S.sb("sq", [128, 1024], BF16), ss=S.sb("ss", [128, 4], F32),
                rstd=S.sb("rstd", [128, 4], F32), xb=S.sb("xb", [128, 4, 1024], BF16), hT=None, ident=ident)
    stage = S.sb("stage", [128, 1024], F32)
    wpq = S.sb("wpq", [128, 8, 2048], BF16)
    load_w_bf16(S, k, wpq, 'wpq', dr['w_pq'], stage, 'stage', 8, 2048, 'wst')
    skb = S.sb("skb", [128, 16, 128], BF16)
    skT = S.sb("skT", [128, 16, 128], BF16)
    for g4 in range(4):
        S.dma('sp', stage[:, 0:512].rearrange("p (a n) -> p a n", n=128), dr['sk'][g4 * 4:(g4 + 1) * 4].rearrange("a p n -> p a n"), writes=['stage'], key='wst')
        k.cp('dve', skb[:, g4 * 4:(g4 + 1) * 4, :], stage[:, 0:512].rearrange("p (a n) -> p a n", n=128), reads=['stage'], writes=['skb'])
    for g4 in range(4):
        pb, pn = bank()
        for a in range(4):
            k.mm(pb[:, a * 128:(a + 1) * 128], lhsT=skb[:, g4 * 4 + a, :], rhs=ident, reads=['skb', 'cst'], writes=[pn])
        k.cp('dve', skT[:, g4 * 4:(g4 + 1) * 4, :].rearrange("p a n -> p (a n)"), pb[:, :], reads=[pn], writes=['skT'])
    qpT = [S.sb("qpT%d" % i, [128, 512], BF16) for i in range(2)]
    s_sb = S.sb("s_sb", [128, 4, 16, 128], F32)
    for blk in range(4):
        T0 = blk * 512
        bufs['hT'] = h2T[:, :, T0:T0 + 512]
        norm_T(S, k, (lambda: bank(0, 4)), dr['x1_d'][T0:T0 + 512, :], pp2[:, 17:25], bufs)
        hT = bufs['hT']
        for hc in range(16):
            pb, pn = bank(0, 4)
            for c in range(8):
                k.mm(pb[:, :], lhsT=wpq[:, c, hc * 128:(hc + 1) * 128], rhs=hT[:, c, :], start=(c == 0), stop=(c == 7), reads=['wpq', 'hT'], writes=[pn])
            qp = qpT[hc % 2]; qpn = 'qpT%d' % (hc % 2)
            k.cp('act', qp[:], pb[:, :], reads=[pn], writes=[qpn])
            for j in range(4):
                sb_, sn_ = banks[4 + j]
                k.mm(sb_[:, (hc % 4) * 128:(hc % 4 + 1) * 128], lhsT=qp[:, j * 128:(j + 1) * 128], rhs=skT[:, hc, :], reads=[qpn, 'skT'], writes=[sn_])
            if hc % 4 == 3:
                for j in range(4):
                    sb_, sn_ = banks[4 + j]
                    k.cp('dve' if j % 2 else 'act', s_sb[:, j, hc - 3:hc + 1, :].rearrange("p a n -> p (a n)"), sb_[:, :], reads=[sn_], writes=['s_sb'])
        S.dma('sp', dr['s_d'][T0:T0 + 512].rearrange("(j p) a n -> p j a n", p=128), s_sb[:], reads=['s_sb'], writes=['s_d'], key='ssd')
    S.pop()

    S.push()
    st = S.sb("st", [128, 16, 128], F32)
    m16 = S.sb("m16", [128, 16, 16], F32)
    tmp = S.sb("tmp", [128, 256], F32)
    cand = S.sb("cand", [128, 8, 256], F32)
    top16 = S.sb("top16", [128, 8, 16], F32)
    thr = S.sb("thr", [128, 8], F32); mx = S.sb("mx", [128, 8], F32); negm = S.sb("negm", [128, 8], F32)
    e16 = S.sb("e16", [128, 8, 16], F32); Zs = S.sb("Zs", [128, 8], F32); rZ = S.sb("rZ", [128, 8], F32)
    Gb = [S.sb("G%d" % i, [128, 16384], BF16) for i in range(2)]
    RC = 16
    Cb = [S.sb("Cb%d" % i, [128, RC, 128], F32) for i in range(2)]
    Eb = [S.sb("Eb%d" % i, [128, RC, 128], BF16) for i in range(2)]
    Mb = [S.sb("Mb%d" % i, [128, RC, 128], BF16) for i in range(2)]
    UTg = [S.sb("UTg%d" % i, [128, 8, 512], BF16) for i in range(2)]
    Vg = [S.sb("Vg%d" % i, [128, 4, 1024], BF16) for i in range(3)]
    a_sb = [S.sb("a_sb%d" % i, [128, 512], F32) for i in range(2)]
    ga = [S.sb("ga%d" % i, [128, 512], BF16) for i in range(2)]
    gaT = [S.sb("gaT%d" % i, [128, 4, 128], BF16) for i in range(2)]
    x1t = S.sb("x1t", [128, 1024], F32)
    ntile = dr.get('_ntile', 16)
    cnt = {'c': 0}

    def topk(nt):
        N0 = nt * 128
        S.dma('sp', st[:], dr['s_d'][N0:N0 + 128], reads=['s_d'], writes=['st'], key='lst')
        for hc in range(16):
            S.op('dve', lambda e, hc=hc: e.max(out=m16[:, hc, 0:8], in_=st[:, hc, :]), reads=['st'], writes=['m16'])
            S.op('dve', lambda e, hc=hc: e.match_replace(out=tmp[:, 0:128], in_to_replace=m16[:, hc, 0:8], in_values=st[:, hc, :], imm_value=-1e30), reads=['st', 'm16'], writes=['tmp'])
            S.op('dve', lambda e, hc=hc: e.max(out=m16[:, hc, 8:16], in_=tmp[:, 0:128]), reads=['tmp'], writes=['m16'])
        for h in range(8):
            k.tt('pool', cand[:, h, :].rearrange("p (a b) -> p a b", b=16),
                 m16[:, 2 * h, :].unsqueeze(2).to_broadcast([128, 16, 16]),
                 m16[:, 2 * h + 1, :].unsqueeze(1).to_broadcast([128, 16, 16]), ALU.add, reads=['m16'], writes=['cand'])
        for h in range(8):
            S.op('dve', lambda e, h=h: e.max(out=top16[:, h, 0:8], in_=cand[:, h, :]), reads=['cand'], writes=['top16'])
            S.op('dve', lambda e, h=h: e.match_replace(out=tmp[:, :], in_to_replace=top16[:, h, 0:8], in_values=cand[:, h, :], imm_value=-1e30), reads=['cand', 'top16'], writes=['tmp'])
            S.op('dve', lambda e, h=h: e.max(out=top16[:, h, 8:16], in_=tmp[:, :]), reads=['tmp'], writes=['top16'])
        S.op('dve', lambda e: e.tensor_reduce(out=thr[:], in_=top16[:], axis=AX.X, op=ALU.min), reads=['top16'], writes=['thr'])
        S.op('dve', lambda e: e.tensor_reduce(out=mx[:], in_=top16[:], axis=AX.X, op=ALU.max), reads=['top16'], writes=['mx'])
        k.ts('dve', negm[:], mx[:], -1.0, None, ALU.mult, reads=['mx'], writes=['negm'])
        for h in range(8):
            k.act(e16[:, h, :], top16[:, h, :], AF.Exp, bias=negm[:, h:h + 1], reads=['top16', 'negm'], writes=['e16'])
        S.op('dve', lambda e: e.reduce_sum(out=Zs[:], in_=e16[:], axis=AX.X), reads=['e16'], writes=['Zs'])
        S.op('dve', lambda e: e.reciprocal(out=rZ[:], in_=Zs[:]), reads=['Zs'], writes=['rZ'])

    def gbuild(nt):
        G = Gb[nt % 2]; gn = 'G%d' % (nt % 2)
        k.memset('pool', G[:], 0.0, writes=[gn])
        yield
        for h in range(8):
            for ic in range(128 // RC):
                b2 = cnt['c'] % 2; cnt['c'] += 1
                C = Cb[b2]; E = Eb[b2]; M = Mb[b2]
                cn, en, mn = 'Cb%d' % b2, 'Eb%d' % b2, 'Mb%d' % b2
                k.tt('pool', C[:], st[:, 2 * h, ic * RC:(ic + 1) * RC].unsqueeze(2).to_broadcast([128, RC, 128]),
                     st[:, 2 * h + 1, :].unsqueeze(1).to_broadcast([128, RC, 128]), ALU.add, reads=['st'], writes=[cn])
                k.act(E[:], C[:], AF.Exp, bias=negm[:, h:h + 1], reads=[cn, 'negm'], writes=[en])
                k.stt('dve', M[:], C[:], thr[:, h:h + 1], E[:], ALU.is_ge, ALU.mult, reads=[cn, en, 'thr'], writes=[mn])
                Gs = G[:, ic * RC * 128:(ic + 1) * RC * 128].rearrange("p (a b) -> p a b", b=128)
                k.stt('dve', Gs, M[:], rZ[:, h:h + 1], Gs, ALU.mult, ALU.add, reads=[mn, 'rZ', gn], writes=[gn])
                yield

    def dense(nt):
        N0 = nt * 128
        G = Gb[nt % 2]; gn = 'G%d' % (nt % 2)
        acc = [banks[6], banks[7]]
        pre = {}; tr = {}

        def stA(eg):
            b2 = eg % 2; v3 = eg % 3
            if not (dr.get('_nodma') and (nt > 0 or eg > 2)):
                S.dma('sp', UTg[b2][:], dr['UT'][:, :, eg * 512:(eg + 1) * 512], reads=['UT'], writes=['UTg%d' % b2], key='ut%d' % b2)
                S.dma('sp', Vg[v3][:], dr['Vb'][eg * 512:(eg + 1) * 512, :].rearrange("(q p) d -> p q d", p=128), reads=['Vb'], writes=['Vg%d' % v3], key='vg%d' % v3)
            pb, pn = bank(0, 3)
            for c in range(8):
                k.mm(pb[:, :], lhsT=h2T[:, c, N0:N0 + 128], rhs=UTg[b2][:, c, :], start=(c == 0), stop=(c == 7), reads=['h2T', 'UTg%d' % b2], writes=[pn])
            k.act(a_sb[b2][:], pb[:, :], AF.Gelu, reads=[pn], writes=['a_sb%d' % b2])
            k.tt('dve', ga[b2][:], a_sb[b2][:], G[:, eg * 512:(eg + 1) * 512], ALU.mult, reads=['a_sb%d' % b2, gn], writes=['ga%d' % b2])

        def stB(eg):
            b2 = eg % 2
            pt, ptn = bank(3, 6)
            for q in range(4):
                k.mm(pt[:, q * 128:(q + 1) * 128], lhsT=ga[b2][:, q * 128:(q + 1) * 128], rhs=ident, reads=['ga%d' % b2, 'cst'], writes=[ptn])
            k.cp('act', gaT[b2][:].rearrange("p q n -> p (q n)"), pt[:, :], reads=[ptn], writes=['gaT%d' % b2])

        def stC(eg):
            b2 = eg % 2; v3 = eg % 3
            for q in range(4):
                for hf in range(2):
                    k.mm(acc[hf][0][:, :], lhsT=gaT[b2][:, q, :], rhs=Vg[v3][:, q, hf * 512:(hf + 1) * 512],
                         start=(eg == 0 and q == 0), stop=(eg == 31 and q == 3), reads=['gaT%d' % b2, 'Vg%d' % v3], writes=[acc[hf][1]])
        for g in range(34):
            if g < 32:
                stA(g)
            if 0 <= g - 1 < 32:
                stB(g - 1)
            if 0 <= g - 2 < 32:
                stC(g - 2)
            yield
        S.dma('sp', x1t[:], dr['x1_d'][N0:N0 + 128, :], reads=['x1_d'], writes=['x1t'], key='lx1')
        for hf in range(2):
            k.tt('dve', x1t[:, hf * 512:(hf + 1) * 512], acc[hf][0][:, :], x1t[:, hf * 512:(hf + 1) * 512], ALU.add, reads=[acc[hf][1], 'x1t'], writes=['x1t'])
        S.dma('sp', dr['x2_d'][N0:N0 + 128, :], x1t[:], reads=['x1t'], writes=['x2_d'], key='sx2')
        yield

    topk(0)
    if dr.get('_nog'):
        def gbuild(nt):
            yield
    for _ in gbuild(0):
        pass
    for nt in range(ntile):
        gb = None
        if nt + 1 < ntile:
            topk(nt + 1)
            gb = gbuild(nt + 1)
        for _ in dense(nt):
            if gb is not None:
                for _r in range(2):
                    try:
                        next(gb)
                    except StopIteration:
                        gb = None
                        break
        if gb is not None:
            for _ in gb:
                pass
    S.pop()
    S.pop()


def phase_final(nc, S, k, dr, bank, cm):
    ident = cm['ident']; pp2 = cm['pp2']
    S.push()
    bufs = dict(xt=S.sb("xt", [128, 4, 1024], F32), sq=S.sb("sq", [128, 1024], BF16), ss=S.sb("ss", [128, 4], F32),
                rstd=S.sb("rstd", [128, 4], F32), xb=S.sb("xb", [128, 4, 1024], BF16), hT=S.sb("hT", [128, 8, 512], BF16), ident=ident)
    stage = S.sb("stage", [128, 1024], F32)
    Wpg = S.sb("Wpg", [128, 8, 1024], BF16); Wpp = S.sb("Wpp", [128, 2, 1024], BF16)
    load_w_bf16(S, k, Wpg, 'Wpg', dr['w_pg'], stage, 'stage', 8, 1024, 'wst')
    load_w_bf16(S, k, Wpp, 'Wpp', dr['w_pp'], stage, 'stage', 2, 1024, 'wst')
    gfin = S.sb("gfin", [128, 1024], F32)
    S.dma('sp', gfin[:], dr['g_fin'].partition_broadcast(128), writes=['gfin'], key='gf')
    pt = S.sb("pt", [128, 4, 256], F32); pb16 = S.sb("pb16", [128, 4, 256], BF16); pT = S.sb("pT", [128, 2, 512], BF16)
    sg = S.sb("sg", [128, 512], F32); tq = S.sb("tq", [128, 512], F32)
    sq2 = S.sb("sq2", [128, 1024], F32); ss2 = S.sb("ss2", [128, 4], F32); rs2 = S.sb("rs2", [128, 4], F32)
    ot = S.sb("ot", [128, 4, 1024], F32)
    xt = bufs['xt']
    for blk in range(4):
        T0 = blk * 512
        norm_T(S, k, bank, dr['x2_d'][T0:T0 + 512, :], pp2[:, 25:33], bufs)
        hT = bufs['hT']
        S.dma('sp', pt[:], dr['p'][T0:T0 + 512, :].rearrange("(j p) d -> p j d", p=128), writes=['pt'], key='lp')
        k.cp('pool', pb16[:], pt[:], reads=['pt'], writes=['pb16'])
        for kt in range(2):
            pb, pn = bank()
            for j in range(4):
                k.mm(pb[:, j * 128:(j + 1) * 128], lhsT=pb16[:, j, kt * 128:(kt + 1) * 128], rhs=ident, reads=['pb16', 'cst'], writes=[pn])
            k.cp('act', pT[:, kt, :], pb[:, :], reads=[pn], writes=['pT'])
        for j in range(4):
            for hf in range(2):
                HS = slice(hf * 512, (hf + 1) * 512)
                pg, pgn = bank(); pq, pqn = bank()
                for c in range(8):
                    k.mm(pg[:, :], lhsT=hT[:, c, j * 128:(j + 1) * 128], rhs=Wpg[:, c, HS], start=(c == 0), stop=(c == 7), reads=['hT', 'Wpg'], writes=[pgn])
                for kt in range(2):
                    k.mm(pq[:, :], lhsT=pT[:, kt, j * 128:(j + 1) * 128], rhs=Wpp[:, kt, HS], start=(kt == 0), stop=(kt == 1), reads=['pT', 'Wpp'], writes=[pqn])
                k.act(sg[:], pg[:, :], AF.Sigmoid, reads=[pgn], writes=['sg'])
                k.tt('dve', tq[:], pq[:, :], sg[:], ALU.mult, reads=[pqn, 'sg'], writes=['tq'])
                k.tt('pool', xt[:, j, HS], xt[:, j, HS], tq[:], ALU.add, reads=['xt', 'tq'], writes=['xt'])
        for j in range(4):
            k.act(sq2[:], xt[:, j, :], AF.Square, reads=['xt'], writes=['sq2'])
            S.op('dve', lambda e, j=j: e.reduce_sum(out=ss2[:, j:j + 1], in_=sq2[:], axis=AX.X), reads=['sq2'], writes=['ss2'])
        k.rsqrt(rs2[:], ss2[:], 1.0 / D, 1e-6, reads=['ss2'], writes=['rs2'])
        for j in range(4):
            k.stt('dve', ot[:, j, :], xt[:, j, :], rs2[:, j:j + 1], gfin[:], ALU.mult, ALU.mult, reads=['xt', 'rs2', 'gfin'], writes=['ot'])
        S.dma('sp', dr['out'][T0:T0 + 512, :].rearrange("(j p) d -> p j d", p=128), ot[:], reads=['ot'], writes=['out'], key='so')
    S.pop()


def phase_h(nc, S, k, dr, bank, cm):
    ident = cm['ident']; pp2 = cm['pp2']
    S.push()
    xtb2 = [S.sb("xtb%d" % i, [128, 4, 1024], F32) for i in range(2)]
    bufs = dict(xt=None, sq=S.sb("sq", [128, 1024], BF16), ss=S.sb("ss", [128, 4], F32),
                rstd=S.sb("rstd", [128, 4], F32), xb=S.sb("xb", [128, 4, 1024], BF16), hT=None, ident=ident)
    hTb = [S.sb("hTb%d" % i, [128, 8, 512], BF16) for i in range(2)]

    def ldx(blk_):
        S.dma('sp', xtb2[blk_ % 2][:], dr['x'][blk_ * 512:(blk_ + 1) * 512, :].rearrange("(j p) d -> p j d", p=128), writes=['xtb%d' % (blk_ % 2)], key='lx%d' % (blk_ % 2))
    ldx(0)
    stage = S.sb("stage", [128, 1024], F32)
    wsh = S.sb("wsh", [128, 8, 384], BF16)
    load_w_bf16(S, k, wsh, 'wsh', dr['w_sh'], stage, 'stage', 8, 384, 'wst')
    ush = [S.sb("ush%d" % i, [128, 3, 512], BF16) for i in range(2)]
    for blk in range(NB):
        T0 = blk * 512
        b2 = blk % 2
        bufs['hT'] = hTb[b2]; bufs['hTn'] = 'hTb%d' % b2
        bufs['xt'] = xtb2[b2]; bufs['xtn'] = 'xtb%d' % b2
        if blk + 1 < NB:
            ldx(blk + 1)
        norm_T(S, k, bank, None, pp2[:, 0:8], bufs)
        S.dma('sp', dr['hT_d'][:, :, T0:T0 + 512], hTb[b2][:], reads=['hTb%d' % b2], writes=['hT_d'], key='sh%d' % b2)
        for tI in range(3):
            pb, pn = bank()
            for c in range(8):
                k.mm(pb[:, :], lhsT=wsh[:, c, tI * 128:(tI + 1) * 128], rhs=hTb[b2][:, c, :], start=(c == 0), stop=(c == 7), reads=['wsh', 'hTb%d' % b2], writes=[pn])
            k.cp('act' if tI % 2 else 'dve', ush[b2][:, tI, :], pb[:, :], reads=[pn], writes=['ush%d' % b2])
        S.dma('sp', dr['ush_d'][:, :, T0:T0 + 512].rearrange("a p n -> p a n"), ush[b2][:], reads=['ush%d' % b2], writes=['ush_d'], key='su%d' % b2)
    S.pop()


def phase_kv(nc, S, k, dr, bank, cm):
    ident = cm['ident']; pp2 = cm['pp2']; ones_f = cm['ones_f']
    S.push()
    hTk = [S.sb("hTk%d" % i, [128, 8, 512], BF16) for i in range(2)]
    stage = S.sb("stage", [128, 1024], F32)
    wki = S.sb("wki", [128, 8, 256], BF16)
    load_w_bf16(S, k, wki, 'wki', dr['w_kvin'], stage, 'stage', 8, 256, 'wst')
    wku = S.sb("wku", [128, 1, 1024], BF16)
    load_w_bf16(S, k, wku, 'wku', dr['w_kvup'], stage, 'stage', 1, 1024, 'wst')
    posi = S.sb("posi", [64, 512], I32); ang = S.sb("ang", [64, 512], F32)
    cosT = S.sb("cosT", [64, 512], F32); sinT = S.sb("sinT", [64, 512], F32); tnf = S.sb("tnf", [64, 512], F32)
    PI = 3.141592653589793
    cks = S.sb("cks", [128, 512], F32); ck2 = S.sb("ck2", [128, 512], BF16); rk_ = S.sb("rk_", [128, 512], F32); ckn = S.sb("ckn", [128, 512], BF16)
    krA = S.sb("krA", [64, 512], F32); krB = S.sb("krB", [64, 512], F32); krR = S.sb("krR", [64, 512], BF16)
    kTs = [S.sb("kTs%d" % i, [128, 512], BF16) for i in range(2)]
    vts = [S.sb("vts%d" % i, [128, 512], BF16) for i in range(2)]
    for blk in range(NB):
        T0 = blk * 512
        S.dma('sp', posi[:], dr['pos_all'][T0:T0 + 512].partition_broadcast(64), writes=['posi'], key='pos')
        k.cp('dve', ang[:], posi[:], reads=['posi'], writes=['ang'])
        k.ts('dve', ang[:], ang[:], pp2[0:64, 41:42], None, ALU.mult, reads=['ang', 'pp2'], writes=['ang'])
        for (dst, dn, shift) in ((sinT, 'sinT', 0.0), (cosT, 'cosT', PI / 2)):
            k.ts('dve', dst[:], ang[:], shift, 1.0 / TWO_PI, ALU.add, ALU.mult, reads=['ang'], writes=[dn])
            k.cp('dve', posi[:], dst[:], reads=[dn], writes=['posi'])
            k.cp('dve', tnf[:], posi[:], reads=['posi'], writes=['tnf'])
            k.ts('dve', dst[:], ang[:], shift, None, ALU.add, reads=['ang'], writes=[dn])
            k.stt('dve', dst[:], tnf[:], -TWO_PI, dst[:], ALU.mult, ALU.add, reads=['tnf', dn], writes=[dn])
            k.ts('dve', dst[:], dst[:], -PI, PI, ALU.max, ALU.min, reads=[dn], writes=[dn])
            k.act(dst[:], dst[:], AF.Sin, reads=[dn], writes=[dn])
        k.ts('dve', sinT[:], sinT[:], pp2[0:64, 42:43], None, ALU.mult, reads=['sinT', 'pp2'], writes=['sinT'])
        hT = hTk[blk % 2]; hTn = 'hTk%d' % (blk % 2)
        if blk == 0:
            S.dma('sp', hTk[0][:], dr['hT_d'][:, :, 0:512], reads=['hT_d'], writes=['hTk0'], key='lh0')
        if blk + 1 < NB:
            nb_ = (blk + 1) % 2
            S.dma('sp', hTk[nb_][:], dr['hT_d'][:, :, T0 + 512:T0 + 1024], reads=['hT_d'], writes=['hTk%d' % nb_], key='lh%d' % nb_)
        pb, pn = bank()
        for c in range(8):
            k.mm(pb[:, :], lhsT=wki[:, c, 0:128], rhs=hT[:, c, :], start=(c == 0), stop=(c == 7), reads=['wki', hTn], writes=[pn])
        k.cp('act', cks[:], pb[:, :], reads=[pn], writes=['cks'])
        for (dst, dn, c0) in ((krA, 'krA', 128), (krB, 'krB', 192)):
            pb, pn = bank()
            for c in range(8):
                k.mm(pb[0:64, :], lhsT=wki[:, c, c0:c0 + 64], rhs=hT[:, c, :], start=(c == 0), stop=(c == 7), reads=['wki', hTn], writes=[pn])
            k.cp('act', dst[:], pb[0:64, :], reads=[pn], writes=[dn])
        k.act(ck2[:], cks[:], AF.Square, reads=['cks'], writes=['ck2'])
        pb, pn = bank()
        k.mm(pb[:, :], lhsT=ones_f[:], rhs=ck2[:], reads=['ones_f', 'ck2'], writes=[pn])
        k.rsqrt(rk_[:], pb[:, :], 1.0 / 128, 1e-6, reads=[pn], writes=['rk_'])
        k.stt('dve', ckn[:], cks[:], pp2[:, 43:44], rk_[:], ALU.mult, ALU.mult, reads=['cks', 'pp2', 'rk_'], writes=['ckn'])
        for hp in range(4):
            pb, pn = bank()
            k.mm(pb[:, :], lhsT=wku[:, 0, hp * 128:(hp + 1) * 128], rhs=ckn[:], reads=['wku', 'ckn'], writes=[pn])
            kt_ = kTs[hp % 2]; ktn = 'kTs%d' % (hp % 2)
            k.cp('act' if hp % 2 else 'dve', kt_[:], pb[:, :], reads=[pn], writes=[ktn])
            S.dma('sp', dr['kTn_d'][2 * hp, :, T0:T0 + 512], kt_[0:64, :], reads=[ktn], writes=['kTn_d'], key='sk%d' % (hp % 2))
            S.dma('sp', dr['kTn_d'][2 * hp + 1, :, T0:T0 + 512], kt_[64:128, :], reads=[ktn], writes=['kTn_d'], key='sk%d' % (hp % 2))
        for j in range(4):
            pb, pn = bank()
            k.mm(pb[:, :], lhsT=ckn[:, j * 128:(j + 1) * 128], rhs=wku[:, 0, 512:1024], reads=['wku', 'ckn'], writes=[pn])
            vt_ = vts[j % 2]; vtn = 'vts%d' % (j % 2)
            k.cp('act' if j % 2 else 'dve', vt_[:], pb[:, :], reads=[pn], writes=[vtn])
            S.dma('sp', dr['vtok_d'][:, :, blk * 4 + j, :].rearrange("h p d -> p h d"), vt_[:].rearrange("p (h d) -> p h d", d=64), reads=[vtn], writes=['vtok_d'], key='sv%d' % (j % 2))
        k.tt('dve', krA[:], krA[:], cosT[:], ALU.mult, reads=['krA', 'cosT'], writes=['krA'])
        k.tt('pool', krB[:], krB[:], sinT[:], ALU.mult, reads=['krB', 'sinT'], writes=['krB'])
        k.tt('dve', krR[:], krA[:], krB[:], ALU.add, reads=['krA', 'krB'], writes=['krR'])
        S.dma('sp', dr['kr_d'][0:16, T0:T0 + 512], krR[0:16, :], reads=['krR'], writes=['kr_d'], key='skr')
        S.dma('sp', dr['kr_d'][16:32, T0:T0 + 512], krR[32:48, :], reads=['krR'], writes=['kr_d'], key='skr')
    S.pop()


def phase_experts(nc, S, k, dr, bank, cm):
    ident = cm['ident']
    S.push()
    uf = [S.sb("uf%d" % i, [128, 1024], F32) for i in range(2)]
    ub = S.sb("ub", [128, 1024], BF16)
    utt = [S.sb("utt%d" % i, [128, 8, 128], BF16) for i in range(2)]
    vf = [S.sb("vf%d" % i, [128, 1024], F32) for i in range(2)]
    vb = [S.sb("vb%d" % i, [128, 1024], BF16) for i in range(2)]
    def ld(et):
        b2 = et % 2
        S.dma('sp', uf[b2][:], dr['u_sh'][et * 128:(et + 1) * 128, :], writes=['uf%d' % b2], key='lu%d' % b2)
        S.dma('sp', vf[b2][:], dr['v_sh'][et * 128:(et + 1) * 128, :], writes=['vf%d' % b2], key='lv%d' % b2)
    ld(0)
    for et in range(128):
        b2 = et % 2
        if et + 1 < 128:
            ld(et + 1)
        k.cp('dve', ub[:], uf[b2][:], reads=['uf%d' % b2], writes=['ub'])
        for g in range(2):
            pb, pn = bank()
            for c4 in range(4):
                c = g * 4 + c4
                k.mm(pb[:, c4 * 128:(c4 + 1) * 128], lhsT=ub[:, c * 128:(c + 1) * 128], rhs=ident, reads=['ub', 'cst'], writes=[pn])
            k.cp('act', utt[b2][:, g * 4:(g + 1) * 4, :].rearrange("p a n -> p (a n)"), pb[:, :], reads=[pn], writes=['utt%d' % b2])
        S.dma('sp', dr['UT'][:, :, et * 128:(et + 1) * 128], utt[b2][:], reads=['utt%d' % b2], writes=['UT'], key='su%d' % b2)
        k.cp('pool', vb[b2][:], vf[b2][:], reads=['vf%d' % b2], writes=['vb%d' % b2])
        S.dma('sp', dr['Vb'][et * 128:(et + 1) * 128, :], vb[b2][:], reads=['vb%d' % b2], writes=['Vb'], key='svb%d' % b2)
    S.pop()


F_IN = [('x', [T, D], F32), ('pos_all', [T], I32),
        ('wa_all', [8, 1024, 384], F32), ('w_sh', [1024, 384], F32), ('pp_all', [8, 128, NPP], F32), ('w2s_all', [8, 128, 64], F32), ('a2s_all', [8, 128, 64], F32),
        ('g2h_all', [8, 128, 64], F32), ('w0row_all', [8, 1, 128], F32), ('cst', [5, 128, 128], F32),
        ('pp2', [128, NPP2], F32), ('w_kvin', [1024, 256], F32), ('w_kvup', [128, 1024], F32), ('u_sh', [16384, 1024], F32), ('v_sh', [16384, 1024], F32),
        ('x_own', [TO, D], F32), ('pos', [TO], I32), ('p', [TO, 256], F32), ('ridx', [128, 2], I32),
        ('w_cq', [1024, 256], F32), ('w_q', [256, 768], F32), ('w_gate', [1024, 2048], F32), ('w_a', [512, 1024], F32), ('w_b', [512, 1024], F32),
        ('w_o', [1024, 1024], F32), ('w_pq', [1024, 2048], F32), ('sk', [16, 128, 128], F32), ('w_pg', [1024, 1024], F32), ('w_pp', [256, 1024], F32),
        ('g_fin', [1024], F32)]


def build_nc():
    nc = bass.Bass("TRN2", target_bir_lowering=False)
    dr = {}
    for nm, sh, dt in F_IN:
        dr[nm] = nc.dram_tensor(nm, list(sh), dt, kind="ExternalInput").ap()
    for nm, sh in [('bon_d', [64, T]), ('g_d', [64, T]), ('qh_d', [128, T]), ('ol_d', [64, T]), ('yrw_own', [512, TO]), ('hh_d', [128, NCH, 64]), ('hT_d', [128, 8, T]), ('ush_d', [3, 128, T]),
                   ('kTn_d', [8, 64, T]), ('kr_d', [32, T]), ('vtok_d', [8, 128, 128, 64]), ('UT', [128, 8, 16384]), ('Vb', [16384, 1024])]:
        dr[nm] = nc.dram_tensor(nm, sh, BF16, kind="Internal").ap()
    dr['x1_d'] = nc.dram_tensor('x1_d', [TO, D], F32, kind="Internal").ap()
    dr['x2_d'] = nc.dram_tensor('x2_d', [TO, D], F32, kind="Internal").ap()
    dr['s_d'] = nc.dram_tensor('s_d', [TO, 16, 128], F32, kind="Internal").ap()
    dr['out'] = nc.dram_tensor('out', [TO, D], F32, kind="ExternalOutput").ap()
    S = Sched(nc)
    with S:
        k = K(S)
        cm, bank = common2(nc, S, k, dr)
        phase_h(nc, S, k, dr, bank, cm)
        for hd in range(8):
            d2 = dict(dr)
            d2['wa'] = dr['wa_all'][hd]; d2['pp'] = dr['pp_all'][hd]; d2['w2s'] = dr['w2s_all'][hd]; d2['a2s'] = dr['a2s_all'][hd]
            d2['g2h'] = dr['g2h_all'][hd]; d2['w0row'] = dr['w0row_all'][hd]
            d2['yrw_own'] = dr['yrw_own'][hd * 64:(hd + 1) * 64, :]
            d2['_banks'] = cm['banks']
            S.push()
            rwkv_phase(nc, S, k, d2)
            S.pop()
            k.epsc = cm['epsc']
        phase_kv(nc, S, k, dr, bank, cm)
        phase_experts(nc, S, k, dr, bank, cm)
        dr2 = dict(dr); dr2['x'] = dr['x_own']
        S.push()
        alloc_attn(S, cm)
        phase_attn(nc, S, k, dr2, bank, cm)
        phase_merge(nc, S, k, dr2, bank, cm)
        S.pop()
        phase_peer(nc, S, k, dr2, bank, cm)
        phase_final(nc, S, k, dr2, bank, cm)
        S.finish()
    return nc


def kernel(**inputs):
    inp = {k_: np.asarray(v) for k_, v in inputs.items()}
    x2d = np.ascontiguousarray(inp['x'][0])
    p1 = [prep_core(inp, h) for h in range(8)]
    shared = {'x': x2d, 'pos_all': np.ascontiguousarray(inp['positions'][0]).astype(np.int32),
              'wa_all': np.stack([np.ascontiguousarray(p['wa'][:, 0:384]) for p in p1]), 'w_sh': np.ascontiguousarray(inp['w_in'][0][:, 1536:1920]), 'pp_all': np.stack([p['pp'] for p in p1]),
              'w2s_all': np.stack([p['w2s'] for p in p1]), 'a2s_all': np.stack([p['a2s'] for p in p1]),
              'g2h_all': np.stack([p['g2h'] for p in p1]), 'w0row_all': np.stack([p['w0row'] for p in p1]),
              'u_sh': np.ascontiguousarray(inp['peer_u'][0]), 'v_sh': np.ascontiguousarray(inp['peer_v'][0])}
    nc = build_nc()
    in_maps = []
    for c in range(8):
        p2 = prep2_core(inp, c)
        m = dict(shared)
        for nm, _, _ in F_IN:
            if nm in m:
                continue
            if nm == 'x_own':
                m[nm] = p2['x']
            elif nm == 'ridx':
                m[nm] = np.stack([np.arange(128) * 8 + c, np.arange(128) * 8 + 7 - c], 1).astype(np.int32)
            else:
                m[nm] = p2[nm]
        in_maps.append(m)
    res = run_bass_kernel_spmd(nc, in_maps, core_ids=list(range(8))).results
    out = np.concatenate([np.asarray(r['out']) for r in res], axis=0)
    return out.reshape(1, T, D).astype(np.float32)
```
